# Optimizing a Trainium2 kernel written in Bass

```python
import math
import jax
import jax.numpy as jnp
from jax import lax
import numpy as np

D_MODEL = 1024
BATCH = 2
SEQ = 16384
DEPTH = 4

HEAD_DIM = 64
N_HEADS = D_MODEL // HEAD_DIM
ATTN_WIDTH = N_HEADS * HEAD_DIM
ATTN_SCALE = HEAD_DIM ** -0.5
N_MIXERS = 4
Q_BLOCK = 128
ROW_CHUNK = 128
REL_BUCKETS = 32
REL_MAX_DIST = 2048
DILATED_PAIRS = ((128, 1), (512, 4), (2048, 16))
SB_CLIP = 60.0
NSA_KV_HEADS = 4
NSA_GROUP = N_HEADS // NSA_KV_HEADS
NSA_KV_WIDTH = NSA_KV_HEADS * HEAD_DIM
NSA_CMP_LEN = 32
NSA_CMP_STRIDE = 16
NSA_CMP_HIDDEN = 256
NSA_SEL_BLOCK = 64
NSA_TOP_N = 16
NSA_WINDOW = 512
MOBA_BLOCK = 256
MOBA_TOP_K = 3
N_EXPERTS = 16
N_GROUPS = 4
EXPERTS_PER_GROUP = N_EXPERTS // N_GROUPS
MOE_TOP_K = 2
D_EXPERT = 256
MOE_BLOCK = 256
DEEPNORM_ALPHA = (2 * DEPTH) ** 0.25
DEEPNORM_BETA = (8 * DEPTH) ** -0.25
LN_EPS = 1e-5
NEG_INF = -1e30
FORCED_SCORE = 1e9

kernel_name = 'hybrid_sparse_attn_grouped_moe_trunk'


def layer_norm(x, g, b):
    xf = x.astype(jnp.float32)
    mu = xf.mean(-1, keepdims=True)
    var = jnp.square(xf - mu).mean(-1, keepdims=True)
    return ((xf - mu) * lax.rsqrt(var + LN_EPS) * g + b).astype(x.dtype)


def rel_bucket(dist):
    n = jnp.maximum(dist, 0)
    exact = REL_BUCKETS // 2
    logf = jnp.log(jnp.maximum(n, 1).astype(jnp.float32) / exact) / math.log(REL_MAX_DIST / exact)
    large = jnp.minimum(exact + (logf * (REL_BUCKETS - exact)).astype(jnp.int32), REL_BUCKETS - 1)
    return jnp.where(n < exact, n, large)


def rel_bias(table, dist):
    return jnp.moveaxis(table[rel_bucket(dist)], -1, 0).astype(jnp.float32)


def masked_softmax(logits, mask):
    logits = jnp.where(mask, logits.astype(jnp.float32), NEG_INF)
    m = logits.max(-1, keepdims=True)
    p = jnp.where(mask, jnp.exp(logits - m), 0.0)
    denom = jnp.maximum(p.sum(-1, keepdims=True), 1e-30)
    return p / denom, m + jnp.log(denom)


def merge_parts(o, lse):
    w = jax.nn.softmax(lse.astype(jnp.float32), axis=1)
    return jnp.einsum('npr,nprd->nrd', w, o.astype(jnp.float32))


def split_heads(t, h):
    b, s, _ = t.shape
    return t.reshape(b, s, h, HEAD_DIM).transpose(0, 2, 1, 3)


def merge_heads(t):
    b, h, s, d = t.shape
    return t.transpose(0, 2, 1, 3).reshape(b, s, h * d)


def dispatch_attention(q_src, pos_src, gid, k_groups, v_groups, kstart, bias_tbl, tbl_idx):
    n_src, n_slots = gid.shape
    n_groups, blk, _ = k_groups.shape
    R = q_src.shape[1]
    N = n_src * n_slots
    flat_g = gid.reshape(N)
    order = jnp.argsort(flat_g).astype(jnp.int32)
    sorted_g = flat_g[order]
    valid = sorted_g < n_groups
    sg = jnp.minimum(sorted_g, n_groups - 1)
    counts = jnp.zeros((n_groups + 1,), jnp.int32).at[flat_g].add(1)[:n_groups]
    padded = (counts + ROW_CHUNK - 1) // ROW_CHUNK * ROW_CHUNK
    pad_end = jnp.cumsum(padded)
    pad_start = pad_end - padded
    start = jnp.cumsum(counts) - counts
    n_chunks = -(-N // ROW_CHUNK) + n_groups
    P = n_chunks * ROW_CHUNK
    dest = jnp.where(valid, pad_start[sg] + jnp.arange(N) - start[sg], P)
    buf = jnp.full((P,), N, jnp.int32).at[dest].set(order, mode='drop')
    chunk_group = jnp.minimum(jnp.searchsorted(pad_end, jnp.arange(n_chunks) * ROW_CHUNK, side='right'),
                              n_groups - 1).astype(jnp.int32)
    q_pad = jnp.concatenate([q_src, jnp.zeros((1, R, HEAD_DIM), q_src.dtype)], axis=0)
    pos_pad = jnp.concatenate([pos_src, jnp.full((1,), -1, pos_src.dtype)], axis=0)
    off = jnp.arange(blk)

    def chunk(args):
        rows, g = args
        src = rows // n_slots
        qi = q_pad[src]
        dist = pos_pad[src][:, None] - (kstart[g] + off)[None, :]
        bias = bias_tbl[tbl_idx[g]][:, rel_bucket(dist)].astype(jnp.float32)
        logits = jnp.einsum('crd,kd->rck', qi, k_groups[g]).astype(jnp.float32) * ATTN_SCALE + bias
        p, lse = masked_softmax(logits, (dist >= 0)[None])
        o = jnp.einsum('rck,kd->crd', p, v_groups[g].astype(jnp.float32))
        return o, lse[..., 0].T

    o, lse = lax.map(chunk, (buf.reshape(n_chunks, ROW_CHUNK), chunk_group))
    o_all = jnp.zeros((N + 1, R, HEAD_DIM), jnp.float32).at[buf].set(o.reshape(P, R, HEAD_DIM))[:N]
    lse_all = jnp.full((N + 1, R), NEG_INF, jnp.float32).at[buf].set(lse.reshape(P, R))[:N]
    return o_all.reshape(n_src, n_slots, R, HEAD_DIM), lse_all.reshape(n_src, n_slots, R)


def dilated_group(q, k, v, table, window, dilation):
    Bsz, H, S_, _ = q.shape
    L = S_ // dilation
    nb = -(-L // Q_BLOCK)
    Lp = nb * Q_BLOCK
    span = window // dilation
    n_prev = -(-span // Q_BLOCK)

    def streams(t):
        t = t.reshape(Bsz, H, L, dilation, HEAD_DIM).transpose(0, 1, 3, 2, 4)
        t = jnp.pad(t, ((0, 0), (0, 0), (0, 0), (0, Lp - L), (0, 0)))
        return t.reshape(Bsz, H, dilation, nb, Q_BLOCK, HEAD_DIM)

    def band(t):
        tp = jnp.pad(t, ((0, 0), (0, 0), (0, 0), (n_prev, 0), (0, 0), (0, 0)))
        return jnp.concatenate([tp[:, :, :, o:o + nb] for o in range(n_prev + 1)], axis=4)

    qs = streams(q)
    kb, vb = band(streams(k)), band(streams(v))
    a = jnp.arange(Q_BLOCK)
    c = jnp.arange((n_prev + 1) * Q_BLOCK)
    steps = n_prev * Q_BLOCK + a[:, None] - c[None, :]
    key_idx = jnp.arange(nb)[:, None] * Q_BLOCK - n_prev * Q_BLOCK + c[None, :]
    mask = ((steps >= 0) & (steps <= span))[None] & (key_idx >= 0)[:, None, :]
    bias = rel_bias(table, steps * dilation)
    logits = jnp.einsum('bhrnqd,bhrnkd->bhrnqk', qs, kb).astype(jnp.float32) * ATTN_SCALE + bias[:, None, None]
    p, lse = masked_softmax(logits, mask)
    o = jnp.einsum('bhrnqk,bhrnkd->bhrnqd', p.astype(v.dtype), vb)
    o = o.reshape(Bsz, H, dilation, Lp, HEAD_DIM)[:, :, :, :L].transpose(0, 1, 3, 2, 4).reshape(Bsz, H, S_, HEAD_DIM)
    lse = lse[..., 0].reshape(Bsz, H, dilation, Lp)[..., :L].transpose(0, 1, 3, 2).reshape(Bsz, H, S_)
    return o, lse


def mixer_dilated(x, w_in, w_out, table):
    proj = x @ w_in
    gw = 3 * ATTN_WIDTH
    outs, lses = [], []
    for g, (window, dilation) in enumerate(DILATED_PAIRS):
        q, k, v = [split_heads(t, N_HEADS) for t in jnp.split(proj[..., g * gw:(g + 1) * gw], 3, axis=-1)]
        o, lse = dilated_group(q, k, v, table, window, dilation)
        outs.append(o)
        lses.append(lse)
    wts = jax.nn.softmax(jnp.stack(lses), axis=0)
    o = jnp.einsum('gbhs,gbhsd->bhsd', wts, jnp.stack(outs).astype(jnp.float32))
    return merge_heads(o.astype(x.dtype)) @ w_out


def mixer_stick_breaking(x, w_in, w_out):
    q, k, v = [split_heads(t, N_HEADS) for t in jnp.split(x @ w_in, 3, axis=-1)]
    Bsz, H, S_, _ = q.shape
    nb = S_ // Q_BLOCK
    nbp = nb + nb % 2
    pad = ((0, 0), (0, 0), (0, (nbp - nb) * Q_BLOCK), (0, 0))

    def blocks(t):
        return jnp.pad(t, pad).reshape(Bsz, H, nbp, Q_BLOCK, HEAD_DIM)

    qb, kb, vb = blocks(q * ATTN_SCALE), blocks(k), blocks(v)
    a = jnp.arange(Q_BLOCK)
    strict = a[None, :] < a[:, None]
    suffix = (a[:, None] >= a[None, :]).astype(jnp.float32)

    z = jnp.clip(jnp.einsum('bhnqd,bhnkd->bhnqk', qb, kb).astype(jnp.float32), -SB_CLIP, SB_CLIP)
    sp = jnp.where(strict, jnp.log1p(jnp.exp(z)), 0.0)
    r_in = jnp.einsum('bhnqj,js->bhnqs', sp, suffix)
    att = jnp.where(strict, jnp.exp(z - r_in), 0.0)
    acc_d = jnp.einsum('bhnqs,bhnsd->bhnqd', att, vb.astype(jnp.float32))
    r_d = r_in[..., 0]

    def pair(p):
        qb2 = nbp - 1 - p
        init_r2 = lax.dynamic_index_in_dim(r_d, qb2, axis=2, keepdims=False)
        init_a2 = lax.dynamic_index_in_dim(acc_d, qb2, axis=2, keepdims=False)

        def step(carry, kstep):
            r_sum, acc, acc_first = carry
            start2 = kstep == p
            acc_first = jnp.where(start2, acc, acc_first)
            r_sum = jnp.where(start2, init_r2, r_sum)
            acc = jnp.where(start2, init_a2, acc)
            first = kstep < p
            qi = jnp.where(first, p, qb2)
            kj = jnp.where(first, p - 1 - kstep, nbp - 2 - kstep)
            qblk = lax.dynamic_index_in_dim(qb, qi, axis=2, keepdims=False)
            kblk = lax.dynamic_index_in_dim(kb, kj, axis=2, keepdims=False)
            vblk = lax.dynamic_index_in_dim(vb, kj, axis=2, keepdims=False)
            zz = jnp.clip(jnp.einsum('bhqd,bhkd->bhqk', qblk, kblk).astype(jnp.float32), -SB_CLIP, SB_CLIP)
            rr = jnp.einsum('bhqj,js->bhqs', jnp.log1p(jnp.exp(zz)), suffix)
            pv = jnp.einsum('bhqs,bhsd->bhqd', jnp.exp(zz - rr), vblk.astype(jnp.float32))
            acc = acc + jnp.exp(-r_sum)[..., None] * pv
            r_sum = r_sum + rr[..., 0]
            return (r_sum, acc, acc_first), None

        init = (lax.dynamic_index_in_dim(r_d, p, axis=2, keepdims=False),
                lax.dynamic_index_in_dim(acc_d, p, axis=2, keepdims=False),
                jnp.zeros((Bsz, H, Q_BLOCK, HEAD_DIM), jnp.float32))
        (_, acc_b, acc_a), _ = lax.scan(step, init, jnp.arange(nbp - 1))
        return acc_a, acc_b

    out_a, out_b = lax.map(pair, jnp.arange(nbp // 2))
    o = jnp.concatenate([out_a, out_b[::-1]], axis=0)
    o = o.transpose(1, 2, 0, 3, 4).reshape(Bsz, H, nbp * Q_BLOCK, HEAD_DIM)[:, :, :S_]
    return merge_heads(o.astype(x.dtype)) @ w_out


def compress_blocks(t, pos, w1, w2):
    Bsz, G, S_, _ = t.shape
    halves = t.reshape(Bsz, G, S_ // NSA_CMP_STRIDE, NSA_CMP_STRIDE, HEAD_DIM)
    blocks = jnp.concatenate([halves[:, :, :-1], halves[:, :, 1:]], axis=3) + pos
    n_cmp = blocks.shape[2]
    flat = blocks.reshape(Bsz, G, n_cmp, NSA_CMP_LEN * HEAD_DIM)
    return jax.nn.gelu(flat @ w1) @ w2


def mixer_nsa(x, w_in, w_out, table, pos_k, w1_k, w2_k, pos_v, w1_v, w2_v):
    Bsz, S_, _ = x.shape
    G, R = NSA_KV_HEADS, NSA_GROUP
    proj = x @ w_in
    q = proj[..., :ATTN_WIDTH].reshape(Bsz, S_, G, R, HEAD_DIM).transpose(0, 2, 3, 1, 4)
    kv = jnp.split(proj[..., ATTN_WIDTH:ATTN_WIDTH + 6 * NSA_KV_WIDTH], 6, axis=-1)
    k_cmp, v_cmp, k_sel, v_sel, k_win, v_win = [split_heads(t, G) for t in kv]
    gates = jax.nn.sigmoid(proj[..., ATTN_WIDTH + 6 * NSA_KV_WIDTH:].astype(jnp.float32))
    gates = gates.reshape(Bsz, S_, 3, G, R).transpose(2, 0, 3, 4, 1)

    kc = compress_blocks(k_cmp, pos_k, w1_k, w2_k)
    vc = compress_blocks(v_cmp, pos_v, w1_v, w2_v)
    n_cmp = kc.shape[2]
    cmp_end = jnp.arange(n_cmp) * NSA_CMP_STRIDE + NSA_CMP_LEN - 1
    n_sel_blocks = S_ // NSA_SEL_BLOCK
    n_top = min(NSA_TOP_N, n_sel_blocks)
    offs = np.arange(1 - NSA_CMP_LEN // NSA_CMP_STRIDE, NSA_SEL_BLOCK // NSA_CMP_STRIDE)
    ov = np.arange(n_sel_blocks)[:, None] * (NSA_SEL_BLOCK // NSA_CMP_STRIDE) + offs[None]
    ov_valid = jnp.asarray((ov >= 0) & (ov < n_cmp))
    ov_idx = jnp.asarray(np.clip(ov, 0, n_cmp - 1))
    kwin = jnp.pad(k_win, ((0, 0), (0, 0), (NSA_WINDOW, 0), (0, 0)))
    vwin = jnp.pad(v_win, ((0, 0), (0, 0), (NSA_WINDOW, 0), (0, 0)))
    table_gr = table.T.reshape(G, R, REL_BUCKETS)
    nb = S_ // Q_BLOCK
    win_len = NSA_WINDOW + Q_BLOCK
    a = jnp.arange(Q_BLOCK)
    win_steps = NSA_WINDOW + a[:, None] - jnp.arange(win_len)[None, :]
    win_bias = rel_bias(table, win_steps).reshape(G, R, Q_BLOCK, win_len)
    sel_j = jnp.arange(n_sel_blocks)
    qb = q.reshape(Bsz, G, R, nb, Q_BLOCK, HEAD_DIM).transpose(3, 0, 1, 2, 4, 5)

    def block(args):
        qi, i = args
        qpos = i * Q_BLOCK + a
        cdist = qpos[:, None] - cmp_end[None, :]
        lc = jnp.einsum('bgrqd,bgcd->bgrqc', qi, kc).astype(jnp.float32) * ATTN_SCALE
        lc = lc + rel_bias(table, cdist).reshape(G, R, Q_BLOCK, n_cmp)
        pc, _ = masked_softmax(lc, cdist >= 0)
        o_cmp = jnp.einsum('bgrqc,bgcd->bgrqd', pc, vc.astype(jnp.float32))
        imp = (pc.sum(2)[..., ov_idx] * ov_valid).sum(-1)
        cur = qpos // NSA_SEL_BLOCK
        forced = (sel_j[None] == 0) | (sel_j[None] == cur[:, None]) | (sel_j[None] == cur[:, None] - 1)
        causal = sel_j[None] * NSA_SEL_BLOCK <= qpos[:, None]
        score = jnp.where(forced, FORCED_SCORE, jnp.where(causal, imp, NEG_INF))
        sel = lax.top_k(score, n_top)[1]
        kw = lax.dynamic_slice_in_dim(kwin, i * Q_BLOCK, win_len, axis=2)
        vw = lax.dynamic_slice_in_dim(vwin, i * Q_BLOCK, win_len, axis=2)
        wmask = (win_steps < NSA_WINDOW) & (win_steps >= 0) & (qpos[:, None] - win_steps >= 0)
        lw = jnp.einsum('bgrqd,bgkd->bgrqk', qi, kw).astype(jnp.float32) * ATTN_SCALE + win_bias
        pw, _ = masked_softmax(lw, wmask)
        o_win = jnp.einsum('bgrqk,bgkd->bgrqd', pw, vw.astype(jnp.float32))
        return o_cmp, o_win, sel

    o_cmp, o_win, sel = lax.map(block, (qb, jnp.arange(nb)))

    def unblock(t):
        return t.transpose(1, 2, 3, 0, 4, 5).reshape(Bsz, G, R, S_, HEAD_DIM)

    o_cmp, o_win = unblock(o_cmp), unblock(o_win)
    sel = sel.transpose(1, 2, 0, 3, 4).reshape(Bsz, G, S_, n_top)
    qpos_all = jnp.arange(S_)
    valid = sel * NSA_SEL_BLOCK <= qpos_all[:, None]
    n_groups = Bsz * G * n_sel_blocks
    bg = jnp.arange(Bsz * G).reshape(Bsz, G, 1, 1)
    gid = jnp.where(valid, bg * n_sel_blocks + sel, n_groups).reshape(Bsz * G * S_, n_top)
    q_src = q.transpose(0, 1, 3, 2, 4).reshape(Bsz * G * S_, R, HEAD_DIM)
    o_s, lse_s = dispatch_attention(
        q_src, jnp.tile(qpos_all, Bsz * G), gid,
        k_sel.reshape(n_groups, NSA_SEL_BLOCK, HEAD_DIM), v_sel.reshape(n_groups, NSA_SEL_BLOCK, HEAD_DIM),
        jnp.tile(jnp.arange(n_sel_blocks) * NSA_SEL_BLOCK, Bsz * G), table_gr,
        (jnp.arange(n_groups) // n_sel_blocks) % G)
    o_sel = merge_parts(o_s, lse_s).reshape(Bsz, G, S_, R, HEAD_DIM).transpose(0, 1, 3, 2, 4)
    out = gates[0][..., None] * o_cmp + gates[1][..., None] * o_sel + gates[2][..., None] * o_win
    return out.transpose(0, 3, 1, 2, 4).reshape(Bsz, S_, ATTN_WIDTH).astype(x.dtype) @ w_out


def mixer_moba(x, w_in, w_out, table):
    Bsz, S_, _ = x.shape
    H = N_HEADS
    q, k, v = [split_heads(t, H) for t in jnp.split(x @ w_in, 3, axis=-1)]
    nblk = -(-S_ // MOBA_BLOCK)
    Sp = nblk * MOBA_BLOCK
    pad = ((0, 0), (0, 0), (0, Sp - S_), (0, 0))
    qblk = jnp.pad(q, pad).reshape(Bsz, H, nblk, MOBA_BLOCK, HEAD_DIM)
    kblk = jnp.pad(k, pad).reshape(Bsz, H, nblk, MOBA_BLOCK, HEAD_DIM)
    vblk = jnp.pad(v, pad).reshape(Bsz, H, nblk, MOBA_BLOCK, HEAD_DIM)
    c = jnp.arange(MOBA_BLOCK)
    own_dist = c[:, None] - c[None, :]
    lo = jnp.einsum('bhnqd,bhnkd->bhnqk', qblk, kblk).astype(jnp.float32) * ATTN_SCALE + rel_bias(table, own_dist)[:, None]
    p_own, lse_own = masked_softmax(lo, own_dist >= 0)
    o_own = jnp.einsum('bhnqk,bhnkd->bhnqd', p_own, vblk.astype(jnp.float32)).reshape(Bsz, H, Sp, HEAD_DIM)[:, :, :S_]
    lse_own = lse_own[..., 0].reshape(Bsz, H, Sp)[:, :, :S_]
    kmean = kblk.mean(axis=3)
    n_top = max(1, min(MOBA_TOP_K, nblk - 1))
    cur = jnp.arange(S_) // MOBA_BLOCK
    gate = jnp.einsum('bhsd,bhnd->bhsn', q, kmean).astype(jnp.float32)
    gate = jnp.where(jnp.arange(nblk)[None, :] < cur[:, None], gate, NEG_INF)
    sel = lax.top_k(gate, n_top)[1]
    valid = sel < cur[:, None]
    n_groups = Bsz * H * nblk
    bh = jnp.arange(Bsz * H).reshape(Bsz, H, 1, 1)
    gid = jnp.where(valid, bh * nblk + sel, n_groups).reshape(Bsz * H * S_, n_top)
    o_sel, lse_sel = dispatch_attention(
        q.reshape(Bsz * H * S_, 1, HEAD_DIM), jnp.tile(jnp.arange(S_), Bsz * H), gid,
        kblk.reshape(n_groups, MOBA_BLOCK, HEAD_DIM), vblk.reshape(n_groups, MOBA_BLOCK, HEAD_DIM),
        jnp.tile(jnp.arange(nblk) * MOBA_BLOCK, Bsz * H), table.T[:, None, :],
        (jnp.arange(n_groups) // nblk) % H)
    o_all = jnp.concatenate([o_own.reshape(Bsz * H * S_, 1, 1, HEAD_DIM), o_sel], axis=1)
    lse_all = jnp.concatenate([lse_own.reshape(Bsz * H * S_, 1, 1), lse_sel], axis=1)
    o = merge_parts(o_all, lse_all).reshape(Bsz, H, S_, HEAD_DIM)
    return merge_heads(o.astype(x.dtype)) @ w_out


def moe_ffn(x, router_w, router_b, w_gate, w_up, w_down):
    Bsz, S_, D_ = x.shape
    T = Bsz * S_
    xt = x.reshape(T, D_)
    scores = jax.nn.sigmoid((xt @ router_w).astype(jnp.float32))
    biased = (scores + router_b.astype(jnp.float32)).reshape(T, N_GROUPS, EXPERTS_PER_GROUP)
    group_score = lax.top_k(biased, MOE_TOP_K)[0].sum(-1)
    best_group = jnp.argmax(group_score, axis=-1).astype(jnp.int32)
    in_group = jnp.take_along_axis(biased, best_group[:, None, None], axis=1)[:, 0]
    local = lax.top_k(in_group, MOE_TOP_K)[1]
    expert_idx = best_group[:, None] * EXPERTS_PER_GROUP + local
    gate = jnp.take_along_axis(scores, expert_idx, axis=1)
    gate = gate / gate.sum(-1, keepdims=True)
    n_assign = T * MOE_TOP_K
    flat_e = expert_idx.reshape(n_assign)
    order = jnp.argsort(flat_e)
    sorted_e = flat_e[order]
    sorted_tok = (order // MOE_TOP_K).astype(jnp.int32)
    sorted_w = gate.reshape(n_assign)[order]
    counts = jnp.zeros((N_EXPERTS,), jnp.int32).at[flat_e].add(1)
    padded = (counts + MOE_BLOCK - 1) // MOE_BLOCK * MOE_BLOCK
    pad_end = jnp.cumsum(padded)
    pad_start = pad_end - padded
    start = jnp.cumsum(counts) - counts
    dest = pad_start[sorted_e] + jnp.arange(n_assign) - start[sorted_e]
    n_blocks = -(-n_assign // MOE_BLOCK) + N_EXPERTS
    P = n_blocks * MOE_BLOCK
    buf_tok = jnp.full((P,), T, jnp.int32).at[dest].set(sorted_tok)
    buf_w = jnp.zeros((P,), x.dtype).at[dest].set(sorted_w.astype(x.dtype))
    block_expert = jnp.minimum(jnp.searchsorted(pad_end, jnp.arange(n_blocks) * MOE_BLOCK, side='right'), N_EXPERTS - 1)
    x_pad = jnp.concatenate([xt, jnp.zeros((1, D_), xt.dtype)], axis=0)
    xb = x_pad[buf_tok].reshape(n_blocks, MOE_BLOCK, D_)

    def expert_block(args):
        xi, e = args
        return (jax.nn.silu(xi @ w_gate[e]) * (xi @ w_up[e])) @ w_down[e]

    yb = lax.map(expert_block, (xb, block_expert)).reshape(P, D_) * buf_w[:, None]
    y = jnp.zeros((T + 1, D_), yb.dtype).at[buf_tok].add(yb)[:T]
    return y.reshape(Bsz, S_, D_)


def setup_inputs(seed: int = 0) -> dict:
    key = jax.random.key(seed)
    ks = jax.random.split(key, 26)
    n_occ = [len(range(m, DEPTH, N_MIXERS)) for m in range(N_MIXERS)]
    beta = DEEPNORM_BETA

    def normal(k, shape, scale):
        return jax.random.normal(k, shape, jnp.float32) * scale

    def col_scale(pieces):
        return jnp.asarray(np.concatenate([np.full((w,), s, np.float32) for w, s in pieces]))

    qkv_pieces = [(ATTN_WIDTH, 1.0), (ATTN_WIDTH, 1.0), (ATTN_WIDTH, beta)]
    dil_scale = col_scale(qkv_pieces * len(DILATED_PAIRS))
    qkv_scale = col_scale(qkv_pieces)
    nsa_scale = col_scale([(ATTN_WIDTH, 1.0)] + [(NSA_KV_WIDTH, 1.0), (NSA_KV_WIDTH, beta)] * 3 + [(3 * N_HEADS, 1.0)])
    din = D_MODEL ** -0.5
    out_scale = ATTN_WIDTH ** -0.5 * beta
    cmp_in = NSA_CMP_LEN * HEAD_DIM
    return {
        'x': normal(ks[0], (BATCH, SEQ, D_MODEL), 1.0),
        'rel_table': normal(ks[1], (REL_BUCKETS, N_HEADS), 0.1),
        'router_w': normal(ks[2], (D_MODEL, N_EXPERTS), din),
        'router_b': normal(ks[3], (N_EXPERTS,), 0.01),
        'ln1_g': 1.0 + normal(ks[4], (DEPTH, D_MODEL), 0.01),
        'ln1_b': normal(ks[5], (DEPTH, D_MODEL), 0.01),
        'ln2_g': 1.0 + normal(ks[6], (DEPTH, D_MODEL), 0.01),
        'ln2_b': normal(ks[7], (DEPTH, D_MODEL), 0.01),
        'exp_w_gate': normal(ks[8], (DEPTH, N_EXPERTS, D_MODEL, D_EXPERT), din),
        'exp_w_up': normal(ks[9], (DEPTH, N_EXPERTS, D_MODEL, D_EXPERT), din),
        'exp_w_down': normal(ks[10], (DEPTH, N_EXPERTS, D_EXPERT, D_MODEL), D_EXPERT ** -0.5 * beta),
        'dil_w_in': normal(ks[11], (n_occ[0], D_MODEL, 3 * len(DILATED_PAIRS) * ATTN_WIDTH), din) * dil_scale,
        'dil_w_out': normal(ks[12], (n_occ[0], ATTN_WIDTH, D_MODEL), out_scale),
        'sb_w_in': normal(ks[13], (n_occ[1], D_MODEL, 3 * ATTN_WIDTH), din) * qkv_scale,
        'sb_w_out': normal(ks[14], (n_occ[1], ATTN_WIDTH, D_MODEL), out_scale),
        'nsa_w_in': normal(ks[15], (n_occ[2], D_MODEL, ATTN_WIDTH + 6 * NSA_KV_WIDTH + 3 * N_HEADS), din) * nsa_scale,
        'nsa_w_out': normal(ks[16], (n_occ[2], ATTN_WIDTH, D_MODEL), out_scale),
        'nsa_cmp_pos_k': normal(ks[17], (n_occ[2], NSA_CMP_LEN, HEAD_DIM), 0.1),
        'nsa_cmp_w1_k': normal(ks[18], (n_occ[2], cmp_in, NSA_CMP_HIDDEN), cmp_in ** -0.5),
        'nsa_cmp_w2_k': normal(ks[19], (n_occ[2], NSA_CMP_HIDDEN, HEAD_DIM), NSA_CMP_HIDDEN ** -0.5),
        'nsa_cmp_pos_v': normal(ks[20], (n_occ[2], NSA_CMP_LEN, HEAD_DIM), 0.1),
        'nsa_cmp_w1_v': normal(ks[21], (n_occ[2], cmp_in, NSA_CMP_HIDDEN), cmp_in ** -0.5),
        'nsa_cmp_w2_v': normal(ks[22], (n_occ[2], NSA_CMP_HIDDEN, HEAD_DIM), NSA_CMP_HIDDEN ** -0.5),
        'moba_w_in': normal(ks[23], (n_occ[3], D_MODEL, 3 * ATTN_WIDTH), din) * qkv_scale,
        'moba_w_out': normal(ks[24], (n_occ[3], ATTN_WIDTH, D_MODEL), out_scale),
    }


def reference(x, rel_table, router_w, router_b, ln1_g, ln1_b, ln2_g, ln2_b,
              exp_w_gate, exp_w_up, exp_w_down, dil_w_in, dil_w_out, sb_w_in, sb_w_out,
              nsa_w_in, nsa_w_out, nsa_cmp_pos_k, nsa_cmp_w1_k, nsa_cmp_w2_k,
              nsa_cmp_pos_v, nsa_cmp_w1_v, nsa_cmp_w2_v, moba_w_in, moba_w_out):
    for layer in range(DEPTH):
        kind, occ = layer % N_MIXERS, layer // N_MIXERS
        if kind == 0:
            mixed = mixer_dilated(x, dil_w_in[occ], dil_w_out[occ], rel_table)
        elif kind == 1:
            mixed = mixer_stick_breaking(x, sb_w_in[occ], sb_w_out[occ])
        elif kind == 2:
            mixed = mixer_nsa(x, nsa_w_in[occ], nsa_w_out[occ], rel_table,
                              nsa_cmp_pos_k[occ], nsa_cmp_w1_k[occ], nsa_cmp_w2_k[occ],
                              nsa_cmp_pos_v[occ], nsa_cmp_w1_v[occ], nsa_cmp_w2_v[occ])
        else:
            mixed = mixer_moba(x, moba_w_in[occ], moba_w_out[occ], rel_table)
        x = layer_norm(DEEPNORM_ALPHA * x + mixed, ln1_g[layer], ln1_b[layer])
        ffn = moe_ffn(x, router_w, router_b, exp_w_gate[layer], exp_w_up[layer], exp_w_down[layer])
        x = layer_norm(DEEPNORM_ALPHA * x + ffn, ln2_g[layer], ln2_b[layer])
    return x
```

```python
import contextlib
import numpy as np
import concourse.bass as bass
import concourse.mybir as mybir
from concourse.bass_utils import run_bass_kernel_spmd

F32 = mybir.dt.float32
BF16 = mybir.dt.bfloat16
I32 = mybir.dt.int32
AF = mybir.ActivationFunctionType
ALU = mybir.AluOpType
AX = mybir.AxisListType

ENGS = ("pe", "act", "dve", "pool", "sp")
NDSEM = 8


class Sched:
    def __init__(self, nc):
        self.nc = nc
        self.ops = {e: [] for e in ENGS}
        self.lastw = {}
        self.readers = {}
        self.ndma = {e: 0 for e in ENGS}
        self.waited = {e: {} for e in ENGS}

    def _add(self, eng, fn, reads, writes, dma):
        idx = len(self.ops[eng])
        deps = set()
        for b in reads:
            if b in self.lastw:
                deps.add(self.lastw[b])
        for b in writes:
            if b in self.lastw:
                deps.add(self.lastw[b])
            for r in self.readers.get(b, ()):
                deps.add(r)
        op = dict(fn=fn, waits=[], dma=None, sig=False)
        me = (eng, idx)
        if dma:
            k = self.ndma[eng]
            self.ndma[eng] += 1
            slot, val = k % NDSEM, 16 * (k // NDSEM + 1)
            op["dma"] = (slot, val)
            me = ("dma", eng, slot, val)
            if k >= NDSEM:
                deps.add(("dma", eng, slot, val - 16))
        best = {}
        for d in deps:
            if d[0] == "dma":
                key = ("dma", d[1], d[2]); v = d[3]
            else:
                key = d[0]; v = d[1]
                if key == eng and eng == "pe" and not dma:
                    continue
            if v > best.get(key, -1):
                best[key] = v
        w = self.waited[eng]
        for key, v in best.items():
            if w.get(key, -1) >= v:
                continue
            w[key] = v
            op["waits"].append((key, v))
            if key not in ("dma",) and not isinstance(key, tuple):
                self.ops[key][v]["sig"] = True
        self.ops[eng].append(op)
        for b in reads:
            self.readers.setdefault(b, []).append(me)
        for b in writes:
            self.lastw[b] = me
            self.readers[b] = []
        return me

    def op(self, eng, fn, reads=(), writes=()):
        return self._add(eng, fn, tuple(reads), tuple(writes), False)

    def dma(self, eng, fn, reads=(), writes=()):
        return self._add(eng, fn, tuple(reads), tuple(writes), True)

    def fence(self):
        targets = []
        for e in ENGS:
            n = len(self.ops[e])
            for i in range(n - 1, -1, -1):
                o = self.ops[e][i]
                if o["fn"] is not None and o["dma"] is None:
                    targets.append((e, i))
                    break
            k = self.ndma[e]
            for slot in range(min(k, NDSEM)):
                cnt = (k - 1 - slot) // NDSEM + 1
                targets.append((("dma", e, slot), 16 * cnt))
        for e in ENGS:
            w = self.waited[e]
            waits = []
            for key, v in targets:
                if key == e and not isinstance(key, tuple):
                    continue
                if w.get(key, -1) >= v:
                    continue
                w[key] = v
                waits.append((key, v))
                if not isinstance(key, tuple):
                    self.ops[key][v]["sig"] = True
            if waits:
                self.ops[e].append(dict(fn=None, waits=waits, dma=None, sig=False))

    def emit(self, final_engine="sp"):
        nc = self.nc
        fin_waits = []
        for e in ENGS:
            n = self.ndma[e]
            for slot in range(min(n, NDSEM)):
                cnt = (n - 1 - slot) // NDSEM + 1
                fin_waits.append((("dma", e, slot), 16 * cnt))
        self.ops[final_engine].append(dict(fn=None, waits=fin_waits, dma=None, sig=False))
        sigcnt = {}
        for e in ENGS:
            c = 0
            arr = []
            for o in self.ops[e]:
                if o["sig"]:
                    c += 1
                arr.append(c)
            sigcnt[e] = arr
        import contextlib
        with contextlib.ExitStack() as st:
            esem = {e: st.enter_context(nc.semaphore(f"s_{e}")) for e in ENGS}
            dsem = {e: [st.enter_context(nc.semaphore(f"d_{e}{i}")) for i in range(NDSEM)]
                    for e in ENGS if self.ndma[e] > 0}
            block = st.enter_context(nc.Block())

            def run(e, eng):
                for i, o in enumerate(self.ops[e]):
                    for key, v in o["waits"]:
                        if isinstance(key, tuple):
                            eng.wait_ge(dsem[key[1]][key[2]], v)
                        else:
                            eng.wait_ge(esem[key], sigcnt[key][v])
                    if o["fn"] is None:
                        continue
                    ins = o["fn"](eng)
                    if o["dma"] is not None:
                        ins.then_inc(dsem[e][o["dma"][0]], 16)
                    elif o["sig"]:
                        ins.then_inc(esem[e], 1)

            if self.ops["pe"]:
                @block.tensor
                def _(eng):
                    run("pe", eng)
            if self.ops["act"]:
                @block.scalar
                def _(eng):
                    run("act", eng)
            if self.ops["dve"]:
                @block.vector
                def _(eng):
                    run("dve", eng)
            if self.ops["pool"]:
                @block.gpsimd
                def _(eng):
                    run("pool", eng)
            if self.ops["sp"]:
                @block.sync
                def _(eng):
                    run("sp", eng)


D = 1024
HD = 64
SCALE = 0.125
NEG = -30000.0


class KB:
    def __init__(self):
        self.nc = bass.Bass("TRN2", target_bir_lowering=False)
        self.S = Sched(self.nc)
        self.st = contextlib.ExitStack()
        self._n = 0

    def din(self, name, shape, dt=F32):
        return self.nc.dram_tensor(name, list(shape), dt, kind="ExternalInput").ap()

    def dout(self, name, shape, dt=F32):
        return self.nc.dram_tensor(name, list(shape), dt, kind="ExternalOutput").ap()

    def sb(self, name, shape, dt, st=None):
        t = (st or self.st).enter_context(self.nc.sbuf_tensor(name, list(shape), dt))
        return t

    def ps(self, name, shape=(128, 512), dt=F32):
        return self.st.enter_context(self.nc.psum_tensor(name, list(shape), dt))

    def finish(self):
        self.S.emit()
        self.st.close()
        return self.nc


def pipeline(units, stages, skews):
    n = len(units)
    if n == 0:
        return
    for t in range(n + max(skews)):
        for s, fn in enumerate(stages):
            u = t - skews[s]
            if 0 <= u < n:
                fn(u, units[u])


def load_w_bf16(kb, name, w_ap, ncols, stg, stg_key):
    S = kb.S
    wb = kb.sb(name, [128, 8, ncols], BF16)
    S.dma("sp", lambda e: e.dma_start(out=stg[:, 0:8 * ncols].rearrange("p (k n) -> p k n", k=8),
                                      in_=w_ap.rearrange("(k p) n -> p k n", p=128)), writes=[stg_key])
    S.op("pool", lambda e: e.tensor_copy(out=wb[:].rearrange("p k n -> p (k n)"), in_=stg[:, 0:8 * ncols]),
         reads=[stg_key], writes=[name])
    return wb


def project(kb, xT, Sq, fm_outs, tm_outs, pss, xs, xb):
    S = kb.S
    xv = xT.rearrange("(k p) t -> p k t", p=128)
    NC = Sq // 512
    pi = 0
    for c in range(NC):
        xsi = xs[c % len(xs)]
        if isinstance(xsi, tuple):
            xsi, xsk = xsi
        else:
            xsk = f"xs{c % len(xs)}"
        xbi = xb[c % len(xb)]
        if isinstance(xbi, tuple):
            xbi, xbk = xbi
        else:
            xbk = f"xb{c % len(xb)}"
        S.dma("sp", lambda e, xsi=xsi, c=c: e.dma_start(out=xsi[:], in_=xv[:, :, c * 512:(c + 1) * 512]), writes=[xsk])
        S.op("dve", lambda e, xsi=xsi, xbi=xbi: e.tensor_copy(out=xbi[:, 0:4, :], in_=xsi[:, 0:4, :]), reads=[xsk], writes=[xbk])
        S.op("act", lambda e, xsi=xsi, xbi=xbi: e.copy(out=xbi[:, 4:8, :], in_=xsi[:, 4:8, :]), reads=[xsk], writes=[xbk])
        for (w, wkey, M, dst, dkey, scale, ev) in fm_outs:
            p, pk = pss[pi % len(pss)]
            pi += 1
            for k in range(8):
                S.op("pe", lambda e, p=p, w=w, k=k, M=M, xbi=xbi: e.matmul(p[0:M, :], lhsT=w[:, k, 0:M], rhs=xbi[:, k, :],
                                                                          start=(k == 0), stop=(k == 7)),
                     reads=[wkey, xbk], writes=[pk])
            if ev == "act":
                S.op("act", lambda e, p=p, M=M, dst=dst, c=c, scale=scale: e.mul(dst[0:M, c * 512:(c + 1) * 512], p[0:M, :], scale),
                     reads=[pk], writes=[dkey])
            else:
                S.op("dve", lambda e, p=p, M=M, dst=dst, c=c, scale=scale: e.tensor_scalar(
                    out=dst[0:M, c * 512:(c + 1) * 512], in0=p[0:M, :], scalar1=scale, scalar2=None, op0=ALU.mult),
                    reads=[pk], writes=[dkey])
        for (w, wkey, N, dst_fn, dkey) in tm_outs:
            p, pk = pss[pi % len(pss)]
            pi += 1
            for tt in range(4):
                for k in range(8):
                    S.op("pe", lambda e, p=p, w=w, k=k, N=N, tt=tt, xbi=xbi: e.matmul(
                        p[:, tt * N:(tt + 1) * N], lhsT=xbi[:, k, tt * 128:(tt + 1) * 128], rhs=w[:, k, 0:N],
                        start=(k == 0), stop=(k == 7)), reads=[wkey, xbk], writes=[pk])
            for tt in range(4):
                for (dap, lo, hi) in dst_fn(c * 4 + tt):
                    S.op("dve", lambda e, p=p, N=N, tt=tt, dap=dap, lo=lo, hi=hi: e.tensor_copy(out=dap, in_=p[:, tt * N + lo:tt * N + hi]),
                         reads=[pk], writes=[dkey])


def build_sb(Sq):
    kb = KB()
    S = kb.S
    NT, NCH = Sq // 128, Sq // 512
    xT = kb.din("xT", [D, Sq])
    wq = kb.din("wq", [D, 256])
    wk = kb.din("wk", [D, 256])
    wv = kb.din("wv", [D, 256])
    cm = kb.din("cm", [128, 4 * 512 + 256])
    oT = kb.dout("oT", [256, Sq], BF16)

    stg = kb.sb("stg", [128, 8 * 256], F32)
    cmf = kb.sb("cmf", [128, 4 * 512 + 256], F32)
    cmb = kb.sb("cmb", [128, 4 * 512 + 256], BF16)
    one1 = kb.sb("one1", [128, 1], F32)
    S.dma("sp", lambda e: e.dma_start(out=cmf[:], in_=cm), writes=["cmf"])
    S.op("pool", lambda e: e.tensor_copy(out=cmb[:], in_=cmf[:]), reads=["cmf"], writes=["cmb"])
    S.op("pool", lambda e: e.memset(one1[:], 1.0), writes=["one1"])
    ones_b = cmb[:, 2048:2176]
    umat_b = cmb[:, 2176:2304]
    wqb = load_w_bf16(kb, "wqb", wq, 256, stg, "stg")
    wkb = load_w_bf16(kb, "wkb", wk, 256, stg, "stg")
    wvb = load_w_bf16(kb, "wvb", wv, 256, stg, "stg")

    QT = kb.sb("QT", [128, Sq], BF16)
    KT = kb.sb("KT", [128, Sq], BF16)
    V = kb.sb("V", [128, NT, 128], BF16)
    xs = [kb.sb(f"xs{i}", [128, 8, 512], F32) for i in range(1)]
    xb = [kb.sb(f"xb{i}", [128, 8, 512], BF16) for i in range(2)]
    NB = 3
    zc = [kb.sb(f"zc{i}", [128, 512], F32) for i in range(NB)]
    ee = [kb.sb(f"ee{i}", [128, 512], F32) for i in range(NB)]
    sp = [kb.sb(f"sp{i}", [128, 512], BF16) for i in range(NB)]
    t1 = [kb.sb(f"t1{i}", [128, 512], F32) for i in range(NB)]
    att = [kb.sb(f"att{i}", [128, 512], BF16) for i in range(NB)]
    osb = [kb.sb(f"osb{i}", [64, 512], BF16) for i in range(2)]
    totb = [kb.sb(f"totb{i}", [128, 512], F32) for i in range(2)]

    psz = [kb.ps(f"psz{i}") for i in range(2)]
    psT = [kb.ps(f"psT{i}") for i in range(2)]
    psL = [kb.ps(f"psL{i}") for i in range(2)]
    pso = [kb.ps(f"pso{i}") for i in range(2)]
    pss = [(psz[0], "psz0"), (psz[1], "psz1"), (psL[0], "psL0"), (psL[1], "psL1")]

    for pair in range(2):
        fm = [(wqb[:, :, pair * 128:(pair + 1) * 128], "wqb", 128, QT, "QT", SCALE, "act"),
              (wkb[:, :, pair * 128:(pair + 1) * 128], "wkb", 128, KT, "KT", 1.0, "dve")]
        tm = [(wvb[:, :, pair * 128:(pair + 1) * 128], "wvb", 128, (lambda t: [(V[:, t, :], 0, 128)]), "V")]
        project(kb, xT, Sq, fm, tm, pss, xs, xb)

        units = []
        for c in range(NCH):
            jmax = 4 * c + 3
            for j in range(jmax, -1, -1):
                for h in range(2):
                    units.append((c, j, h, jmax))

        def s1(u, un):
            c, j, h, jmax = un
            pz, pzk = psz[u % 2], f"psz{u % 2}"
            hp = slice(h * 64, (h + 1) * 64)
            S.op("pe", lambda e: e.matmul(pz[:], lhsT=KT[hp, j * 128:(j + 1) * 128], rhs=QT[hp, c * 512:(c + 1) * 512],
                                          start=True, stop=True), reads=["KT", "QT"], writes=[pzk])

        def s2(u, un):
            c, j, h, jmax = un
            pz, pzk = psz[u % 2], f"psz{u % 2}"
            b = u % NB
            S.op("dve", lambda e: e.tensor_scalar(out=zc[b][:], in0=pz[:], scalar1=-60.0, scalar2=60.0,
                                                  op0=ALU.max, op1=ALU.min), reads=[pzk], writes=[f"zc{b}"])
            S.op("act", lambda e: e.activation(out=ee[b][:], in_=zc[b][:], func=AF.Exp), reads=[f"zc{b}"], writes=[f"ee{b}"])
            S.op("act", lambda e: e.activation(out=sp[b][:], in_=ee[b][:], func=AF.Ln, bias=one1[:, 0:1]),
                 reads=[f"ee{b}", "one1"], writes=[f"sp{b}"])
            if j >= 4 * c:
                o = j - 4 * c
                S.op("pool", lambda e: e.tensor_tensor(out=sp[b][:], in0=sp[b][:], in1=cmb[:, o * 512:(o + 1) * 512], op=ALU.mult),
                     reads=[f"sp{b}", "cmb"], writes=[f"sp{b}"])

        def s3(u, un):
            c, j, h, jmax = un
            b = u % NB
            pl, plk = psL[u % 2], f"psL{u % 2}"
            p1, p1k = psT[u % 2], f"psT{u % 2}"
            S.op("pe", lambda e: e.matmul(pl[:], lhsT=umat_b, rhs=sp[b][:], start=True, stop=True),
                 reads=[f"sp{b}", "cmb"], writes=[plk])
            S.op("pe", lambda e: e.matmul(p1[:], lhsT=ones_b, rhs=sp[b][:], start=True, stop=True),
                 reads=[f"sp{b}", "cmb"], writes=[p1k])

        def s4(u, un):
            c, j, h, jmax = un
            b = u % NB
            pl, plk = psL[u % 2], f"psL{u % 2}"
            p1, p1k = psT[u % 2], f"psT{u % 2}"
            S.op("dve", lambda e: e.tensor_tensor(out=t1[b][:], in0=zc[b][:], in1=pl[:], op=ALU.subtract),
                 reads=[f"zc{b}", plk], writes=[f"t1{b}"])
            if j != jmax:
                S.op("pool", lambda e: e.tensor_tensor(out=t1[b][:], in0=t1[b][:], in1=totb[h][:], op=ALU.subtract),
                     reads=[f"t1{b}", f"totb{h}"], writes=[f"t1{b}"])
                S.op("dve", lambda e: e.tensor_tensor(out=totb[h][:], in0=totb[h][:], in1=p1[:], op=ALU.add),
                     reads=[f"totb{h}", p1k], writes=[f"totb{h}"])
            else:
                S.op("dve", lambda e: e.tensor_copy(out=totb[h][:], in_=p1[:]), reads=[p1k], writes=[f"totb{h}"])
            S.op("act", lambda e: e.activation(out=att[b][:], in_=t1[b][:], func=AF.Exp), reads=[f"t1{b}"], writes=[f"att{b}"])
            if j >= 4 * c:
                o = j - 4 * c
                S.op("pool", lambda e: e.tensor_tensor(out=att[b][:], in0=att[b][:], in1=cmb[:, o * 512:(o + 1) * 512], op=ALU.mult),
                     reads=[f"att{b}", "cmb"], writes=[f"att{b}"])

        def s5(u, un):
            c, j, h, jmax = un
            b = u % NB
            S.op("pe", lambda e: e.matmul(pso[h][0:64, :], lhsT=V[:, j, h * 64:(h + 1) * 64], rhs=att[b][:],
                                          start=(j == jmax), stop=(j == 0)), reads=[f"att{b}", "V"], writes=[f"pso{h}"])
            if j == 0:
                ob = osb[h]
                S.op("act", lambda e: e.copy(out=ob[:], in_=pso[h][0:64, :]), reads=[f"pso{h}"], writes=[f"osb{h}"])
                r0 = pair * 128 + h * 64
                S.dma("sp", lambda e: e.dma_start(out=oT[r0:r0 + 64, c * 512:(c + 1) * 512], in_=ob[:]), reads=[f"osb{h}"])

        n_u = len(units)
        for t in range(n_u + 2):
            if t % 2 == 0:
                for u in (t, t + 1):
                    if u < n_u:
                        s1(u, units[u])
            for fn_, sk_ in ((s2, 0), (s3, 1), (s4, 1), (s5, 2)):
                u = t - sk_
                if 0 <= u < n_u:
                    fn_(u, units[u])
    return kb.finish()


def sb_consts():
    cm = np.zeros((128, 4 * 512 + 256), np.float32)
    k = np.arange(128)[:, None]
    q = np.arange(512)[None, :]
    for o in range(4):
        cm[:, o * 512:(o + 1) * 512] = ((o * 128 + k) < q).astype(np.float32)
    cm[:, 2048:2176] = 1.0
    jj = np.arange(128)[:, None]
    ss = np.arange(128)[None, :]
    cm[:, 2176:2304] = (jj >= ss).astype(np.float32)
    return cm


class SoftmaxPipe:
    def __init__(self, kb, nS=3, nP=3):
        import os
        nP = int(os.environ.get('SM_NP', nP))
        self.kb = kb
        self.psS = [kb.ps(f"psS{i}") for i in range(nS)]
        self.PT = [kb.sb(f"PT{i}", [128, 512], BF16) for i in range(nP)]
        self.nS, self.nP = nS, nP
        self.cnt = 0

    def run(self, units):
        S = self.kb.S
        base = self.cnt
        self.cnt += len(units)

        def s1(u, un):
            i = (base + u) % self.nS
            ps, pk = self.psS[i], f"psS{i}"
            n = len(un["mm"])
            kp, N = un.get("kp", 128), un["N"]
            for m, mmx in enumerate(un["mm"]):
                (lhsT, rhs, reads) = mmx[:3]
                r0, r1 = mmx[3] if len(mmx) > 3 else (0, kp)
                S.op("pe", lambda e, lhsT=lhsT, rhs=rhs, m=m, r0=r0, r1=r1: e.matmul(ps[r0:r1, 0:N], lhsT=lhsT, rhs=rhs, start=(m == 0), stop=(m == n - 1)),
                     reads=reads, writes=[pk])

        def s2(u, un):
            i = (base + u) % self.nS
            ps, pk = self.psS[i], f"psS{i}"
            b = (base + u) % self.nP
            kp, N = un.get("kp", 128), un["N"]
            cb = un.get("cb")
            if cb is None:
                S.op("act", lambda e: e.activation(out=self.PT[b][0:kp, 0:N], in_=ps[0:kp, 0:N], func=AF.Exp),
                     reads=[pk], writes=[f"PT{b}"])
            else:
                S.op("act", lambda e: e.activation(out=self.PT[b][0:kp, 0:N], in_=ps[0:kp, 0:N], func=AF.Exp, bias=cb[0]),
                     reads=[pk, cb[1]], writes=[f"PT{b}"])
            if un.get("post"):
                un["post"](self.PT[b], f"PT{b}")

        def s3(u, un):
            b = (base + u) % self.nP
            kp, N = un.get("kp", 128), un["N"]
            for pv in un["pv"]:
                (lhsT_v, reads, acc, acck, start, stop) = pv[:6]
                c0, c1 = pv[6] if len(pv) > 6 else (0, N)
                S.op("pe", lambda e, lhsT_v=lhsT_v, acc=acc, start=start, stop=stop, c0=c0, c1=c1: e.matmul(
                    acc, lhsT=lhsT_v, rhs=self.PT[b][0:kp, c0:c1], start=start, stop=stop),
                    reads=[f"PT{b}"] + list(reads), writes=[acck])
            if un.get("fin"):
                un["fin"]()

        import os
        pipeline(units, [s1, s2, s3], [int(v) for v in os.environ.get('SM_SKEW', '0,0,1').split(',')])


class SoftmaxPairPipe:
    def __init__(self, kb, nU=2, nP=3):
        self.kb = kb
        self.psS = [[kb.ps(f"psS{u}_{h}") for h in range(2)] for u in range(nU)]
        self.PT = [[kb.sb(f"PT{u}_{h}", [128, 512], BF16) for h in range(2)] for u in range(nP)]
        self.nU, self.nP = nU, nP
        self.cnt = 0

    def run(self, units):
        S = self.kb.S
        base = self.cnt
        self.cnt += len(units)

        def s1(u, un):
            i = (base + u) % self.nU
            N = un["N"]
            cnt = [0, 0]
            tot = [sum(1 for m in un["mm"] if m[3] == h) for h in range(2)]
            for (lhsT, rhs, reads, h) in un["mm"]:
                ps, pk = self.psS[i][h], f"psS{i}_{h}"
                first, last = cnt[h] == 0, cnt[h] == tot[h] - 1
                cnt[h] += 1
                S.op("pe", lambda e, lhsT=lhsT, rhs=rhs, ps=ps, first=first, last=last: e.matmul(ps[:, 0:N], lhsT=lhsT, rhs=rhs, start=first, stop=last),
                     reads=reads, writes=[pk])

        def s2(u, un):
            i = (base + u) % self.nU
            b = (base + u) % self.nP
            N = un["N"]
            for h in range(2):
                ps, pk = self.psS[i][h], f"psS{i}_{h}"
                cb = un["cb"][h]
                if cb is None:
                    S.op("act", lambda e, ps=ps, h=h: e.activation(out=self.PT[b][h][:, 0:N], in_=ps[:, 0:N], func=AF.Exp),
                         reads=[pk], writes=[f"PT{b}_{h}"])
                else:
                    S.op("act", lambda e, ps=ps, h=h, cb=cb: e.activation(out=self.PT[b][h][:, 0:N], in_=ps[:, 0:N], func=AF.Exp, bias=cb[0]),
                         reads=[pk, cb[1]], writes=[f"PT{b}_{h}"])

        def s3(u, un):
            b = (base + u) % self.nP
            N = un["N"]
            for (lhsT_v, reads, acc, acck, start, stop, h) in un["pv"]:
                S.op("pe", lambda e, lhsT_v=lhsT_v, acc=acc, start=start, stop=stop, h=h: e.matmul(
                    acc, lhsT=lhsT_v, rhs=self.PT[b][h][:, 0:N], start=start, stop=stop),
                    reads=[f"PT{b}_{h}"] + list(reads), writes=[acck])
            if un.get("fin"):
                un["fin"]()

        pipeline(units, [s1, s2, s3], [0, 0, 1])


class Normalizer:
    def __init__(self, kb):
        self.kb = kb
        self.rdt = kb.sb("nz_rdt", [128, 512], F32)
        self.num = kb.sb("nz_num", [64, 512], F32)
        self.onesf = kb.sb("nz_ones", [128, 64], F32)
        self.psB = kb.ps("psB")
        kb.S.op("pool", lambda e: e.memset(self.onesf[:], 1.0), writes=["nz_ones"])

    def bcast_recip(self, acc, acck, N):
        S = self.kb.S
        S.op("dve", lambda e: e.reciprocal(out=self.rdt[64:65, 0:N], in_=acc[64:65, 0:N]), reads=[acck], writes=["nz_rdt"])
        S.op("pe", lambda e: e.matmul(self.psB[0:64, 0:N], lhsT=self.onesf[64:65, 0:64], rhs=self.rdt[64:65, 0:N],
                                      start=True, stop=True), reads=["nz_rdt", "nz_ones"], writes=["psB"])

    def normalize(self, acc, acck, N, out_ap, outk):
        S = self.kb.S
        self.bcast_recip(acc, acck, N)
        S.op("act", lambda e: e.copy(out=self.num[:, 0:N], in_=acc[0:64, 0:N]), reads=[acck], writes=["nz_num"])
        S.op("dve", lambda e: e.tensor_tensor(out=out_ap, in0=self.num[:, 0:N], in1=self.psB[0:64, 0:N], op=ALU.mult),
             reads=["nz_num", "psB"], writes=[outk])


NOFF = 16


def build_moba(Sq):
    kb = KB()
    S = kb.S
    NT, NCH, NBLK = Sq // 128, Sq // 512, Sq // 256
    GW = max(NBLK, 8)
    xT = kb.din("xT", [D, Sq])
    wq = kb.din("wq", [D, 256])
    wk = kb.din("wk", [D, 256])
    wv = kb.din("wv", [D, 256])
    bss = kb.din("bss", [4, 128, 2432])
    cfar = kb.din("cfar", [128, 4])
    oT = kb.dout("oT", [256, Sq], BF16)

    stg = kb.sb("stg", [128, 8 * 512], F32)
    wqb = load_w_bf16(kb, "wqb", wq, 256, stg, "stg")
    wkb = load_w_bf16(kb, "wkb", wk, 256, stg, "stg")
    wvb = load_w_bf16(kb, "wvb", wv, 256, stg, "stg")
    cf = kb.sb("cf", [128, 4], F32)
    S.dma("sp", lambda e: e.dma_start(out=cf[:], in_=cfar), writes=["cf"])
    identf = kb.sb("identf", [128, 128], F32)
    identb = kb.sb("identb", [128, 128], BF16)
    onesf = kb.sb("onesf", [128, 128], F32)
    S.op("pool", lambda e: e.memset(onesf[:], 1.0), writes=["onesf"])
    S.op("pool", lambda e: e.affine_select(out=identf[:], in_=onesf[:], pattern=[[-1, 128]], compare_op=ALU.is_equal,
                                           fill=0.0, base=0, channel_multiplier=1), reads=["onesf"], writes=["identf"])
    S.op("pool", lambda e: e.tensor_copy(out=identb[:], in_=identf[:]), reads=["identf"], writes=["identb"])

    QT = kb.sb("QT", [128, Sq], BF16)
    KT = kb.sb("KT", [128, Sq], BF16)
    Vh = [kb.sb(f"V{h}", [128, NT, 65], BF16) for h in range(2)]
    for h in range(2):
        S.op("pool", lambda e, h=h: e.memset(Vh[h][:, :, 64:65], 1.0), writes=["V"])
    maskT = kb.sb("maskT", [128, Sq], BF16)
    bs = kb.sb("bs", [128, 2, 2432], BF16)
    xs = [(stg[:].rearrange("p (k t) -> p k t", k=8), "stg")]
    xb = [kb.sb(f"xb{i}", [128, 8, 512], BF16) for i in range(2)]
    km = kb.sb("km", [128, NBLK], F32)
    kmb = kb.sb("kmb", [128, NBLK], BF16)
    gbuf = kb.sb("gbuf", [128, GW], F32)
    m8 = kb.sb("m8", [128, 8], F32)
    mb2 = kb.sb("mb2", [128, 128], F32)
    osb = [kb.sb(f"osb{i}", [64, 512], BF16) for i in range(2)]

    sp_ = SoftmaxPairPipe(kb)
    nz = Normalizer(kb)
    pso = [[kb.ps(f"pso{h}_0")] for h in range(2)]
    pss = [(sp_.psS[0][0], "psS0_0"), (sp_.psS[0][1], "psS0_1"), (sp_.psS[1][0], "psS1_0")]
    import os
    for i in range(int(os.environ.get("DUMMY_WARM", "0"))):
        S.op("pe", lambda e: e.matmul(sp_.psS[0][0][:, 0:256], lhsT=identb[:], rhs=wqb[:, 0, :], start=True, stop=True),
             reads=["identb", "wqb"], writes=["psS0_0"])

    for pair in range(2):
        fm = [(wqb[:, :, pair * 128:(pair + 1) * 128], "wqb", 128, QT, "QT", SCALE, "act"),
              (wkb[:, :, pair * 128:(pair + 1) * 128], "wkb", 128, KT, "KT", 1.0, "dve")]
        tm = [(wvb[:, :, pair * 128:(pair + 1) * 128], "wvb", 128,
               (lambda t: [(Vh[0][:, t, 0:64], 0, 64), (Vh[1][:, t, 0:64], 64, 128)]), "V")]
        project(kb, xT, Sq, fm, tm, pss, xs, xb)
        for h in range(2):
            for w0 in range(0, 2432, 2048):
                wn = min(2048, 2432 - w0)
                S.dma("sp", lambda e, h=h, pair=pair, w0=w0, wn=wn: e.dma_start(out=stg[:, 0:wn], in_=bss[pair * 2 + h, :, w0:w0 + wn]), writes=["stg"])
                S.op("pool", lambda e, h=h, w0=w0, wn=wn: e.tensor_copy(out=bs[:, h, w0:w0 + wn], in_=stg[:, 0:wn]), reads=["stg"], writes=["bs"])
        S.op("dve", lambda e: e.tensor_reduce(out=km[:], in_=KT[:].rearrange("p (b k) -> p b k", k=256), axis=AX.X, op=ALU.add),
             reads=["KT"], writes=["km"])
        S.op("dve", lambda e: e.tensor_copy(out=kmb[:], in_=km[:]), reads=["km"], writes=["kmb"])
        for qt in range(NT):
            nv = qt // 2
            S.op("pool", lambda e: e.memset(mb2[:], NEG), writes=["mb2"])
            for h in range(2):
                hp = slice(h * 64, (h + 1) * 64)
                c0 = h * 64
                pg, pgk = pss[(qt * 2 + h) % 3]
                if nv > 3:
                    S.op("pe", lambda e, pg=pg, hp=hp, qt=qt: e.matmul(pg[:, 0:NBLK], lhsT=QT[hp, qt * 128:(qt + 1) * 128], rhs=kmb[hp, :],
                                                                       start=True, stop=True), reads=["QT", "kmb"], writes=[pgk])
                    S.op("pool", lambda e: e.memset(gbuf[:], -1e30), writes=["gbuf"])
                    S.op("dve", lambda e, pg=pg, nv=nv: e.tensor_copy(out=gbuf[:, 0:nv], in_=pg[:, 0:nv]), reads=[pgk], writes=["gbuf"])
                    S.op("dve", lambda e: e.max(out=m8[:], in_=gbuf[:]), reads=["gbuf"], writes=["m8"])
                    S.op("dve", lambda e, c0=c0: e.tensor_scalar(out=mb2[:, c0:c0 + NBLK], in0=gbuf[:, 0:NBLK], scalar1=m8[:, 2:3], scalar2=NEG,
                                                                 op0=ALU.is_lt, op1=ALU.mult), reads=["gbuf", "m8"], writes=["mb2"])
                elif nv > 0:
                    S.op("pool", lambda e, nv=nv, c0=c0: e.memset(mb2[:, c0:c0 + nv], 0.0), writes=["mb2"])
                S.op("pool", lambda e, nv=nv, c0=c0: e.memset(mb2[:, c0 + nv:c0 + nv + 1], 0.0), writes=["mb2"])
            pt, ptk = pss[(qt * 2 + 2) % 3]
            S.op("pe", lambda e, pt=pt: e.transpose(pt[:, 0:128], mb2[:], identf[:]), reads=["mb2", "identf"], writes=[ptk])
            S.op("act", lambda e, pt=pt, qt=qt: e.copy(out=maskT[:, qt * 128:(qt + 1) * 128], in_=pt[:, 0:128]),
                 reads=[ptk], writes=["maskT"])
        units = []
        for c in range(NCH):
            jmax = 4 * c + 3
            qs = slice(c * 512, (c + 1) * 512)
            for j in range(jmax + 1):
                o = 4 * c - j
                x = j // 2
                mm, cbs, pv = [], [], []
                for h in range(2):
                    hp = slice(h * 64, (h + 1) * 64)
                    mm.append((KT[hp, j * 128:(j + 1) * 128], QT[hp, qs], ["KT", "QT"], h))
                for h in range(2):
                    hp = slice(h * 64, (h + 1) * 64)
                    mm.append((identb[hp, h * 64 + x:h * 64 + x + 1].to_broadcast([64, 128]), maskT[hp, qs], ["identb", "maskT"], h))
                for h in range(2):
                    if o <= 12:
                        mm.append((identb[:], bs[:, h, (o + 3) * 128:(o + 3) * 128 + 512], ["identb", "bs"], h))
                        cbs.append(None)
                    else:
                        cbs.append((cf[:, pair * 2 + h:pair * 2 + h + 1], "cf"))
                    pv.append((Vh[h][:, j, :], ["V"], pso[h][0][0:65, :], f"pso{h}_0", j == 0, j == jmax, h))
                un = dict(mm=mm, N=512, cb=cbs, pv=pv)
                if j == jmax:
                    def fin(c=c, pair=pair):
                        for h in range(2):
                            ob = osb[h]
                            nz.normalize(pso[h][0], f"pso{h}_0", 512, ob[:], f"osb{h}")
                            r0 = pair * 128 + h * 64
                            S.dma("sp", lambda e, ob=ob, r0=r0: e.dma_start(out=oT[r0:r0 + 64, c * 512:(c + 1) * 512], in_=ob[:]), reads=[f"osb{h}"])
                    un["fin"] = fin
                units.append(un)
        sp_.run(units)
    return kb.finish()


def rel_bucket_np(dist):
    n = np.maximum(dist, 0)
    exact = 16
    logf = np.log(np.maximum(n, 1).astype(np.float32) / exact) / np.float32(np.log(2048 / exact))
    large = np.minimum(exact + (logf * 16).astype(np.int32), 31)
    return np.where(n < exact, n, large)


def moba_bias_tiles(rel_table, heads):
    k = np.arange(128)[:, None]
    q = np.arange(512)[None, :]
    out = np.empty((len(heads), NOFF, 128, 512), np.float32)
    for oi in range(NOFF):
        o = oi - 3
        dist = o * 128 + q - k
        bk = rel_bucket_np(dist)
        for i, h in enumerate(heads):
            out[i, oi] = np.where(dist >= 0, rel_table[bk, h], np.float32(NEG))
    return out


DIL = ((128, 1), (512, 4), (2048, 16))


def build_dil(Sq):
    kb = KB()
    S = kb.S
    NT, NCH = Sq // 128, Sq // 512
    xTg = [kb.din(f"xT{g}", [D, Sq]) for g in range(3)]
    wA = kb.din("wA", [3, 4, D, 192])
    btA = kb.din("btA", [3, 4, 128, 256])
    oT = kb.dout("oT", [256, Sq], BF16)

    stg = kb.sb("stg", [128, 8 * 192], F32)
    identf = kb.sb("identf", [128, 128], F32)
    identb = kb.sb("identb", [128, 128], BF16)
    onesf = kb.sb("onesf", [128, 128], F32)
    S.op("pool", lambda e: e.memset(onesf[:], 1.0), writes=["onesf"])
    S.op("pool", lambda e: e.affine_select(out=identf[:], in_=onesf[:], pattern=[[-1, 128]], compare_op=ALU.is_equal,
                                           fill=0.0, base=0, channel_multiplier=1), reads=["onesf"], writes=["identf"])
    S.op("pool", lambda e: e.tensor_copy(out=identb[:], in_=identf[:]), reads=["identf"], writes=["identb"])
    QT = kb.sb("QT", [64, Sq], BF16)
    KT = kb.sb("KT", [64, Sq], BF16)
    V = kb.sb("V", [128, NT, 65], BF16)
    S.op("pool", lambda e: e.memset(V[:, :, 64:65], 1.0), writes=["V"])
    accS = kb.sb("accS", [65, Sq], F32)
    wab = kb.sb("wab", [128, 8, 192], BF16)
    btf = kb.sb("btf", [128, 256], F32)
    btb = kb.sb("btb", [128, 256], BF16)
    xs = [kb.sb(f"xs{i}", [128, 8, 512], F32) for i in range(1)]
    xb = [kb.sb(f"xb{i}", [128, 8, 512], BF16) for i in range(2)]
    osb = [kb.sb(f"osb{i}", [64, 512], BF16) for i in range(2)]
    sp_ = SoftmaxPipe(kb)
    nz = Normalizer(kb)
    pso = [kb.ps(f"pso{i}") for i in range(2)]
    pss = [(sp_.psS[0], "psS0"), (sp_.psS[1], "psS1"), (sp_.psS[2], "psS2")]

    for hh in range(4):
        for g, (win, d) in enumerate(DIL):
            L = Sq // d
            Lt = L // 128
            S.dma("sp", lambda e, g=g, hh=hh: e.dma_start(out=stg[:].rearrange("p (k n) -> p k n", k=8),
                                                         in_=wA[g, hh].rearrange("(k p) n -> p k n", p=128)), writes=["stg"])
            S.op("pool", lambda e: e.tensor_copy(out=wab[:].rearrange("p k n -> p (k n)"), in_=stg[:]), reads=["stg"], writes=["wab"])
            S.dma("sp", lambda e, g=g, hh=hh: e.dma_start(out=btf[:], in_=btA[g, hh]), writes=["btf"])
            S.op("pool", lambda e: e.tensor_copy(out=btb[:], in_=btf[:]), reads=["btf"], writes=["btb"])
            fm = [(wab[:, :, 0:64], "wab", 64, QT, "QT", SCALE, "act"), (wab[:, :, 64:128], "wab", 64, KT, "KT", 1.0, "dve")]
            tm = [(wab[:, :, 128:192], "wab", 64, (lambda t: [(V[:, t, 0:64], 0, 64)]), "V")]
            project(kb, xTg[g], Sq, fm, tm, pss, xs, xb)
            accv = accS[:].rearrange("p (i r) -> p r i", r=d)
            units = []
            for T in range(NT):
                r, jl = T // Lt, T % Lt
                has_next = jl + 1 < Lt
                N = 256 if has_next else 128
                mm = [(KT[:, T * 128:(T + 1) * 128], QT[:, T * 128:T * 128 + N], ["KT", "QT"]),
                      (identb[:], btb[:, 0:N], ["identb", "btb"])]
                pv = [(V[:, T, :], ["V"], pso[T % 2][0:65, 0:128], f"pso{T % 2}", jl == 0, True, (0, 128))]
                if has_next:
                    pv.append((V[:, T, :], ["V"], pso[(T + 1) % 2][0:65, 0:128], f"pso{(T + 1) % 2}", True, False, (128, 256)))

                def fin(T=T, r=r, jl=jl, g=g, accv=accv):
                    dst = accv[:, r, jl * 128:(jl + 1) * 128]
                    acc, acck = pso[T % 2], f"pso{T % 2}"
                    if g == 0:
                        S.op("dve", lambda e: e.tensor_copy(out=dst, in_=acc[0:65, 0:128]), reads=[acck], writes=["accS"])
                    else:
                        S.op("dve", lambda e: e.tensor_tensor(out=dst, in0=dst, in1=acc[0:65, 0:128], op=ALU.add),
                             reads=[acck, "accS"], writes=["accS"])
                units.append(dict(mm=mm, N=N, kp=128, pv=pv, fin=fin))
            sp_.run(units)
        for c in range(NCH):
            ob = osb[c % 2]
            cs = slice(c * 512, (c + 1) * 512)
            nz.bcast_recip(accS[:, cs], "accS", 512)
            S.op("dve", lambda e, ob=ob, cs=cs: e.tensor_tensor(out=ob[:], in0=accS[0:64, cs], in1=nz.psB[0:64, :], op=ALU.mult),
                 reads=["accS", "psB"], writes=[f"osb{c % 2}"])
            S.dma("sp", lambda e, ob=ob, cs=cs, hh=hh: e.dma_start(out=oT[hh * 64:(hh + 1) * 64, cs], in_=ob[:]), reads=[f"osb{c % 2}"])
    return kb.finish()


def dil_bias_tiles(rel_table, heads):
    k = np.arange(128)[:, None]
    q = np.arange(256)[None, :]
    steps = q - k
    out = np.empty((3, len(heads), 128, 256), np.float32)
    for g, (win, d) in enumerate(DIL):
        span = win // d
        ok = (steps >= 0) & (steps <= span)
        bk = rel_bucket_np(steps * d)
        for i, h in enumerate(heads):
            out[g, i] = np.where(ok, rel_table[bk, h], np.float32(NEG))
    return out


def dil_perm(Sq, d):
    L = Sq // d
    return (np.arange(d)[:, None] + d * np.arange(L)[None, :]).reshape(-1)


def nsa_consts(Sq):
    NSEL = Sq // 64
    n_cmp = Sq // 16 - 1
    NCT = (n_cmp + 127) // 128
    c = np.arange(NCT * 128)[:, None]
    j = np.arange(NSEL)[None, :]
    ov = ((c >= 4 * j - 1) & (c <= 4 * j + 3) & (c < n_cmp)).astype(np.float32)
    return ov


def nsa_bias_strips(rel_table, heads):
    k = np.arange(128)[:, None]
    H = len(heads)
    def strip(dist, ok):
        bk = rel_bucket_np(dist)
        out = np.empty((H,) + dist.shape, np.float32)
        for i, h in enumerate(heads):
            out[i] = np.where(ok, rel_table[bk, h], np.float32(NEG))
        return out
    ds = np.arange(2432)[None, :] - 384 - k
    dw = np.arange(1408)[None, :] - 384 - k
    dc = np.arange(3584)[None, :] - 16 * k - 31
    return strip(ds, ds >= 0), strip(dw, (dw >= 0) & (dw <= 511)), strip(dc, dc >= 0)


def build_nsa(Sq):
    kb = KB()
    S = kb.S
    NT, NCH, NSEL = Sq // 128, Sq // 512, Sq // 64
    n_cmp = Sq // 16 - 1
    NCT = (n_cmp + 127) // 128
    NCC = NCT * 128
    NH = (NSEL + 127) // 128
    NSP = min(NSEL, 128)
    SW = max(NSEL, 8)
    xT = kb.din("xT", [D, Sq])
    wq = kb.din("wq", [D, 256])
    wqs = kb.din("wqs", [D, 256])
    wkv = kb.din("wkv", [D, 384])
    wgp = kb.din("wgp", [D, 76])
    w1 = kb.din("w1", [128, 32, 256])
    posT = kb.din("posT", [128, 32])
    w2 = kb.din("w2", [128, 2, 128])
    ovm = kb.din("ovm", [NCC, NSEL])
    bss = kb.din("bss", [4, 128, 2432])
    bsw = kb.din("bsw", [4, 128, 1408])
    bsc = kb.din("bsc", [4, 128, 3584])
    cfar = kb.din("cfar", [128, 4])
    oT = kb.dout("oT", [256, Sq], BF16)

    identf = kb.sb("identf", [128, 128], F32)
    identb = kb.sb("identb", [128, 128], BF16)
    onesf = kb.sb("onesf", [128, 128], F32)
    S.op("pool", lambda e: e.memset(onesf[:], 1.0), writes=["onesf"])
    S.op("pool", lambda e: e.affine_select(out=identf[:], in_=onesf[:], pattern=[[-1, 128]], compare_op=ALU.is_equal,
                                           fill=0.0, base=0, channel_multiplier=1), reads=["onesf"], writes=["identf"])
    S.op("pool", lambda e: e.tensor_copy(out=identb[:], in_=identf[:]), reads=["identf"], writes=["identb"])
    cf = kb.sb("cf", [128, 4], F32)
    S.dma("sp", lambda e: e.dma_start(out=cf[:], in_=cfar), writes=["cf"])
    selg = kb.sb("selg", [128, 12 * 64], F32)
    stg = kb.sb("stg", [128, 8 * 512], F32)
    S.op("pool", lambda e: e.memset(stg[:, 0:768], 1.0), writes=["stg"])
    S.op("pool", lambda e: e.affine_select(out=selg[:].rearrange("p (i m) -> p i m", m=64), in_=stg[:, 0:768].rearrange("p (i m) -> p i m", m=64),
                                           pattern=[[-1, 12], [0, 64]], compare_op=ALU.is_equal, fill=0.0, base=-64,
                                           channel_multiplier=1), reads=["stg"], writes=["selg"])
    zerob = kb.sb("zerob", [128, 128], BF16)
    S.op("pool", lambda e: e.memset(zerob[:], 0.0), writes=["zerob"])
    wqb = load_w_bf16(kb, "wqb", wq, 256, stg, "stg")
    wqsb = load_w_bf16(kb, "wqsb", wqs, 256, stg, "stg")
    wgb = load_w_bf16(kb, "wgb", wgp, 76, stg, "stg")
    w2b = kb.sb("w2b", [128, 2, 128], BF16)
    S.dma("sp", lambda e: e.dma_start(out=stg[:, 0:256].rearrange("p (a b) -> p a b", a=2), in_=w2), writes=["stg"])
    S.op("pool", lambda e: e.tensor_copy(out=w2b[:].rearrange("p a b -> p (a b)"), in_=stg[:, 0:256]), reads=["stg"], writes=["w2b"])
    ovb = kb.sb("ovb", [128, NCT, NSEL], BF16)
    for ct in range(NCT):
        S.dma("sp", lambda e, ct=ct: e.dma_start(out=stg[:, 0:NSEL], in_=ovm[ct * 128:(ct + 1) * 128, :]), writes=["stg"])
        S.op("pool", lambda e, ct=ct: e.tensor_copy(out=ovb[:, ct, :], in_=stg[:, 0:NSEL]), reads=["stg"], writes=["ovb"])

    KK = kb.sb("KK", [128, Sq], BF16)
    Vs = kb.sb("Vs", [128, NT, 76], BF16)
    Vw = kb.sb("Vw", [128, NT, 76], BF16)
    vc = kb.sb("vc", [128, NCT, 76], BF16)
    kcT = kb.sb("kcT", [64, NCC], BF16)
    for t_, k_ in ((Vs, "Vs"), (Vw, "Vw"), (vc, "vc")):
        S.op("pool", lambda e, t_=t_: e.memset(t_[:, :, 64:76], 1.0), writes=[k_])
    xs = [(stg[:].rearrange("p (k t) -> p k t", k=8), "stg")]
    xb0 = kb.sb("xb0", [128, 8, 512], BF16)
    xb = [(xb0, "xb0")]
    sp_ = SoftmaxPipe(kb, nS=2, nP=2)
    pss = [(sp_.psS[0], "psS0"), (sp_.psS[1], "psS1")]
    accs = [kb.ps(f"acc{i}") for i in range(2)]
    psB = kb.ps("psB")
    psI = [kb.ps(f"psI{i}") for i in range(2)]
    psR = kb.ps("psR")

    with contextlib.ExitStack() as st0:
        KVc = kb.sb("KVc", [128, Sq], BF16, st0)
        wkvb = kb.sb("wkvb", [128, 8, 384], BF16, st0)
        S.dma("sp", lambda e: e.dma_start(out=stg[:, 0:3072].rearrange("p (k n) -> p k n", k=8), in_=wkv.rearrange("(k p) n -> p k n", p=128)), writes=["stg"])
        S.op("pool", lambda e: e.tensor_copy(out=wkvb[:].rearrange("p k n -> p (k n)"), in_=stg[:, 0:3072]), reads=["stg"], writes=["wkvb"])
        w1b = kb.sb("w1b", [128, 32, 256], BF16, st0)
        hT = kb.sb("hT", [128, 2, 2, NCC], BF16, st0)
        posb = kb.sb("posb", [128, 32], BF16, st0)
        pbias = kb.sb("pbias", [128, 4], F32, st0)
        gx = kb.sb("gx", [128, 512], F32, st0)
        gu = kb.sb("gu", [128, 512], F32, st0)
        for q4 in range(4):
            S.dma("sp", lambda e, q4=q4: e.dma_start(out=stg[:, 0:2048].rearrange("p (a b) -> p a b", a=8), in_=w1[:, q4 * 8:(q4 + 1) * 8, :]),
                  writes=["stg"])
            S.op("pool", lambda e, q4=q4: e.tensor_copy(out=w1b[:, q4 * 8:(q4 + 1) * 8, :].rearrange("p a b -> p (a b)"), in_=stg[:, 0:2048]),
                 reads=["stg"], writes=["w1b"])
        S.dma("sp", lambda e: e.dma_start(out=stg[:, 0:32], in_=posT), writes=["stg"])
        S.op("pool", lambda e: e.tensor_copy(out=posb[:], in_=stg[:, 0:32]), reads=["stg"], writes=["posb"])
        S.op("pool", lambda e: e.memset(hT[:], 0.0), writes=["hT"])
        S.op("pool", lambda e: e.memset(kcT[:], 0.0), writes=["kcT"])
        fm = [(wkvb[:, :, 0:128], "wkvb", 128, KVc, "KVc", 1.0, "act"),
              (wkvb[:, :, 128:256], "wkvb", 128, KK, "KK", 1.0, "dve")]
        tm = [(wkvb[:, :, 256:384], "wkvb", 128, (lambda t: [(Vs[:, t, 0:64], 0, 64), (Vw[:, t, 0:64], 64, 128)]), "V")]
        project(kb, xT, Sq, fm, tm, pss + [(accs[0], "acc0"), (accs[1], "acc1")], xs, xb)
        for kv in range(2):
            rows = slice(kv * 64, (kv + 1) * 64)
            for hc in range(2):
                for p in range(32):
                    S.op("pe", lambda e, rows=rows, hc=hc, p=p, kv=kv: e.matmul(
                        psR[:, kv * 2 + hc:kv * 2 + hc + 1], lhsT=w1b[rows, p, hc * 128:(hc + 1) * 128], rhs=posb[rows, p:p + 1],
                        start=(p == 0), stop=(p == 31)), reads=["w1b", "posb"], writes=["psR"])
        S.op("act", lambda e: e.copy(out=pbias[:], in_=psR[:, 0:4]), reads=["psR"], writes=["pbias"])
        ccs = [(c0, min(512, n_cmp - c0)) for c0 in range(0, n_cmp, 512)]
        KVv = KVc[:].rearrange("p (c s) -> p s c", s=16)
        ui = 0
        for kv in range(2):
            rows = slice(kv * 64, (kv + 1) * 64)
            for hc in range(2):
                for (c0, cn) in ccs:
                    ps_, pk_ = pss[ui % 2]
                    ui += 1
                    for p in range(32):
                        S.op("pe", lambda e, ps_=ps_, rows=rows, hc=hc, p=p, c0=c0, cn=cn: e.matmul(
                            ps_[:, 0:cn], lhsT=w1b[rows, p, hc * 128:(hc + 1) * 128],
                            rhs=KVv[rows, p % 16, c0 + p // 16:c0 + p // 16 + cn], start=(p == 0), stop=(p == 31)),
                            reads=["w1b", "KVc"], writes=[pk_])
                    col = kv * 2 + hc
                    S.op("act", lambda e, ps_=ps_, cn=cn, col=col: e.activation(out=gx[:, 0:cn], in_=ps_[:, 0:cn], func=AF.Identity,
                                                                                bias=pbias[:, col:col + 1]), reads=[pk_, "pbias"], writes=["gx"])
                    S.op("dve", lambda e, cn=cn: e.tensor_tensor(out=gu[:, 0:cn], in0=gx[:, 0:cn], in1=gx[:, 0:cn], op=ALU.mult), reads=["gx"], writes=["gu"])
                    S.op("dve", lambda e, cn=cn: e.tensor_scalar(out=gu[:, 0:cn], in0=gu[:, 0:cn], scalar1=0.044715, scalar2=1.0, op0=ALU.mult, op1=ALU.add),
                         reads=["gu"], writes=["gu"])
                    S.op("dve", lambda e, cn=cn: e.tensor_tensor(out=gu[:, 0:cn], in0=gu[:, 0:cn], in1=gx[:, 0:cn], op=ALU.mult), reads=["gu", "gx"], writes=["gu"])
                    S.op("act", lambda e, cn=cn: e.activation(out=gu[:, 0:cn], in_=gu[:, 0:cn], func=AF.Tanh, scale=0.7978845608028654), reads=["gu"], writes=["gu"])
                    S.op("dve", lambda e, cn=cn: e.tensor_scalar(out=gu[:, 0:cn], in0=gu[:, 0:cn], scalar1=1.0, scalar2=0.5, op0=ALU.add, op1=ALU.mult),
                         reads=["gu"], writes=["gu"])
                    S.op("dve", lambda e, cn=cn, kv=kv, hc=hc, c0=c0: e.tensor_tensor(out=hT[:, kv, hc, c0:c0 + cn], in0=gu[:, 0:cn], in1=gx[:, 0:cn], op=ALU.mult),
                         reads=["gu", "gx"], writes=["hT"])
        for c0 in range(0, NCC, 512):
            cn = min(512, NCC - c0)
            ps_, pk_ = pss[ui % 2]
            ui += 1
            for hc in range(2):
                S.op("pe", lambda e, ps_=ps_, hc=hc, c0=c0, cn=cn: e.matmul(ps_[0:64, 0:cn], lhsT=w2b[:, hc, 0:64], rhs=hT[:, 0, hc, c0:c0 + cn],
                                                                            start=(hc == 0), stop=(hc == 1)), reads=["w2b", "hT"], writes=[pk_])
            S.op("act", lambda e, ps_=ps_, c0=c0, cn=cn: e.copy(out=kcT[:, c0:c0 + cn], in_=ps_[0:64, 0:cn]), reads=[pk_], writes=["kcT"])
        for ct in range(NCT):
            ps_, pk_ = pss[ui % 2]
            ui += 1
            for hc in range(2):
                S.op("pe", lambda e, ps_=ps_, hc=hc, ct=ct: e.matmul(ps_[:, 0:64], lhsT=hT[:, 1, hc, ct * 128:(ct + 1) * 128], rhs=w2b[:, hc, 64:128],
                                                                     start=(hc == 0), stop=(hc == 1)), reads=["w2b", "hT"], writes=[pk_])
            S.op("dve", lambda e, ps_=ps_, ct=ct: e.tensor_copy(out=vc[:, ct, 0:64], in_=ps_[:, 0:64]), reads=[pk_], writes=["vc"])

    S.fence()
    strips = {}
    for nm, src, W in (("bs_s", bss, 2432), ("bs_w", bsw, 1408), ("bs_c", bsc, 3584)):
        t_ = kb.sb(nm, [128, 4, W], BF16)
        strips[nm] = t_
        for r in range(4):
            for w0 in range(0, W, 2048):
                wn = min(2048, W - w0)
                S.dma("sp", lambda e, src=src, r=r, w0=w0, wn=wn: e.dma_start(out=stg[:, 0:wn], in_=src[r, :, w0:w0 + wn]), writes=["stg"])
                S.op("pool", lambda e, t_=t_, r=r, w0=w0, wn=wn: e.tensor_copy(out=t_[:, r, w0:w0 + wn], in_=stg[:, 0:wn]), reads=["stg"], writes=[nm])
    bs_s, bs_w, bs_c = strips["bs_s"], strips["bs_w"], strips["bs_c"]
    Qc = [kb.sb(f"Qc{i}", [128, 512], BF16) for i in range(4)]
    gsb = kb.sb("gsb", [128, 512], F32)
    fgd = kb.sb("fgd", [128, 512], F32)
    numt = kb.sb("numt", [64, 512], F32)
    tmpc = kb.sb("tmpc", [64, 512], F32)
    res = [kb.sb(f"res{r}", [64, 512], F32) for r in range(4)]
    osb = [kb.sb(f"osb{i}", [64, 512], BF16) for i in range(2)]
    impsum = [kb.sb(f"imps{t}", [128, SW], F32) for t in range(4)]
    sc2 = kb.sb("sc2", [128, SW], F32)
    mbias = kb.sb("mbias", [128, SW], F32)
    m8a = kb.sb("m8a", [128, 8], F32)
    m8b = kb.sb("m8b", [128, 8], F32)
    thr = kb.sb("thr", [128, 1], F32)
    rdcol = kb.sb("rdcol", [128, 4], F32)
    maskT = kb.sb("maskT", [128, NH, 512], BF16)
    xv = xT.rearrange("(k p) t -> p k t", p=128)

    def qsel(r):
        return [Qc[0][0:64, :], Qc[2][0:64, :], Qc[1][0:64, :], Qc[3][0:64, :]][r], ["Qc0", "Qc2", "Qc1", "Qc3"][r]

    def qwin(r):
        return [Qc[2][64:128, :], Qc[0][64:128, :], Qc[3][64:128, :], Qc[1][64:128, :]][r], ["Qc2", "Qc0", "Qc3", "Qc1"][r]

    acc_i = [0]

    def finalize(acc, acck, br, r, first, last, c):
        i = br * 4 + r
        S.op("dve", lambda e: e.tensor_scalar(out=fgd[64:76, :], in0=acc[64:76, :], scalar1=1e-30, scalar2=None, op0=ALU.max),
             reads=[acck], writes=["fgd"])
        S.op("dve", lambda e: e.reciprocal(out=fgd[64:76, :], in_=fgd[64:76, :]), reads=["fgd"], writes=["fgd"])
        if br == 0:
            for tt in range(4):
                S.op("pe", lambda e, tt=tt: e.matmul(psR[:, tt:tt + 1], lhsT=fgd[64:65, tt * 128:(tt + 1) * 128], rhs=onesf[64:65, 0:1],
                                                     start=True, stop=True), reads=["fgd", "onesf"], writes=["psR"])
            S.op("act", lambda e: e.copy(out=rdcol[:], in_=psR[:, 0:4]), reads=["psR"], writes=["rdcol"])
        S.op("dve", lambda e: e.tensor_tensor(out=fgd[64:76, :], in0=fgd[64:76, :], in1=gsb[64:76, :], op=ALU.mult),
             reads=["fgd", "gsb"], writes=["fgd"])
        S.op("pe", lambda e: e.matmul(psB[0:64, :], lhsT=selg[64:76, i * 64:(i + 1) * 64], rhs=fgd[64:76, :], start=True, stop=True),
             reads=["fgd", "selg"], writes=["psB"])
        S.op("act", lambda e: e.copy(out=numt[:], in_=acc[0:64, :]), reads=[acck], writes=["numt"])
        if first:
            S.op("dve", lambda e: e.tensor_tensor(out=res[r][:], in0=numt[:], in1=psB[0:64, :], op=ALU.mult),
                 reads=["numt", "psB"], writes=[f"res{r}"])
        else:
            S.op("dve", lambda e: e.tensor_tensor(out=tmpc[:], in0=numt[:], in1=psB[0:64, :], op=ALU.mult),
                 reads=["numt", "psB"], writes=["tmpc"])
            if not last:
                S.op("pool", lambda e: e.tensor_tensor(out=res[r][:], in0=res[r][:], in1=tmpc[:], op=ALU.add),
                     reads=["tmpc", f"res{r}"], writes=[f"res{r}"])
            else:
                ob = osb[r % 2]
                S.op("pool", lambda e: e.tensor_tensor(out=ob[:], in0=res[r][:], in1=tmpc[:], op=ALU.add),
                     reads=["tmpc", f"res{r}"], writes=[f"osb{r % 2}"])
                S.dma("sp", lambda e: e.dma_start(out=oT[r * 64:(r + 1) * 64, c * 512:(c + 1) * 512], in_=ob[:]), reads=[f"osb{r % 2}"])

    for c in range(NCH):
        qs = slice(c * 512, (c + 1) * 512)
        xbi, xbk = xb0, "xb0"
        S.dma("sp", lambda e, c=c: e.dma_start(out=xs[0][0], in_=xv[:, :, c * 512:(c + 1) * 512]), writes=["stg"])
        S.op("dve", lambda e, xbi=xbi: e.tensor_copy(out=xbi[:, 0:4, :], in_=xs[0][0][:, 0:4, :]), reads=["stg"], writes=[xbk])
        S.op("act", lambda e, xbi=xbi: e.copy(out=xbi[:, 4:8, :], in_=xs[0][0][:, 4:8, :]), reads=["stg"], writes=[xbk])
        for qi, (wt, wkey, cols) in enumerate(((wqb, "wqb", 0), (wqb, "wqb", 128), (wqsb, "wqsb", 0), (wqsb, "wqsb", 128))):
            ps_, pk_ = pss[qi % 2]
            for k in range(8):
                S.op("pe", lambda e, ps_=ps_, wt=wt, cols=cols, k=k, xbi=xbi: e.matmul(ps_[:], lhsT=wt[:, k, cols:cols + 128], rhs=xbi[:, k, :],
                                                                                     start=(k == 0), stop=(k == 7)), reads=[wkey, xbk], writes=[pk_])
            S.op("act", lambda e, ps_=ps_, qi=qi: e.mul(Qc[qi][:], ps_[:], SCALE), reads=[pk_], writes=[f"Qc{qi}"])
        for k in range(8):
            S.op("pe", lambda e, k=k, xbi=xbi: e.matmul(psB[0:76, :], lhsT=wgb[:, k, 0:76], rhs=xbi[:, k, :], start=(k == 0), stop=(k == 7)),
                 reads=["wgb", xbk], writes=["psB"])
        S.op("act", lambda e: e.activation(out=gsb[64:76, :], in_=psB[64:76, :], func=AF.Sigmoid), reads=["psB"], writes=["gsb"])

        ctmax = min(NCT - 1, (32 * c + 30) // 128)
        for r in range(4):
            acc, acck = accs[acc_i[0] % 2], f"acc{acc_i[0] % 2}"
            acc_i[0] += 1
            qa, qk = qsel(r)
            units = []
            for ct in range(ctmax + 1):
                o = c - 4 * ct
                mm = [(kcT[:, ct * 128:(ct + 1) * 128], qa, ["kcT", qk])]
                cb = None
                if o <= 6:
                    mm.append((identb[:], bs_c[:, r, o * 512:(o + 1) * 512], ["identb", "bs_c"]))
                else:
                    cb = (cf[:, r:r + 1], "cf")
                un = dict(mm=mm, N=512, cb=cb, pv=[(vc[:, ct, :], ["vc"], acc[0:76, :], acck, ct == 0, ct == ctmax)])
                un["imp"] = (ct, ctmax)
                units.append(un)
            for bnk in range(2):
                S.op("pe", lambda e, bnk=bnk: e.matmul(psI[bnk][:], lhsT=zerob[:], rhs=bs_c[:, 0, 0:512], start=True, stop=False),
                     reads=["zerob", "bs_c"], writes=[f"psI{bnk}"])
            run_cmp(kb, sp_, units, psI, ovb, NSEL)
            finalize(acc, acck, 0, r, True, False, c)
            for tt in range(4):
                src = psI[tt // 2][:, (tt % 2) * 256:(tt % 2) * 256 + NSEL]
                if r == 0:
                    S.op("dve", lambda e, tt=tt, src=src: e.tensor_scalar(out=impsum[tt][:, 0:NSEL], in0=src, scalar1=rdcol[:, tt:tt + 1], scalar2=None,
                                                                          op0=ALU.mult), reads=[f"psI{tt // 2}", "rdcol"], writes=[f"imps{tt}"])
                else:
                    S.op("dve", lambda e, tt=tt, src=src: e.scalar_tensor_tensor(out=impsum[tt][:, 0:NSEL], in0=src, scalar=rdcol[:, tt:tt + 1],
                                                                                 in1=impsum[tt][:, 0:NSEL], op0=ALU.mult, op1=ALU.add),
                         reads=[f"psI{tt // 2}", "rdcol", f"imps{tt}"], writes=[f"imps{tt}"])
        for tt in range(4):
            qt = 4 * c + tt
            sc = impsum[tt]
            sk = f"imps{tt}"
            if SW > NSEL:
                S.op("pool", lambda e, sc=sc: e.memset(sc[:, NSEL:SW], -1e30), writes=[sk])
            if 2 * qt + 2 < NSEL:
                S.op("pool", lambda e, sc=sc, qt=qt: e.memset(sc[:, 2 * qt + 2:NSEL], -1e30), writes=[sk])
            S.op("pool", lambda e, sc=sc, qt=qt: e.memset(sc[0:64, 2 * qt + 1:2 * qt + 2], -1e30), writes=[sk])
            S.op("pool", lambda e, sc=sc: e.memset(sc[:, 0:1], 1e9), writes=[sk])
            lo = max(2 * qt - 1, 0)
            S.op("pool", lambda e, sc=sc, qt=qt, lo=lo: e.memset(sc[0:64, lo:2 * qt + 1], 1e9), writes=[sk])
            S.op("pool", lambda e, sc=sc, qt=qt: e.memset(sc[64:128, 2 * qt:2 * qt + 2], 1e9), writes=[sk])
            S.op("dve", lambda e, sc=sc: e.max(out=m8a[:], in_=sc[:]), reads=[sk], writes=["m8a"])
            S.op("dve", lambda e, sc=sc: e.match_replace(out=sc2[:], in_to_replace=m8a[:], in_values=sc[:], imm_value=-1e30),
                 reads=[sk, "m8a"], writes=["sc2"])
            S.op("dve", lambda e: e.max(out=m8b[:], in_=sc2[:]), reads=["sc2"], writes=["m8b"])
            S.op("dve", lambda e: e.tensor_scalar(out=thr[:], in0=m8b[:, 7:8], scalar1=-1e29, scalar2=None, op0=ALU.max),
                 reads=["m8b"], writes=["thr"])
            S.op("dve", lambda e, sc=sc: e.tensor_scalar(out=mbias[:], in0=sc[:], scalar1=thr[:, 0:1], scalar2=NEG, op0=ALU.is_lt, op1=ALU.mult),
                 reads=[sk, "thr"], writes=["mbias"])
            for jh in range(NH):
                ps_, pk_ = pss[(tt * NH + jh) % 2]
                S.op("pe", lambda e, ps_=ps_, jh=jh: e.transpose(ps_[0:NSP, 0:128], mbias[:, jh * 128:jh * 128 + NSP], identf[:]),
                     reads=["mbias", "identf"], writes=[pk_])
                S.op("act", lambda e, ps_=ps_, jh=jh, tt=tt: e.copy(out=maskT[0:NSP, jh, tt * 128:(tt + 1) * 128], in_=ps_[0:NSP, 0:128]),
                     reads=[pk_], writes=["maskT"])
        jmax = 4 * c + 3
        for r in range(4):
            acc, acck = accs[acc_i[0] % 2], f"acc{acc_i[0] % 2}"
            acc_i[0] += 1
            qa, qk = qsel(r)
            units = []
            for j in range(jmax + 1):
                o = 4 * c - j
                mm = [(KK[0:64, j * 128:(j + 1) * 128], qa, ["KK", qk]),
                      (identb[0:NSP, 2 * (j % 64):2 * (j % 64) + 1].to_broadcast([NSP, 64]), maskT[0:NSP, j // 64, :], ["identb", "maskT"], (0, 64)),
                      (identb[0:NSP, 2 * (j % 64) + 1:2 * (j % 64) + 2].to_broadcast([NSP, 64]), maskT[0:NSP, j // 64, :], ["identb", "maskT"], (64, 128))]
                cb = None
                if o <= 12:
                    mm.append((identb[:], bs_s[:, r, (o + 3) * 128:(o + 3) * 128 + 512], ["identb", "bs_s"]))
                else:
                    cb = (cf[:, r:r + 1], "cf")
                units.append(dict(mm=mm, N=512, cb=cb, pv=[(Vs[:, j, :], ["Vs"], acc[0:76, :], acck, j == 0, j == jmax)]))
            sp_.run(units)
            finalize(acc, acck, 1, r, False, False, c)
        for r in range(4):
            acc, acck = accs[acc_i[0] % 2], f"acc{acc_i[0] % 2}"
            acc_i[0] += 1
            qa, qk = qwin(r)
            units = []
            j0 = max(0, 4 * c - 4)
            for j in range(j0, jmax + 1):
                o = 4 * c - j
                mm = [(KK[64:128, j * 128:(j + 1) * 128], qa, ["KK", qk]),
                      (identb[:], bs_w[:, r, (o + 3) * 128:(o + 3) * 128 + 512], ["identb", "bs_w"])]
                units.append(dict(mm=mm, N=512, pv=[(Vw[:, j, :], ["Vw"], acc[0:76, :], acck, j == j0, j == jmax)]))
            sp_.run(units)
            finalize(acc, acck, 2, r, False, True, c)
    return kb.finish()


def run_cmp(kb, sp_, units, psI, ovb, NSEL):
    S = kb.S
    for un in units:
        ct, ctmax = un["imp"]

        def post(PTb, PTk, ct=ct, ctmax=ctmax):
            for tt in range(4):
                S.op("pe", lambda e, tt=tt: e.matmul(psI[tt // 2][:, (tt % 2) * 256:(tt % 2) * 256 + NSEL],
                                                     lhsT=PTb[:, tt * 128:(tt + 1) * 128], rhs=ovb[:, ct, :],
                                                     start=False, stop=(ct == ctmax and tt % 2 == 1)),
                     reads=[PTk, "ovb"], writes=[f"psI{tt // 2}"])
        un["post"] = post
    sp_.run(units)


ALPHA = 8 ** 0.25
LN_EPS = 1e-5
D = 1024
NE = 16
DE = 256


def build_post(T, stage=9):
    nc = bass.Bass("TRN2", target_bir_lowering=False)
    TG = min(1024, T)
    NG = T // TG
    NT = TG // 128
    NCH = TG // 512
    oT = nc.dram_tensor("oT", [D, T], BF16, kind="ExternalInput").ap()
    xin = nc.dram_tensor("xin", [T, D], F32, kind="ExternalInput").ap()
    w_out = nc.dram_tensor("w_out", [D, D], F32, kind="ExternalInput").ap()
    lnp = nc.dram_tensor("lnp", [4, D], F32, kind="ExternalInput").ap()
    rw = nc.dram_tensor("rw", [D, NE], F32, kind="ExternalInput").ap()
    rb = nc.dram_tensor("rb", [1, NE], F32, kind="ExternalInput").ap()
    wg = nc.dram_tensor("wg", [NE, D, DE], F32, kind="ExternalInput").ap()
    wu = nc.dram_tensor("wu", [NE, D, DE], F32, kind="ExternalInput").ap()
    wd = nc.dram_tensor("wd", [NE, DE, D], F32, kind="ExternalInput").ap()
    xout = nc.dram_tensor("xout", [T, D], F32, kind="ExternalOutput").ap()

    S = Sched(nc)
    with contextlib.ExitStack() as st:
        def sb(name, shape, dt):
            return st.enter_context(nc.sbuf_tensor(name, shape, dt))

        def ps(name, shape, dt=F32):
            return st.enter_context(nc.psum_tensor(name, shape, dt))

        ident = sb("ident", [128, 128], F32)
        lnb = sb("lnb", [128, 4, D], F32)
        rbb = sb("rbb", [128, NE], F32)
        rwt = sb("rwt", [128, 8, NE], F32)
        wob = sb("wob", [128, 8, D], BF16)
        stg = [sb(f"stg{i}", [128, 2048], F32) for i in range(3)]
        wb = [[sb(f"wb{j}_{i}", [128, 2048], BF16) for i in range(3)] for j in range(2)]
        x1T = sb("x1T", [128, 8, TG], BF16)
        x1Tf = sb("x1Tf", [128, 8, 128], F32)
        yacc = sb("yacc", [128, NT, D], F32)
        xt = [sb(f"xt{i}", [128, D], F32) for i in range(2)]
        ot = [sb(f"ot{i}", [128, 8, 128], BF16) for i in range(2)]
        r = sb("r", [128, D], F32)
        x1 = sb("x1", [128, D], F32)
        stats = sb("stats", [128, 2, 6], F32)
        mv = sb("mv", [128, 2], F32)
        rstd = sb("rstd", [128, 1], F32)
        G = sb("G", [128, NT, NE], F32)
        rt = [sb(f"rt{i}", [128, NE], F32) for i in range(6)]
        rs = [sb(f"rs{i}", [128, 4], F32) for i in range(4)]
        sg = [sb(f"sg{i}", [128, 512], F32) for i in range(2)]
        aT = [[sb(f"aT{j}_{i}", [128, 512], BF16) for i in range(2)] for j in range(2)]
        outt = [sb(f"outt{i}", [128, D], F32) for i in range(2)]

        psD = [[ps(f"psD{j}_{i}", [128, 512]) for i in range(2)] for j in range(2)]
        psG = [ps(f"psG{i}", [128, 512]) for i in range(2)]
        psU = [ps(f"psU{i}", [128, 512]) for i in range(2)]

        S.op("pool", lambda e: e.memset(ident[:], 0.0), writes=["ident"])
        S.op("pool", lambda e: e.memset(r[:, 0:128], 1.0), writes=["r"])
        S.op("pool", lambda e: e.affine_select(out=ident[:], in_=r[:, 0:128], pattern=[[-1, 128]],
                                               compare_op=ALU.is_equal, fill=0.0, base=0, channel_multiplier=1),
             reads=["r"], writes=["ident"])
        S.dma("sp", lambda e: e.dma_start(out=lnb[:], in_=lnp.partition_broadcast(128)), writes=["lnb"])
        S.dma("sp", lambda e: e.dma_start(out=rbb[:], in_=rb[0, :].partition_broadcast(128)), writes=["rbb"])
        S.dma("sp", lambda e: e.dma_start(out=rwt[:], in_=rw.rearrange("(k p) e -> p k e", p=128)), writes=["rwt"])
        wo_v = w_out.rearrange("(k p) n -> p k n", p=128)
        for i in range(4):
            sname = f"stg{i % 3}"
            S.dma("sp", lambda e, i=i: e.dma_start(out=stg[i % 3][:].rearrange("p (k n) -> p k n", k=2),
                                                   in_=wo_v[:, 2 * i:2 * i + 2, :]), writes=[sname])
            S.op("pool", lambda e, i=i: e.tensor_copy(out=wob[:, 2 * i:2 * i + 2, :].rearrange("p k n -> p (k n)"),
                                                      in_=stg[i % 3][:]), reads=[sname], writes=["wob"])

        xin_v = xin.rearrange("(n p) d -> n p d", p=128)
        xout_v = xout.rearrange("(n p) d -> n p d", p=128)
        oT_v = oT.rearrange("(k p) t -> p k t", p=128)
        wg_v = wg.rearrange("e (k p) f -> e p k f", p=128)
        wu_v = wu.rearrange("e (k p) f -> e p k f", p=128)
        wd_v = wd.rearrange("e (k p) n -> e p k n", p=128)

        def layer_norm(src_ap_fn, src_key, dst_ap, dst_key, gi, tag):
            for h in range(2):
                S.op("dve", lambda e, h=h: e.bn_stats(out=stats[:, h, :], in_=src_ap_fn()[:, h * 512:(h + 1) * 512]),
                     reads=[src_key], writes=["stats"])
            S.op("dve", lambda e: e.bn_aggr(out=mv[:], in_=stats[:].rearrange("p a b -> p (a b)")),
                 reads=["stats"], writes=["mv"])
            S.op("dve", lambda e: e.tensor_scalar(out=rstd[:], in0=mv[:, 1:2], scalar1=LN_EPS, scalar2=None,
                                                  op0=ALU.add), reads=["mv"], writes=["rstd"])
            S.op("act", lambda e: e.sqrt(rstd[:], rstd[:]), reads=["rstd"], writes=["rstd"])
            S.op("dve", lambda e: e.reciprocal(out=rstd[:], in_=rstd[:]), reads=["rstd"], writes=["rstd"])
            S.op("dve", lambda e: e.tensor_scalar(out=dst_ap, in0=src_ap_fn(), scalar1=mv[:, 0:1], scalar2=rstd[:, 0:1],
                                                  op0=ALU.subtract, op1=ALU.mult),
                 reads=[src_key, "mv", "rstd"], writes=[dst_key])
            S.op("pool", lambda e: e.tensor_tensor(out=dst_ap, in0=dst_ap, in1=lnb[:, gi, :], op=ALU.mult),
                 reads=[dst_key, "lnb"], writes=[dst_key])
            S.op("pool", lambda e: e.tensor_tensor(out=dst_ap, in0=dst_ap, in1=lnb[:, gi + 1, :], op=ALU.add),
                 reads=[dst_key, "lnb"], writes=[dst_key])

        wcount = 0
        for g in range(NG if stage >= 1 else 0):
            t0 = g * TG
            for t in range(NT):
                gt = g * NT + t
                xi, oi = xt[gt % 2], ot[gt % 2]
                xk, ok = f"xt{gt % 2}", f"ot{gt % 2}"
                S.dma("sp", lambda e, xi=xi, gt=gt: e.dma_start(out=xi[:], in_=xin_v[gt]), writes=[xk])
                S.dma("sp", lambda e, oi=oi, gt=gt: e.dma_start(out=oi[:], in_=oT_v[:, :, gt * 128:(gt + 1) * 128]),
                      writes=[ok])
                pY = psD[0]
                for h in range(2):
                    for k in range(8):
                        S.op("pe", lambda e, h=h, k=k, oi=oi: e.matmul(pY[h][:], lhsT=oi[:, k, :],
                                                                       rhs=wob[:, k, h * 512:(h + 1) * 512],
                                                                       start=(k == 0), stop=(k == 7)),
                             reads=[ok, "wob"], writes=[f"psD0_{h}"])
                for h in range(2):
                    S.op("dve", lambda e, h=h, xi=xi: e.scalar_tensor_tensor(
                        out=r[:, h * 512:(h + 1) * 512], in0=xi[:, h * 512:(h + 1) * 512], scalar=ALPHA,
                        in1=pY[h][:], op0=ALU.mult, op1=ALU.add), reads=[xk, f"psD0_{h}"], writes=["r"])
                if stage < 1.2: continue
                layer_norm(lambda: r[:], "r", x1[:], "x1", 0, "ln1")
                S.op("act", lambda e, t=t: e.mul(yacc[:, t, :], x1[:], ALPHA), reads=["x1"], writes=[f"yacc{t}"])
                if stage < 1.3: continue
                for k in range(8):
                    S.op("pe", lambda e, k=k: e.transpose(psD[1][k // 4][:, (k % 4) * 128:(k % 4 + 1) * 128],
                                                          x1[:, k * 128:(k + 1) * 128], ident[:]),
                         reads=["x1", "ident"], writes=[f"psD1_{k // 4}"])
                for h in range(2 if stage >= 1.32 else 0):
                    S.op("act", lambda e, h=h: e.copy(out=x1Tf[:, 4 * h:4 * h + 4, :].rearrange("p k t -> p (k t)"),
                                                      in_=psD[1][h][:]), reads=[f"psD1_{h}"], writes=["x1Tf"])
                    if stage < 1.33: continue
                    S.op("pool", lambda e, h=h, t=t: e.tensor_copy(
                        out=x1T[:, 4 * h:4 * h + 4, t * 128:(t + 1) * 128],
                        in_=x1Tf[:, 4 * h:4 * h + 4, :]), reads=["x1Tf"], writes=["x1T"])
                if stage < 1.4: continue
                for k in range(8):
                    S.op("pe", lambda e, k=k: e.matmul(psG[0][:, 0:NE], lhsT=x1Tf[:, k, :], rhs=rwt[:, k, :],
                                                       start=(k == 0), stop=(k == 7)),
                         reads=["x1Tf", "rwt"], writes=["psG0"])
                if stage < 1.5: continue
                sc, bi, eq, b2, sel, ws = rt
                m1, m2, gs, gsel = rs
                S.op("act", lambda e: e.activation(out=sc[:], in_=psG[0][:, 0:NE], func=AF.Sigmoid),
                     reads=["psG0"], writes=["sc"])
                S.op("dve", lambda e: e.tensor_tensor(out=bi[:], in0=sc[:], in1=rbb[:], op=ALU.add),
                     reads=["sc", "rbb"], writes=["bi"])
                v3 = lambda a: a[:].rearrange("p (g j) -> p g j", g=4)
                S.op("dve", lambda e: e.tensor_reduce(out=m1[:], in_=v3(bi), axis=AX.X, op=ALU.max),
                     reads=["bi"], writes=["m1"])
                S.op("dve", lambda e: e.tensor_tensor(out=v3(eq), in0=v3(bi), in1=m1[:].unsqueeze(2).to_broadcast([128, 4, 4]),
                                                      op=ALU.is_equal), reads=["bi", "m1"], writes=["eq"])
                S.op("dve", lambda e: e.scalar_tensor_tensor(out=b2[:], in0=eq[:], scalar=-1e9, in1=bi[:],
                                                             op0=ALU.mult, op1=ALU.add), reads=["eq", "bi"], writes=["b2"])
                S.op("dve", lambda e: e.tensor_reduce(out=m2[:], in_=v3(b2), axis=AX.X, op=ALU.max),
                     reads=["b2"], writes=["m2"])
                S.op("dve", lambda e: e.tensor_tensor(out=gs[:], in0=m1[:], in1=m2[:], op=ALU.add),
                     reads=["m1", "m2"], writes=["gs"])
                S.op("dve", lambda e: e.tensor_reduce(out=rstd[:], in_=gs[:], axis=AX.X, op=ALU.max),
                     reads=["gs"], writes=["rstd"])
                S.op("dve", lambda e: e.tensor_scalar(out=gsel[:], in0=gs[:], scalar1=rstd[:, 0:1], scalar2=None,
                                                      op0=ALU.is_ge), reads=["gs", "rstd"], writes=["gsel"])
                S.op("dve", lambda e: e.tensor_tensor(out=v3(sel), in0=v3(bi), in1=m2[:].unsqueeze(2).to_broadcast([128, 4, 4]),
                                                      op=ALU.is_ge), reads=["bi", "m2"], writes=["sel"])
                S.op("dve", lambda e: e.tensor_tensor(out=v3(sel), in0=v3(sel), in1=gsel[:].unsqueeze(2).to_broadcast([128, 4, 4]),
                                                      op=ALU.mult), reads=["sel", "gsel"], writes=["sel"])
                S.op("dve", lambda e: e.tensor_tensor(out=ws[:], in0=sel[:], in1=sc[:], op=ALU.mult),
                     reads=["sel", "sc"], writes=["ws"])
                S.op("dve", lambda e: e.tensor_reduce(out=rstd[:], in_=ws[:], axis=AX.X, op=ALU.add),
                     reads=["ws"], writes=["rstd"])
                S.op("dve", lambda e: e.reciprocal(out=rstd[:], in_=rstd[:]), reads=["rstd"], writes=["rstd"])
                S.op("dve", lambda e, t=t: e.tensor_scalar(out=G[:, t, :], in0=ws[:], scalar1=rstd[:, 0:1], scalar2=None,
                                                           op0=ALU.mult), reads=["ws", "rstd"], writes=[f"G{t}"])
            for ex in range(NE if stage >= 2 else 0):
                wset = wb[wcount % 2]
                wk = [f"wb{wcount % 2}_{i}" for i in range(3)]
                wcount += 1
                srcs = [wg_v[ex], wu_v[ex], wd_v[ex]]
                for i in range(3):
                    kk = 8 if i < 2 else 2
                    S.dma("sp", lambda e, i=i, kk=kk, src=srcs[i]: e.dma_start(
                        out=stg[i][:].rearrange("p (k n) -> p k n", k=kk), in_=src), writes=[f"stg{i}"])
                    S.op("pool", lambda e, i=i, wset=wset: e.tensor_copy(out=wset[i][:], in_=stg[i][:]),
                         reads=[f"stg{i}"], writes=[wk[i]])
                wgb = wset[0][:].rearrange("p (k f) -> p k f", k=8)
                wub = wset[1][:].rearrange("p (k f) -> p k f", k=8)
                wdb = wset[2][:].rearrange("p (k n) -> p k n", k=2)
                for c in range(NCH):
                    pi = (ex * NCH + c) % 2
                    for fh in range(2):
                        for (pt, pk, wv, wkey) in ((psG[fh], f"psG{fh}", wgb, wk[0]), (psU[fh], f"psU{fh}", wub, wk[1])):
                            for k in range(8):
                                S.op("pe", lambda e, pt=pt, wv=wv, k=k, fh=fh, c=c: e.matmul(
                                    pt[:], lhsT=wv[:, k, fh * 128:(fh + 1) * 128], rhs=x1T[:, k, c * 512:(c + 1) * 512],
                                    start=(k == 0), stop=(k == 7)), reads=[wkey, "x1T"], writes=[pk])
                        S.op("act", lambda e, fh=fh: e.activation(out=sg[fh][:], in_=psG[fh][:], func=AF.Silu),
                             reads=[f"psG{fh}"], writes=[f"sg{fh}"])
                        S.op("dve", lambda e, fh=fh, pi=pi: e.tensor_tensor(out=aT[pi][fh][:], in0=sg[fh][:], in1=psU[fh][:],
                                                                           op=ALU.mult),
                             reads=[f"sg{fh}", f"psU{fh}"], writes=[f"aT{pi}_{fh}"])
                    for tt in range(4):
                        t = c * 4 + tt
                        dj = (c * 4 + tt) % 2
                        for h in range(2):
                            for fh in range(2):
                                S.op("pe", lambda e, dj=dj, h=h, fh=fh, tt=tt, pi=pi, wdb=wdb: e.matmul(
                                    psD[dj][h][:], lhsT=aT[pi][fh][:, tt * 128:(tt + 1) * 128],
                                    rhs=wdb[:, fh, h * 512:(h + 1) * 512], start=(fh == 0), stop=(fh == 1)),
                                    reads=[f"aT{pi}_{fh}", wk[2]], writes=[f"psD{dj}_{h}"])
                            S.op("dve", lambda e, dj=dj, h=h, t=t, ex=ex: e.scalar_tensor_tensor(
                                out=yacc[:, t, h * 512:(h + 1) * 512], in0=psD[dj][h][:], scalar=G[:, t, ex:ex + 1],
                                in1=yacc[:, t, h * 512:(h + 1) * 512], op0=ALU.mult, op1=ALU.add),
                                reads=[f"psD{dj}_{h}", f"G{t}", f"yacc{t}"], writes=[f"yacc{t}"])
            for t in range(NT if stage >= 3 else 0):
                gt = g * NT + t
                oo = outt[gt % 2]
                layer_norm(lambda t=t: yacc[:, t, :], f"yacc{t}", oo[:], f"outt{gt % 2}", 2, "ln2")
                S.dma("sp", lambda e, oo=oo, gt=gt: e.dma_start(out=xout_v[gt], in_=oo[:]), reads=[f"outt{gt % 2}"])
        if stage < 3:
            S.dma('sp', lambda e: e.dma_start(out=xout_v[0], in_=lnb[:, 0, :]), reads=['lnb'])
        S.emit()
    return nc


_NCS = {}


def _nc(name, fn):
    if name not in _NCS:
        _NCS[name] = fn()
    return _NCS[name]


def _c(a):
    return np.ascontiguousarray(a)


def _nsa_in_map(xb_, G, w_in, tbl, inp, ov):
    heads = list(range(G * 4, G * 4 + 4))
    wq = _c(w_in[:, G * 256:(G + 1) * 256])
    wqs = _c(np.concatenate([wq[:, 64:128], wq[:, 0:64], wq[:, 192:256], wq[:, 128:192]], axis=1))
    kvp = lambda i: w_in[:, 1024 + i * 256 + G * 64: 1024 + i * 256 + (G + 1) * 64]
    wkv = _c(np.concatenate([kvp(0), kvp(1), kvp(2), kvp(4), kvp(3), kvp(5)], axis=1))
    wgp = np.zeros((1024, 76), np.float32)
    for br in range(3):
        for r in range(4):
            wgp[:, 64 + br * 4 + r] = w_in[:, 2560 + br * 16 + G * 4 + r]
    w1 = np.concatenate([inp['nsa_cmp_w1_k'][0].reshape(32, 64, 256).transpose(1, 0, 2),
                         inp['nsa_cmp_w1_v'][0].reshape(32, 64, 256).transpose(1, 0, 2)], axis=0)
    posT = np.concatenate([inp['nsa_cmp_pos_k'][0].T, inp['nsa_cmp_pos_v'][0].T], axis=0)
    w2 = np.concatenate([inp['nsa_cmp_w2_k'][0].reshape(2, 128, 64).transpose(1, 0, 2),
                         inp['nsa_cmp_w2_v'][0].reshape(2, 128, 64).transpose(1, 0, 2)], axis=2)
    bss, bsw, bsc = nsa_bias_strips(tbl, heads)
    return dict(xT=xb_, wq=wq, wqs=wqs, wkv=wkv, wgp=wgp, w1=_c(w1), posT=_c(posT), w2=_c(w2), ovm=ov, bss=bss, bsw=bsw, bsc=bsc,
                cfar=_c(np.broadcast_to(tbl[31, heads][None, :], (128, 4))))


def kernel(**inputs):
    inp = {k: np.asarray(v) for k, v in inputs.items()}
    x = inp['x']
    B, Sq, _ = x.shape
    tbl = inp['rel_table']
    T = B * Sq // 8
    cur = x
    for layer in range(4):
        kind = layer % 4
        xTs = [_c(cur[b].T) for b in range(B)]
        maps = []
        if kind == 0:
            nca = _nc('dil', lambda: build_dil(Sq))
            w_in, w_out = inp['dil_w_in'][0], inp['dil_w_out'][0]
            xperm = [[_c(xTs[b][:, dil_perm(Sq, d)]) for (_, d) in DIL] for b in range(B)]
            for c in range(8):
                b, hg = c // 4, c % 4
                heads = list(range(hg * 4, hg * 4 + 4))
                m = {f"xT{g}": xperm[b][g] for g in range(3)}
                wA = np.empty((3, 4, 1024, 192), np.float32)
                for g in range(3):
                    for i, h in enumerate(heads):
                        for j in range(3):
                            wA[g, i, :, j * 64:(j + 1) * 64] = w_in[:, g * 3072 + j * 1024 + h * 64: g * 3072 + j * 1024 + (h + 1) * 64]
                m["wA"] = wA
                m["btA"] = dil_bias_tiles(tbl, heads)
                maps.append(m)
        elif kind == 1:
            nca = _nc('sb', lambda: build_sb(Sq))
            w_in, w_out = inp['sb_w_in'][0], inp['sb_w_out'][0]
            cm = sb_consts()
            for c in range(8):
                b, hg = c // 4, c % 4
                cols = slice(hg * 256, (hg + 1) * 256)
                maps.append(dict(xT=xTs[b], wq=_c(w_in[:, 0:1024][:, cols]), wk=_c(w_in[:, 1024:2048][:, cols]),
                                 wv=_c(w_in[:, 2048:3072][:, cols]), cm=cm))
        elif kind == 2:
            nca = _nc('nsa', lambda: build_nsa(Sq))
            w_in, w_out = inp['nsa_w_in'][0], inp['nsa_w_out'][0]
            ov = nsa_consts(Sq)
            for c in range(8):
                maps.append(_nsa_in_map(xTs[c // 4], c % 4, w_in, tbl, inp, ov))
        else:
            nca = _nc('moba', lambda: build_moba(Sq))
            w_in, w_out = inp['moba_w_in'][0], inp['moba_w_out'][0]
            for c in range(8):
                b, hg = c // 4, c % 4
                cols = slice(hg * 256, (hg + 1) * 256)
                heads = list(range(hg * 4, hg * 4 + 4))
                maps.append(dict(xT=xTs[b], wq=_c(w_in[:, 0:1024][:, cols]), wk=_c(w_in[:, 1024:2048][:, cols]),
                                 wv=_c(w_in[:, 2048:3072][:, cols]), bss=nsa_bias_strips(tbl, heads)[0],
                                 cfar=_c(np.broadcast_to(tbl[31, heads][None, :], (128, 4)))))
        res = run_bass_kernel_spmd(nca, maps, core_ids=list(range(8)))
        oTf = [np.concatenate([res.results[b * 4 + i]['oT'] for i in range(4)], axis=0) for b in range(B)]
        del res, maps, xTs
        ncp = _nc('post', lambda: build_post(T))
        lnp = _c(np.stack([inp['ln1_g'][layer], inp['ln1_b'][layer], inp['ln2_g'][layer], inp['ln2_b'][layer]]))
        curf = cur.reshape(B * Sq, 1024)
        pmaps = []
        for c in range(8):
            b, s0 = (c * T) // Sq, (c * T) % Sq
            pmaps.append(dict(oT=_c(oTf[b][:, s0:s0 + T]), xin=_c(curf[c * T:(c + 1) * T]), w_out=_c(w_out), lnp=lnp,
                              rw=inp['router_w'], rb=_c(inp['router_b'][None, :]), wg=inp['exp_w_gate'][layer],
                              wu=inp['exp_w_up'][layer], wd=inp['exp_w_down'][layer]))
        res = run_bass_kernel_spmd(ncp, pmaps, core_ids=list(range(8)))
        cur = np.concatenate([res.results[c]['xout'] for c in range(8)], axis=0).reshape(B, Sq, 1024)
        del res, pmaps
    return np.asarray(cur, dtype=np.float32)
```

```python
import contextlib
import numpy as np
import concourse.bass as bass
import concourse.mybir as mybir
from concourse.bass_utils import run_bass_kernel_spmd

F32 = mybir.dt.float32
BF16 = mybir.dt.bfloat16
I32 = mybir.dt.int32
AF = mybir.ActivationFunctionType
ALU = mybir.AluOpType
AX = mybir.AxisListType

ENGS = ("pe", "act", "dve", "pool", "sp")
NDSEM = 8


class Sched:
    def __init__(self, nc):
        self.nc = nc
        self.ops = {e: [] for e in ENGS}
        self.lastw = {}
        self.readers = {}
        self.ndma = {e: 0 for e in ENGS}
        self.waited = {e: {} for e in ENGS}

    def _add(self, eng, fn, reads, writes, dma):
        idx = len(self.ops[eng])
        deps = set()
        for b in reads:
            if b in self.lastw:
                deps.add(self.lastw[b])
        for b in writes:
            if b in self.lastw:
                deps.add(self.lastw[b])
            for r in self.readers.get(b, ()):
                deps.add(r)
        op = dict(fn=fn, waits=[], dma=None, sig=False)
        me = (eng, idx)
        if dma:
            k = self.ndma[eng]
            self.ndma[eng] += 1
            slot, val = k % NDSEM, 16 * (k // NDSEM + 1)
            op["dma"] = (slot, val)
            me = ("dma", eng, slot, val)
            if k >= NDSEM:
                deps.add(("dma", eng, slot, val - 16))
        best = {}
        for d in deps:
            if d[0] == "dma":
                key = ("dma", d[1], d[2]); v = d[3]
            else:
                key = d[0]; v = d[1]
                if key == eng and eng == "pe" and not dma:
                    continue
            if v > best.get(key, -1):
                best[key] = v
        w = self.waited[eng]
        for key, v in best.items():
            if w.get(key, -1) >= v:
                continue
            w[key] = v
            op["waits"].append((key, v))
            if key not in ("dma",) and not isinstance(key, tuple):
                self.ops[key][v]["sig"] = True
        self.ops[eng].append(op)
        for b in reads:
            self.readers.setdefault(b, []).append(me)
        for b in writes:
            self.lastw[b] = me
            self.readers[b] = []
        return me

    def op(self, eng, fn, reads=(), writes=()):
        return self._add(eng, fn, tuple(reads), tuple(writes), False)

    def dma(self, eng, fn, reads=(), writes=()):
        return self._add(eng, fn, tuple(reads), tuple(writes), True)

    def fence(self):
        targets = []
        for e in ENGS:
            n = len(self.ops[e])
            for i in range(n - 1, -1, -1):
                o = self.ops[e][i]
                if o["fn"] is not None and o["dma"] is None:
                    targets.append((e, i))
                    break
            k = self.ndma[e]
            for slot in range(min(k, NDSEM)):
                cnt = (k - 1 - slot) // NDSEM + 1
                targets.append((("dma", e, slot), 16 * cnt))
        for e in ENGS:
            w = self.waited[e]
            waits = []
            for key, v in targets:
                if key == e and not isinstance(key, tuple):
                    continue
                if w.get(key, -1) >= v:
                    continue
                w[key] = v
                waits.append((key, v))
                if not isinstance(key, tuple):
                    self.ops[key][v]["sig"] = True
            if waits:
                self.ops[e].append(dict(fn=None, waits=waits, dma=None, sig=False))

    def emit(self, final_engine="sp"):
        nc = self.nc
        fin_waits = []
        for e in ENGS:
            n = self.ndma[e]
            for slot in range(min(n, NDSEM)):
                cnt = (n - 1 - slot) // NDSEM + 1
                fin_waits.append((("dma", e, slot), 16 * cnt))
        self.ops[final_engine].append(dict(fn=None, waits=fin_waits, dma=None, sig=False))
        sigcnt = {}
        for e in ENGS:
            c = 0
            arr = []
            for o in self.ops[e]:
                if o["sig"]:
                    c += 1
                arr.append(c)
            sigcnt[e] = arr
        import contextlib
        with contextlib.ExitStack() as st:
            esem = {e: st.enter_context(nc.semaphore(f"s_{e}")) for e in ENGS}
            dsem = {e: [st.enter_context(nc.semaphore(f"d_{e}{i}")) for i in range(NDSEM)]
                    for e in ENGS if self.ndma[e] > 0}
            block = st.enter_context(nc.Block())

            def run(e, eng):
                for i, o in enumerate(self.ops[e]):
                    for key, v in o["waits"]:
                        if isinstance(key, tuple):
                            eng.wait_ge(dsem[key[1]][key[2]], v)
                        else:
                            eng.wait_ge(esem[key], sigcnt[key][v])
                    if o["fn"] is None:
                        continue
                    ins = o["fn"](eng)
                    if o["dma"] is not None:
                        ins.then_inc(dsem[e][o["dma"][0]], 16)
                    elif o["sig"]:
                        ins.then_inc(esem[e], 1)

            if self.ops["pe"]:
                @block.tensor
                def _(eng):
                    run("pe", eng)
            if self.ops["act"]:
                @block.scalar
                def _(eng):
                    run("act", eng)
            if self.ops["dve"]:
                @block.vector
                def _(eng):
                    run("dve", eng)
            if self.ops["pool"]:
                @block.gpsimd
                def _(eng):
                    run("pool", eng)
            if self.ops["sp"]:
                @block.sync
                def _(eng):
                    run("sp", eng)


D = 1024
HD = 64
SCALE = 0.125
NEG = -30000.0


class KB:
    def __init__(self):
        self.nc = bass.Bass("TRN2", target_bir_lowering=False)
        self.S = Sched(self.nc)
        self.st = contextlib.ExitStack()
        self._n = 0

    def din(self, name, shape, dt=F32):
        return self.nc.dram_tensor(name, list(shape), dt, kind="ExternalInput").ap()

    def dout(self, name, shape, dt=F32):
        return self.nc.dram_tensor(name, list(shape), dt, kind="ExternalOutput").ap()

    def sb(self, name, shape, dt, st=None):
        t = (st or self.st).enter_context(self.nc.sbuf_tensor(name, list(shape), dt))
        return t

    def ps(self, name, shape=(128, 512), dt=F32):
        return self.st.enter_context(self.nc.psum_tensor(name, list(shape), dt))

    def finish(self):
        self.S.emit()
        self.st.close()
        return self.nc


def pipeline(units, stages, skews):
    n = len(units)
    if n == 0:
        return
    for t in range(n + max(skews)):
        for s, fn in enumerate(stages):
            u = t - skews[s]
            if 0 <= u < n:
                fn(u, units[u])


def load_w_bf16(kb, name, w_ap, ncols, stg, stg_key):
    S = kb.S
    wb = kb.sb(name, [128, 8, ncols], BF16)
    S.dma("sp", lambda e: e.dma_start(out=stg[:, 0:8 * ncols].rearrange("p (k n) -> p k n", k=8),
                                      in_=w_ap.rearrange("(k p) n -> p k n", p=128)), writes=[stg_key])
    S.op("pool", lambda e: e.tensor_copy(out=wb[:].rearrange("p k n -> p (k n)"), in_=stg[:, 0:8 * ncols]),
         reads=[stg_key], writes=[name])
    return wb


def project(kb, xT, Sq, fm_outs, tm_outs, pss, xs, xb):
    S = kb.S
    xv = xT.rearrange("(k p) t -> p k t", p=128)
    NC = Sq // 512
    pi = 0
    for c in range(NC):
        xsi = xs[c % len(xs)]
        if isinstance(xsi, tuple):
            xsi, xsk = xsi
        else:
            xsk = f"xs{c % len(xs)}"
        xbi = xb[c % len(xb)]
        if isinstance(xbi, tuple):
            xbi, xbk = xbi
        else:
            xbk = f"xb{c % len(xb)}"
        S.dma("sp", lambda e, xsi=xsi, c=c: e.dma_start(out=xsi[:], in_=xv[:, :, c * 512:(c + 1) * 512]), writes=[xsk])
        S.op("dve", lambda e, xsi=xsi, xbi=xbi: e.tensor_copy(out=xbi[:, 0:4, :], in_=xsi[:, 0:4, :]), reads=[xsk], writes=[xbk])
        S.op("act", lambda e, xsi=xsi, xbi=xbi: e.copy(out=xbi[:, 4:8, :], in_=xsi[:, 4:8, :]), reads=[xsk], writes=[xbk])
        for (w, wkey, M, dst, dkey, scale, ev) in fm_outs:
            p, pk = pss[pi % len(pss)]
            pi += 1
            for k in range(8):
                S.op("pe", lambda e, p=p, w=w, k=k, M=M, xbi=xbi: e.matmul(p[0:M, :], lhsT=w[:, k, 0:M], rhs=xbi[:, k, :],
                                                                          start=(k == 0), stop=(k == 7)),
                     reads=[wkey, xbk], writes=[pk])
            if ev == "act":
                S.op("act", lambda e, p=p, M=M, dst=dst, c=c, scale=scale: e.mul(dst[0:M, c * 512:(c + 1) * 512], p[0:M, :], scale),
                     reads=[pk], writes=[dkey])
            else:
                S.op("dve", lambda e, p=p, M=M, dst=dst, c=c, scale=scale: e.tensor_scalar(
                    out=dst[0:M, c * 512:(c + 1) * 512], in0=p[0:M, :], scalar1=scale, scalar2=None, op0=ALU.mult),
                    reads=[pk], writes=[dkey])
        for (w, wkey, N, dst_fn, dkey) in tm_outs:
            p, pk = pss[pi % len(pss)]
            pi += 1
            for tt in range(4):
                for k in range(8):
                    S.op("pe", lambda e, p=p, w=w, k=k, N=N, tt=tt, xbi=xbi: e.matmul(
                        p[:, tt * N:(tt + 1) * N], lhsT=xbi[:, k, tt * 128:(tt + 1) * 128], rhs=w[:, k, 0:N],
                        start=(k == 0), stop=(k == 7)), reads=[wkey, xbk], writes=[pk])
            for tt in range(4):
                for (dap, lo, hi) in dst_fn(c * 4 + tt):
                    S.op("dve", lambda e, p=p, N=N, tt=tt, dap=dap, lo=lo, hi=hi: e.tensor_copy(out=dap, in_=p[:, tt * N + lo:tt * N + hi]),
                         reads=[pk], writes=[dkey])


def build_sb(Sq):
    kb = KB()
    S = kb.S
    NT, NCH = Sq // 128, Sq // 512
    xT = kb.din("xT", [D, Sq])
    wq = kb.din("wq", [D, 256])
    wk = kb.din("wk", [D, 256])
    wv = kb.din("wv", [D, 256])
    cm = kb.din("cm", [128, 4 * 512 + 256])
    oT = kb.dout("oT", [256, Sq], BF16)

    stg = kb.sb("stg", [128, 8 * 256], F32)
    cmf = kb.sb("cmf", [128, 4 * 512 + 256], F32)
    cmb = kb.sb("cmb", [128, 4 * 512 + 256], BF16)
    one1 = kb.sb("one1", [128, 1], F32)
    S.dma("sp", lambda e: e.dma_start(out=cmf[:], in_=cm), writes=["cmf"])
    S.op("pool", lambda e: e.tensor_copy(out=cmb[:], in_=cmf[:]), reads=["cmf"], writes=["cmb"])
    S.op("pool", lambda e: e.memset(one1[:], 1.0), writes=["one1"])
    ones_b = cmb[:, 2048:2176]
    umat_b = cmb[:, 2176:2304]
    wqb = load_w_bf16(kb, "wqb", wq, 256, stg, "stg")
    wkb = load_w_bf16(kb, "wkb", wk, 256, stg, "stg")
    wvb = load_w_bf16(kb, "wvb", wv, 256, stg, "stg")

    QT = kb.sb("QT", [128, Sq], BF16)
    KT = kb.sb("KT", [128, Sq], BF16)
    V = kb.sb("V", [128, NT, 128], BF16)
    xs = [kb.sb(f"xs{i}", [128, 8, 512], F32) for i in range(1)]
    xb = [kb.sb(f"xb{i}", [128, 8, 512], BF16) for i in range(2)]
    NB = 3
    NZ = 6
    zc = [kb.sb(f"zc{i}", [128, 512], F32) for i in range(NZ)]
    ee = [kb.sb(f"ee{i}", [128, 512], F32) for i in range(2)]
    sp = [kb.sb(f"sp{i}", [128, 512], BF16) for i in range(NB)]
    t1 = [kb.sb(f"t1{i}", [128, 512], F32) for i in range(NB)]
    att = [kb.sb(f"att{i}", [128, 512], BF16) for i in range(NB)]
    osb = [kb.sb(f"osb{i}", [64, 512], BF16) for i in range(2)]
    totb = [kb.sb(f"totb{i}", [128, 512], F32) for i in range(2)]

    psz = [kb.ps(f"psz{i}") for i in range(2)]
    psT = [kb.ps(f"psT{i}") for i in range(2)]
    psL = [kb.ps(f"psL{i}") for i in range(2)]
    pso = [kb.ps(f"pso{i}") for i in range(2)]
    pss = [(psz[0], "psz0"), (psz[1], "psz1"), (psL[0], "psL0"), (psL[1], "psL1")]

    for pair in range(2):
        fm = [(wqb[:, :, pair * 128:(pair + 1) * 128], "wqb", 128, QT, "QT", SCALE, "act"),
              (wkb[:, :, pair * 128:(pair + 1) * 128], "wkb", 128, KT, "KT", 1.0, "dve")]
        tm = [(wvb[:, :, pair * 128:(pair + 1) * 128], "wvb", 128, (lambda t: [(V[:, t, :], 0, 128)]), "V")]
        project(kb, xT, Sq, fm, tm, pss, xs, xb)

        units = []
        for c in range(NCH):
            jmax = 4 * c + 3
            for j in range(jmax, -1, -1):
                for h in range(2):
                    units.append((c, j, h, jmax))

        def s1(u, un):
            c, j, h, jmax = un
            pz, pzk = psz[u % 2], f"psz{u % 2}"
            hp = slice(h * 64, (h + 1) * 64)
            S.op("pe", lambda e: e.matmul(pz[:], lhsT=KT[hp, j * 128:(j + 1) * 128], rhs=QT[hp, c * 512:(c + 1) * 512],
                                          start=True, stop=True), reads=["KT", "QT"], writes=[pzk])

        def s2a(u, un):
            c, j, h, jmax = un
            pz, pzk = psz[u % 2], f"psz{u % 2}"
            bz = u % NZ
            S.op("dve", lambda e: e.tensor_scalar(out=zc[bz][:], in0=pz[:], scalar1=-60.0, scalar2=60.0,
                                                  op0=ALU.max, op1=ALU.min), reads=[pzk], writes=[f"zc{bz}"])

        def s2b(u, un):
            c, j, h, jmax = un
            bz, be, b = u % NZ, u % 2, u % NB
            S.op("act", lambda e: e.activation(out=ee[be][:], in_=zc[bz][:], func=AF.Exp), reads=[f"zc{bz}"], writes=[f"ee{be}"])
            S.op("act", lambda e: e.activation(out=sp[b][:], in_=ee[be][:], func=AF.Ln, bias=one1[:, 0:1]),
                 reads=[f"ee{be}", "one1"], writes=[f"sp{b}"])
            if j >= 4 * c:
                o = j - 4 * c
                S.op("pool", lambda e: e.tensor_tensor(out=sp[b][:], in0=sp[b][:], in1=cmb[:, o * 512:(o + 1) * 512], op=ALU.mult),
                     reads=[f"sp{b}", "cmb"], writes=[f"sp{b}"])

        def s3b(u, un):
            c, j, h, jmax = un
            bz = u % NZ
            if j != jmax:
                S.op("pool", lambda e: e.tensor_tensor(out=zc[bz][:], in0=zc[bz][:], in1=totb[h][:], op=ALU.subtract),
                     reads=[f"zc{bz}", f"totb{h}"], writes=[f"zc{bz}"])

        def s3(u, un):
            c, j, h, jmax = un
            b = u % NB
            pl, plk = psL[u % 2], f"psL{u % 2}"
            p1, p1k = psT[u % 2], f"psT{u % 2}"
            S.op("pe", lambda e: e.matmul(pl[:], lhsT=umat_b, rhs=sp[b][:], start=True, stop=True),
                 reads=[f"sp{b}", "cmb"], writes=[plk])
            S.op("pe", lambda e: e.matmul(p1[:], lhsT=ones_b, rhs=sp[b][:], start=True, stop=True),
                 reads=[f"sp{b}", "cmb"], writes=[p1k])

        def s4a(u, un):
            c, j, h, jmax = un
            bz, b = u % NZ, u % NB
            pl, plk = psL[u % 2], f"psL{u % 2}"
            p1, p1k = psT[u % 2], f"psT{u % 2}"
            S.op("dve", lambda e: e.tensor_tensor(out=t1[b][:], in0=zc[bz][:], in1=pl[:], op=ALU.subtract),
                 reads=[f"zc{bz}", plk], writes=[f"t1{b}"])
            if j != jmax:
                S.op("dve", lambda e: e.tensor_tensor(out=totb[h][:], in0=totb[h][:], in1=p1[:], op=ALU.add),
                     reads=[f"totb{h}", p1k], writes=[f"totb{h}"])
            else:
                S.op("dve", lambda e: e.tensor_copy(out=totb[h][:], in_=p1[:]), reads=[p1k], writes=[f"totb{h}"])

        def s4b(u, un):
            c, j, h, jmax = un
            b = u % NB
            S.op("act", lambda e: e.activation(out=att[b][:], in_=t1[b][:], func=AF.Exp), reads=[f"t1{b}"], writes=[f"att{b}"])
            if j >= 4 * c:
                o = j - 4 * c
                S.op("pool", lambda e: e.tensor_tensor(out=att[b][:], in0=att[b][:], in1=cmb[:, o * 512:(o + 1) * 512], op=ALU.mult),
                     reads=[f"att{b}", "cmb"], writes=[f"att{b}"])

        def s5(u, un):
            c, j, h, jmax = un
            b = u % NB
            S.op("pe", lambda e: e.matmul(pso[h][0:64, :], lhsT=V[:, j, h * 64:(h + 1) * 64], rhs=att[b][:],
                                          start=(j == jmax), stop=(j == 0)), reads=[f"att{b}", "V"], writes=[f"pso{h}"])
            if j == 0:
                ob = osb[h]
                S.op("act", lambda e: e.copy(out=ob[:], in_=pso[h][0:64, :]), reads=[f"pso{h}"], writes=[f"osb{h}"])
                r0 = pair * 128 + h * 64
                S.dma("sp", lambda e: e.dma_start(out=oT[r0:r0 + 64, c * 512:(c + 1) * 512], in_=ob[:]), reads=[f"osb{h}"])

        n_u = len(units)
        stages = ((s2a, 0), (s2b, 2), (s3, 3), (s4a, 4), (s3b, 3), (s4b, 5), (s5, 6))
        for t in range(n_u + 7):
            if t % 2 == 0:
                for u in (t, t + 1):
                    if u < n_u:
                        s1(u, units[u])
            for fn_, sk_ in stages:
                u = t - sk_
                if 0 <= u < n_u:
                    fn_(u, units[u])
    return kb.finish()


def sb_consts():
    cm = np.zeros((128, 4 * 512 + 256), np.float32)
    k = np.arange(128)[:, None]
    q = np.arange(512)[None, :]
    for o in range(4):
        cm[:, o * 512:(o + 1) * 512] = ((o * 128 + k) < q).astype(np.float32)
    cm[:, 2048:2176] = 1.0
    jj = np.arange(128)[:, None]
    ss = np.arange(128)[None, :]
    cm[:, 2176:2304] = (jj >= ss).astype(np.float32)
    return cm


class SoftmaxPipe:
    def __init__(self, kb, nS=3, nP=3):
        import os
        nP = int(os.environ.get('SM_NP', nP))
        self.kb = kb
        self.psS = [kb.ps(f"psS{i}") for i in range(nS)]
        self.PT = [kb.sb(f"PT{i}", [128, 512], BF16) for i in range(nP)]
        self.nS, self.nP = nS, nP
        self.cnt = 0

    def run(self, units):
        S = self.kb.S
        base = self.cnt
        self.cnt += len(units)

        def s1(u, un):
            i = (base + u) % self.nS
            ps, pk = self.psS[i], f"psS{i}"
            n = len(un["mm"])
            kp, N = un.get("kp", 128), un["N"]
            for m, mmx in enumerate(un["mm"]):
                (lhsT, rhs, reads) = mmx[:3]
                r0, r1 = mmx[3] if len(mmx) > 3 else (0, kp)
                S.op("pe", lambda e, lhsT=lhsT, rhs=rhs, m=m, r0=r0, r1=r1: e.matmul(ps[r0:r1, 0:N], lhsT=lhsT, rhs=rhs, start=(m == 0), stop=(m == n - 1)),
                     reads=reads, writes=[pk])

        def s2(u, un):
            i = (base + u) % self.nS
            ps, pk = self.psS[i], f"psS{i}"
            b = (base + u) % self.nP
            kp, N = un.get("kp", 128), un["N"]
            cb = un.get("cb")
            if cb is None:
                S.op("act", lambda e: e.activation(out=self.PT[b][0:kp, 0:N], in_=ps[0:kp, 0:N], func=AF.Exp),
                     reads=[pk], writes=[f"PT{b}"])
            else:
                S.op("act", lambda e: e.activation(out=self.PT[b][0:kp, 0:N], in_=ps[0:kp, 0:N], func=AF.Exp, bias=cb[0]),
                     reads=[pk, cb[1]], writes=[f"PT{b}"])
            if un.get("post"):
                un["post"](self.PT[b], f"PT{b}")

        def s3(u, un):
            b = (base + u) % self.nP
            kp, N = un.get("kp", 128), un["N"]
            for pv in un["pv"]:
                (lhsT_v, reads, acc, acck, start, stop) = pv[:6]
                c0, c1 = pv[6] if len(pv) > 6 else (0, N)
                S.op("pe", lambda e, lhsT_v=lhsT_v, acc=acc, start=start, stop=stop, c0=c0, c1=c1: e.matmul(
                    acc, lhsT=lhsT_v, rhs=self.PT[b][0:kp, c0:c1], start=start, stop=stop),
                    reads=[f"PT{b}"] + list(reads), writes=[acck])
            if un.get("fin"):
                un["fin"]()

        import os
        pipeline(units, [s1, s2, s3], [int(v) for v in os.environ.get('SM_SKEW', '0,1,2').split(',')])


class SoftmaxPairPipe:
    def __init__(self, kb, nU=2, nP=3):
        import os
        nP = int(os.environ.get('SMP_NP', 4))
        self.kb = kb
        self.psS = [[kb.ps(f"psS{u}_{h}") for h in range(2)] for u in range(nU)]
        self.PT = [[kb.sb(f"PT{u}_{h}", [128, 512], BF16) for h in range(2)] for u in range(nP)]
        self.nU, self.nP = nU, nP
        self.cnt = 0

    def run(self, units):
        S = self.kb.S
        base = self.cnt
        self.cnt += len(units)

        def s1(u, un):
            i = (base + u) % self.nU
            N = un["N"]
            cnt = [0, 0]
            tot = [sum(1 for m in un["mm"] if m[3] == h) for h in range(2)]
            for (lhsT, rhs, reads, h) in un["mm"]:
                ps, pk = self.psS[i][h], f"psS{i}_{h}"
                first, last = cnt[h] == 0, cnt[h] == tot[h] - 1
                cnt[h] += 1
                S.op("pe", lambda e, lhsT=lhsT, rhs=rhs, ps=ps, first=first, last=last: e.matmul(ps[:, 0:N], lhsT=lhsT, rhs=rhs, start=first, stop=last),
                     reads=reads, writes=[pk])

        def s2(u, un):
            i = (base + u) % self.nU
            b = (base + u) % self.nP
            N = un["N"]
            for h in range(2):
                ps, pk = self.psS[i][h], f"psS{i}_{h}"
                cb = un["cb"][h]
                if cb is None:
                    S.op("act", lambda e, ps=ps, h=h: e.activation(out=self.PT[b][h][:, 0:N], in_=ps[:, 0:N], func=AF.Exp),
                         reads=[pk], writes=[f"PT{b}_{h}"])
                else:
                    S.op("act", lambda e, ps=ps, h=h, cb=cb: e.activation(out=self.PT[b][h][:, 0:N], in_=ps[:, 0:N], func=AF.Exp, bias=cb[0]),
                         reads=[pk, cb[1]], writes=[f"PT{b}_{h}"])

        def s3(u, un):
            b = (base + u) % self.nP
            N = un["N"]
            for (lhsT_v, reads, acc, acck, start, stop, h) in un["pv"]:
                S.op("pe", lambda e, lhsT_v=lhsT_v, acc=acc, start=start, stop=stop, h=h: e.matmul(
                    acc, lhsT=lhsT_v, rhs=self.PT[b][h][:, 0:N], start=start, stop=stop),
                    reads=[f"PT{b}_{h}"] + list(reads), writes=[acck])
            if un.get("fin"):
                un["fin"]()

        import os
        pipeline(units, [s1, s2, s3], [int(v) for v in os.environ.get('SMP_SKEW', '0,1,2').split(',')])


class Normalizer:
    def __init__(self, kb):
        self.kb = kb
        self.rdt = kb.sb("nz_rdt", [128, 512], F32)
        self.num = kb.sb("nz_num", [64, 512], F32)
        self.onesf = kb.sb("nz_ones", [128, 64], F32)
        self.psB = kb.ps("psB")
        kb.S.op("pool", lambda e: e.memset(self.onesf[:], 1.0), writes=["nz_ones"])

    def bcast_recip(self, acc, acck, N):
        S = self.kb.S
        S.op("dve", lambda e: e.reciprocal(out=self.rdt[64:65, 0:N], in_=acc[64:65, 0:N]), reads=[acck], writes=["nz_rdt"])
        S.op("pe", lambda e: e.matmul(self.psB[0:64, 0:N], lhsT=self.onesf[64:65, 0:64], rhs=self.rdt[64:65, 0:N],
                                      start=True, stop=True), reads=["nz_rdt", "nz_ones"], writes=["psB"])

    def normalize(self, acc, acck, N, out_ap, outk):
        S = self.kb.S
        self.bcast_recip(acc, acck, N)
        S.op("act", lambda e: e.copy(out=self.num[:, 0:N], in_=acc[0:64, 0:N]), reads=[acck], writes=["nz_num"])
        S.op("dve", lambda e: e.tensor_tensor(out=out_ap, in0=self.num[:, 0:N], in1=self.psB[0:64, 0:N], op=ALU.mult),
             reads=["nz_num", "psB"], writes=[outk])


NOFF = 16


def build_moba(Sq):
    kb = KB()
    S = kb.S
    NT, NCH, NBLK = Sq // 128, Sq // 512, Sq // 256
    GW = max(NBLK, 8)
    xT = kb.din("xT", [D, Sq])
    wq = kb.din("wq", [D, 256])
    wk = kb.din("wk", [D, 256])
    wv = kb.din("wv", [D, 256])
    bss = kb.din("bss", [4, 128, 2432])
    cfar = kb.din("cfar", [128, 4])
    oT = kb.dout("oT", [256, Sq], BF16)

    stg = kb.sb("stg", [128, 8 * 512], F32)
    wqb = load_w_bf16(kb, "wqb", wq, 256, stg, "stg")
    wkb = load_w_bf16(kb, "wkb", wk, 256, stg, "stg")
    wvb = load_w_bf16(kb, "wvb", wv, 256, stg, "stg")
    cf = kb.sb("cf", [128, 4], F32)
    S.dma("sp", lambda e: e.dma_start(out=cf[:], in_=cfar), writes=["cf"])
    identf = kb.sb("identf", [128, 128], F32)
    identb = kb.sb("identb", [128, 128], BF16)
    onesf = kb.sb("onesf", [128, 128], F32)
    S.op("pool", lambda e: e.memset(onesf[:], 1.0), writes=["onesf"])
    S.op("pool", lambda e: e.affine_select(out=identf[:], in_=onesf[:], pattern=[[-1, 128]], compare_op=ALU.is_equal,
                                           fill=0.0, base=0, channel_multiplier=1), reads=["onesf"], writes=["identf"])
    S.op("pool", lambda e: e.tensor_copy(out=identb[:], in_=identf[:]), reads=["identf"], writes=["identb"])

    QT = kb.sb("QT", [128, Sq], BF16)
    KT = kb.sb("KT", [128, Sq], BF16)
    Vh = [kb.sb(f"V{h}", [128, NT, 65], BF16) for h in range(2)]
    for h in range(2):
        S.op("pool", lambda e, h=h: e.memset(Vh[h][:, :, 64:65], 1.0), writes=["V"])
    maskT = kb.sb("maskT", [128, Sq], BF16)
    bs = kb.sb("bs", [128, 2, 2432], BF16)
    xs = [(stg[:].rearrange("p (k t) -> p k t", k=8), "stg")]
    xb = [kb.sb(f"xb{i}", [128, 8, 512], BF16) for i in range(2)]
    km = kb.sb("km", [128, NBLK], F32)
    kmb = kb.sb("kmb", [128, NBLK], BF16)
    gbuf = kb.sb("gbuf", [128, GW], F32)
    m8 = kb.sb("m8", [128, 8], F32)
    mb2 = kb.sb("mb2", [128, 128], F32)
    osb = [kb.sb(f"osb{i}", [64, 512], BF16) for i in range(2)]

    sp_ = SoftmaxPairPipe(kb)
    nz = Normalizer(kb)
    pso = [[kb.ps(f"pso{h}_0")] for h in range(2)]
    pss = [(sp_.psS[0][0], "psS0_0"), (sp_.psS[0][1], "psS0_1"), (sp_.psS[1][0], "psS1_0")]
    import os
    for i in range(int(os.environ.get("DUMMY_WARM", "0"))):
        S.op("pe", lambda e: e.matmul(sp_.psS[0][0][:, 0:256], lhsT=identb[:], rhs=wqb[:, 0, :], start=True, stop=True),
             reads=["identb", "wqb"], writes=["psS0_0"])

    for pair in range(2):
        fm = [(wqb[:, :, pair * 128:(pair + 1) * 128], "wqb", 128, QT, "QT", SCALE, "act"),
              (wkb[:, :, pair * 128:(pair + 1) * 128], "wkb", 128, KT, "KT", 1.0, "dve")]
        tm = [(wvb[:, :, pair * 128:(pair + 1) * 128], "wvb", 128,
               (lambda t: [(Vh[0][:, t, 0:64], 0, 64), (Vh[1][:, t, 0:64], 64, 128)]), "V")]
        project(kb, xT, Sq, fm, tm, pss, xs, xb)
        for h in range(2):
            for w0 in range(0, 2432, 2048):
                wn = min(2048, 2432 - w0)
                S.dma("sp", lambda e, h=h, pair=pair, w0=w0, wn=wn: e.dma_start(out=stg[:, 0:wn], in_=bss[pair * 2 + h, :, w0:w0 + wn]), writes=["stg"])
                S.op("pool", lambda e, h=h, w0=w0, wn=wn: e.tensor_copy(out=bs[:, h, w0:w0 + wn], in_=stg[:, 0:wn]), reads=["stg"], writes=["bs"])
        S.op("dve", lambda e: e.tensor_reduce(out=km[:], in_=KT[:].rearrange("p (b k) -> p b k", k=256), axis=AX.X, op=ALU.add),
             reads=["KT"], writes=["km"])
        S.op("dve", lambda e: e.tensor_copy(out=kmb[:], in_=km[:]), reads=["km"], writes=["kmb"])
        for qt in range(NT):
            nv = qt // 2
            S.op("pool", lambda e: e.memset(mb2[:], NEG), writes=["mb2"])
            for h in range(2):
                hp = slice(h * 64, (h + 1) * 64)
                c0 = h * 64
                pg, pgk = pss[(qt * 2 + h) % 3]
                if nv > 3:
                    S.op("pe", lambda e, pg=pg, hp=hp, qt=qt: e.matmul(pg[:, 0:NBLK], lhsT=QT[hp, qt * 128:(qt + 1) * 128], rhs=kmb[hp, :],
                                                                       start=True, stop=True), reads=["QT", "kmb"], writes=[pgk])
                    S.op("pool", lambda e: e.memset(gbuf[:], -1e30), writes=["gbuf"])
                    S.op("dve", lambda e, pg=pg, nv=nv: e.tensor_copy(out=gbuf[:, 0:nv], in_=pg[:, 0:nv]), reads=[pgk], writes=["gbuf"])
                    S.op("dve", lambda e: e.max(out=m8[:], in_=gbuf[:]), reads=["gbuf"], writes=["m8"])
                    S.op("dve", lambda e, c0=c0: e.tensor_scalar(out=mb2[:, c0:c0 + NBLK], in0=gbuf[:, 0:NBLK], scalar1=m8[:, 2:3], scalar2=NEG,
                                                                 op0=ALU.is_lt, op1=ALU.mult), reads=["gbuf", "m8"], writes=["mb2"])
                elif nv > 0:
                    S.op("pool", lambda e, nv=nv, c0=c0: e.memset(mb2[:, c0:c0 + nv], 0.0), writes=["mb2"])
                S.op("pool", lambda e, nv=nv, c0=c0: e.memset(mb2[:, c0 + nv:c0 + nv + 1], 0.0), writes=["mb2"])
            pt, ptk = pss[(qt * 2 + 2) % 3]
            S.op("pe", lambda e, pt=pt: e.transpose(pt[:, 0:128], mb2[:], identf[:]), reads=["mb2", "identf"], writes=[ptk])
            S.op("act", lambda e, pt=pt, qt=qt: e.copy(out=maskT[:, qt * 128:(qt + 1) * 128], in_=pt[:, 0:128]),
                 reads=[ptk], writes=["maskT"])
        units = []
        for c in range(NCH):
            jmax = 4 * c + 3
            qs = slice(c * 512, (c + 1) * 512)
            for j in range(jmax + 1):
                o = 4 * c - j
                x = j // 2
                mm, cbs, pv = [], [], []
                for h in range(2):
                    hp = slice(h * 64, (h + 1) * 64)
                    mm.append((KT[hp, j * 128:(j + 1) * 128], QT[hp, qs], ["KT", "QT"], h))
                for h in range(2):
                    hp = slice(h * 64, (h + 1) * 64)
                    mm.append((identb[hp, h * 64 + x:h * 64 + x + 1].to_broadcast([64, 128]), maskT[hp, qs], ["identb", "maskT"], h))
                for h in range(2):
                    if o <= 12:
                        mm.append((identb[:], bs[:, h, (o + 3) * 128:(o + 3) * 128 + 512], ["identb", "bs"], h))
                        cbs.append(None)
                    else:
                        cbs.append((cf[:, pair * 2 + h:pair * 2 + h + 1], "cf"))
                    pv.append((Vh[h][:, j, :], ["V"], pso[h][0][0:65, :], f"pso{h}_0", j == 0, j == jmax, h))
                un = dict(mm=mm, N=512, cb=cbs, pv=pv)
                if j == jmax:
                    def fin(c=c, pair=pair):
                        for h in range(2):
                            ob = osb[h]
                            nz.normalize(pso[h][0], f"pso{h}_0", 512, ob[:], f"osb{h}")
                            r0 = pair * 128 + h * 64
                            S.dma("sp", lambda e, ob=ob, r0=r0: e.dma_start(out=oT[r0:r0 + 64, c * 512:(c + 1) * 512], in_=ob[:]), reads=[f"osb{h}"])
                    un["fin"] = fin
                units.append(un)
        sp_.run(units)
    return kb.finish()


def rel_bucket_np(dist):
    n = np.maximum(dist, 0)
    exact = 16
    logf = np.log(np.maximum(n, 1).astype(np.float32) / exact) / np.float32(np.log(2048 / exact))
    large = np.minimum(exact + (logf * 16).astype(np.int32), 31)
    return np.where(n < exact, n, large)


def moba_bias_tiles(rel_table, heads):
    k = np.arange(128)[:, None]
    q = np.arange(512)[None, :]
    out = np.empty((len(heads), NOFF, 128, 512), np.float32)
    for oi in range(NOFF):
        o = oi - 3
        dist = o * 128 + q - k
        bk = rel_bucket_np(dist)
        for i, h in enumerate(heads):
            out[i, oi] = np.where(dist >= 0, rel_table[bk, h], np.float32(NEG))
    return out


DIL = ((128, 1), (512, 4), (2048, 16))


def build_dil(Sq):
    kb = KB()
    S = kb.S
    NT, NCH = Sq // 128, Sq // 512
    xTg = [kb.din(f"xT{g}", [D, Sq]) for g in range(3)]
    wA = kb.din("wA", [3, 4, D, 192])
    btA = kb.din("btA", [3, 4, 128, 256])
    oT = kb.dout("oT", [256, Sq], BF16)

    stg = kb.sb("stg", [128, 8 * 192], F32)
    identf = kb.sb("identf", [128, 128], F32)
    identb = kb.sb("identb", [128, 128], BF16)
    onesf = kb.sb("onesf", [128, 128], F32)
    S.op("pool", lambda e: e.memset(onesf[:], 1.0), writes=["onesf"])
    S.op("pool", lambda e: e.affine_select(out=identf[:], in_=onesf[:], pattern=[[-1, 128]], compare_op=ALU.is_equal,
                                           fill=0.0, base=0, channel_multiplier=1), reads=["onesf"], writes=["identf"])
    S.op("pool", lambda e: e.tensor_copy(out=identb[:], in_=identf[:]), reads=["identf"], writes=["identb"])
    QT = kb.sb("QT", [64, Sq], BF16)
    KT = kb.sb("KT", [64, Sq], BF16)
    V = kb.sb("V", [128, NT, 65], BF16)
    S.op("pool", lambda e: e.memset(V[:, :, 64:65], 1.0), writes=["V"])
    accS = kb.sb("accS", [65, Sq], F32)
    wab = kb.sb("wab", [128, 8, 192], BF16)
    btf = kb.sb("btf", [128, 256], F32)
    btb = kb.sb("btb", [128, 256], BF16)
    xs = [kb.sb(f"xs{i}", [128, 8, 512], F32) for i in range(1)]
    xb = [kb.sb(f"xb{i}", [128, 8, 512], BF16) for i in range(2)]
    osb = [kb.sb(f"osb{i}", [64, 512], BF16) for i in range(2)]
    sp_ = SoftmaxPipe(kb)
    nz = Normalizer(kb)
    pso = [kb.ps(f"pso{i}") for i in range(2)]
    pss = [(sp_.psS[0], "psS0"), (sp_.psS[1], "psS1"), (sp_.psS[2], "psS2")]

    for hh in range(4):
        for g, (win, d) in enumerate(DIL):
            L = Sq // d
            Lt = L // 128
            S.dma("sp", lambda e, g=g, hh=hh: e.dma_start(out=stg[:].rearrange("p (k n) -> p k n", k=8),
                                                         in_=wA[g, hh].rearrange("(k p) n -> p k n", p=128)), writes=["stg"])
            S.op("pool", lambda e: e.tensor_copy(out=wab[:].rearrange("p k n -> p (k n)"), in_=stg[:]), reads=["stg"], writes=["wab"])
            S.dma("sp", lambda e, g=g, hh=hh: e.dma_start(out=btf[:], in_=btA[g, hh]), writes=["btf"])
            S.op("pool", lambda e: e.tensor_copy(out=btb[:], in_=btf[:]), reads=["btf"], writes=["btb"])
            fm = [(wab[:, :, 0:64], "wab", 64, QT, "QT", SCALE, "act"), (wab[:, :, 64:128], "wab", 64, KT, "KT", 1.0, "dve")]
            tm = [(wab[:, :, 128:192], "wab", 64, (lambda t: [(V[:, t, 0:64], 0, 64)]), "V")]
            project(kb, xTg[g], Sq, fm, tm, pss, xs, xb)
            accv = accS[:].rearrange("p (i r) -> p r i", r=d)
            units = []
            for T in range(NT):
                r, jl = T // Lt, T % Lt
                has_next = jl + 1 < Lt
                N = 256 if has_next else 128
                mm = [(KT[:, T * 128:(T + 1) * 128], QT[:, T * 128:T * 128 + N], ["KT", "QT"]),
                      (identb[:], btb[:, 0:N], ["identb", "btb"])]
                pv = [(V[:, T, :], ["V"], pso[T % 2][0:65, 0:128], f"pso{T % 2}", jl == 0, True, (0, 128))]
                if has_next:
                    pv.append((V[:, T, :], ["V"], pso[(T + 1) % 2][0:65, 0:128], f"pso{(T + 1) % 2}", True, False, (128, 256)))

                def fin(T=T, r=r, jl=jl, g=g, accv=accv):
                    dst = accv[:, r, jl * 128:(jl + 1) * 128]
                    acc, acck = pso[T % 2], f"pso{T % 2}"
                    if g == 0:
                        S.op("dve", lambda e: e.tensor_copy(out=dst, in_=acc[0:65, 0:128]), reads=[acck], writes=["accS"])
                    else:
                        S.op("dve", lambda e: e.tensor_tensor(out=dst, in0=dst, in1=acc[0:65, 0:128], op=ALU.add),
                             reads=[acck, "accS"], writes=["accS"])
                units.append(dict(mm=mm, N=N, kp=128, pv=pv, fin=fin))
            sp_.run(units)
        for c in range(NCH):
            ob = osb[c % 2]
            cs = slice(c * 512, (c + 1) * 512)
            nz.bcast_recip(accS[:, cs], "accS", 512)
            S.op("dve", lambda e, ob=ob, cs=cs: e.tensor_tensor(out=ob[:], in0=accS[0:64, cs], in1=nz.psB[0:64, :], op=ALU.mult),
                 reads=["accS", "psB"], writes=[f"osb{c % 2}"])
            S.dma("sp", lambda e, ob=ob, cs=cs, hh=hh: e.dma_start(out=oT[hh * 64:(hh + 1) * 64, cs], in_=ob[:]), reads=[f"osb{c % 2}"])
    return kb.finish()


def dil_bias_tiles(rel_table, heads):
    k = np.arange(128)[:, None]
    q = np.arange(256)[None, :]
    steps = q - k
    out = np.empty((3, len(heads), 128, 256), np.float32)
    for g, (win, d) in enumerate(DIL):
        span = win // d
        ok = (steps >= 0) & (steps <= span)
        bk = rel_bucket_np(steps * d)
        for i, h in enumerate(heads):
            out[g, i] = np.where(ok, rel_table[bk, h], np.float32(NEG))
    return out


def dil_perm(Sq, d):
    L = Sq // d
    return (np.arange(d)[:, None] + d * np.arange(L)[None, :]).reshape(-1)


def nsa_consts(Sq):
    NSEL = Sq // 64
    n_cmp = Sq // 16 - 1
    NCT = (n_cmp + 127) // 128
    c = np.arange(NCT * 128)[:, None]
    j = np.arange(NSEL)[None, :]
    ov = ((c >= 4 * j - 1) & (c <= 4 * j + 3) & (c < n_cmp)).astype(np.float32)
    return ov


def nsa_bias_strips(rel_table, heads):
    k = np.arange(128)[:, None]
    H = len(heads)
    def strip(dist, ok):
        bk = rel_bucket_np(dist)
        out = np.empty((H,) + dist.shape, np.float32)
        for i, h in enumerate(heads):
            out[i] = np.where(ok, rel_table[bk, h], np.float32(NEG))
        return out
    ds = np.arange(2432)[None, :] - 384 - k
    dw = np.arange(1408)[None, :] - 384 - k
    dc = np.arange(3584)[None, :] - 16 * k - 31
    return strip(ds, ds >= 0), strip(dw, (dw >= 0) & (dw <= 511)), strip(dc, dc >= 0)


def build_nsa(Sq):
    kb = KB()
    S = kb.S
    NT, NCH, NSEL = Sq // 128, Sq // 512, Sq // 64
    n_cmp = Sq // 16 - 1
    NCT = (n_cmp + 127) // 128
    NCC = NCT * 128
    NH = (NSEL + 127) // 128
    NSP = min(NSEL, 128)
    SW = max(NSEL, 8)
    xT = kb.din("xT", [D, Sq])
    wq = kb.din("wq", [D, 256])
    wqs = kb.din("wqs", [D, 256])
    wkv = kb.din("wkv", [D, 384])
    wgp = kb.din("wgp", [D, 76])
    w1 = kb.din("w1", [128, 32, 256])
    posT = kb.din("posT", [128, 32])
    w2 = kb.din("w2", [128, 2, 128])
    ovm = kb.din("ovm", [NCC, NSEL])
    bss = kb.din("bss", [4, 128, 2432])
    bsw = kb.din("bsw", [4, 128, 1408])
    bsc = kb.din("bsc", [4, 128, 3584])
    cfar = kb.din("cfar", [128, 4])
    oT = kb.dout("oT", [256, Sq], BF16)

    identf = kb.sb("identf", [128, 128], F32)
    identb = kb.sb("identb", [128, 128], BF16)
    onesf = kb.sb("onesf", [128, 128], F32)
    S.op("pool", lambda e: e.memset(onesf[:], 1.0), writes=["onesf"])
    S.op("pool", lambda e: e.affine_select(out=identf[:], in_=onesf[:], pattern=[[-1, 128]], compare_op=ALU.is_equal,
                                           fill=0.0, base=0, channel_multiplier=1), reads=["onesf"], writes=["identf"])
    S.op("pool", lambda e: e.tensor_copy(out=identb[:], in_=identf[:]), reads=["identf"], writes=["identb"])
    cf = kb.sb("cf", [128, 4], F32)
    S.dma("sp", lambda e: e.dma_start(out=cf[:], in_=cfar), writes=["cf"])
    selg = kb.sb("selg", [128, 12 * 64], F32)
    stg = kb.sb("stg", [128, 8 * 512], F32)
    S.op("pool", lambda e: e.memset(stg[:, 0:768], 1.0), writes=["stg"])
    S.op("pool", lambda e: e.affine_select(out=selg[:].rearrange("p (i m) -> p i m", m=64), in_=stg[:, 0:768].rearrange("p (i m) -> p i m", m=64),
                                           pattern=[[-1, 12], [0, 64]], compare_op=ALU.is_equal, fill=0.0, base=-64,
                                           channel_multiplier=1), reads=["stg"], writes=["selg"])
    zerob = kb.sb("zerob", [128, 128], BF16)
    S.op("pool", lambda e: e.memset(zerob[:], 0.0), writes=["zerob"])
    wqb = load_w_bf16(kb, "wqb", wq, 256, stg, "stg")
    wqsb = load_w_bf16(kb, "wqsb", wqs, 256, stg, "stg")
    wgb = load_w_bf16(kb, "wgb", wgp, 76, stg, "stg")
    w2b = kb.sb("w2b", [128, 2, 128], BF16)
    S.dma("sp", lambda e: e.dma_start(out=stg[:, 0:256].rearrange("p (a b) -> p a b", a=2), in_=w2), writes=["stg"])
    S.op("pool", lambda e: e.tensor_copy(out=w2b[:].rearrange("p a b -> p (a b)"), in_=stg[:, 0:256]), reads=["stg"], writes=["w2b"])
    ovb = kb.sb("ovb", [128, NCT, NSEL], BF16)
    for ct in range(NCT):
        S.dma("sp", lambda e, ct=ct: e.dma_start(out=stg[:, 0:NSEL], in_=ovm[ct * 128:(ct + 1) * 128, :]), writes=["stg"])
        S.op("pool", lambda e, ct=ct: e.tensor_copy(out=ovb[:, ct, :], in_=stg[:, 0:NSEL]), reads=["stg"], writes=["ovb"])

    KK = kb.sb("KK", [128, Sq], BF16)
    Vs = kb.sb("Vs", [128, NT, 76], BF16)
    Vw = kb.sb("Vw", [128, NT, 76], BF16)
    vc = kb.sb("vc", [128, NCT, 76], BF16)
    kcT = kb.sb("kcT", [64, NCC], BF16)
    for t_, k_ in ((Vs, "Vs"), (Vw, "Vw"), (vc, "vc")):
        S.op("pool", lambda e, t_=t_: e.memset(t_[:, :, 64:76], 1.0), writes=[k_])
    xs = [(stg[:].rearrange("p (k t) -> p k t", k=8), "stg")]
    xb0 = kb.sb("xb0", [128, 8, 512], BF16)
    xb = [(xb0, "xb0")]
    sp_ = SoftmaxPipe(kb, nS=2, nP=2)
    pss = [(sp_.psS[0], "psS0"), (sp_.psS[1], "psS1")]
    accs = [kb.ps(f"acc{i}") for i in range(2)]
    psB = kb.ps("psB")
    psI = [kb.ps(f"psI{i}") for i in range(2)]
    psR = kb.ps("psR")

    with contextlib.ExitStack() as st0:
        KVc = kb.sb("KVc", [128, Sq], BF16, st0)
        wkvb = kb.sb("wkvb", [128, 8, 384], BF16, st0)
        S.dma("sp", lambda e: e.dma_start(out=stg[:, 0:3072].rearrange("p (k n) -> p k n", k=8), in_=wkv.rearrange("(k p) n -> p k n", p=128)), writes=["stg"])
        S.op("pool", lambda e: e.tensor_copy(out=wkvb[:].rearrange("p k n -> p (k n)"), in_=stg[:, 0:3072]), reads=["stg"], writes=["wkvb"])
        w1b = kb.sb("w1b", [128, 32, 256], BF16, st0)
        hT = kb.sb("hT", [128, 2, 2, NCC], BF16, st0)
        posb = kb.sb("posb", [128, 32], BF16, st0)
        pbias = kb.sb("pbias", [128, 4], F32, st0)
        gx = kb.sb("gx", [128, 512], F32, st0)
        gu = kb.sb("gu", [128, 512], F32, st0)
        for q4 in range(4):
            S.dma("sp", lambda e, q4=q4: e.dma_start(out=stg[:, 0:2048].rearrange("p (a b) -> p a b", a=8), in_=w1[:, q4 * 8:(q4 + 1) * 8, :]),
                  writes=["stg"])
            S.op("pool", lambda e, q4=q4: e.tensor_copy(out=w1b[:, q4 * 8:(q4 + 1) * 8, :].rearrange("p a b -> p (a b)"), in_=stg[:, 0:2048]),
                 reads=["stg"], writes=["w1b"])
        S.dma("sp", lambda e: e.dma_start(out=stg[:, 0:32], in_=posT), writes=["stg"])
        S.op("pool", lambda e: e.tensor_copy(out=posb[:], in_=stg[:, 0:32]), reads=["stg"], writes=["posb"])
        S.op("pool", lambda e: e.memset(hT[:], 0.0), writes=["hT"])
        S.op("pool", lambda e: e.memset(kcT[:], 0.0), writes=["kcT"])
        fm = [(wkvb[:, :, 0:128], "wkvb", 128, KVc, "KVc", 1.0, "act"),
              (wkvb[:, :, 128:256], "wkvb", 128, KK, "KK", 1.0, "dve")]
        tm = [(wkvb[:, :, 256:384], "wkvb", 128, (lambda t: [(Vs[:, t, 0:64], 0, 64), (Vw[:, t, 0:64], 64, 128)]), "V")]
        project(kb, xT, Sq, fm, tm, pss + [(accs[0], "acc0"), (accs[1], "acc1")], xs, xb)
        for kv in range(2):
            rows = slice(kv * 64, (kv + 1) * 64)
            for hc in range(2):
                for p in range(32):
                    S.op("pe", lambda e, rows=rows, hc=hc, p=p, kv=kv: e.matmul(
                        psR[:, kv * 2 + hc:kv * 2 + hc + 1], lhsT=w1b[rows, p, hc * 128:(hc + 1) * 128], rhs=posb[rows, p:p + 1],
                        start=(p == 0), stop=(p == 31)), reads=["w1b", "posb"], writes=["psR"])
        S.op("act", lambda e: e.copy(out=pbias[:], in_=psR[:, 0:4]), reads=["psR"], writes=["pbias"])
        ccs = [(c0, min(512, n_cmp - c0)) for c0 in range(0, n_cmp, 512)]
        KVv = KVc[:].rearrange("p (c s) -> p s c", s=16)
        ui = 0
        for kv in range(2):
            rows = slice(kv * 64, (kv + 1) * 64)
            for hc in range(2):
                for (c0, cn) in ccs:
                    ps_, pk_ = pss[ui % 2]
                    ui += 1
                    for p in range(32):
                        S.op("pe", lambda e, ps_=ps_, rows=rows, hc=hc, p=p, c0=c0, cn=cn: e.matmul(
                            ps_[:, 0:cn], lhsT=w1b[rows, p, hc * 128:(hc + 1) * 128],
                            rhs=KVv[rows, p % 16, c0 + p // 16:c0 + p // 16 + cn], start=(p == 0), stop=(p == 31)),
                            reads=["w1b", "KVc"], writes=[pk_])
                    col = kv * 2 + hc
                    S.op("act", lambda e, ps_=ps_, cn=cn, col=col: e.activation(out=gx[:, 0:cn], in_=ps_[:, 0:cn], func=AF.Identity,
                                                                                bias=pbias[:, col:col + 1]), reads=[pk_, "pbias"], writes=["gx"])
                    S.op("dve", lambda e, cn=cn: e.tensor_tensor(out=gu[:, 0:cn], in0=gx[:, 0:cn], in1=gx[:, 0:cn], op=ALU.mult), reads=["gx"], writes=["gu"])
                    S.op("dve", lambda e, cn=cn: e.tensor_scalar(out=gu[:, 0:cn], in0=gu[:, 0:cn], scalar1=0.044715, scalar2=1.0, op0=ALU.mult, op1=ALU.add),
                         reads=["gu"], writes=["gu"])
                    S.op("dve", lambda e, cn=cn: e.tensor_tensor(out=gu[:, 0:cn], in0=gu[:, 0:cn], in1=gx[:, 0:cn], op=ALU.mult), reads=["gu", "gx"], writes=["gu"])
                    S.op("act", lambda e, cn=cn: e.activation(out=gu[:, 0:cn], in_=gu[:, 0:cn], func=AF.Tanh, scale=0.7978845608028654), reads=["gu"], writes=["gu"])
                    S.op("dve", lambda e, cn=cn: e.tensor_scalar(out=gu[:, 0:cn], in0=gu[:, 0:cn], scalar1=1.0, scalar2=0.5, op0=ALU.add, op1=ALU.mult),
                         reads=["gu"], writes=["gu"])
                    S.op("dve", lambda e, cn=cn, kv=kv, hc=hc, c0=c0: e.tensor_tensor(out=hT[:, kv, hc, c0:c0 + cn], in0=gu[:, 0:cn], in1=gx[:, 0:cn], op=ALU.mult),
                         reads=["gu", "gx"], writes=["hT"])
        for c0 in range(0, NCC, 512):
            cn = min(512, NCC - c0)
            ps_, pk_ = pss[ui % 2]
            ui += 1
            for hc in range(2):
                S.op("pe", lambda e, ps_=ps_, hc=hc, c0=c0, cn=cn: e.matmul(ps_[0:64, 0:cn], lhsT=w2b[:, hc, 0:64], rhs=hT[:, 0, hc, c0:c0 + cn],
                                                                            start=(hc == 0), stop=(hc == 1)), reads=["w2b", "hT"], writes=[pk_])
            S.op("act", lambda e, ps_=ps_, c0=c0, cn=cn: e.copy(out=kcT[:, c0:c0 + cn], in_=ps_[0:64, 0:cn]), reads=[pk_], writes=["kcT"])
        for ct in range(NCT):
            ps_, pk_ = pss[ui % 2]
            ui += 1
            for hc in range(2):
                S.op("pe", lambda e, ps_=ps_, hc=hc, ct=ct: e.matmul(ps_[:, 0:64], lhsT=hT[:, 1, hc, ct * 128:(ct + 1) * 128], rhs=w2b[:, hc, 64:128],
                                                                     start=(hc == 0), stop=(hc == 1)), reads=["w2b", "hT"], writes=[pk_])
            S.op("dve", lambda e, ps_=ps_, ct=ct: e.tensor_copy(out=vc[:, ct, 0:64], in_=ps_[:, 0:64]), reads=[pk_], writes=["vc"])

    S.fence()
    strips = {}
    for nm, src, W in (("bs_s", bss, 2432), ("bs_w", bsw, 1408), ("bs_c", bsc, 3584)):
        t_ = kb.sb(nm, [128, 4, W], BF16)
        strips[nm] = t_
        for r in range(4):
            for w0 in range(0, W, 2048):
                wn = min(2048, W - w0)
                S.dma("sp", lambda e, src=src, r=r, w0=w0, wn=wn: e.dma_start(out=stg[:, 0:wn], in_=src[r, :, w0:w0 + wn]), writes=["stg"])
                S.op("pool", lambda e, t_=t_, r=r, w0=w0, wn=wn: e.tensor_copy(out=t_[:, r, w0:w0 + wn], in_=stg[:, 0:wn]), reads=["stg"], writes=[nm])
    bs_s, bs_w, bs_c = strips["bs_s"], strips["bs_w"], strips["bs_c"]
    Qc = [kb.sb(f"Qc{i}", [128, 512], BF16) for i in range(4)]
    gsb = kb.sb("gsb", [128, 512], F32)
    fgd = kb.sb("fgd", [128, 512], F32)
    numt = kb.sb("numt", [64, 512], F32)
    tmpc = kb.sb("tmpc", [64, 512], F32)
    res = [kb.sb(f"res{r}", [64, 512], F32) for r in range(4)]
    osb = [kb.sb(f"osb{i}", [64, 512], BF16) for i in range(2)]
    impsum = [kb.sb(f"imps{t}", [128, SW], F32) for t in range(4)]
    sc2 = kb.sb("sc2", [128, SW], F32)
    mbias = kb.sb("mbias", [128, SW], F32)
    m8a = kb.sb("m8a", [128, 8], F32)
    m8b = kb.sb("m8b", [128, 8], F32)
    thr = kb.sb("thr", [128, 1], F32)
    rdcol = kb.sb("rdcol", [128, 4], F32)
    maskT = kb.sb("maskT", [128, NH, 512], BF16)
    xv = xT.rearrange("(k p) t -> p k t", p=128)

    def qsel(r):
        return [Qc[0][0:64, :], Qc[2][0:64, :], Qc[1][0:64, :], Qc[3][0:64, :]][r], ["Qc0", "Qc2", "Qc1", "Qc3"][r]

    def qwin(r):
        return [Qc[2][64:128, :], Qc[0][64:128, :], Qc[3][64:128, :], Qc[1][64:128, :]][r], ["Qc2", "Qc0", "Qc3", "Qc1"][r]

    acc_i = [0]

    def finalize(acc, acck, br, r, first, last, c):
        i = br * 4 + r
        S.op("dve", lambda e: e.tensor_scalar(out=fgd[64:76, :], in0=acc[64:76, :], scalar1=1e-30, scalar2=None, op0=ALU.max),
             reads=[acck], writes=["fgd"])
        S.op("dve", lambda e: e.reciprocal(out=fgd[64:76, :], in_=fgd[64:76, :]), reads=["fgd"], writes=["fgd"])
        if br == 0:
            for tt in range(4):
                S.op("pe", lambda e, tt=tt: e.matmul(psR[:, tt:tt + 1], lhsT=fgd[64:65, tt * 128:(tt + 1) * 128], rhs=onesf[64:65, 0:1],
                                                     start=True, stop=True), reads=["fgd", "onesf"], writes=["psR"])
            S.op("act", lambda e: e.copy(out=rdcol[:], in_=psR[:, 0:4]), reads=["psR"], writes=["rdcol"])
        S.op("dve", lambda e: e.tensor_tensor(out=fgd[64:76, :], in0=fgd[64:76, :], in1=gsb[64:76, :], op=ALU.mult),
             reads=["fgd", "gsb"], writes=["fgd"])
        S.op("pe", lambda e: e.matmul(psB[0:64, :], lhsT=selg[64:76, i * 64:(i + 1) * 64], rhs=fgd[64:76, :], start=True, stop=True),
             reads=["fgd", "selg"], writes=["psB"])
        S.op("act", lambda e: e.copy(out=numt[:], in_=acc[0:64, :]), reads=[acck], writes=["numt"])
        if first:
            S.op("dve", lambda e: e.tensor_tensor(out=res[r][:], in0=numt[:], in1=psB[0:64, :], op=ALU.mult),
                 reads=["numt", "psB"], writes=[f"res{r}"])
        else:
            S.op("dve", lambda e: e.tensor_tensor(out=tmpc[:], in0=numt[:], in1=psB[0:64, :], op=ALU.mult),
                 reads=["numt", "psB"], writes=["tmpc"])
            if not last:
                S.op("pool", lambda e: e.tensor_tensor(out=res[r][:], in0=res[r][:], in1=tmpc[:], op=ALU.add),
                     reads=["tmpc", f"res{r}"], writes=[f"res{r}"])
            else:
                ob = osb[r % 2]
                S.op("pool", lambda e: e.tensor_tensor(out=ob[:], in0=res[r][:], in1=tmpc[:], op=ALU.add),
                     reads=["tmpc", f"res{r}"], writes=[f"osb{r % 2}"])
                S.dma("sp", lambda e: e.dma_start(out=oT[r * 64:(r + 1) * 64, c * 512:(c + 1) * 512], in_=ob[:]), reads=[f"osb{r % 2}"])

    for c in range(NCH):
        qs = slice(c * 512, (c + 1) * 512)
        xbi, xbk = xb0, "xb0"
        S.dma("sp", lambda e, c=c: e.dma_start(out=xs[0][0], in_=xv[:, :, c * 512:(c + 1) * 512]), writes=["stg"])
        S.op("dve", lambda e, xbi=xbi: e.tensor_copy(out=xbi[:, 0:4, :], in_=xs[0][0][:, 0:4, :]), reads=["stg"], writes=[xbk])
        S.op("act", lambda e, xbi=xbi: e.copy(out=xbi[:, 4:8, :], in_=xs[0][0][:, 4:8, :]), reads=["stg"], writes=[xbk])
        for qi, (wt, wkey, cols) in enumerate(((wqb, "wqb", 0), (wqb, "wqb", 128), (wqsb, "wqsb", 0), (wqsb, "wqsb", 128))):
            ps_, pk_ = pss[qi % 2]
            for k in range(8):
                S.op("pe", lambda e, ps_=ps_, wt=wt, cols=cols, k=k, xbi=xbi: e.matmul(ps_[:], lhsT=wt[:, k, cols:cols + 128], rhs=xbi[:, k, :],
                                                                                     start=(k == 0), stop=(k == 7)), reads=[wkey, xbk], writes=[pk_])
            S.op("act", lambda e, ps_=ps_, qi=qi: e.mul(Qc[qi][:], ps_[:], SCALE), reads=[pk_], writes=[f"Qc{qi}"])
        for k in range(8):
            S.op("pe", lambda e, k=k, xbi=xbi: e.matmul(psB[0:76, :], lhsT=wgb[:, k, 0:76], rhs=xbi[:, k, :], start=(k == 0), stop=(k == 7)),
                 reads=["wgb", xbk], writes=["psB"])
        S.op("act", lambda e: e.activation(out=gsb[64:76, :], in_=psB[64:76, :], func=AF.Sigmoid), reads=["psB"], writes=["gsb"])

        ctmax = min(NCT - 1, (32 * c + 30) // 128)
        for r in range(4):
            acc, acck = accs[acc_i[0] % 2], f"acc{acc_i[0] % 2}"
            acc_i[0] += 1
            qa, qk = qsel(r)
            units = []
            for ct in range(ctmax + 1):
                o = c - 4 * ct
                mm = [(kcT[:, ct * 128:(ct + 1) * 128], qa, ["kcT", qk])]
                cb = None
                if o <= 6:
                    mm.append((identb[:], bs_c[:, r, o * 512:(o + 1) * 512], ["identb", "bs_c"]))
                else:
                    cb = (cf[:, r:r + 1], "cf")
                un = dict(mm=mm, N=512, cb=cb, pv=[(vc[:, ct, :], ["vc"], acc[0:76, :], acck, ct == 0, ct == ctmax)])
                un["imp"] = (ct, ctmax)
                units.append(un)
            for bnk in range(2):
                S.op("pe", lambda e, bnk=bnk: e.matmul(psI[bnk][:], lhsT=zerob[:], rhs=bs_c[:, 0, 0:512], start=True, stop=False),
                     reads=["zerob", "bs_c"], writes=[f"psI{bnk}"])
            run_cmp(kb, sp_, units, psI, ovb, NSEL)
            finalize(acc, acck, 0, r, True, False, c)
            for tt in range(4):
                src = psI[tt // 2][:, (tt % 2) * 256:(tt % 2) * 256 + NSEL]
                if r == 0:
                    S.op("dve", lambda e, tt=tt, src=src: e.tensor_scalar(out=impsum[tt][:, 0:NSEL], in0=src, scalar1=rdcol[:, tt:tt + 1], scalar2=None,
                                                                          op0=ALU.mult), reads=[f"psI{tt // 2}", "rdcol"], writes=[f"imps{tt}"])
                else:
                    S.op("dve", lambda e, tt=tt, src=src: e.scalar_tensor_tensor(out=impsum[tt][:, 0:NSEL], in0=src, scalar=rdcol[:, tt:tt + 1],
                                                                                 in1=impsum[tt][:, 0:NSEL], op0=ALU.mult, op1=ALU.add),
                         reads=[f"psI{tt // 2}", "rdcol", f"imps{tt}"], writes=[f"imps{tt}"])
        for tt in range(4):
            qt = 4 * c + tt
            sc = impsum[tt]
            sk = f"imps{tt}"
            if SW > NSEL:
                S.op("pool", lambda e, sc=sc: e.memset(sc[:, NSEL:SW], -1e30), writes=[sk])
            if 2 * qt + 2 < NSEL:
                S.op("pool", lambda e, sc=sc, qt=qt: e.memset(sc[:, 2 * qt + 2:NSEL], -1e30), writes=[sk])
            S.op("pool", lambda e, sc=sc, qt=qt: e.memset(sc[0:64, 2 * qt + 1:2 * qt + 2], -1e30), writes=[sk])
            S.op("pool", lambda e, sc=sc: e.memset(sc[:, 0:1], 1e9), writes=[sk])
            lo = max(2 * qt - 1, 0)
            S.op("pool", lambda e, sc=sc, qt=qt, lo=lo: e.memset(sc[0:64, lo:2 * qt + 1], 1e9), writes=[sk])
            S.op("pool", lambda e, sc=sc, qt=qt: e.memset(sc[64:128, 2 * qt:2 * qt + 2], 1e9), writes=[sk])
            S.op("dve", lambda e, sc=sc: e.max(out=m8a[:], in_=sc[:]), reads=[sk], writes=["m8a"])
            S.op("dve", lambda e, sc=sc: e.match_replace(out=sc2[:], in_to_replace=m8a[:], in_values=sc[:], imm_value=-1e30),
                 reads=[sk, "m8a"], writes=["sc2"])
            S.op("dve", lambda e: e.max(out=m8b[:], in_=sc2[:]), reads=["sc2"], writes=["m8b"])
            S.op("dve", lambda e: e.tensor_scalar(out=thr[:], in0=m8b[:, 7:8], scalar1=-1e29, scalar2=None, op0=ALU.max),
                 reads=["m8b"], writes=["thr"])
            S.op("dve", lambda e, sc=sc: e.tensor_scalar(out=mbias[:], in0=sc[:], scalar1=thr[:, 0:1], scalar2=NEG, op0=ALU.is_lt, op1=ALU.mult),
                 reads=[sk, "thr"], writes=["mbias"])
            for jh in range(NH):
                ps_, pk_ = pss[(tt * NH + jh) % 2]
                S.op("pe", lambda e, ps_=ps_, jh=jh: e.transpose(ps_[0:NSP, 0:128], mbias[:, jh * 128:jh * 128 + NSP], identf[:]),
                     reads=["mbias", "identf"], writes=[pk_])
                S.op("act", lambda e, ps_=ps_, jh=jh, tt=tt: e.copy(out=maskT[0:NSP, jh, tt * 128:(tt + 1) * 128], in_=ps_[0:NSP, 0:128]),
                     reads=[pk_], writes=["maskT"])
        jmax = 4 * c + 3
        for r in range(4):
            acc, acck = accs[acc_i[0] % 2], f"acc{acc_i[0] % 2}"
            acc_i[0] += 1
            qa, qk = qsel(r)
            units = []
            for j in range(jmax + 1):
                o = 4 * c - j
                mm = [(KK[0:64, j * 128:(j + 1) * 128], qa, ["KK", qk]),
                      (identb[0:NSP, 2 * (j % 64):2 * (j % 64) + 1].to_broadcast([NSP, 64]), maskT[0:NSP, j // 64, :], ["identb", "maskT"], (0, 64)),
                      (identb[0:NSP, 2 * (j % 64) + 1:2 * (j % 64) + 2].to_broadcast([NSP, 64]), maskT[0:NSP, j // 64, :], ["identb", "maskT"], (64, 128))]
                cb = None
                if o <= 12:
                    mm.append((identb[:], bs_s[:, r, (o + 3) * 128:(o + 3) * 128 + 512], ["identb", "bs_s"]))
                else:
                    cb = (cf[:, r:r + 1], "cf")
                units.append(dict(mm=mm, N=512, cb=cb, pv=[(Vs[:, j, :], ["Vs"], acc[0:76, :], acck, j == 0, j == jmax)]))
            sp_.run(units)
            finalize(acc, acck, 1, r, False, False, c)
        for r in range(4):
            acc, acck = accs[acc_i[0] % 2], f"acc{acc_i[0] % 2}"
            acc_i[0] += 1
            qa, qk = qwin(r)
            units = []
            j0 = max(0, 4 * c - 4)
            for j in range(j0, jmax + 1):
                o = 4 * c - j
                mm = [(KK[64:128, j * 128:(j + 1) * 128], qa, ["KK", qk]),
                      (identb[:], bs_w[:, r, (o + 3) * 128:(o + 3) * 128 + 512], ["identb", "bs_w"])]
                units.append(dict(mm=mm, N=512, pv=[(Vw[:, j, :], ["Vw"], acc[0:76, :], acck, j == j0, j == jmax)]))
            sp_.run(units)
            finalize(acc, acck, 2, r, False, True, c)
    return kb.finish()


def run_cmp(kb, sp_, units, psI, ovb, NSEL):
    S = kb.S
    for un in units:
        ct, ctmax = un["imp"]

        def post(PTb, PTk, ct=ct, ctmax=ctmax):
            for tt in range(4):
                S.op("pe", lambda e, tt=tt: e.matmul(psI[tt // 2][:, (tt % 2) * 256:(tt % 2) * 256 + NSEL],
                                                     lhsT=PTb[:, tt * 128:(tt + 1) * 128], rhs=ovb[:, ct, :],
                                                     start=False, stop=(ct == ctmax and tt % 2 == 1)),
                     reads=[PTk, "ovb"], writes=[f"psI{tt // 2}"])
        un["post"] = post
    sp_.run(units)


ALPHA = 8 ** 0.25
LN_EPS = 1e-5
D = 1024
NE = 16
DE = 256


def build_post(T, stage=9):
    nc = bass.Bass("TRN2", target_bir_lowering=False)
    TG = min(1024, T)
    NG = T // TG
    NT = TG // 128
    NCH = TG // 512
    oT = nc.dram_tensor("oT", [D, T], BF16, kind="ExternalInput").ap()
    xin = nc.dram_tensor("xin", [T, D], F32, kind="ExternalInput").ap()
    w_out = nc.dram_tensor("w_out", [D, D], F32, kind="ExternalInput").ap()
    lnp = nc.dram_tensor("lnp", [4, D], F32, kind="ExternalInput").ap()
    rw = nc.dram_tensor("rw", [D, NE], F32, kind="ExternalInput").ap()
    rb = nc.dram_tensor("rb", [1, NE], F32, kind="ExternalInput").ap()
    wg = nc.dram_tensor("wg", [NE, D, DE], F32, kind="ExternalInput").ap()
    wu = nc.dram_tensor("wu", [NE, D, DE], F32, kind="ExternalInput").ap()
    wd = nc.dram_tensor("wd", [NE, DE, D], F32, kind="ExternalInput").ap()
    xout = nc.dram_tensor("xout", [T, D], F32, kind="ExternalOutput").ap()

    S = Sched(nc)
    with contextlib.ExitStack() as st:
        def sb(name, shape, dt):
            return st.enter_context(nc.sbuf_tensor(name, shape, dt))

        def ps(name, shape, dt=F32):
            return st.enter_context(nc.psum_tensor(name, shape, dt))

        ident = sb("ident", [128, 128], F32)
        lnb = sb("lnb", [128, 4, D], F32)
        rbb = sb("rbb", [128, NE], F32)
        rwt = sb("rwt", [128, 8, NE], F32)
        wob = sb("wob", [128, 8, D], BF16)
        stg = [sb(f"stg{i}", [128, 2048], F32) for i in range(3)]
        wb = [[sb(f"wb{j}_{i}", [128, 2048], BF16) for i in range(3)] for j in range(2)]
        x1T = sb("x1T", [128, 8, TG], BF16)
        x1Tf = sb("x1Tf", [128, 8, 128], F32)
        yacc = sb("yacc", [128, NT, D], F32)
        xt = [sb(f"xt{i}", [128, D], F32) for i in range(2)]
        ot = [sb(f"ot{i}", [128, 8, 128], BF16) for i in range(2)]
        r = sb("r", [128, D], F32)
        x1 = sb("x1", [128, D], F32)
        stats = sb("stats", [128, 2, 6], F32)
        mv = sb("mv", [128, 2], F32)
        rstd = sb("rstd", [128, 1], F32)
        G = sb("G", [128, NT, NE], F32)
        rt = [sb(f"rt{i}", [128, NE], F32) for i in range(6)]
        rs = [sb(f"rs{i}", [128, 4], F32) for i in range(4)]
        sg = [sb(f"sg{i}", [128, 512], F32) for i in range(2)]
        aT = [[sb(f"aT{j}_{i}", [128, 512], BF16) for i in range(2)] for j in range(2)]
        outt = [sb(f"outt{i}", [128, D], F32) for i in range(2)]

        psD = [[ps(f"psD{j}_{i}", [128, 512]) for i in range(2)] for j in range(2)]
        psG = [ps(f"psG{i}", [128, 512]) for i in range(2)]
        psU = [ps(f"psU{i}", [128, 512]) for i in range(2)]

        S.op("pool", lambda e: e.memset(ident[:], 0.0), writes=["ident"])
        S.op("pool", lambda e: e.memset(r[:, 0:128], 1.0), writes=["r"])
        S.op("pool", lambda e: e.affine_select(out=ident[:], in_=r[:, 0:128], pattern=[[-1, 128]],
                                               compare_op=ALU.is_equal, fill=0.0, base=0, channel_multiplier=1),
             reads=["r"], writes=["ident"])
        S.dma("sp", lambda e: e.dma_start(out=lnb[:], in_=lnp.partition_broadcast(128)), writes=["lnb"])
        S.dma("sp", lambda e: e.dma_start(out=rbb[:], in_=rb[0, :].partition_broadcast(128)), writes=["rbb"])
        S.dma("sp", lambda e: e.dma_start(out=rwt[:], in_=rw.rearrange("(k p) e -> p k e", p=128)), writes=["rwt"])
        wo_v = w_out.rearrange("(k p) n -> p k n", p=128)
        for i in range(4):
            sname = f"stg{i % 3}"
            S.dma("sp", lambda e, i=i: e.dma_start(out=stg[i % 3][:].rearrange("p (k n) -> p k n", k=2),
                                                   in_=wo_v[:, 2 * i:2 * i + 2, :]), writes=[sname])
            S.op("pool", lambda e, i=i: e.tensor_copy(out=wob[:, 2 * i:2 * i + 2, :].rearrange("p k n -> p (k n)"),
                                                      in_=stg[i % 3][:]), reads=[sname], writes=["wob"])

        xin_v = xin.rearrange("(n p) d -> n p d", p=128)
        xout_v = xout.rearrange("(n p) d -> n p d", p=128)
        oT_v = oT.rearrange("(k p) t -> p k t", p=128)
        wg_v = wg.rearrange("e (k p) f -> e p k f", p=128)
        wu_v = wu.rearrange("e (k p) f -> e p k f", p=128)
        wd_v = wd.rearrange("e (k p) n -> e p k n", p=128)

        def layer_norm(src_ap_fn, src_key, dst_ap, dst_key, gi, tag):
            for h in range(2):
                S.op("dve", lambda e, h=h: e.bn_stats(out=stats[:, h, :], in_=src_ap_fn()[:, h * 512:(h + 1) * 512]),
                     reads=[src_key], writes=["stats"])
            S.op("dve", lambda e: e.bn_aggr(out=mv[:], in_=stats[:].rearrange("p a b -> p (a b)")),
                 reads=["stats"], writes=["mv"])
            S.op("dve", lambda e: e.tensor_scalar(out=rstd[:], in0=mv[:, 1:2], scalar1=LN_EPS, scalar2=None,
                                                  op0=ALU.add), reads=["mv"], writes=["rstd"])
            S.op("act", lambda e: e.sqrt(rstd[:], rstd[:]), reads=["rstd"], writes=["rstd"])
            S.op("dve", lambda e: e.reciprocal(out=rstd[:], in_=rstd[:]), reads=["rstd"], writes=["rstd"])
            S.op("dve", lambda e: e.tensor_scalar(out=dst_ap, in0=src_ap_fn(), scalar1=mv[:, 0:1], scalar2=rstd[:, 0:1],
                                                  op0=ALU.subtract, op1=ALU.mult),
                 reads=[src_key, "mv", "rstd"], writes=[dst_key])
            S.op("pool", lambda e: e.tensor_tensor(out=dst_ap, in0=dst_ap, in1=lnb[:, gi, :], op=ALU.mult),
                 reads=[dst_key, "lnb"], writes=[dst_key])
            S.op("pool", lambda e: e.tensor_tensor(out=dst_ap, in0=dst_ap, in1=lnb[:, gi + 1, :], op=ALU.add),
                 reads=[dst_key, "lnb"], writes=[dst_key])

        wcount = 0
        for g in range(NG if stage >= 1 else 0):
            t0 = g * TG
            for t in range(NT):
                gt = g * NT + t
                xi, oi = xt[gt % 2], ot[gt % 2]
                xk, ok = f"xt{gt % 2}", f"ot{gt % 2}"
                S.dma("sp", lambda e, xi=xi, gt=gt: e.dma_start(out=xi[:], in_=xin_v[gt]), writes=[xk])
                S.dma("sp", lambda e, oi=oi, gt=gt: e.dma_start(out=oi[:], in_=oT_v[:, :, gt * 128:(gt + 1) * 128]),
                      writes=[ok])
                pY = psD[0]
                for h in range(2):
                    for k in range(8):
                        S.op("pe", lambda e, h=h, k=k, oi=oi: e.matmul(pY[h][:], lhsT=oi[:, k, :],
                                                                       rhs=wob[:, k, h * 512:(h + 1) * 512],
                                                                       start=(k == 0), stop=(k == 7)),
                             reads=[ok, "wob"], writes=[f"psD0_{h}"])
                for h in range(2):
                    S.op("dve", lambda e, h=h, xi=xi: e.scalar_tensor_tensor(
                        out=r[:, h * 512:(h + 1) * 512], in0=xi[:, h * 512:(h + 1) * 512], scalar=ALPHA,
                        in1=pY[h][:], op0=ALU.mult, op1=ALU.add), reads=[xk, f"psD0_{h}"], writes=["r"])
                if stage < 1.2: continue
                layer_norm(lambda: r[:], "r", x1[:], "x1", 0, "ln1")
                S.op("act", lambda e, t=t: e.mul(yacc[:, t, :], x1[:], ALPHA), reads=["x1"], writes=[f"yacc{t}"])
                if stage < 1.3: continue
                for k in range(8):
                    S.op("pe", lambda e, k=k: e.transpose(psD[1][k // 4][:, (k % 4) * 128:(k % 4 + 1) * 128],
                                                          x1[:, k * 128:(k + 1) * 128], ident[:]),
                         reads=["x1", "ident"], writes=[f"psD1_{k // 4}"])
                for h in range(2 if stage >= 1.32 else 0):
                    S.op("act", lambda e, h=h: e.copy(out=x1Tf[:, 4 * h:4 * h + 4, :].rearrange("p k t -> p (k t)"),
                                                      in_=psD[1][h][:]), reads=[f"psD1_{h}"], writes=["x1Tf"])
                    if stage < 1.33: continue
                    S.op("pool", lambda e, h=h, t=t: e.tensor_copy(
                        out=x1T[:, 4 * h:4 * h + 4, t * 128:(t + 1) * 128],
                        in_=x1Tf[:, 4 * h:4 * h + 4, :]), reads=["x1Tf"], writes=["x1T"])
                if stage < 1.4: continue
                for k in range(8):
                    S.op("pe", lambda e, k=k: e.matmul(psG[0][:, 0:NE], lhsT=x1Tf[:, k, :], rhs=rwt[:, k, :],
                                                       start=(k == 0), stop=(k == 7)),
                         reads=["x1Tf", "rwt"], writes=["psG0"])
                if stage < 1.5: continue
                sc, bi, eq, b2, sel, ws = rt
                m1, m2, gs, gsel = rs
                S.op("act", lambda e: e.activation(out=sc[:], in_=psG[0][:, 0:NE], func=AF.Sigmoid),
                     reads=["psG0"], writes=["sc"])
                S.op("dve", lambda e: e.tensor_tensor(out=bi[:], in0=sc[:], in1=rbb[:], op=ALU.add),
                     reads=["sc", "rbb"], writes=["bi"])
                v3 = lambda a: a[:].rearrange("p (g j) -> p g j", g=4)
                S.op("dve", lambda e: e.tensor_reduce(out=m1[:], in_=v3(bi), axis=AX.X, op=ALU.max),
                     reads=["bi"], writes=["m1"])
                S.op("dve", lambda e: e.tensor_tensor(out=v3(eq), in0=v3(bi), in1=m1[:].unsqueeze(2).to_broadcast([128, 4, 4]),
                                                      op=ALU.is_equal), reads=["bi", "m1"], writes=["eq"])
                S.op("dve", lambda e: e.scalar_tensor_tensor(out=b2[:], in0=eq[:], scalar=-1e9, in1=bi[:],
                                                             op0=ALU.mult, op1=ALU.add), reads=["eq", "bi"], writes=["b2"])
                S.op("dve", lambda e: e.tensor_reduce(out=m2[:], in_=v3(b2), axis=AX.X, op=ALU.max),
                     reads=["b2"], writes=["m2"])
                S.op("dve", lambda e: e.tensor_tensor(out=gs[:], in0=m1[:], in1=m2[:], op=ALU.add),
                     reads=["m1", "m2"], writes=["gs"])
                S.op("dve", lambda e: e.tensor_reduce(out=rstd[:], in_=gs[:], axis=AX.X, op=ALU.max),
                     reads=["gs"], writes=["rstd"])
                S.op("dve", lambda e: e.tensor_scalar(out=gsel[:], in0=gs[:], scalar1=rstd[:, 0:1], scalar2=None,
                                                      op0=ALU.is_ge), reads=["gs", "rstd"], writes=["gsel"])
                S.op("dve", lambda e: e.tensor_tensor(out=v3(sel), in0=v3(bi), in1=m2[:].unsqueeze(2).to_broadcast([128, 4, 4]),
                                                      op=ALU.is_ge), reads=["bi", "m2"], writes=["sel"])
                S.op("dve", lambda e: e.tensor_tensor(out=v3(sel), in0=v3(sel), in1=gsel[:].unsqueeze(2).to_broadcast([128, 4, 4]),
                                                      op=ALU.mult), reads=["sel", "gsel"], writes=["sel"])
                S.op("dve", lambda e: e.tensor_tensor(out=ws[:], in0=sel[:], in1=sc[:], op=ALU.mult),
                     reads=["sel", "sc"], writes=["ws"])
                S.op("dve", lambda e: e.tensor_reduce(out=rstd[:], in_=ws[:], axis=AX.X, op=ALU.add),
                     reads=["ws"], writes=["rstd"])
                S.op("dve", lambda e: e.reciprocal(out=rstd[:], in_=rstd[:]), reads=["rstd"], writes=["rstd"])
                S.op("dve", lambda e, t=t: e.tensor_scalar(out=G[:, t, :], in0=ws[:], scalar1=rstd[:, 0:1], scalar2=None,
                                                           op0=ALU.mult), reads=["ws", "rstd"], writes=[f"G{t}"])
            for ex in range(NE if stage >= 2 else 0):
                wset = wb[wcount % 2]
                wk = [f"wb{wcount % 2}_{i}" for i in range(3)]
                wcount += 1
                srcs = [wg_v[ex], wu_v[ex], wd_v[ex]]
                for i in range(3):
                    kk = 8 if i < 2 else 2
                    S.dma("sp", lambda e, i=i, kk=kk, src=srcs[i]: e.dma_start(
                        out=stg[i][:].rearrange("p (k n) -> p k n", k=kk), in_=src), writes=[f"stg{i}"])
                    S.op("pool", lambda e, i=i, wset=wset: e.tensor_copy(out=wset[i][:], in_=stg[i][:]),
                         reads=[f"stg{i}"], writes=[wk[i]])
                wgb = wset[0][:].rearrange("p (k f) -> p k f", k=8)
                wub = wset[1][:].rearrange("p (k f) -> p k f", k=8)
                wdb = wset[2][:].rearrange("p (k n) -> p k n", k=2)
                for c in range(NCH):
                    pi = (ex * NCH + c) % 2
                    for fh in range(2):
                        for (pt, pk, wv, wkey) in ((psG[fh], f"psG{fh}", wgb, wk[0]), (psU[fh], f"psU{fh}", wub, wk[1])):
                            for k in range(8):
                                S.op("pe", lambda e, pt=pt, wv=wv, k=k, fh=fh, c=c: e.matmul(
                                    pt[:], lhsT=wv[:, k, fh * 128:(fh + 1) * 128], rhs=x1T[:, k, c * 512:(c + 1) * 512],
                                    start=(k == 0), stop=(k == 7)), reads=[wkey, "x1T"], writes=[pk])
                        S.op("act", lambda e, fh=fh: e.activation(out=sg[fh][:], in_=psG[fh][:], func=AF.Silu),
                             reads=[f"psG{fh}"], writes=[f"sg{fh}"])
                        S.op("dve", lambda e, fh=fh, pi=pi: e.tensor_tensor(out=aT[pi][fh][:], in0=sg[fh][:], in1=psU[fh][:],
                                                                           op=ALU.mult),
                             reads=[f"sg{fh}", f"psU{fh}"], writes=[f"aT{pi}_{fh}"])
                    for tt in range(4):
                        t = c * 4 + tt
                        dj = (c * 4 + tt) % 2
                        for h in range(2):
                            for fh in range(2):
                                S.op("pe", lambda e, dj=dj, h=h, fh=fh, tt=tt, pi=pi, wdb=wdb: e.matmul(
                                    psD[dj][h][:], lhsT=aT[pi][fh][:, tt * 128:(tt + 1) * 128],
                                    rhs=wdb[:, fh, h * 512:(h + 1) * 512], start=(fh == 0), stop=(fh == 1)),
                                    reads=[f"aT{pi}_{fh}", wk[2]], writes=[f"psD{dj}_{h}"])
                            S.op("dve", lambda e, dj=dj, h=h, t=t, ex=ex: e.scalar_tensor_tensor(
                                out=yacc[:, t, h * 512:(h + 1) * 512], in0=psD[dj][h][:], scalar=G[:, t, ex:ex + 1],
                                in1=yacc[:, t, h * 512:(h + 1) * 512], op0=ALU.mult, op1=ALU.add),
                                reads=[f"psD{dj}_{h}", f"G{t}", f"yacc{t}"], writes=[f"yacc{t}"])
            for t in range(NT if stage >= 3 else 0):
                gt = g * NT + t
                oo = outt[gt % 2]
                layer_norm(lambda t=t: yacc[:, t, :], f"yacc{t}", oo[:], f"outt{gt % 2}", 2, "ln2")
                S.dma("sp", lambda e, oo=oo, gt=gt: e.dma_start(out=xout_v[gt], in_=oo[:]), reads=[f"outt{gt % 2}"])
        if stage < 3:
            S.dma('sp', lambda e: e.dma_start(out=xout_v[0], in_=lnb[:, 0, :]), reads=['lnb'])
        S.emit()
    return nc


_NCS = {}


def _nc(name, fn):
    if name not in _NCS:
        _NCS[name] = fn()
    return _NCS[name]


def _c(a):
    return np.ascontiguousarray(a)


def _nsa_in_map(xb_, G, w_in, tbl, inp, ov):
    heads = list(range(G * 4, G * 4 + 4))
    wq = _c(w_in[:, G * 256:(G + 1) * 256])
    wqs = _c(np.concatenate([wq[:, 64:128], wq[:, 0:64], wq[:, 192:256], wq[:, 128:192]], axis=1))
    kvp = lambda i: w_in[:, 1024 + i * 256 + G * 64: 1024 + i * 256 + (G + 1) * 64]
    wkv = _c(np.concatenate([kvp(0), kvp(1), kvp(2), kvp(4), kvp(3), kvp(5)], axis=1))
    wgp = np.zeros((1024, 76), np.float32)
    for br in range(3):
        for r in range(4):
            wgp[:, 64 + br * 4 + r] = w_in[:, 2560 + br * 16 + G * 4 + r]
    w1 = np.concatenate([inp['nsa_cmp_w1_k'][0].reshape(32, 64, 256).transpose(1, 0, 2),
                         inp['nsa_cmp_w1_v'][0].reshape(32, 64, 256).transpose(1, 0, 2)], axis=0)
    posT = np.concatenate([inp['nsa_cmp_pos_k'][0].T, inp['nsa_cmp_pos_v'][0].T], axis=0)
    w2 = np.concatenate([inp['nsa_cmp_w2_k'][0].reshape(2, 128, 64).transpose(1, 0, 2),
                         inp['nsa_cmp_w2_v'][0].reshape(2, 128, 64).transpose(1, 0, 2)], axis=2)
    bss, bsw, bsc = nsa_bias_strips(tbl, heads)
    return dict(xT=xb_, wq=wq, wqs=wqs, wkv=wkv, wgp=wgp, w1=_c(w1), posT=_c(posT), w2=_c(w2), ovm=ov, bss=bss, bsw=bsw, bsc=bsc,
                cfar=_c(np.broadcast_to(tbl[31, heads][None, :], (128, 4))))


def kernel(**inputs):
    inp = {k: np.asarray(v) for k, v in inputs.items()}
    x = inp['x']
    B, Sq, _ = x.shape
    tbl = inp['rel_table']
    T = B * Sq // 8
    cur = x
    for layer in range(4):
        kind = layer % 4
        xTs = [_c(cur[b].T) for b in range(B)]
        maps = []
        if kind == 0:
            nca = _nc('dil', lambda: build_dil(Sq))
            w_in, w_out = inp['dil_w_in'][0], inp['dil_w_out'][0]
            xperm = [[_c(xTs[b][:, dil_perm(Sq, d)]) for (_, d) in DIL] for b in range(B)]
            for c in range(8):
                b, hg = c // 4, c % 4
                heads = list(range(hg * 4, hg * 4 + 4))
                m = {f"xT{g}": xperm[b][g] for g in range(3)}
                wA = np.empty((3, 4, 1024, 192), np.float32)
                for g in range(3):
                    for i, h in enumerate(heads):
                        for j in range(3):
                            wA[g, i, :, j * 64:(j + 1) * 64] = w_in[:, g * 3072 + j * 1024 + h * 64: g * 3072 + j * 1024 + (h + 1) * 64]
                m["wA"] = wA
                m["btA"] = dil_bias_tiles(tbl, heads)
                maps.append(m)
        elif kind == 1:
            nca = _nc('sb', lambda: build_sb(Sq))
            w_in, w_out = inp['sb_w_in'][0], inp['sb_w_out'][0]
            cm = sb_consts()
            for c in range(8):
                b, hg = c // 4, c % 4
                cols = slice(hg * 256, (hg + 1) * 256)
                maps.append(dict(xT=xTs[b], wq=_c(w_in[:, 0:1024][:, cols]), wk=_c(w_in[:, 1024:2048][:, cols]),
                                 wv=_c(w_in[:, 2048:3072][:, cols]), cm=cm))
        elif kind == 2:
            nca = _nc('nsa', lambda: build_nsa(Sq))
            w_in, w_out = inp['nsa_w_in'][0], inp['nsa_w_out'][0]
            ov = nsa_consts(Sq)
            for c in range(8):
                maps.append(_nsa_in_map(xTs[c // 4], c % 4, w_in, tbl, inp, ov))
        else:
            nca = _nc('moba', lambda: build_moba(Sq))
            w_in, w_out = inp['moba_w_in'][0], inp['moba_w_out'][0]
            for c in range(8):
                b, hg = c // 4, c % 4
                cols = slice(hg * 256, (hg + 1) * 256)
                heads = list(range(hg * 4, hg * 4 + 4))
                maps.append(dict(xT=xTs[b], wq=_c(w_in[:, 0:1024][:, cols]), wk=_c(w_in[:, 1024:2048][:, cols]),
                                 wv=_c(w_in[:, 2048:3072][:, cols]), bss=nsa_bias_strips(tbl, heads)[0],
                                 cfar=_c(np.broadcast_to(tbl[31, heads][None, :], (128, 4)))))
        res = run_bass_kernel_spmd(nca, maps, core_ids=list(range(8)))
        oTf = [np.concatenate([res.results[b * 4 + i]['oT'] for i in range(4)], axis=0) for b in range(B)]
        del res, maps, xTs
        ncp = _nc('post', lambda: build_post(T))
        lnp = _c(np.stack([inp['ln1_g'][layer], inp['ln1_b'][layer], inp['ln2_g'][layer], inp['ln2_b'][layer]]))
        curf = cur.reshape(B * Sq, 1024)
        pmaps = []
        for c in range(8):
            b, s0 = (c * T) // Sq, (c * T) % Sq
            pmaps.append(dict(oT=_c(oTf[b][:, s0:s0 + T]), xin=_c(curf[c * T:(c + 1) * T]), w_out=_c(w_out), lnp=lnp,
                              rw=inp['router_w'], rb=_c(inp['router_b'][None, :]), wg=inp['exp_w_gate'][layer],
                              wu=inp['exp_w_up'][layer], wd=inp['exp_w_down'][layer]))
        res = run_bass_kernel_spmd(ncp, pmaps, core_ids=list(range(8)))
        cur = np.concatenate([res.results[c]['xout'] for c in range(8)], axis=0).reshape(B, Sq, 1024)
        del res, pmaps
    return np.asarray(cur, dtype=np.float32)
```

```python
import contextlib
import numpy as np
import concourse.bass as bass
import concourse.mybir as mybir
from concourse.bass_utils import run_bass_kernel_spmd

F32 = mybir.dt.float32
BF16 = mybir.dt.bfloat16
I32 = mybir.dt.int32
AF = mybir.ActivationFunctionType
ALU = mybir.AluOpType
AX = mybir.AxisListType

ENGS = ("pe", "act", "dve", "pool", "sp")
NDSEM = 8


class Sched:
    def __init__(self, nc):
        self.nc = nc
        self.ops = {e: [] for e in ENGS}
        self.lastw = {}
        self.readers = {}
        self.ndma = {e: 0 for e in ENGS}
        self.waited = {e: {} for e in ENGS}

    def _add(self, eng, fn, reads, writes, dma):
        idx = len(self.ops[eng])
        deps = set()
        for b in reads:
            if b in self.lastw:
                deps.add(self.lastw[b])
        for b in writes:
            if b in self.lastw:
                deps.add(self.lastw[b])
            for r in self.readers.get(b, ()):
                deps.add(r)
        op = dict(fn=fn, waits=[], dma=None, sig=False)
        me = (eng, idx)
        if dma:
            k = self.ndma[eng]
            self.ndma[eng] += 1
            slot, val = k % NDSEM, 16 * (k // NDSEM + 1)
            op["dma"] = (slot, val)
            me = ("dma", eng, slot, val)
            if k >= NDSEM:
                deps.add(("dma", eng, slot, val - 16))
        best = {}
        for d in deps:
            if d[0] == "dma":
                key = ("dma", d[1], d[2]); v = d[3]
            else:
                key = d[0]; v = d[1]
                if key == eng and eng == "pe" and not dma:
                    continue
            if v > best.get(key, -1):
                best[key] = v
        w = self.waited[eng]
        for key, v in best.items():
            if w.get(key, -1) >= v:
                continue
            w[key] = v
            op["waits"].append((key, v))
            if key not in ("dma",) and not isinstance(key, tuple):
                self.ops[key][v]["sig"] = True
        self.ops[eng].append(op)
        for b in reads:
            self.readers.setdefault(b, []).append(me)
        for b in writes:
            self.lastw[b] = me
            self.readers[b] = []
        return me

    def op(self, eng, fn, reads=(), writes=()):
        return self._add(eng, fn, tuple(reads), tuple(writes), False)

    def dma(self, eng, fn, reads=(), writes=()):
        return self._add(eng, fn, tuple(reads), tuple(writes), True)

    def fence(self):
        targets = []
        for e in ENGS:
            n = len(self.ops[e])
            for i in range(n - 1, -1, -1):
                o = self.ops[e][i]
                if o["fn"] is not None and o["dma"] is None:
                    targets.append((e, i))
                    break
            k = self.ndma[e]
            for slot in range(min(k, NDSEM)):
                cnt = (k - 1 - slot) // NDSEM + 1
                targets.append((("dma", e, slot), 16 * cnt))
        for e in ENGS:
            w = self.waited[e]
            waits = []
            for key, v in targets:
                if key == e and not isinstance(key, tuple):
                    continue
                if w.get(key, -1) >= v:
                    continue
                w[key] = v
                waits.append((key, v))
                if not isinstance(key, tuple):
                    self.ops[key][v]["sig"] = True
            if waits:
                self.ops[e].append(dict(fn=None, waits=waits, dma=None, sig=False))

    def emit(self, final_engine="sp"):
        nc = self.nc
        fin_waits = []
        for e in ENGS:
            n = self.ndma[e]
            for slot in range(min(n, NDSEM)):
                cnt = (n - 1 - slot) // NDSEM + 1
                fin_waits.append((("dma", e, slot), 16 * cnt))
        self.ops[final_engine].append(dict(fn=None, waits=fin_waits, dma=None, sig=False))
        sigcnt = {}
        for e in ENGS:
            c = 0
            arr = []
            for o in self.ops[e]:
                if o["sig"]:
                    c += 1
                arr.append(c)
            sigcnt[e] = arr
        import contextlib
        with contextlib.ExitStack() as st:
            esem = {e: st.enter_context(nc.semaphore(f"s_{e}")) for e in ENGS}
            dsem = {e: [st.enter_context(nc.semaphore(f"d_{e}{i}")) for i in range(NDSEM)]
                    for e in ENGS if self.ndma[e] > 0}
            block = st.enter_context(nc.Block())

            def run(e, eng):
                for i, o in enumerate(self.ops[e]):
                    for key, v in o["waits"]:
                        if isinstance(key, tuple):
                            eng.wait_ge(dsem[key[1]][key[2]], v)
                        else:
                            eng.wait_ge(esem[key], sigcnt[key][v])
                    if o["fn"] is None:
                        continue
                    ins = o["fn"](eng)
                    if o["dma"] is not None:
                        ins.then_inc(dsem[e][o["dma"][0]], 16)
                    elif o["sig"]:
                        ins.then_inc(esem[e], 1)

            if self.ops["pe"]:
                @block.tensor
                def _(eng):
                    run("pe", eng)
            if self.ops["act"]:
                @block.scalar
                def _(eng):
                    run("act", eng)
            if self.ops["dve"]:
                @block.vector
                def _(eng):
                    run("dve", eng)
            if self.ops["pool"]:
                @block.gpsimd
                def _(eng):
                    run("pool", eng)
            if self.ops["sp"]:
                @block.sync
                def _(eng):
                    run("sp", eng)


D = 1024
HD = 64
SCALE = 0.125
NEG = -30000.0


class KB:
    def __init__(self):
        self.nc = bass.Bass("TRN2", target_bir_lowering=False)
        self.S = Sched(self.nc)
        self.st = contextlib.ExitStack()
        self._n = 0

    def din(self, name, shape, dt=F32):
        return self.nc.dram_tensor(name, list(shape), dt, kind="ExternalInput").ap()

    def dout(self, name, shape, dt=F32):
        return self.nc.dram_tensor(name, list(shape), dt, kind="ExternalOutput").ap()

    def sb(self, name, shape, dt, st=None):
        t = (st or self.st).enter_context(self.nc.sbuf_tensor(name, list(shape), dt))
        return t

    def ps(self, name, shape=(128, 512), dt=F32):
        return self.st.enter_context(self.nc.psum_tensor(name, list(shape), dt))

    def finish(self):
        self.S.emit()
        self.st.close()
        return self.nc


def pipeline(units, stages, skews):
    n = len(units)
    if n == 0:
        return
    for t in range(n + max(skews)):
        for s, fn in enumerate(stages):
            u = t - skews[s]
            if 0 <= u < n:
                fn(u, units[u])


def load_w_bf16(kb, name, w_ap, ncols, stg, stg_key):
    S = kb.S
    wb = kb.sb(name, [128, 8, ncols], BF16)
    S.dma("sp", lambda e: e.dma_start(out=stg[:, 0:8 * ncols].rearrange("p (k n) -> p k n", k=8),
                                      in_=w_ap.rearrange("(k p) n -> p k n", p=128)), writes=[stg_key])
    S.op("pool", lambda e: e.tensor_copy(out=wb[:].rearrange("p k n -> p (k n)"), in_=stg[:, 0:8 * ncols]),
         reads=[stg_key], writes=[name])
    return wb


def project(kb, xT, Sq, fm_outs, tm_outs, pss, xs, xb):
    S = kb.S
    xv = xT.rearrange("(k p) t -> p k t", p=128)
    NC = Sq // 512
    pi = 0
    for c in range(NC):
        xsi = xs[c % len(xs)]
        if isinstance(xsi, tuple):
            xsi, xsk = xsi
        else:
            xsk = f"xs{c % len(xs)}"
        xbi = xb[c % len(xb)]
        if isinstance(xbi, tuple):
            xbi, xbk = xbi
        else:
            xbk = f"xb{c % len(xb)}"
        S.dma("sp", lambda e, xsi=xsi, c=c: e.dma_start(out=xsi[:], in_=xv[:, :, c * 512:(c + 1) * 512]), writes=[xsk])
        S.op("dve", lambda e, xsi=xsi, xbi=xbi: e.tensor_copy(out=xbi[:, 0:4, :], in_=xsi[:, 0:4, :]), reads=[xsk], writes=[xbk])
        S.op("act", lambda e, xsi=xsi, xbi=xbi: e.copy(out=xbi[:, 4:8, :], in_=xsi[:, 4:8, :]), reads=[xsk], writes=[xbk])
        for (w, wkey, M, dst, dkey, scale, ev) in fm_outs:
            p, pk = pss[pi % len(pss)]
            pi += 1
            for k in range(8):
                S.op("pe", lambda e, p=p, w=w, k=k, M=M, xbi=xbi: e.matmul(p[0:M, :], lhsT=w[:, k, 0:M], rhs=xbi[:, k, :],
                                                                          start=(k == 0), stop=(k == 7)),
                     reads=[wkey, xbk], writes=[pk])
            if callable(ev):
                ev(p, pk, c)
            elif ev == "act":
                S.op("act", lambda e, p=p, M=M, dst=dst, c=c, scale=scale: e.mul(dst[0:M, c * 512:(c + 1) * 512], p[0:M, :], scale),
                     reads=[pk], writes=[dkey])
            else:
                S.op("dve", lambda e, p=p, M=M, dst=dst, c=c, scale=scale: e.tensor_scalar(
                    out=dst[0:M, c * 512:(c + 1) * 512], in0=p[0:M, :], scalar1=scale, scalar2=None, op0=ALU.mult),
                    reads=[pk], writes=[dkey])
        for (w, wkey, N, dst_fn, dkey) in tm_outs:
            p, pk = pss[pi % len(pss)]
            pi += 1
            for tt in range(4):
                for k in range(8):
                    S.op("pe", lambda e, p=p, w=w, k=k, N=N, tt=tt, xbi=xbi: e.matmul(
                        p[:, tt * N:(tt + 1) * N], lhsT=xbi[:, k, tt * 128:(tt + 1) * 128], rhs=w[:, k, 0:N],
                        start=(k == 0), stop=(k == 7)), reads=[wkey, xbk], writes=[pk])
            for tt in range(4):
                for (dap, lo, hi) in dst_fn(c * 4 + tt):
                    S.op("dve", lambda e, p=p, N=N, tt=tt, dap=dap, lo=lo, hi=hi: e.tensor_copy(out=dap, in_=p[:, tt * N + lo:tt * N + hi]),
                         reads=[pk], writes=[dkey])


def build_sb(Sq):
    kb = KB()
    S = kb.S
    NT, NCH = Sq // 128, Sq // 512
    xT = kb.din("xT", [D, Sq])
    wq = kb.din("wq", [D, 256])
    wk = kb.din("wk", [D, 256])
    wv = kb.din("wv", [D, 256])
    cm = kb.din("cm", [128, 4 * 512 + 256])
    oT = kb.dout("oT", [256, Sq], BF16)

    stg = kb.sb("stg", [128, 8 * 256], F32)
    cmf = kb.sb("cmf", [128, 4 * 512 + 256], F32)
    cmb = kb.sb("cmb", [128, 4 * 512 + 256], BF16)
    one1 = kb.sb("one1", [128, 1], F32)
    S.dma("sp", lambda e: e.dma_start(out=cmf[:], in_=cm), writes=["cmf"])
    S.op("pool", lambda e: e.tensor_copy(out=cmb[:], in_=cmf[:]), reads=["cmf"], writes=["cmb"])
    S.op("pool", lambda e: e.memset(one1[:], 1.0), writes=["one1"])
    ones_b = cmb[:, 2048:2176]
    umat_b = cmb[:, 2176:2304]
    wqb = load_w_bf16(kb, "wqb", wq, 256, stg, "stg")
    wkb = load_w_bf16(kb, "wkb", wk, 256, stg, "stg")
    wvb = load_w_bf16(kb, "wvb", wv, 256, stg, "stg")

    QT = kb.sb("QT", [128, Sq], BF16)
    KT = kb.sb("KT", [128, Sq], BF16)
    V = kb.sb("V", [128, NT, 128], BF16)
    xs = [kb.sb(f"xs{i}", [128, 8, 512], F32) for i in range(1)]
    xb = [kb.sb(f"xb{i}", [128, 8, 512], BF16) for i in range(2)]
    NB = 3
    NZ = 6
    zc = [kb.sb(f"zc{i}", [128, 512], F32) for i in range(NZ)]
    ee = [kb.sb(f"ee{i}", [128, 512], F32) for i in range(2)]
    sp = [kb.sb(f"sp{i}", [128, 512], BF16) for i in range(NB)]
    t1 = [kb.sb(f"t1{i}", [128, 512], F32) for i in range(NB)]
    att = [kb.sb(f"att{i}", [128, 512], BF16) for i in range(NB)]
    osb = [kb.sb(f"osb{i}", [64, 512], BF16) for i in range(2)]
    totb = [kb.sb(f"totb{i}", [128, 512], F32) for i in range(2)]

    psz = [kb.ps(f"psz{i}") for i in range(2)]
    psT = [kb.ps(f"psT{i}") for i in range(2)]
    psL = [kb.ps(f"psL{i}") for i in range(2)]
    pso = [kb.ps(f"pso{i}") for i in range(2)]
    pss = [(psz[0], "psz0"), (psz[1], "psz1"), (psL[0], "psL0"), (psL[1], "psL1")]

    for pair in range(2):
        fm = [(wqb[:, :, pair * 128:(pair + 1) * 128], "wqb", 128, QT, "QT", SCALE, "act"),
              (wkb[:, :, pair * 128:(pair + 1) * 128], "wkb", 128, KT, "KT", 1.0, "dve")]
        tm = [(wvb[:, :, pair * 128:(pair + 1) * 128], "wvb", 128, (lambda t: [(V[:, t, :], 0, 128)]), "V")]
        project(kb, xT, Sq, fm, tm, pss, xs, xb)

        units = []
        for c in range(NCH):
            jmax = 4 * c + 3
            for j in range(jmax, -1, -1):
                for h in range(2):
                    units.append((c, j, h, jmax))

        def s1(u, un):
            c, j, h, jmax = un
            pz, pzk = psz[u % 2], f"psz{u % 2}"
            hp = slice(h * 64, (h + 1) * 64)
            S.op("pe", lambda e: e.matmul(pz[:], lhsT=KT[hp, j * 128:(j + 1) * 128], rhs=QT[hp, c * 512:(c + 1) * 512],
                                          start=True, stop=True), reads=["KT", "QT"], writes=[pzk])

        def s2a(u, un):
            c, j, h, jmax = un
            pz, pzk = psz[u % 2], f"psz{u % 2}"
            bz = u % NZ
            S.op("dve", lambda e: e.tensor_scalar(out=zc[bz][:], in0=pz[:], scalar1=-60.0, scalar2=60.0,
                                                  op0=ALU.max, op1=ALU.min), reads=[pzk], writes=[f"zc{bz}"])

        def s2b(u, un):
            c, j, h, jmax = un
            bz, be, b = u % NZ, u % 2, u % NB
            S.op("act", lambda e: e.activation(out=ee[be][:], in_=zc[bz][:], func=AF.Exp), reads=[f"zc{bz}"], writes=[f"ee{be}"])
            S.op("act", lambda e: e.activation(out=sp[b][:], in_=ee[be][:], func=AF.Ln, bias=one1[:, 0:1]),
                 reads=[f"ee{be}", "one1"], writes=[f"sp{b}"])
            if j >= 4 * c:
                o = j - 4 * c
                S.op("pool", lambda e: e.tensor_tensor(out=sp[b][:], in0=sp[b][:], in1=cmb[:, o * 512:(o + 1) * 512], op=ALU.mult),
                     reads=[f"sp{b}", "cmb"], writes=[f"sp{b}"])

        def s3b(u, un):
            c, j, h, jmax = un
            bz = u % NZ
            if j != jmax:
                S.op("pool", lambda e: e.tensor_tensor(out=zc[bz][:], in0=zc[bz][:], in1=totb[h][:], op=ALU.subtract),
                     reads=[f"zc{bz}", f"totb{h}"], writes=[f"zc{bz}"])

        def s3(u, un):
            c, j, h, jmax = un
            b = u % NB
            pl, plk = psL[u % 2], f"psL{u % 2}"
            p1, p1k = psT[u % 2], f"psT{u % 2}"
            S.op("pe", lambda e: e.matmul(pl[:], lhsT=umat_b, rhs=sp[b][:], start=True, stop=True),
                 reads=[f"sp{b}", "cmb"], writes=[plk])
            S.op("pe", lambda e: e.matmul(p1[:], lhsT=ones_b, rhs=sp[b][:], start=True, stop=True),
                 reads=[f"sp{b}", "cmb"], writes=[p1k])

        def s4a(u, un):
            c, j, h, jmax = un
            bz, b = u % NZ, u % NB
            pl, plk = psL[u % 2], f"psL{u % 2}"
            p1, p1k = psT[u % 2], f"psT{u % 2}"
            S.op("dve", lambda e: e.tensor_tensor(out=t1[b][:], in0=zc[bz][:], in1=pl[:], op=ALU.subtract),
                 reads=[f"zc{bz}", plk], writes=[f"t1{b}"])
            if j != jmax:
                S.op("dve", lambda e: e.tensor_tensor(out=totb[h][:], in0=totb[h][:], in1=p1[:], op=ALU.add),
                     reads=[f"totb{h}", p1k], writes=[f"totb{h}"])
            else:
                S.op("dve", lambda e: e.tensor_copy(out=totb[h][:], in_=p1[:]), reads=[p1k], writes=[f"totb{h}"])

        def s4b(u, un):
            c, j, h, jmax = un
            b = u % NB
            S.op("act", lambda e: e.activation(out=att[b][:], in_=t1[b][:], func=AF.Exp), reads=[f"t1{b}"], writes=[f"att{b}"])
            if j >= 4 * c:
                o = j - 4 * c
                S.op("pool", lambda e: e.tensor_tensor(out=att[b][:], in0=att[b][:], in1=cmb[:, o * 512:(o + 1) * 512], op=ALU.mult),
                     reads=[f"att{b}", "cmb"], writes=[f"att{b}"])

        def s5(u, un):
            c, j, h, jmax = un
            b = u % NB
            S.op("pe", lambda e: e.matmul(pso[h][0:64, :], lhsT=V[:, j, h * 64:(h + 1) * 64], rhs=att[b][:],
                                          start=(j == jmax), stop=(j == 0)), reads=[f"att{b}", "V"], writes=[f"pso{h}"])
            if j == 0:
                ob = osb[h]
                S.op("act", lambda e: e.copy(out=ob[:], in_=pso[h][0:64, :]), reads=[f"pso{h}"], writes=[f"osb{h}"])
                r0 = pair * 128 + h * 64
                S.dma("sp", lambda e: e.dma_start(out=oT[r0:r0 + 64, c * 512:(c + 1) * 512], in_=ob[:]), reads=[f"osb{h}"])

        n_u = len(units)
        stages = ((s2a, 0), (s2b, 2), (s3, 3), (s4a, 4), (s3b, 3), (s4b, 5), (s5, 6))
        for t in range(n_u + 7):
            if t % 2 == 0:
                for u in (t, t + 1):
                    if u < n_u:
                        s1(u, units[u])
            for fn_, sk_ in stages:
                u = t - sk_
                if 0 <= u < n_u:
                    fn_(u, units[u])
    return kb.finish()


def sb_consts():
    cm = np.zeros((128, 4 * 512 + 256), np.float32)
    k = np.arange(128)[:, None]
    q = np.arange(512)[None, :]
    for o in range(4):
        cm[:, o * 512:(o + 1) * 512] = ((o * 128 + k) < q).astype(np.float32)
    cm[:, 2048:2176] = 1.0
    jj = np.arange(128)[:, None]
    ss = np.arange(128)[None, :]
    cm[:, 2176:2304] = (jj >= ss).astype(np.float32)
    return cm


class SoftmaxPipe:
    def __init__(self, kb, nS=3, nP=3):
        import os
        nP = int(os.environ.get('SM_NP', nP))
        self.kb = kb
        self.psS = [kb.ps(f"psS{i}") for i in range(nS)]
        self.PT = [kb.sb(f"PT{i}", [128, 512], BF16) for i in range(nP)]
        self.nS, self.nP = nS, nP
        self.cnt = 0

    def run(self, units):
        S = self.kb.S
        base = self.cnt
        self.cnt += len(units)

        def s1(u, un):
            i = (base + u) % self.nS
            ps, pk = self.psS[i], f"psS{i}"
            n = len(un["mm"])
            kp, N = un.get("kp", 128), un["N"]
            for m, mmx in enumerate(un["mm"]):
                (lhsT, rhs, reads) = mmx[:3]
                r0, r1 = mmx[3] if len(mmx) > 3 else (0, kp)
                S.op("pe", lambda e, lhsT=lhsT, rhs=rhs, m=m, r0=r0, r1=r1: e.matmul(ps[r0:r1, 0:N], lhsT=lhsT, rhs=rhs, start=(m == 0), stop=(m == n - 1)),
                     reads=reads, writes=[pk])

        def s2(u, un):
            i = (base + u) % self.nS
            ps, pk = self.psS[i], f"psS{i}"
            b = (base + u) % self.nP
            kp, N = un.get("kp", 128), un["N"]
            cb = un.get("cb")
            if cb is None:
                S.op("act", lambda e: e.activation(out=self.PT[b][0:kp, 0:N], in_=ps[0:kp, 0:N], func=AF.Exp),
                     reads=[pk], writes=[f"PT{b}"])
            else:
                S.op("act", lambda e: e.activation(out=self.PT[b][0:kp, 0:N], in_=ps[0:kp, 0:N], func=AF.Exp, bias=cb[0]),
                     reads=[pk, cb[1]], writes=[f"PT{b}"])
            if un.get("post"):
                un["post"](self.PT[b], f"PT{b}")

        def s3(u, un):
            b = (base + u) % self.nP
            kp, N = un.get("kp", 128), un["N"]
            for pv in un["pv"]:
                (lhsT_v, reads, acc, acck, start, stop) = pv[:6]
                c0, c1 = pv[6] if len(pv) > 6 else (0, N)
                S.op("pe", lambda e, lhsT_v=lhsT_v, acc=acc, start=start, stop=stop, c0=c0, c1=c1: e.matmul(
                    acc, lhsT=lhsT_v, rhs=self.PT[b][0:kp, c0:c1], start=start, stop=stop),
                    reads=[f"PT{b}"] + list(reads), writes=[acck])
            if un.get("fin"):
                un["fin"]()

        import os
        pipeline(units, [s1, s2, s3], [int(v) for v in os.environ.get('SM_SKEW', '0,1,2').split(',')])


class SoftmaxPairPipe:
    def __init__(self, kb, nU=2, nP=3):
        import os
        nP = int(os.environ.get('SMP_NP', 4))
        self.kb = kb
        self.psS = [[kb.ps(f"psS{u}_{h}") for h in range(2)] for u in range(nU)]
        self.PT = [[kb.sb(f"PT{u}_{h}", [128, 512], BF16) for h in range(2)] for u in range(nP)]
        self.nU, self.nP = nU, nP
        self.cnt = 0

    def run(self, units):
        S = self.kb.S
        base = self.cnt
        self.cnt += len(units)

        def s1(u, un):
            i = (base + u) % self.nU
            N = un["N"]
            cnt = [0, 0]
            tot = [sum(1 for m in un["mm"] if m[3] == h) for h in range(2)]
            for mmx in un["mm"]:
                (lhsT, rhs, reads, h) = mmx[:4]
                r0, r1 = mmx[4] if len(mmx) > 4 else (0, 128)
                ps, pk = self.psS[i][h], f"psS{i}_{h}"
                first, last = cnt[h] == 0, cnt[h] == tot[h] - 1
                cnt[h] += 1
                S.op("pe", lambda e, lhsT=lhsT, rhs=rhs, ps=ps, first=first, last=last, r0=r0, r1=r1: e.matmul(ps[r0:r1, 0:N], lhsT=lhsT, rhs=rhs, start=first, stop=last),
                     reads=reads, writes=[pk])

        def s2(u, un):
            i = (base + u) % self.nU
            b = (base + u) % self.nP
            N = un["N"]
            for h in un.get("slots", (0, 1)):
                ps, pk = self.psS[i][h], f"psS{i}_{h}"
                cb = un["cb"][h]
                if cb is None:
                    S.op("act", lambda e, ps=ps, h=h: e.activation(out=self.PT[b][h][:, 0:N], in_=ps[:, 0:N], func=AF.Exp),
                         reads=[pk], writes=[f"PT{b}_{h}"])
                else:
                    S.op("act", lambda e, ps=ps, h=h, cb=cb: e.activation(out=self.PT[b][h][:, 0:N], in_=ps[:, 0:N], func=AF.Exp, bias=cb[0]),
                         reads=[pk, cb[1]], writes=[f"PT{b}_{h}"])
            if un.get("post"):
                un["post"](self.PT[b][0], f"PT{b}_0")

        def s3(u, un):
            b = (base + u) % self.nP
            N = un["N"]
            for (lhsT_v, reads, acc, acck, start, stop, h) in un["pv"]:
                S.op("pe", lambda e, lhsT_v=lhsT_v, acc=acc, start=start, stop=stop, h=h: e.matmul(
                    acc, lhsT=lhsT_v, rhs=self.PT[b][h][:, 0:N], start=start, stop=stop),
                    reads=[f"PT{b}_{h}"] + list(reads), writes=[acck])
            if un.get("fin"):
                un["fin"]()

        import os
        pipeline(units, [s1, s2, s3], [int(v) for v in os.environ.get('SMP_SKEW', '0,1,2').split(',')])


class Normalizer:
    def __init__(self, kb):
        self.kb = kb
        self.rdt = kb.sb("nz_rdt", [128, 512], F32)
        self.num = kb.sb("nz_num", [64, 512], F32)
        self.onesf = kb.sb("nz_ones", [128, 64], F32)
        self.psB = kb.ps("psB")
        kb.S.op("pool", lambda e: e.memset(self.onesf[:], 1.0), writes=["nz_ones"])

    def bcast_recip(self, acc, acck, N):
        S = self.kb.S
        S.op("dve", lambda e: e.reciprocal(out=self.rdt[64:65, 0:N], in_=acc[64:65, 0:N]), reads=[acck], writes=["nz_rdt"])
        S.op("pe", lambda e: e.matmul(self.psB[0:64, 0:N], lhsT=self.onesf[64:65, 0:64], rhs=self.rdt[64:65, 0:N],
                                      start=True, stop=True), reads=["nz_rdt", "nz_ones"], writes=["psB"])

    def normalize(self, acc, acck, N, out_ap, outk):
        S = self.kb.S
        self.bcast_recip(acc, acck, N)
        S.op("act", lambda e: e.copy(out=self.num[:, 0:N], in_=acc[0:64, 0:N]), reads=[acck], writes=["nz_num"])
        S.op("dve", lambda e: e.tensor_tensor(out=out_ap, in0=self.num[:, 0:N], in1=self.psB[0:64, 0:N], op=ALU.mult),
             reads=["nz_num", "psB"], writes=[outk])


NOFF = 16


def build_moba(Sq):
    kb = KB()
    S = kb.S
    NT, NCH, NBLK = Sq // 128, Sq // 512, Sq // 256
    GW = max(NBLK, 8)
    xT = kb.din("xT", [D, Sq])
    wq = kb.din("wq", [D, 256])
    wk = kb.din("wk", [D, 256])
    wv = kb.din("wv", [D, 256])
    bss = kb.din("bss", [4, 128, 2432])
    cfar = kb.din("cfar", [128, 4])
    oT = kb.dout("oT", [256, Sq], BF16)

    stg = kb.sb("stg", [128, 8 * 512], F32)
    wqb = load_w_bf16(kb, "wqb", wq, 256, stg, "stg")
    wkb = load_w_bf16(kb, "wkb", wk, 256, stg, "stg")
    wvb = load_w_bf16(kb, "wvb", wv, 256, stg, "stg")
    cf = kb.sb("cf", [128, 4], F32)
    S.dma("sp", lambda e: e.dma_start(out=cf[:], in_=cfar), writes=["cf"])
    identf = kb.sb("identf", [128, 128], F32)
    identb = kb.sb("identb", [128, 128], BF16)
    onesf = kb.sb("onesf", [128, 128], F32)
    S.op("pool", lambda e: e.memset(onesf[:], 1.0), writes=["onesf"])
    S.op("pool", lambda e: e.affine_select(out=identf[:], in_=onesf[:], pattern=[[-1, 128]], compare_op=ALU.is_equal,
                                           fill=0.0, base=0, channel_multiplier=1), reads=["onesf"], writes=["identf"])
    S.op("pool", lambda e: e.tensor_copy(out=identb[:], in_=identf[:]), reads=["identf"], writes=["identb"])

    QT = kb.sb("QT", [128, Sq], BF16)
    KT = kb.sb("KT", [128, Sq], BF16)
    Vh = [kb.sb(f"V{h}", [128, NT, 65], BF16) for h in range(2)]
    for h in range(2):
        S.op("pool", lambda e, h=h: e.memset(Vh[h][:, :, 64:65], 1.0), writes=["V"])
    maskT = kb.sb("maskT", [128, Sq], BF16)
    bs = kb.sb("bs", [128, 2, 2432], BF16)
    xs = [(stg[:].rearrange("p (k t) -> p k t", k=8), "stg")]
    xb = [kb.sb(f"xb{i}", [128, 8, 512], BF16) for i in range(2)]
    km = kb.sb("km", [128, NBLK], F32)
    kmb = kb.sb("kmb", [128, NBLK], BF16)
    gbuf = kb.sb("gbuf", [128, GW], F32)
    m8 = kb.sb("m8", [128, 8], F32)
    mb2 = kb.sb("mb2", [128, 128], F32)
    osb = [kb.sb(f"osb{i}", [64, 512], BF16) for i in range(2)]

    sp_ = SoftmaxPairPipe(kb)
    nz = Normalizer(kb)
    pso = [[kb.ps(f"pso{h}_0")] for h in range(2)]
    pss = [(sp_.psS[0][0], "psS0_0"), (sp_.psS[0][1], "psS0_1"), (sp_.psS[1][0], "psS1_0")]
    import os
    for i in range(int(os.environ.get("DUMMY_WARM", "0"))):
        S.op("pe", lambda e: e.matmul(sp_.psS[0][0][:, 0:256], lhsT=identb[:], rhs=wqb[:, 0, :], start=True, stop=True),
             reads=["identb", "wqb"], writes=["psS0_0"])

    for pair in range(2):
        fm = [(wqb[:, :, pair * 128:(pair + 1) * 128], "wqb", 128, QT, "QT", SCALE, "act"),
              (wkb[:, :, pair * 128:(pair + 1) * 128], "wkb", 128, KT, "KT", 1.0, "dve")]
        tm = [(wvb[:, :, pair * 128:(pair + 1) * 128], "wvb", 128,
               (lambda t: [(Vh[0][:, t, 0:64], 0, 64), (Vh[1][:, t, 0:64], 64, 128)]), "V")]
        project(kb, xT, Sq, fm, tm, pss, xs, xb)
        for h in range(2):
            for w0 in range(0, 2432, 2048):
                wn = min(2048, 2432 - w0)
                S.dma("sp", lambda e, h=h, pair=pair, w0=w0, wn=wn: e.dma_start(out=stg[:, 0:wn], in_=bss[pair * 2 + h, :, w0:w0 + wn]), writes=["stg"])
                S.op("pool", lambda e, h=h, w0=w0, wn=wn: e.tensor_copy(out=bs[:, h, w0:w0 + wn], in_=stg[:, 0:wn]), reads=["stg"], writes=["bs"])
        S.op("dve", lambda e: e.tensor_reduce(out=km[:], in_=KT[:].rearrange("p (b k) -> p b k", k=256), axis=AX.X, op=ALU.add),
             reads=["KT"], writes=["km"])
        S.op("dve", lambda e: e.tensor_copy(out=kmb[:], in_=km[:]), reads=["km"], writes=["kmb"])
        for qt in range(NT):
            nv = qt // 2
            S.op("pool", lambda e: e.memset(mb2[:], NEG), writes=["mb2"])
            for h in range(2):
                hp = slice(h * 64, (h + 1) * 64)
                c0 = h * 64
                pg, pgk = pss[(qt * 2 + h) % 3]
                if nv > 3:
                    S.op("pe", lambda e, pg=pg, hp=hp, qt=qt: e.matmul(pg[:, 0:NBLK], lhsT=QT[hp, qt * 128:(qt + 1) * 128], rhs=kmb[hp, :],
                                                                       start=True, stop=True), reads=["QT", "kmb"], writes=[pgk])
                    S.op("pool", lambda e: e.memset(gbuf[:], -1e30), writes=["gbuf"])
                    S.op("dve", lambda e, pg=pg, nv=nv: e.tensor_copy(out=gbuf[:, 0:nv], in_=pg[:, 0:nv]), reads=[pgk], writes=["gbuf"])
                    S.op("dve", lambda e: e.max(out=m8[:], in_=gbuf[:]), reads=["gbuf"], writes=["m8"])
                    S.op("dve", lambda e, c0=c0: e.tensor_scalar(out=mb2[:, c0:c0 + NBLK], in0=gbuf[:, 0:NBLK], scalar1=m8[:, 2:3], scalar2=NEG,
                                                                 op0=ALU.is_lt, op1=ALU.mult), reads=["gbuf", "m8"], writes=["mb2"])
                elif nv > 0:
                    S.op("pool", lambda e, nv=nv, c0=c0: e.memset(mb2[:, c0:c0 + nv], 0.0), writes=["mb2"])
                S.op("pool", lambda e, nv=nv, c0=c0: e.memset(mb2[:, c0 + nv:c0 + nv + 1], 0.0), writes=["mb2"])
            pt, ptk = pss[(qt * 2 + 2) % 3]
            S.op("pe", lambda e, pt=pt: e.transpose(pt[:, 0:128], mb2[:], identf[:]), reads=["mb2", "identf"], writes=[ptk])
            S.op("act", lambda e, pt=pt, qt=qt: e.copy(out=maskT[:, qt * 128:(qt + 1) * 128], in_=pt[:, 0:128]),
                 reads=[ptk], writes=["maskT"])
        units = []
        for c in range(NCH):
            jmax = 4 * c + 3
            qs = slice(c * 512, (c + 1) * 512)
            for j in range(jmax + 1):
                o = 4 * c - j
                x = j // 2
                mm, cbs, pv = [], [], []
                for h in range(2):
                    hp = slice(h * 64, (h + 1) * 64)
                    mm.append((KT[hp, j * 128:(j + 1) * 128], QT[hp, qs], ["KT", "QT"], h))
                for h in range(2):
                    hp = slice(h * 64, (h + 1) * 64)
                    mm.append((identb[hp, h * 64 + x:h * 64 + x + 1].to_broadcast([64, 128]), maskT[hp, qs], ["identb", "maskT"], h))
                for h in range(2):
                    if o <= 12:
                        mm.append((identb[:], bs[:, h, (o + 3) * 128:(o + 3) * 128 + 512], ["identb", "bs"], h))
                        cbs.append(None)
                    else:
                        cbs.append((cf[:, pair * 2 + h:pair * 2 + h + 1], "cf"))
                    pv.append((Vh[h][:, j, :], ["V"], pso[h][0][0:65, :], f"pso{h}_0", j == 0, j == jmax, h))
                un = dict(mm=mm, N=512, cb=cbs, pv=pv)
                if j == jmax:
                    def fin(c=c, pair=pair):
                        for h in range(2):
                            ob = osb[h]
                            nz.normalize(pso[h][0], f"pso{h}_0", 512, ob[:], f"osb{h}")
                            r0 = pair * 128 + h * 64
                            S.dma("sp", lambda e, ob=ob, r0=r0: e.dma_start(out=oT[r0:r0 + 64, c * 512:(c + 1) * 512], in_=ob[:]), reads=[f"osb{h}"])
                    un["fin"] = fin
                units.append(un)
        sp_.run(units)
    return kb.finish()


def rel_bucket_np(dist):
    n = np.maximum(dist, 0)
    exact = 16
    logf = np.log(np.maximum(n, 1).astype(np.float32) / exact) / np.float32(np.log(2048 / exact))
    large = np.minimum(exact + (logf * 16).astype(np.int32), 31)
    return np.where(n < exact, n, large)


def moba_bias_tiles(rel_table, heads):
    k = np.arange(128)[:, None]
    q = np.arange(512)[None, :]
    out = np.empty((len(heads), NOFF, 128, 512), np.float32)
    for oi in range(NOFF):
        o = oi - 3
        dist = o * 128 + q - k
        bk = rel_bucket_np(dist)
        for i, h in enumerate(heads):
            out[i, oi] = np.where(dist >= 0, rel_table[bk, h], np.float32(NEG))
    return out


DIL = ((128, 1), (512, 4), (2048, 16))


def build_dil(Sq):
    kb = KB()
    S = kb.S
    NT, NCH = Sq // 128, Sq // 512
    xTg = [kb.din(f"xT{g}", [D, Sq]) for g in range(3)]
    wA = kb.din("wA", [3, 4, D, 192])
    btA = kb.din("btA", [3, 4, 128, 256])
    oT = kb.dout("oT", [256, Sq], BF16)

    stg = kb.sb("stg", [128, 8 * 192], F32)
    identf = kb.sb("identf", [128, 128], F32)
    identb = kb.sb("identb", [128, 128], BF16)
    onesf = kb.sb("onesf", [128, 128], F32)
    S.op("pool", lambda e: e.memset(onesf[:], 1.0), writes=["onesf"])
    S.op("pool", lambda e: e.affine_select(out=identf[:], in_=onesf[:], pattern=[[-1, 128]], compare_op=ALU.is_equal,
                                           fill=0.0, base=0, channel_multiplier=1), reads=["onesf"], writes=["identf"])
    S.op("pool", lambda e: e.tensor_copy(out=identb[:], in_=identf[:]), reads=["identf"], writes=["identb"])
    QT = kb.sb("QT", [64, Sq], BF16)
    KT = kb.sb("KT", [64, Sq], BF16)
    V = kb.sb("V", [128, NT, 65], BF16)
    S.op("pool", lambda e: e.memset(V[:, :, 64:65], 1.0), writes=["V"])
    accS = kb.sb("accS", [65, Sq], F32)
    wab = kb.sb("wab", [128, 8, 192], BF16)
    btf = kb.sb("btf", [128, 256], F32)
    btb = kb.sb("btb", [128, 256], BF16)
    xs = [kb.sb(f"xs{i}", [128, 8, 512], F32) for i in range(1)]
    xb = [kb.sb(f"xb{i}", [128, 8, 512], BF16) for i in range(2)]
    osb = [kb.sb(f"osb{i}", [64, 512], BF16) for i in range(2)]
    sp_ = SoftmaxPipe(kb)
    nz = Normalizer(kb)
    pso = [kb.ps(f"pso{i}") for i in range(2)]
    pss = [(sp_.psS[0], "psS0"), (sp_.psS[1], "psS1"), (sp_.psS[2], "psS2")]

    for hh in range(4):
        for g, (win, d) in enumerate(DIL):
            L = Sq // d
            Lt = L // 128
            S.dma("sp", lambda e, g=g, hh=hh: e.dma_start(out=stg[:].rearrange("p (k n) -> p k n", k=8),
                                                         in_=wA[g, hh].rearrange("(k p) n -> p k n", p=128)), writes=["stg"])
            S.op("pool", lambda e: e.tensor_copy(out=wab[:].rearrange("p k n -> p (k n)"), in_=stg[:]), reads=["stg"], writes=["wab"])
            S.dma("sp", lambda e, g=g, hh=hh: e.dma_start(out=btf[:], in_=btA[g, hh]), writes=["btf"])
            S.op("pool", lambda e: e.tensor_copy(out=btb[:], in_=btf[:]), reads=["btf"], writes=["btb"])
            fm = [(wab[:, :, 0:64], "wab", 64, QT, "QT", SCALE, "act"), (wab[:, :, 64:128], "wab", 64, KT, "KT", 1.0, "dve")]
            tm = [(wab[:, :, 128:192], "wab", 64, (lambda t: [(V[:, t, 0:64], 0, 64)]), "V")]
            project(kb, xTg[g], Sq, fm, tm, pss, xs, xb)
            accv = accS[:].rearrange("p (i r) -> p r i", r=d)
            units = []
            for T in range(NT):
                r, jl = T // Lt, T % Lt
                has_next = jl + 1 < Lt
                N = 256 if has_next else 128
                mm = [(KT[:, T * 128:(T + 1) * 128], QT[:, T * 128:T * 128 + N], ["KT", "QT"]),
                      (identb[:], btb[:, 0:N], ["identb", "btb"])]
                pv = [(V[:, T, :], ["V"], pso[T % 2][0:65, 0:128], f"pso{T % 2}", jl == 0, True, (0, 128))]
                if has_next:
                    pv.append((V[:, T, :], ["V"], pso[(T + 1) % 2][0:65, 0:128], f"pso{(T + 1) % 2}", True, False, (128, 256)))

                def fin(T=T, r=r, jl=jl, g=g, accv=accv):
                    dst = accv[:, r, jl * 128:(jl + 1) * 128]
                    acc, acck = pso[T % 2], f"pso{T % 2}"
                    if g == 0:
                        S.op("dve", lambda e: e.tensor_copy(out=dst, in_=acc[0:65, 0:128]), reads=[acck], writes=["accS"])
                    else:
                        S.op("dve", lambda e: e.tensor_tensor(out=dst, in0=dst, in1=acc[0:65, 0:128], op=ALU.add),
                             reads=[acck, "accS"], writes=["accS"])
                units.append(dict(mm=mm, N=N, kp=128, pv=pv, fin=fin))
            sp_.run(units)
        for c in range(NCH):
            ob = osb[c % 2]
            cs = slice(c * 512, (c + 1) * 512)
            nz.bcast_recip(accS[:, cs], "accS", 512)
            S.op("dve", lambda e, ob=ob, cs=cs: e.tensor_tensor(out=ob[:], in0=accS[0:64, cs], in1=nz.psB[0:64, :], op=ALU.mult),
                 reads=["accS", "psB"], writes=[f"osb{c % 2}"])
            S.dma("sp", lambda e, ob=ob, cs=cs, hh=hh: e.dma_start(out=oT[hh * 64:(hh + 1) * 64, cs], in_=ob[:]), reads=[f"osb{c % 2}"])
    return kb.finish()


def dil_bias_tiles(rel_table, heads):
    k = np.arange(128)[:, None]
    q = np.arange(256)[None, :]
    steps = q - k
    out = np.empty((3, len(heads), 128, 256), np.float32)
    for g, (win, d) in enumerate(DIL):
        span = win // d
        ok = (steps >= 0) & (steps <= span)
        bk = rel_bucket_np(steps * d)
        for i, h in enumerate(heads):
            out[g, i] = np.where(ok, rel_table[bk, h], np.float32(NEG))
    return out


def dil_perm(Sq, d):
    L = Sq // d
    return (np.arange(d)[:, None] + d * np.arange(L)[None, :]).reshape(-1)


def nsa_consts(Sq):
    NSEL = Sq // 64
    n_cmp = Sq // 16 - 1
    NCT = (n_cmp + 127) // 128
    c = np.arange(NCT * 128)[:, None]
    j = np.arange(NSEL)[None, :]
    ov = ((c >= 4 * j - 1) & (c <= 4 * j + 3) & (c < n_cmp)).astype(np.float32)
    return ov


def nsa_bias_strips(rel_table, heads):
    k = np.arange(128)[:, None]
    H = len(heads)
    def strip(dist, ok):
        bk = rel_bucket_np(dist)
        out = np.empty((H,) + dist.shape, np.float32)
        for i, h in enumerate(heads):
            out[i] = np.where(ok, rel_table[bk, h], np.float32(NEG))
        return out
    ds = np.arange(2432)[None, :] - 384 - k
    dw = np.arange(1408)[None, :] - 384 - k
    dc = np.arange(3584)[None, :] - 16 * k - 31
    return strip(ds, ds >= 0), strip(dw, (dw >= 0) & (dw <= 511)), strip(dc, dc >= 0)


def build_nsa(Sq):
    kb = KB()
    S = kb.S
    NT, NCH, NSEL = Sq // 128, Sq // 512, Sq // 64
    n_cmp = Sq // 16 - 1
    NCT = (n_cmp + 127) // 128
    NCC = NCT * 128
    NH = (NSEL + 127) // 128
    NSP = min(NSEL, 128)
    SW = max(NSEL, 8)
    xT = kb.din("xT", [D, Sq])
    wq = kb.din("wq", [D, 256])
    wkv = kb.din("wkv", [D, 512])
    wgp = kb.din("wgp", [D, 76])
    w1 = kb.din("w1", [128, 32, 256])
    posT = kb.din("posT", [128, 32])
    w2 = kb.din("w2", [128, 2, 128])
    ovm = kb.din("ovm", [NCC, NSEL])
    bss = kb.din("bss", [4, 128, 2432])
    bsw = kb.din("bsw", [4, 128, 1408])
    bsc = kb.din("bsc", [4, 128, 3584])
    cfar = kb.din("cfar", [128, 4])
    oT = kb.dout("oT", [256, Sq], BF16)

    identf = kb.sb("identf", [128, 128], F32)
    identb = kb.sb("identb", [128, 128], BF16)
    onesf = kb.sb("onesf", [128, 128], F32)
    S.op("pool", lambda e: e.memset(onesf[:], 1.0), writes=["onesf"])
    S.op("pool", lambda e: e.affine_select(out=identf[:], in_=onesf[:], pattern=[[-1, 128]], compare_op=ALU.is_equal,
                                           fill=0.0, base=0, channel_multiplier=1), reads=["onesf"], writes=["identf"])
    S.op("pool", lambda e: e.tensor_copy(out=identb[:], in_=identf[:]), reads=["identf"], writes=["identb"])
    cf = kb.sb("cf", [128, 4], F32)
    S.dma("sp", lambda e: e.dma_start(out=cf[:], in_=cfar), writes=["cf"])
    selgb = kb.sb("selgb", [128, 12 * 64], BF16)
    stg = kb.sb("stg", [128, 8 * 512], F32)
    zerob = kb.sb("zerob", [128, 128], BF16)
    S.op("pool", lambda e: e.memset(zerob[:], 0.0), writes=["zerob"])
    wqb = load_w_bf16(kb, "wqb", wq, 256, stg, "stg")
    wgb = load_w_bf16(kb, "wgb", wgp, 76, stg, "stg")
    w2b = kb.sb("w2b", [128, 2, 128], BF16)
    S.dma("sp", lambda e: e.dma_start(out=stg[:, 0:256].rearrange("p (a b) -> p a b", a=2), in_=w2), writes=["stg"])
    S.op("pool", lambda e: e.tensor_copy(out=w2b[:].rearrange("p a b -> p (a b)"), in_=stg[:, 0:256]), reads=["stg"], writes=["w2b"])
    ovb = kb.sb("ovb", [128, NCT, NSEL], BF16)
    for ct in range(NCT):
        S.dma("sp", lambda e, ct=ct: e.dma_start(out=stg[:, 0:NSEL], in_=ovm[ct * 128:(ct + 1) * 128, :]), writes=["stg"])
        S.op("pool", lambda e, ct=ct: e.tensor_copy(out=ovb[:, ct, :], in_=stg[:, 0:NSEL]), reads=["stg"], writes=["ovb"])

    KKs = kb.sb("KKs", [128, Sq // 2], BF16)
    KKw = kb.sb("KKw", [128, Sq // 2], BF16)
    Vs = kb.sb("Vs", [128, NT, 76], BF16)
    Vw = kb.sb("Vw", [128, NT, 76], BF16)
    vc = kb.sb("vc", [128, NCT, 76], BF16)
    kcT = kb.sb("kcT", [64, NCC], BF16)
    for t_, k_ in ((Vs, "Vs"), (Vw, "Vw"), (vc, "vc")):
        S.op("pool", lambda e, t_=t_: e.memset(t_[:, :, 64:76], 1.0), writes=[k_])
    xs = [(stg[:].rearrange("p (k t) -> p k t", k=8), "stg")]
    xb0 = kb.sb("xb0", [128, 8, 512], BF16)
    xb = [(xb0, "xb0")]
    sp_ = SoftmaxPairPipe(kb, nU=2, nP=2)
    pss = [(sp_.psS[0][0], "psS0_0"), (sp_.psS[0][1], "psS0_1"), (sp_.psS[1][0], "psS1_0"), (sp_.psS[1][1], "psS1_1")]
    accs = [kb.ps("acc0")]
    psB = kb.ps("psB")
    psI = [kb.ps(f"psI{i}") for i in range(2)]
    psR = psB

    with contextlib.ExitStack() as st0:
        selg = kb.sb("selg", [128, 12 * 64], F32, st0)
        S.op("pool", lambda e: e.memset(stg[:, 0:768], 1.0), writes=["stg"])
        S.op("pool", lambda e: e.affine_select(out=selg[:].rearrange("p (i m) -> p i m", m=64), in_=stg[:, 0:768].rearrange("p (i m) -> p i m", m=64),
                                               pattern=[[-1, 12], [0, 64]], compare_op=ALU.is_equal, fill=0.0, base=-64,
                                               channel_multiplier=1), reads=["stg"], writes=["selg"])
        S.op("pool", lambda e: e.tensor_copy(out=selgb[:], in_=selg[:]), reads=["selg"], writes=["selgb"])
        KVc = kb.sb("KVc", [128, Sq], BF16, st0)
        wkvb = kb.sb("wkvb", [128, 8, 512], BF16, st0)
        S.dma("sp", lambda e: e.dma_start(out=stg[:, 0:4096].rearrange("p (k n) -> p k n", k=8), in_=wkv.rearrange("(k p) n -> p k n", p=128)), writes=["stg"])
        S.op("pool", lambda e: e.tensor_copy(out=wkvb[:].rearrange("p k n -> p (k n)"), in_=stg[:, 0:4096]), reads=["stg"], writes=["wkvb"])
        w1b = kb.sb("w1b", [128, 32, 256], BF16, st0)
        hT = kb.sb("hT", [128, 2, 2, NCC], BF16, st0)
        posb = kb.sb("posb", [128, 32], BF16, st0)
        pbias = kb.sb("pbias", [128, 4], F32, st0)
        gx = kb.sb("gx", [128, 512], F32, st0)
        gu = kb.sb("gu", [128, 512], F32, st0)
        for q4 in range(4):
            S.dma("sp", lambda e, q4=q4: e.dma_start(out=stg[:, 0:2048].rearrange("p (a b) -> p a b", a=8), in_=w1[:, q4 * 8:(q4 + 1) * 8, :]),
                  writes=["stg"])
            S.op("pool", lambda e, q4=q4: e.tensor_copy(out=w1b[:, q4 * 8:(q4 + 1) * 8, :].rearrange("p a b -> p (a b)"), in_=stg[:, 0:2048]),
                 reads=["stg"], writes=["w1b"])
        S.dma("sp", lambda e: e.dma_start(out=stg[:, 0:32], in_=posT), writes=["stg"])
        S.op("pool", lambda e: e.tensor_copy(out=posb[:], in_=stg[:, 0:32]), reads=["stg"], writes=["posb"])
        S.op("pool", lambda e: e.memset(hT[:], 0.0), writes=["hT"])
        S.op("pool", lambda e: e.memset(kcT[:], 0.0), writes=["kcT"])
        def scatter(dst, dkey, eng):
            def ev(p, pk, c):
                for tt in range(4):
                    rows = slice(0, 64) if tt % 2 == 0 else slice(64, 128)
                    m = (4 * c + tt) // 2
                    if eng == "act":
                        S.op("act", lambda e, rows=rows, m=m, tt=tt, p=p: e.copy(out=dst[rows, m * 128:(m + 1) * 128], in_=p[rows, tt * 128:(tt + 1) * 128]),
                             reads=[pk], writes=[dkey])
                    else:
                        S.op("dve", lambda e, rows=rows, m=m, tt=tt, p=p: e.tensor_copy(out=dst[rows, m * 128:(m + 1) * 128], in_=p[rows, tt * 128:(tt + 1) * 128]),
                             reads=[pk], writes=[dkey])
            return ev
        fm = [(wkvb[:, :, 0:128], "wkvb", 128, KVc, "KVc", 1.0, "act"),
              (wkvb[:, :, 128:256], "wkvb", 128, KKs, "KKs", 1.0, scatter(KKs, "KKs", "dve")),
              (wkvb[:, :, 256:384], "wkvb", 128, KKw, "KKw", 1.0, scatter(KKw, "KKw", "act"))]
        tm = [(wkvb[:, :, 384:512], "wkvb", 128, (lambda t: [(Vs[:, t, 0:64], 0, 64), (Vw[:, t, 0:64], 64, 128)]), "V")]
        project(kb, xT, Sq, fm, tm, pss + [(accs[0], "acc0")], xs, xb)
        for kv in range(2):
            rows = slice(kv * 64, (kv + 1) * 64)
            for hc in range(2):
                for p in range(32):
                    S.op("pe", lambda e, rows=rows, hc=hc, p=p, kv=kv: e.matmul(
                        psR[:, kv * 2 + hc:kv * 2 + hc + 1], lhsT=w1b[rows, p, hc * 128:(hc + 1) * 128], rhs=posb[rows, p:p + 1],
                        start=(p == 0), stop=(p == 31)), reads=["w1b", "posb"], writes=["psB"])
        S.op("act", lambda e: e.copy(out=pbias[:], in_=psR[:, 0:4]), reads=["psB"], writes=["pbias"])
        ccs = [(c0, min(512, n_cmp - c0)) for c0 in range(0, n_cmp, 512)]
        KVv = KVc[:].rearrange("p (c s) -> p s c", s=16)
        ui = 0
        for kv in range(2):
            rows = slice(kv * 64, (kv + 1) * 64)
            for hc in range(2):
                for (c0, cn) in ccs:
                    ps_, pk_ = pss[ui % 2]
                    ui += 1
                    for p in range(32):
                        S.op("pe", lambda e, ps_=ps_, rows=rows, hc=hc, p=p, c0=c0, cn=cn: e.matmul(
                            ps_[:, 0:cn], lhsT=w1b[rows, p, hc * 128:(hc + 1) * 128],
                            rhs=KVv[rows, p % 16, c0 + p // 16:c0 + p // 16 + cn], start=(p == 0), stop=(p == 31)),
                            reads=["w1b", "KVc"], writes=[pk_])
                    col = kv * 2 + hc
                    S.op("act", lambda e, ps_=ps_, cn=cn, col=col: e.activation(out=gx[:, 0:cn], in_=ps_[:, 0:cn], func=AF.Identity,
                                                                                bias=pbias[:, col:col + 1]), reads=[pk_, "pbias"], writes=["gx"])
                    S.op("dve", lambda e, cn=cn: e.tensor_tensor(out=gu[:, 0:cn], in0=gx[:, 0:cn], in1=gx[:, 0:cn], op=ALU.mult), reads=["gx"], writes=["gu"])
                    S.op("dve", lambda e, cn=cn: e.tensor_scalar(out=gu[:, 0:cn], in0=gu[:, 0:cn], scalar1=0.044715, scalar2=1.0, op0=ALU.mult, op1=ALU.add),
                         reads=["gu"], writes=["gu"])
                    S.op("dve", lambda e, cn=cn: e.tensor_tensor(out=gu[:, 0:cn], in0=gu[:, 0:cn], in1=gx[:, 0:cn], op=ALU.mult), reads=["gu", "gx"], writes=["gu"])
                    S.op("act", lambda e, cn=cn: e.activation(out=gu[:, 0:cn], in_=gu[:, 0:cn], func=AF.Tanh, scale=0.7978845608028654), reads=["gu"], writes=["gu"])
                    S.op("dve", lambda e, cn=cn: e.tensor_scalar(out=gu[:, 0:cn], in0=gu[:, 0:cn], scalar1=1.0, scalar2=0.5, op0=ALU.add, op1=ALU.mult),
                         reads=["gu"], writes=["gu"])
                    S.op("dve", lambda e, cn=cn, kv=kv, hc=hc, c0=c0: e.tensor_tensor(out=hT[:, kv, hc, c0:c0 + cn], in0=gu[:, 0:cn], in1=gx[:, 0:cn], op=ALU.mult),
                         reads=["gu", "gx"], writes=["hT"])
        for c0 in range(0, NCC, 512):
            cn = min(512, NCC - c0)
            ps_, pk_ = pss[ui % 2]
            ui += 1
            for hc in range(2):
                S.op("pe", lambda e, ps_=ps_, hc=hc, c0=c0, cn=cn: e.matmul(ps_[0:64, 0:cn], lhsT=w2b[:, hc, 0:64], rhs=hT[:, 0, hc, c0:c0 + cn],
                                                                            start=(hc == 0), stop=(hc == 1)), reads=["w2b", "hT"], writes=[pk_])
            S.op("act", lambda e, ps_=ps_, c0=c0, cn=cn: e.copy(out=kcT[:, c0:c0 + cn], in_=ps_[0:64, 0:cn]), reads=[pk_], writes=["kcT"])
        for ct in range(NCT):
            ps_, pk_ = pss[ui % 2]
            ui += 1
            for hc in range(2):
                S.op("pe", lambda e, ps_=ps_, hc=hc, ct=ct: e.matmul(ps_[:, 0:64], lhsT=hT[:, 1, hc, ct * 128:(ct + 1) * 128], rhs=w2b[:, hc, 64:128],
                                                                     start=(hc == 0), stop=(hc == 1)), reads=["w2b", "hT"], writes=[pk_])
            S.op("dve", lambda e, ps_=ps_, ct=ct: e.tensor_copy(out=vc[:, ct, 0:64], in_=ps_[:, 0:64]), reads=[pk_], writes=["vc"])

    S.fence()
    strips = {}
    for nm, src, W in (("bs_s", bss, 2432), ("bs_w", bsw, 1408), ("bs_c", bsc, 3584)):
        t_ = kb.sb(nm, [128, 4, W], BF16)
        strips[nm] = t_
        for r in range(4):
            for w0 in range(0, W, 2048):
                wn = min(2048, W - w0)
                S.dma("sp", lambda e, src=src, r=r, w0=w0, wn=wn: e.dma_start(out=stg[:, 0:wn], in_=src[r, :, w0:w0 + wn]), writes=["stg"])
                S.op("pool", lambda e, t_=t_, r=r, w0=w0, wn=wn: e.tensor_copy(out=t_[:, r, w0:w0 + wn], in_=stg[:, 0:wn]), reads=["stg"], writes=[nm])
    bs_s, bs_w, bs_c = strips["bs_s"], strips["bs_w"], strips["bs_c"]
    Qc = [kb.sb(f"Qc{i}", [128, 512], BF16) for i in range(4)]
    ng_ = kb.sb("ng_", [128, 512], F32)
    tf_ = kb.sb("tf_", [128, 512], F32)
    gsb, fgd = ng_, tf_
    fgdb = kb.sb("fgdb", [128, 512], BF16)
    numt, tmpc = ng_, tf_
    res = [kb.sb(f"res{r}", [64, 512], F32) for r in range(4)]
    osb0 = kb.sb("osb0", [64, 512], BF16)
    osb = [osb0, osb0]
    impsum = [kb.sb(f"imps{t}", [128, SW], F32) for t in range(4)]
    sc2 = kb.sb("sc2", [128, SW], F32)
    mbias = kb.sb("mbias", [128, SW], F32)
    m8a = kb.sb("m8a", [128, 8], F32)
    m8b = kb.sb("m8b", [128, 8], F32)
    thr = kb.sb("thr", [128, 1], F32)
    rdcol = kb.sb("rdcol", [128, 4], F32)
    maskT = kb.sb("maskT", [128, NH, 512], BF16)
    xv = xT.rearrange("(k p) t -> p k t", p=128)

    def qsel(r):
        return [Qc[0][0:64, :], Qc[2][0:64, :], Qc[1][0:64, :], Qc[3][0:64, :]][r], ["Qc0", "Qc2", "Qc1", "Qc3"][r]

    def qwin(r):
        return [Qc[2][64:128, :], Qc[0][64:128, :], Qc[3][64:128, :], Qc[1][64:128, :]][r], ["Qc2", "Qc0", "Qc3", "Qc1"][r]

    acc_i = [0]

    def finalize(acc, acck, br, r, first, last, c):
        i = br * 4 + r
        S.op("dve", lambda e: e.tensor_scalar(out=fgd[64:76, :], in0=acc[64:76, :], scalar1=1e-30, scalar2=None, op0=ALU.max),
             reads=[acck], writes=["fgd"])
        S.op("dve", lambda e: e.reciprocal(out=fgd[64:76, :], in_=fgd[64:76, :]), reads=["fgd"], writes=["fgd"])
        if br == 0:
            for tt in range(4):
                S.op("pe", lambda e, tt=tt: e.matmul(psR[:, tt:tt + 1], lhsT=fgd[64:65, tt * 128:(tt + 1) * 128], rhs=onesf[64:65, 0:1],
                                                     start=True, stop=True), reads=["fgd", "onesf"], writes=["psB"])
            S.op("act", lambda e: e.copy(out=rdcol[:], in_=psR[:, 0:4]), reads=["psB"], writes=["rdcol"])
        S.op("dve", lambda e: e.tensor_tensor(out=fgdb[64:76, :], in0=fgd[64:76, :], in1=gsb[64:76, :], op=ALU.mult),
             reads=["fgd", "gsb"], writes=["fgdb"])
        S.op("pe", lambda e: e.matmul(psB[0:64, :], lhsT=selgb[64:76, i * 64:(i + 1) * 64], rhs=fgdb[64:76, :], start=True, stop=True),
             reads=["fgdb", "selgb"], writes=["psB"])
        S.op("act", lambda e: e.copy(out=numt[0:64, :], in_=acc[0:64, :]), reads=[acck], writes=["numt"])
        if first:
            S.op("dve", lambda e: e.tensor_tensor(out=res[r][:], in0=numt[0:64, :], in1=psB[0:64, :], op=ALU.mult),
                 reads=["numt", "psB"], writes=[f"res{r}"])
        else:
            S.op("dve", lambda e: e.tensor_tensor(out=tmpc[0:64, :], in0=numt[0:64, :], in1=psB[0:64, :], op=ALU.mult),
                 reads=["numt", "psB"], writes=["tmpc"])
            if not last:
                S.op("pool", lambda e: e.tensor_tensor(out=res[r][:], in0=res[r][:], in1=tmpc[0:64, :], op=ALU.add),
                     reads=["tmpc", f"res{r}"], writes=[f"res{r}"])
            else:
                ob = osb[r % 2]
                S.op("pool", lambda e: e.tensor_tensor(out=ob[:], in0=res[r][:], in1=tmpc[0:64, :], op=ALU.add),
                     reads=["tmpc", f"res{r}"], writes=["osb0"])
                S.dma("sp", lambda e: e.dma_start(out=oT[r * 64:(r + 1) * 64, c * 512:(c + 1) * 512], in_=ob[:]), reads=["osb0"])

    for c in range(NCH):
        qs = slice(c * 512, (c + 1) * 512)
        xbi, xbk = xb0, "xb0"
        S.dma("sp", lambda e, c=c: e.dma_start(out=xs[0][0], in_=xv[:, :, c * 512:(c + 1) * 512]), writes=["stg"])
        S.op("dve", lambda e, xbi=xbi: e.tensor_copy(out=xbi[:, 0:4, :], in_=xs[0][0][:, 0:4, :]), reads=["stg"], writes=[xbk])
        S.op("act", lambda e, xbi=xbi: e.copy(out=xbi[:, 4:8, :], in_=xs[0][0][:, 4:8, :]), reads=["stg"], writes=[xbk])
        for qi in range(4):
            ps_, pk_ = pss[qi % 4]
            base = (qi % 2) * 128
            if qi < 2:
                for k in range(8):
                    S.op("pe", lambda e, ps_=ps_, base=base, k=k, xbi=xbi: e.matmul(ps_[:], lhsT=wqb[:, k, base:base + 128], rhs=xbi[:, k, :],
                                                                                  start=(k == 0), stop=(k == 7)), reads=["wqb", xbk], writes=[pk_])
            else:
                for half in range(2):
                    cols = base + 64 * (1 - half)
                    for k in range(8):
                        S.op("pe", lambda e, ps_=ps_, cols=cols, half=half, k=k, xbi=xbi: e.matmul(
                            ps_[half * 64:(half + 1) * 64, :], lhsT=wqb[:, k, cols:cols + 64], rhs=xbi[:, k, :],
                            start=(k == 0), stop=(k == 7)), reads=["wqb", xbk], writes=[pk_])
            S.op("act", lambda e, ps_=ps_, qi=qi: e.mul(Qc[qi][:], ps_[:], SCALE), reads=[pk_], writes=[f"Qc{qi}"])
        for k in range(8):
            S.op("pe", lambda e, k=k, xbi=xbi: e.matmul(psB[0:76, :], lhsT=wgb[:, k, 0:76], rhs=xbi[:, k, :], start=(k == 0), stop=(k == 7)),
                 reads=["wgb", xbk], writes=["psB"])
        S.op("act", lambda e: e.activation(out=gsb[64:76, :], in_=psB[64:76, :], func=AF.Sigmoid), reads=["psB"], writes=["gsb"])

        ctmax = min(NCT - 1, (32 * c + 30) // 128)
        for r in range(4):
            acc, acck = accs[0], "acc0"
            acc_i[0] += 1
            qa, qk = qsel(r)
            units = []
            for ct in range(ctmax + 1):
                o = c - 4 * ct
                mm = [(kcT[:, ct * 128:(ct + 1) * 128], qa, ["kcT", qk], 0)]
                cb = None
                if o <= 6:
                    mm.append((identb[:], bs_c[:, r, o * 512:(o + 1) * 512], ["identb", "bs_c"], 0))
                else:
                    cb = (cf[:, r:r + 1], "cf")
                un = dict(mm=mm, N=512, cb=[cb, None], slots=(0,), pv=[(vc[:, ct, :], ["vc"], acc[0:76, :], acck, ct == 0, ct == ctmax, 0)])
                un["imp"] = (ct, ctmax)
                units.append(un)
            for bnk in range(2):
                S.op("pe", lambda e, bnk=bnk: e.matmul(psI[bnk][:], lhsT=zerob[:], rhs=bs_c[:, 0, 0:512], start=True, stop=False),
                     reads=["zerob", "bs_c"], writes=[f"psI{bnk}"])
            run_cmp(kb, sp_, units, psI, ovb, NSEL)
            finalize(acc, acck, 0, r, True, False, c)
            for tt in range(4):
                src = psI[tt // 2][:, (tt % 2) * 256:(tt % 2) * 256 + NSEL]
                if r == 0:
                    S.op("dve", lambda e, tt=tt, src=src: e.tensor_scalar(out=impsum[tt][:, 0:NSEL], in0=src, scalar1=rdcol[:, tt:tt + 1], scalar2=None,
                                                                          op0=ALU.mult), reads=[f"psI{tt // 2}", "rdcol"], writes=[f"imps{tt}"])
                else:
                    S.op("dve", lambda e, tt=tt, src=src: e.scalar_tensor_tensor(out=impsum[tt][:, 0:NSEL], in0=src, scalar=rdcol[:, tt:tt + 1],
                                                                                 in1=impsum[tt][:, 0:NSEL], op0=ALU.mult, op1=ALU.add),
                         reads=[f"psI{tt // 2}", "rdcol", f"imps{tt}"], writes=[f"imps{tt}"])
        for tt in range(4):
            qt = 4 * c + tt
            sc = impsum[tt]
            sk = f"imps{tt}"
            if SW > NSEL:
                S.op("pool", lambda e, sc=sc: e.memset(sc[:, NSEL:SW], -1e30), writes=[sk])
            if 2 * qt + 2 < NSEL:
                S.op("pool", lambda e, sc=sc, qt=qt: e.memset(sc[:, 2 * qt + 2:NSEL], -1e30), writes=[sk])
            S.op("pool", lambda e, sc=sc, qt=qt: e.memset(sc[0:64, 2 * qt + 1:2 * qt + 2], -1e30), writes=[sk])
            S.op("pool", lambda e, sc=sc: e.memset(sc[:, 0:1], 1e9), writes=[sk])
            lo = max(2 * qt - 1, 0)
            S.op("pool", lambda e, sc=sc, qt=qt, lo=lo: e.memset(sc[0:64, lo:2 * qt + 1], 1e9), writes=[sk])
            S.op("pool", lambda e, sc=sc, qt=qt: e.memset(sc[64:128, 2 * qt:2 * qt + 2], 1e9), writes=[sk])
            S.op("dve", lambda e, sc=sc: e.max(out=m8a[:], in_=sc[:]), reads=[sk], writes=["m8a"])
            S.op("dve", lambda e, sc=sc: e.match_replace(out=sc2[:], in_to_replace=m8a[:], in_values=sc[:], imm_value=-1e30),
                 reads=[sk, "m8a"], writes=["sc2"])
            S.op("dve", lambda e: e.max(out=m8b[:], in_=sc2[:]), reads=["sc2"], writes=["m8b"])
            S.op("dve", lambda e: e.tensor_scalar(out=thr[:], in0=m8b[:, 7:8], scalar1=-1e29, scalar2=None, op0=ALU.max),
                 reads=["m8b"], writes=["thr"])
            S.op("dve", lambda e, sc=sc: e.tensor_scalar(out=mbias[:], in0=sc[:], scalar1=thr[:, 0:1], scalar2=NEG, op0=ALU.is_lt, op1=ALU.mult),
                 reads=[sk, "thr"], writes=["mbias"])
            for jh in range(NH):
                ps_, pk_ = pss[(tt * NH + jh) % 2]
                S.op("pe", lambda e, ps_=ps_, jh=jh: e.transpose(ps_[0:NSP, 0:128], mbias[:, jh * 128:jh * 128 + NSP], identf[:]),
                     reads=["mbias", "identf"], writes=[pk_])
                S.op("act", lambda e, ps_=ps_, jh=jh, tt=tt: e.copy(out=maskT[0:NSP, jh, tt * 128:(tt + 1) * 128], in_=ps_[0:NSP, 0:128]),
                     reads=[pk_], writes=["maskT"])
        jmax = 4 * c + 3
        for r in range(4):
            acc, acck = accs[0], "acc0"
            (qa0, qk0), (qa1, qk1) = qsel(r), qwin(r)
            units = []
            npair = (jmax + 1) // 2
            for m in range(npair):
                mm, cbs, pv = [], [], []
                mm.append((KKs[0:64, m * 128:(m + 1) * 128], qa0, ["KKs", qk0], 0))
                mm.append((KKs[64:128, m * 128:(m + 1) * 128], qa1, ["KKs", qk1], 1))
                for sl in range(2):
                    j = 2 * m + sl
                    col = 2 * (j % 64)
                    mm.append((identb[0:NSP, col:col + 1].to_broadcast([NSP, 64]), maskT[0:NSP, j // 64, :], ["identb", "maskT"], sl, (0, 64)))
                    mm.append((identb[0:NSP, col + 1:col + 2].to_broadcast([NSP, 64]), maskT[0:NSP, j // 64, :], ["identb", "maskT"], sl, (64, 128)))
                for sl in range(2):
                    j = 2 * m + sl
                    o = 4 * c - j
                    if o <= 12:
                        mm.append((identb[:], bs_s[:, r, (o + 3) * 128:(o + 3) * 128 + 512], ["identb", "bs_s"], sl))
                        cbs.append(None)
                    else:
                        cbs.append((cf[:, r:r + 1], "cf"))
                    pv.append((Vs[:, j, :], ["Vs"], acc[0:76, :], acck, j == 0, j == jmax, sl))
                units.append(dict(mm=mm, N=512, cb=cbs, pv=pv))
            sp_.run(units)
            finalize(acc, acck, 1, r, False, False, c)
        for r in range(4):
            acc, acck = accs[0], "acc0"
            (qa0, qk0), (qa1, qk1) = qsel(r), qwin(r)
            units = []
            j0 = max(0, 4 * c - 4)
            for m in range(j0 // 2, (jmax + 1) // 2):
                mm, pv = [], []
                mm.append((KKw[0:64, m * 128:(m + 1) * 128], qa0, ["KKw", qk0], 0))
                mm.append((KKw[64:128, m * 128:(m + 1) * 128], qa1, ["KKw", qk1], 1))
                for sl in range(2):
                    j = 2 * m + sl
                    o = 4 * c - j
                    mm.append((identb[:], bs_w[:, r, (o + 3) * 128:(o + 3) * 128 + 512], ["identb", "bs_w"], sl))
                    pv.append((Vw[:, j, :], ["Vw"], acc[0:76, :], acck, j == j0, j == jmax, sl))
                units.append(dict(mm=mm, N=512, cb=[None, None], pv=pv))
            sp_.run(units)
            finalize(acc, acck, 2, r, False, True, c)
    return kb.finish()


def run_cmp(kb, sp_, units, psI, ovb, NSEL):
    S = kb.S
    for un in units:
        ct, ctmax = un["imp"]

        def post(PTb, PTk, ct=ct, ctmax=ctmax):
            for tt in range(4):
                S.op("pe", lambda e, tt=tt: e.matmul(psI[tt // 2][:, (tt % 2) * 256:(tt % 2) * 256 + NSEL],
                                                     lhsT=PTb[:, tt * 128:(tt + 1) * 128], rhs=ovb[:, ct, :],
                                                     start=False, stop=(ct == ctmax and tt % 2 == 1)),
                     reads=[PTk, "ovb"], writes=[f"psI{tt // 2}"])
        un["post"] = post
    sp_.run(units)


ALPHA = 8 ** 0.25
LN_EPS = 1e-5
D = 1024
NE = 16
DE = 256


def build_post(T, stage=9):
    nc = bass.Bass("TRN2", target_bir_lowering=False)
    TG = min(1024, T)
    NG = T // TG
    NT = TG // 128
    NCH = TG // 512
    oT = nc.dram_tensor("oT", [D, T], BF16, kind="ExternalInput").ap()
    xin = nc.dram_tensor("xin", [T, D], F32, kind="ExternalInput").ap()
    w_out = nc.dram_tensor("w_out", [D, D], F32, kind="ExternalInput").ap()
    lnp = nc.dram_tensor("lnp", [4, D], F32, kind="ExternalInput").ap()
    rw = nc.dram_tensor("rw", [D, NE], F32, kind="ExternalInput").ap()
    rb = nc.dram_tensor("rb", [1, NE], F32, kind="ExternalInput").ap()
    wg = nc.dram_tensor("wg", [NE, D, DE], F32, kind="ExternalInput").ap()
    wu = nc.dram_tensor("wu", [NE, D, DE], F32, kind="ExternalInput").ap()
    wd = nc.dram_tensor("wd", [NE, DE, D], F32, kind="ExternalInput").ap()
    xout = nc.dram_tensor("xout", [T, D], F32, kind="ExternalOutput").ap()

    S = Sched(nc)
    with contextlib.ExitStack() as st:
        def sb(name, shape, dt):
            return st.enter_context(nc.sbuf_tensor(name, shape, dt))

        def ps(name, shape, dt=F32):
            return st.enter_context(nc.psum_tensor(name, shape, dt))

        ident = sb("ident", [128, 128], F32)
        lnb = sb("lnb", [128, 4, D], F32)
        rbb = sb("rbb", [128, NE], F32)
        rwt = sb("rwt", [128, 8, NE], F32)
        wob = sb("wob", [128, 8, D], BF16)
        stg = [sb(f"stg{i}", [128, 2048], F32) for i in range(3)]
        wb = [[sb(f"wb{j}_{i}", [128, 2048], BF16) for i in range(3)] for j in range(2)]
        x1T = sb("x1T", [128, 8, TG], BF16)
        x1Tf = sb("x1Tf", [128, 8, 128], F32)
        yacc = sb("yacc", [128, NT, D], F32)
        xt = [sb(f"xt{i}", [128, D], F32) for i in range(2)]
        ot = [sb(f"ot{i}", [128, 8, 128], BF16) for i in range(2)]
        r = sb("r", [128, D], F32)
        x1 = sb("x1", [128, D], F32)
        stats = sb("stats", [128, 2, 6], F32)
        mv = sb("mv", [128, 2], F32)
        rstd = sb("rstd", [128, 1], F32)
        G = sb("G", [128, NT, NE], F32)
        rt = [sb(f"rt{i}", [128, NE], F32) for i in range(6)]
        rs = [sb(f"rs{i}", [128, 4], F32) for i in range(4)]
        sg = [sb(f"sg{i}", [128, 512], F32) for i in range(2)]
        aT = [[sb(f"aT{j}_{i}", [128, 512], BF16) for i in range(2)] for j in range(2)]
        outt = [sb(f"outt{i}", [128, D], F32) for i in range(2)]

        psD = [[ps(f"psD{j}_{i}", [128, 512]) for i in range(2)] for j in range(2)]
        psG = [ps(f"psG{i}", [128, 512]) for i in range(2)]
        psU = [ps(f"psU{i}", [128, 512]) for i in range(2)]

        S.op("pool", lambda e: e.memset(ident[:], 0.0), writes=["ident"])
        S.op("pool", lambda e: e.memset(r[:, 0:128], 1.0), writes=["r"])
        S.op("pool", lambda e: e.affine_select(out=ident[:], in_=r[:, 0:128], pattern=[[-1, 128]],
                                               compare_op=ALU.is_equal, fill=0.0, base=0, channel_multiplier=1),
             reads=["r"], writes=["ident"])
        S.dma("sp", lambda e: e.dma_start(out=lnb[:], in_=lnp.partition_broadcast(128)), writes=["lnb"])
        S.dma("sp", lambda e: e.dma_start(out=rbb[:], in_=rb[0, :].partition_broadcast(128)), writes=["rbb"])
        S.dma("sp", lambda e: e.dma_start(out=rwt[:], in_=rw.rearrange("(k p) e -> p k e", p=128)), writes=["rwt"])
        wo_v = w_out.rearrange("(k p) n -> p k n", p=128)
        for i in range(4):
            sname = f"stg{i % 3}"
            S.dma("sp", lambda e, i=i: e.dma_start(out=stg[i % 3][:].rearrange("p (k n) -> p k n", k=2),
                                                   in_=wo_v[:, 2 * i:2 * i + 2, :]), writes=[sname])
            S.op("pool", lambda e, i=i: e.tensor_copy(out=wob[:, 2 * i:2 * i + 2, :].rearrange("p k n -> p (k n)"),
                                                      in_=stg[i % 3][:]), reads=[sname], writes=["wob"])

        xin_v = xin.rearrange("(n p) d -> n p d", p=128)
        xout_v = xout.rearrange("(n p) d -> n p d", p=128)
        oT_v = oT.rearrange("(k p) t -> p k t", p=128)
        wg_v = wg.rearrange("e (k p) f -> e p k f", p=128)
        wu_v = wu.rearrange("e (k p) f -> e p k f", p=128)
        wd_v = wd.rearrange("e (k p) n -> e p k n", p=128)

        def layer_norm(src_ap_fn, src_key, dst_ap, dst_key, gi, tag):
            for h in range(2):
                S.op("dve", lambda e, h=h: e.bn_stats(out=stats[:, h, :], in_=src_ap_fn()[:, h * 512:(h + 1) * 512]),
                     reads=[src_key], writes=["stats"])
            S.op("dve", lambda e: e.bn_aggr(out=mv[:], in_=stats[:].rearrange("p a b -> p (a b)")),
                 reads=["stats"], writes=["mv"])
            S.op("dve", lambda e: e.tensor_scalar(out=rstd[:], in0=mv[:, 1:2], scalar1=LN_EPS, scalar2=None,
                                                  op0=ALU.add), reads=["mv"], writes=["rstd"])
            S.op("act", lambda e: e.sqrt(rstd[:], rstd[:]), reads=["rstd"], writes=["rstd"])
            S.op("dve", lambda e: e.reciprocal(out=rstd[:], in_=rstd[:]), reads=["rstd"], writes=["rstd"])
            S.op("dve", lambda e: e.tensor_scalar(out=dst_ap, in0=src_ap_fn(), scalar1=mv[:, 0:1], scalar2=rstd[:, 0:1],
                                                  op0=ALU.subtract, op1=ALU.mult),
                 reads=[src_key, "mv", "rstd"], writes=[dst_key])
            S.op("pool", lambda e: e.tensor_tensor(out=dst_ap, in0=dst_ap, in1=lnb[:, gi, :], op=ALU.mult),
                 reads=[dst_key, "lnb"], writes=[dst_key])
            S.op("pool", lambda e: e.tensor_tensor(out=dst_ap, in0=dst_ap, in1=lnb[:, gi + 1, :], op=ALU.add),
                 reads=[dst_key, "lnb"], writes=[dst_key])

        wcount = 0
        for g in range(NG if stage >= 1 else 0):
            t0 = g * TG
            for t in range(NT):
                gt = g * NT + t
                xi, oi = xt[gt % 2], ot[gt % 2]
                xk, ok = f"xt{gt % 2}", f"ot{gt % 2}"
                S.dma("sp", lambda e, xi=xi, gt=gt: e.dma_start(out=xi[:], in_=xin_v[gt]), writes=[xk])
                S.dma("sp", lambda e, oi=oi, gt=gt: e.dma_start(out=oi[:], in_=oT_v[:, :, gt * 128:(gt + 1) * 128]),
                      writes=[ok])
                pY = psD[0]
                for h in range(2):
                    for k in range(8):
                        S.op("pe", lambda e, h=h, k=k, oi=oi: e.matmul(pY[h][:], lhsT=oi[:, k, :],
                                                                       rhs=wob[:, k, h * 512:(h + 1) * 512],
                                                                       start=(k == 0), stop=(k == 7)),
                             reads=[ok, "wob"], writes=[f"psD0_{h}"])
                for h in range(2):
                    S.op("dve", lambda e, h=h, xi=xi: e.scalar_tensor_tensor(
                        out=r[:, h * 512:(h + 1) * 512], in0=xi[:, h * 512:(h + 1) * 512], scalar=ALPHA,
                        in1=pY[h][:], op0=ALU.mult, op1=ALU.add), reads=[xk, f"psD0_{h}"], writes=["r"])
                if stage < 1.2: continue
                layer_norm(lambda: r[:], "r", x1[:], "x1", 0, "ln1")
                S.op("act", lambda e, t=t: e.mul(yacc[:, t, :], x1[:], ALPHA), reads=["x1"], writes=[f"yacc{t}"])
                if stage < 1.3: continue
                for k in range(8):
                    S.op("pe", lambda e, k=k: e.transpose(psD[1][k // 4][:, (k % 4) * 128:(k % 4 + 1) * 128],
                                                          x1[:, k * 128:(k + 1) * 128], ident[:]),
                         reads=["x1", "ident"], writes=[f"psD1_{k // 4}"])
                for h in range(2 if stage >= 1.32 else 0):
                    S.op("act", lambda e, h=h: e.copy(out=x1Tf[:, 4 * h:4 * h + 4, :].rearrange("p k t -> p (k t)"),
                                                      in_=psD[1][h][:]), reads=[f"psD1_{h}"], writes=["x1Tf"])
                    if stage < 1.33: continue
                    S.op("pool", lambda e, h=h, t=t: e.tensor_copy(
                        out=x1T[:, 4 * h:4 * h + 4, t * 128:(t + 1) * 128],
                        in_=x1Tf[:, 4 * h:4 * h + 4, :]), reads=["x1Tf"], writes=["x1T"])
                if stage < 1.4: continue
                for k in range(8):
                    S.op("pe", lambda e, k=k: e.matmul(psG[0][:, 0:NE], lhsT=x1Tf[:, k, :], rhs=rwt[:, k, :],
                                                       start=(k == 0), stop=(k == 7)),
                         reads=["x1Tf", "rwt"], writes=["psG0"])
                if stage < 1.5: continue
                sc, bi, eq, b2, sel, ws = rt
                m1, m2, gs, gsel = rs
                S.op("act", lambda e: e.activation(out=sc[:], in_=psG[0][:, 0:NE], func=AF.Sigmoid),
                     reads=["psG0"], writes=["sc"])
                S.op("dve", lambda e: e.tensor_tensor(out=bi[:], in0=sc[:], in1=rbb[:], op=ALU.add),
                     reads=["sc", "rbb"], writes=["bi"])
                v3 = lambda a: a[:].rearrange("p (g j) -> p g j", g=4)
                S.op("dve", lambda e: e.tensor_reduce(out=m1[:], in_=v3(bi), axis=AX.X, op=ALU.max),
                     reads=["bi"], writes=["m1"])
                S.op("dve", lambda e: e.tensor_tensor(out=v3(eq), in0=v3(bi), in1=m1[:].unsqueeze(2).to_broadcast([128, 4, 4]),
                                                      op=ALU.is_equal), reads=["bi", "m1"], writes=["eq"])
                S.op("dve", lambda e: e.scalar_tensor_tensor(out=b2[:], in0=eq[:], scalar=-1e9, in1=bi[:],
                                                             op0=ALU.mult, op1=ALU.add), reads=["eq", "bi"], writes=["b2"])
                S.op("dve", lambda e: e.tensor_reduce(out=m2[:], in_=v3(b2), axis=AX.X, op=ALU.max),
                     reads=["b2"], writes=["m2"])
                S.op("dve", lambda e: e.tensor_tensor(out=gs[:], in0=m1[:], in1=m2[:], op=ALU.add),
                     reads=["m1", "m2"], writes=["gs"])
                S.op("dve", lambda e: e.tensor_reduce(out=rstd[:], in_=gs[:], axis=AX.X, op=ALU.max),
                     reads=["gs"], writes=["rstd"])
                S.op("dve", lambda e: e.tensor_scalar(out=gsel[:], in0=gs[:], scalar1=rstd[:, 0:1], scalar2=None,
                                                      op0=ALU.is_ge), reads=["gs", "rstd"], writes=["gsel"])
                S.op("dve", lambda e: e.tensor_tensor(out=v3(sel), in0=v3(bi), in1=m2[:].unsqueeze(2).to_broadcast([128, 4, 4]),
                                                      op=ALU.is_ge), reads=["bi", "m2"], writes=["sel"])
                S.op("dve", lambda e: e.tensor_tensor(out=v3(sel), in0=v3(sel), in1=gsel[:].unsqueeze(2).to_broadcast([128, 4, 4]),
                                                      op=ALU.mult), reads=["sel", "gsel"], writes=["sel"])
                S.op("dve", lambda e: e.tensor_tensor(out=ws[:], in0=sel[:], in1=sc[:], op=ALU.mult),
                     reads=["sel", "sc"], writes=["ws"])
                S.op("dve", lambda e: e.tensor_reduce(out=rstd[:], in_=ws[:], axis=AX.X, op=ALU.add),
                     reads=["ws"], writes=["rstd"])
                S.op("dve", lambda e: e.reciprocal(out=rstd[:], in_=rstd[:]), reads=["rstd"], writes=["rstd"])
                S.op("dve", lambda e, t=t: e.tensor_scalar(out=G[:, t, :], in0=ws[:], scalar1=rstd[:, 0:1], scalar2=None,
                                                           op0=ALU.mult), reads=["ws", "rstd"], writes=[f"G{t}"])
            for ex in range(NE if stage >= 2 else 0):
                wset = wb[wcount % 2]
                wk = [f"wb{wcount % 2}_{i}" for i in range(3)]
                wcount += 1
                srcs = [wg_v[ex], wu_v[ex], wd_v[ex]]
                for i in range(3):
                    kk = 8 if i < 2 else 2
                    S.dma("sp", lambda e, i=i, kk=kk, src=srcs[i]: e.dma_start(
                        out=stg[i][:].rearrange("p (k n) -> p k n", k=kk), in_=src), writes=[f"stg{i}"])
                    S.op("pool", lambda e, i=i, wset=wset: e.tensor_copy(out=wset[i][:], in_=stg[i][:]),
                         reads=[f"stg{i}"], writes=[wk[i]])
                wgb = wset[0][:].rearrange("p (k f) -> p k f", k=8)
                wub = wset[1][:].rearrange("p (k f) -> p k f", k=8)
                wdb = wset[2][:].rearrange("p (k n) -> p k n", k=2)
                for c in range(NCH):
                    pi = (ex * NCH + c) % 2
                    for fh in range(2):
                        for (pt, pk, wv, wkey) in ((psG[fh], f"psG{fh}", wgb, wk[0]), (psU[fh], f"psU{fh}", wub, wk[1])):
                            for k in range(8):
                                S.op("pe", lambda e, pt=pt, wv=wv, k=k, fh=fh, c=c: e.matmul(
                                    pt[:], lhsT=wv[:, k, fh * 128:(fh + 1) * 128], rhs=x1T[:, k, c * 512:(c + 1) * 512],
                                    start=(k == 0), stop=(k == 7)), reads=[wkey, "x1T"], writes=[pk])
                        S.op("act", lambda e, fh=fh: e.activation(out=sg[fh][:], in_=psG[fh][:], func=AF.Silu),
                             reads=[f"psG{fh}"], writes=[f"sg{fh}"])
                        S.op("dve", lambda e, fh=fh, pi=pi: e.tensor_tensor(out=aT[pi][fh][:], in0=sg[fh][:], in1=psU[fh][:],
                                                                           op=ALU.mult),
                             reads=[f"sg{fh}", f"psU{fh}"], writes=[f"aT{pi}_{fh}"])
                    for tt in range(4):
                        t = c * 4 + tt
                        dj = (c * 4 + tt) % 2
                        for h in range(2):
                            for fh in range(2):
                                S.op("pe", lambda e, dj=dj, h=h, fh=fh, tt=tt, pi=pi, wdb=wdb: e.matmul(
                                    psD[dj][h][:], lhsT=aT[pi][fh][:, tt * 128:(tt + 1) * 128],
                                    rhs=wdb[:, fh, h * 512:(h + 1) * 512], start=(fh == 0), stop=(fh == 1)),
                                    reads=[f"aT{pi}_{fh}", wk[2]], writes=[f"psD{dj}_{h}"])
                            S.op("dve", lambda e, dj=dj, h=h, t=t, ex=ex: e.scalar_tensor_tensor(
                                out=yacc[:, t, h * 512:(h + 1) * 512], in0=psD[dj][h][:], scalar=G[:, t, ex:ex + 1],
                                in1=yacc[:, t, h * 512:(h + 1) * 512], op0=ALU.mult, op1=ALU.add),
                                reads=[f"psD{dj}_{h}", f"G{t}", f"yacc{t}"], writes=[f"yacc{t}"])
            for t in range(NT if stage >= 3 else 0):
                gt = g * NT + t
                oo = outt[gt % 2]
                layer_norm(lambda t=t: yacc[:, t, :], f"yacc{t}", oo[:], f"outt{gt % 2}", 2, "ln2")
                S.dma("sp", lambda e, oo=oo, gt=gt: e.dma_start(out=xout_v[gt], in_=oo[:]), reads=[f"outt{gt % 2}"])
        if stage < 3:
            S.dma('sp', lambda e: e.dma_start(out=xout_v[0], in_=lnb[:, 0, :]), reads=['lnb'])
        S.emit()
    return nc


_NCS = {}


def _nc(name, fn):
    if name not in _NCS:
        _NCS[name] = fn()
    return _NCS[name]


def _c(a):
    return np.ascontiguousarray(a)


def _nsa_in_map(xb_, G, w_in, tbl, inp, ov):
    heads = list(range(G * 4, G * 4 + 4))
    wq = _c(w_in[:, G * 256:(G + 1) * 256])
    wqs = _c(np.concatenate([wq[:, 64:128], wq[:, 0:64], wq[:, 192:256], wq[:, 128:192]], axis=1))
    kvp = lambda i: w_in[:, 1024 + i * 256 + G * 64: 1024 + i * 256 + (G + 1) * 64]
    wkv = _c(np.concatenate([kvp(0), kvp(1), kvp(2), kvp(2), kvp(4), kvp(4), kvp(3), kvp(5)], axis=1))
    wgp = np.zeros((1024, 76), np.float32)
    for br in range(3):
        for r in range(4):
            wgp[:, 64 + br * 4 + r] = w_in[:, 2560 + br * 16 + G * 4 + r]
    w1 = np.concatenate([inp['nsa_cmp_w1_k'][0].reshape(32, 64, 256).transpose(1, 0, 2),
                         inp['nsa_cmp_w1_v'][0].reshape(32, 64, 256).transpose(1, 0, 2)], axis=0)
    posT = np.concatenate([inp['nsa_cmp_pos_k'][0].T, inp['nsa_cmp_pos_v'][0].T], axis=0)
    w2 = np.concatenate([inp['nsa_cmp_w2_k'][0].reshape(2, 128, 64).transpose(1, 0, 2),
                         inp['nsa_cmp_w2_v'][0].reshape(2, 128, 64).transpose(1, 0, 2)], axis=2)
    bss, bsw, bsc = nsa_bias_strips(tbl, heads)
    return dict(xT=xb_, wq=wq, wkv=wkv, wgp=wgp, w1=_c(w1), posT=_c(posT), w2=_c(w2), ovm=ov, bss=bss, bsw=bsw, bsc=bsc,
                cfar=_c(np.broadcast_to(tbl[31, heads][None, :], (128, 4))))


def kernel(**inputs):
    inp = {k: np.asarray(v) for k, v in inputs.items()}
    x = inp['x']
    B, Sq, _ = x.shape
    tbl = inp['rel_table']
    T = B * Sq // 8
    cur = x
    for layer in range(4):
        kind = layer % 4
        xTs = [_c(cur[b].T) for b in range(B)]
        maps = []
        if kind == 0:
            nca = _nc('dil', lambda: build_dil(Sq))
            w_in, w_out = inp['dil_w_in'][0], inp['dil_w_out'][0]
            xperm = [[_c(xTs[b][:, dil_perm(Sq, d)]) for (_, d) in DIL] for b in range(B)]
            for c in range(8):
                b, hg = c // 4, c % 4
                heads = list(range(hg * 4, hg * 4 + 4))
                m = {f"xT{g}": xperm[b][g] for g in range(3)}
                wA = np.empty((3, 4, 1024, 192), np.float32)
                for g in range(3):
                    for i, h in enumerate(heads):
                        for j in range(3):
                            wA[g, i, :, j * 64:(j + 1) * 64] = w_in[:, g * 3072 + j * 1024 + h * 64: g * 3072 + j * 1024 + (h + 1) * 64]
                m["wA"] = wA
                m["btA"] = dil_bias_tiles(tbl, heads)
                maps.append(m)
        elif kind == 1:
            nca = _nc('sb', lambda: build_sb(Sq))
            w_in, w_out = inp['sb_w_in'][0], inp['sb_w_out'][0]
            cm = sb_consts()
            for c in range(8):
                b, hg = c // 4, c % 4
                cols = slice(hg * 256, (hg + 1) * 256)
                maps.append(dict(xT=xTs[b], wq=_c(w_in[:, 0:1024][:, cols]), wk=_c(w_in[:, 1024:2048][:, cols]),
                                 wv=_c(w_in[:, 2048:3072][:, cols]), cm=cm))
        elif kind == 2:
            nca = _nc('nsa', lambda: build_nsa(Sq))
            w_in, w_out = inp['nsa_w_in'][0], inp['nsa_w_out'][0]
            ov = nsa_consts(Sq)
            for c in range(8):
                maps.append(_nsa_in_map(xTs[c // 4], c % 4, w_in, tbl, inp, ov))
        else:
            nca = _nc('moba', lambda: build_moba(Sq))
            w_in, w_out = inp['moba_w_in'][0], inp['moba_w_out'][0]
            for c in range(8):
                b, hg = c // 4, c % 4
                cols = slice(hg * 256, (hg + 1) * 256)
                heads = list(range(hg * 4, hg * 4 + 4))
                maps.append(dict(xT=xTs[b], wq=_c(w_in[:, 0:1024][:, cols]), wk=_c(w_in[:, 1024:2048][:, cols]),
                                 wv=_c(w_in[:, 2048:3072][:, cols]), bss=nsa_bias_strips(tbl, heads)[0],
                                 cfar=_c(np.broadcast_to(tbl[31, heads][None, :], (128, 4)))))
        res = run_bass_kernel_spmd(nca, maps, core_ids=list(range(8)))
        oTf = [np.concatenate([res.results[b * 4 + i]['oT'] for i in range(4)], axis=0) for b in range(B)]
        del res, maps, xTs
        ncp = _nc('post', lambda: build_post(T))
        lnp = _c(np.stack([inp['ln1_g'][layer], inp['ln1_b'][layer], inp['ln2_g'][layer], inp['ln2_b'][layer]]))
        curf = cur.reshape(B * Sq, 1024)
        pmaps = []
        for c in range(8):
            b, s0 = (c * T) // Sq, (c * T) % Sq
            pmaps.append(dict(oT=_c(oTf[b][:, s0:s0 + T]), xin=_c(curf[c * T:(c + 1) * T]), w_out=_c(w_out), lnp=lnp,
                              rw=inp['router_w'], rb=_c(inp['router_b'][None, :]), wg=inp['exp_w_gate'][layer],
                              wu=inp['exp_w_up'][layer], wd=inp['exp_w_down'][layer]))
        res = run_bass_kernel_spmd(ncp, pmaps, core_ids=list(range(8)))
        cur = np.concatenate([res.results[c]['xout'] for c in range(8)], axis=0).reshape(B, Sq, 1024)
        del res, pmaps
    return np.asarray(cur, dtype=np.float32)
```

```python
import contextlib
import numpy as np
import concourse.bass as bass
import concourse.mybir as mybir
from concourse.bass_utils import run_bass_kernel_spmd

F32 = mybir.dt.float32
BF16 = mybir.dt.bfloat16
I32 = mybir.dt.int32
AF = mybir.ActivationFunctionType
ALU = mybir.AluOpType
AX = mybir.AxisListType

ENGS = ("pe", "act", "dve", "pool", "sp")
NDSEM = 8


class Sched:
    def __init__(self, nc):
        self.nc = nc
        self.ops = {e: [] for e in ENGS}
        self.lastw = {}
        self.readers = {}
        self.ndma = {e: 0 for e in ENGS}
        self.waited = {e: {} for e in ENGS}

    def _add(self, eng, fn, reads, writes, dma):
        idx = len(self.ops[eng])
        deps = set()
        for b in reads:
            if b in self.lastw:
                deps.add(self.lastw[b])
        for b in writes:
            if b in self.lastw:
                deps.add(self.lastw[b])
            for r in self.readers.get(b, ()):
                deps.add(r)
        op = dict(fn=fn, waits=[], dma=None, sig=False)
        me = (eng, idx)
        if dma:
            k = self.ndma[eng]
            self.ndma[eng] += 1
            slot, val = k % NDSEM, 16 * (k // NDSEM + 1)
            op["dma"] = (slot, val)
            me = ("dma", eng, slot, val)
            if k >= NDSEM:
                deps.add(("dma", eng, slot, val - 16))
        best = {}
        for d in deps:
            if d[0] == "dma":
                key = ("dma", d[1], d[2]); v = d[3]
            else:
                key = d[0]; v = d[1]
                if key == eng and eng == "pe" and not dma:
                    continue
            if v > best.get(key, -1):
                best[key] = v
        w = self.waited[eng]
        for key, v in best.items():
            if w.get(key, -1) >= v:
                continue
            w[key] = v
            op["waits"].append((key, v))
            if key not in ("dma",) and not isinstance(key, tuple):
                self.ops[key][v]["sig"] = True
        self.ops[eng].append(op)
        for b in reads:
            self.readers.setdefault(b, []).append(me)
        for b in writes:
            self.lastw[b] = me
            self.readers[b] = []
        return me

    def op(self, eng, fn, reads=(), writes=()):
        return self._add(eng, fn, tuple(reads), tuple(writes), False)

    def dma(self, eng, fn, reads=(), writes=()):
        return self._add(eng, fn, tuple(reads), tuple(writes), True)

    def fence(self):
        targets = []
        for e in ENGS:
            n = len(self.ops[e])
            for i in range(n - 1, -1, -1):
                o = self.ops[e][i]
                if o["fn"] is not None and o["dma"] is None:
                    targets.append((e, i))
                    break
            k = self.ndma[e]
            for slot in range(min(k, NDSEM)):
                cnt = (k - 1 - slot) // NDSEM + 1
                targets.append((("dma", e, slot), 16 * cnt))
        for e in ENGS:
            w = self.waited[e]
            waits = []
            for key, v in targets:
                if key == e and not isinstance(key, tuple):
                    continue
                if w.get(key, -1) >= v:
                    continue
                w[key] = v
                waits.append((key, v))
                if not isinstance(key, tuple):
                    self.ops[key][v]["sig"] = True
            if waits:
                self.ops[e].append(dict(fn=None, waits=waits, dma=None, sig=False))

    def emit(self, final_engine="sp"):
        nc = self.nc
        fin_waits = []
        for e in ENGS:
            n = self.ndma[e]
            for slot in range(min(n, NDSEM)):
                cnt = (n - 1 - slot) // NDSEM + 1
                fin_waits.append((("dma", e, slot), 16 * cnt))
        self.ops[final_engine].append(dict(fn=None, waits=fin_waits, dma=None, sig=False))
        sigcnt = {}
        for e in ENGS:
            c = 0
            arr = []
            for o in self.ops[e]:
                if o["sig"]:
                    c += 1
                arr.append(c)
            sigcnt[e] = arr
        import contextlib
        with contextlib.ExitStack() as st:
            esem = {e: st.enter_context(nc.semaphore(f"s_{e}")) for e in ENGS}
            dsem = {e: [st.enter_context(nc.semaphore(f"d_{e}{i}")) for i in range(NDSEM)]
                    for e in ENGS if self.ndma[e] > 0}
            block = st.enter_context(nc.Block())

            def run(e, eng):
                for i, o in enumerate(self.ops[e]):
                    for key, v in o["waits"]:
                        if isinstance(key, tuple):
                            eng.wait_ge(dsem[key[1]][key[2]], v)
                        else:
                            eng.wait_ge(esem[key], sigcnt[key][v])
                    if o["fn"] is None:
                        continue
                    ins = o["fn"](eng)
                    if o["dma"] is not None:
                        ins.then_inc(dsem[e][o["dma"][0]], 16)
                    elif o["sig"]:
                        ins.then_inc(esem[e], 1)

            if self.ops["pe"]:
                @block.tensor
                def _(eng):
                    run("pe", eng)
            if self.ops["act"]:
                @block.scalar
                def _(eng):
                    run("act", eng)
            if self.ops["dve"]:
                @block.vector
                def _(eng):
                    run("dve", eng)
            if self.ops["pool"]:
                @block.gpsimd
                def _(eng):
                    run("pool", eng)
            if self.ops["sp"]:
                @block.sync
                def _(eng):
                    run("sp", eng)


D = 1024
HD = 64
SCALE = 0.125
NEG = -30000.0


class KB:
    def __init__(self):
        self.nc = bass.Bass("TRN2", target_bir_lowering=False)
        self.S = Sched(self.nc)
        self.st = contextlib.ExitStack()
        self._n = 0

    def din(self, name, shape, dt=F32):
        return self.nc.dram_tensor(name, list(shape), dt, kind="ExternalInput").ap()

    def dout(self, name, shape, dt=F32):
        return self.nc.dram_tensor(name, list(shape), dt, kind="ExternalOutput").ap()

    def sb(self, name, shape, dt, st=None):
        t = (st or self.st).enter_context(self.nc.sbuf_tensor(name, list(shape), dt))
        return t

    def ps(self, name, shape=(128, 512), dt=F32):
        return self.st.enter_context(self.nc.psum_tensor(name, list(shape), dt))

    def finish(self):
        self.S.emit()
        self.st.close()
        return self.nc


def pipeline(units, stages, skews):
    n = len(units)
    if n == 0:
        return
    for t in range(n + max(skews)):
        for s, fn in enumerate(stages):
            u = t - skews[s]
            if 0 <= u < n:
                fn(u, units[u])


def load_w_bf16(kb, name, w_ap, ncols, stg, stg_key):
    S = kb.S
    wb = kb.sb(name, [128, 8, ncols], BF16)
    S.dma("sp", lambda e: e.dma_start(out=stg[:, 0:8 * ncols].rearrange("p (k n) -> p k n", k=8),
                                      in_=w_ap.rearrange("(k p) n -> p k n", p=128)), writes=[stg_key])
    S.op("pool", lambda e: e.tensor_copy(out=wb[:].rearrange("p k n -> p (k n)"), in_=stg[:, 0:8 * ncols]),
         reads=[stg_key], writes=[name])
    return wb


def project(kb, xT, Sq, fm_outs, tm_outs, pss, xs, xb):
    S = kb.S
    xv = xT.rearrange("(k p) t -> p k t", p=128)
    NC = Sq // 512
    pi = 0
    for c in range(NC):
        xsi = xs[c % len(xs)]
        if isinstance(xsi, tuple):
            xsi, xsk = xsi
        else:
            xsk = f"xs{c % len(xs)}"
        xbi = xb[c % len(xb)]
        if isinstance(xbi, tuple):
            xbi, xbk = xbi
        else:
            xbk = f"xb{c % len(xb)}"
        S.dma("sp", lambda e, xsi=xsi, c=c: e.dma_start(out=xsi[:], in_=xv[:, :, c * 512:(c + 1) * 512]), writes=[xsk])
        S.op("dve", lambda e, xsi=xsi, xbi=xbi: e.tensor_copy(out=xbi[:, 0:4, :], in_=xsi[:, 0:4, :]), reads=[xsk], writes=[xbk])
        S.op("act", lambda e, xsi=xsi, xbi=xbi: e.copy(out=xbi[:, 4:8, :], in_=xsi[:, 4:8, :]), reads=[xsk], writes=[xbk])
        for (w, wkey, M, dst, dkey, scale, ev) in fm_outs:
            p, pk = pss[pi % len(pss)]
            pi += 1
            for k in range(8):
                S.op("pe", lambda e, p=p, w=w, k=k, M=M, xbi=xbi: e.matmul(p[0:M, :], lhsT=w[:, k, 0:M], rhs=xbi[:, k, :],
                                                                          start=(k == 0), stop=(k == 7)),
                     reads=[wkey, xbk], writes=[pk])
            if callable(ev):
                ev(p, pk, c)
            elif ev == "act":
                S.op("act", lambda e, p=p, M=M, dst=dst, c=c, scale=scale: e.mul(dst[0:M, c * 512:(c + 1) * 512], p[0:M, :], scale),
                     reads=[pk], writes=[dkey])
            else:
                S.op("dve", lambda e, p=p, M=M, dst=dst, c=c, scale=scale: e.tensor_scalar(
                    out=dst[0:M, c * 512:(c + 1) * 512], in0=p[0:M, :], scalar1=scale, scalar2=None, op0=ALU.mult),
                    reads=[pk], writes=[dkey])
        for (w, wkey, N, dst_fn, dkey) in tm_outs:
            p, pk = pss[pi % len(pss)]
            pi += 1
            for tt in range(4):
                for k in range(8):
                    S.op("pe", lambda e, p=p, w=w, k=k, N=N, tt=tt, xbi=xbi: e.matmul(
                        p[:, tt * N:(tt + 1) * N], lhsT=xbi[:, k, tt * 128:(tt + 1) * 128], rhs=w[:, k, 0:N],
                        start=(k == 0), stop=(k == 7)), reads=[wkey, xbk], writes=[pk])
            for tt in range(4):
                for (dap, lo, hi) in dst_fn(c * 4 + tt):
                    S.op("dve", lambda e, p=p, N=N, tt=tt, dap=dap, lo=lo, hi=hi: e.tensor_copy(out=dap, in_=p[:, tt * N + lo:tt * N + hi]),
                         reads=[pk], writes=[dkey])


def build_sb(Sq):
    kb = KB()
    S = kb.S
    NT, NCH = Sq // 128, Sq // 512
    xT = kb.din("xT", [D, Sq])
    wq = kb.din("wq", [D, 256])
    wk = kb.din("wk", [D, 256])
    wv = kb.din("wv", [D, 256])
    cm = kb.din("cm", [128, 4 * 512 + 256])
    oT = kb.dout("oT", [256, Sq], BF16)

    stg = kb.sb("stg", [128, 8 * 256], F32)
    cmf = kb.sb("cmf", [128, 4 * 512 + 256], F32)
    cmb = kb.sb("cmb", [128, 4 * 512 + 256], BF16)
    one1 = kb.sb("one1", [128, 1], F32)
    S.dma("sp", lambda e: e.dma_start(out=cmf[:], in_=cm), writes=["cmf"])
    S.op("pool", lambda e: e.tensor_copy(out=cmb[:], in_=cmf[:]), reads=["cmf"], writes=["cmb"])
    S.op("pool", lambda e: e.memset(one1[:], 1.0), writes=["one1"])
    ones_b = cmb[:, 2048:2176]
    umat_b = cmb[:, 2176:2304]
    wqb = load_w_bf16(kb, "wqb", wq, 256, stg, "stg")
    wkb = load_w_bf16(kb, "wkb", wk, 256, stg, "stg")
    wvb = load_w_bf16(kb, "wvb", wv, 256, stg, "stg")

    QT = kb.sb("QT", [128, Sq], BF16)
    KT = kb.sb("KT", [128, Sq], BF16)
    V = kb.sb("V", [128, NT, 128], BF16)
    xs = [kb.sb(f"xs{i}", [128, 8, 512], F32) for i in range(1)]
    xb = [kb.sb(f"xb{i}", [128, 8, 512], BF16) for i in range(2)]
    NB = 3
    NZ = 6
    zc = [kb.sb(f"zc{i}", [128, 512], F32) for i in range(NZ)]
    ee = [kb.sb(f"ee{i}", [128, 512], F32) for i in range(2)]
    sp = [kb.sb(f"sp{i}", [128, 512], BF16) for i in range(NB)]
    t1 = [kb.sb(f"t1{i}", [128, 512], F32) for i in range(NB)]
    att = [kb.sb(f"att{i}", [128, 512], BF16) for i in range(NB)]
    osb = [kb.sb(f"osb{i}", [64, 512], BF16) for i in range(2)]
    totb = [kb.sb(f"totb{i}", [128, 512], F32) for i in range(2)]

    psz = [kb.ps(f"psz{i}") for i in range(2)]
    psT = [kb.ps(f"psT{i}") for i in range(2)]
    psL = [kb.ps(f"psL{i}") for i in range(2)]
    pso = [kb.ps(f"pso{i}") for i in range(2)]
    pss = [(psz[0], "psz0"), (psz[1], "psz1"), (psL[0], "psL0"), (psL[1], "psL1")]

    for pair in range(2):
        fm = [(wqb[:, :, pair * 128:(pair + 1) * 128], "wqb", 128, QT, "QT", SCALE, "act"),
              (wkb[:, :, pair * 128:(pair + 1) * 128], "wkb", 128, KT, "KT", 1.0, "dve")]
        tm = [(wvb[:, :, pair * 128:(pair + 1) * 128], "wvb", 128, (lambda t: [(V[:, t, :], 0, 128)]), "V")]
        project(kb, xT, Sq, fm, tm, pss, xs, xb)

        units = []
        for c in range(NCH):
            jmax = 4 * c + 3
            for j in range(jmax, -1, -1):
                for h in range(2):
                    units.append((c, j, h, jmax))

        def s1(u, un):
            c, j, h, jmax = un
            pz, pzk = psz[u % 2], f"psz{u % 2}"
            hp = slice(h * 64, (h + 1) * 64)
            S.op("pe", lambda e: e.matmul(pz[:], lhsT=KT[hp, j * 128:(j + 1) * 128], rhs=QT[hp, c * 512:(c + 1) * 512],
                                          start=True, stop=True), reads=["KT", "QT"], writes=[pzk])

        def s2a(u, un):
            c, j, h, jmax = un
            pz, pzk = psz[u % 2], f"psz{u % 2}"
            bz = u % NZ
            S.op("dve", lambda e: e.tensor_scalar(out=zc[bz][:], in0=pz[:], scalar1=-60.0, scalar2=60.0,
                                                  op0=ALU.max, op1=ALU.min), reads=[pzk], writes=[f"zc{bz}"])

        def s2b(u, un):
            c, j, h, jmax = un
            bz, be, b = u % NZ, u % 2, u % NB
            S.op("act", lambda e: e.activation(out=ee[be][:], in_=zc[bz][:], func=AF.Exp), reads=[f"zc{bz}"], writes=[f"ee{be}"])
            S.op("act", lambda e: e.activation(out=sp[b][:], in_=ee[be][:], func=AF.Ln, bias=one1[:, 0:1]),
                 reads=[f"ee{be}", "one1"], writes=[f"sp{b}"])
            if j >= 4 * c:
                o = j - 4 * c
                S.op("pool", lambda e: e.tensor_tensor(out=sp[b][:], in0=sp[b][:], in1=cmb[:, o * 512:(o + 1) * 512], op=ALU.mult),
                     reads=[f"sp{b}", "cmb"], writes=[f"sp{b}"])

        def s3b(u, un):
            c, j, h, jmax = un
            bz = u % NZ
            if j != jmax:
                S.op("pool", lambda e: e.tensor_tensor(out=zc[bz][:], in0=zc[bz][:], in1=totb[h][:], op=ALU.subtract),
                     reads=[f"zc{bz}", f"totb{h}"], writes=[f"zc{bz}"])

        def s3(u, un):
            c, j, h, jmax = un
            b = u % NB
            pl, plk = psL[u % 2], f"psL{u % 2}"
            p1, p1k = psT[u % 2], f"psT{u % 2}"
            S.op("pe", lambda e: e.matmul(pl[:], lhsT=umat_b, rhs=sp[b][:], start=True, stop=True),
                 reads=[f"sp{b}", "cmb"], writes=[plk])
            S.op("pe", lambda e: e.matmul(p1[:], lhsT=ones_b, rhs=sp[b][:], start=True, stop=True),
                 reads=[f"sp{b}", "cmb"], writes=[p1k])

        def s4a(u, un):
            c, j, h, jmax = un
            bz, b = u % NZ, u % NB
            pl, plk = psL[u % 2], f"psL{u % 2}"
            p1, p1k = psT[u % 2], f"psT{u % 2}"
            S.op("dve", lambda e: e.tensor_tensor(out=t1[b][:], in0=zc[bz][:], in1=pl[:], op=ALU.subtract),
                 reads=[f"zc{bz}", plk], writes=[f"t1{b}"])
            if j != jmax:
                S.op("dve", lambda e: e.tensor_tensor(out=totb[h][:], in0=totb[h][:], in1=p1[:], op=ALU.add),
                     reads=[f"totb{h}", p1k], writes=[f"totb{h}"])
            else:
                S.op("dve", lambda e: e.tensor_copy(out=totb[h][:], in_=p1[:]), reads=[p1k], writes=[f"totb{h}"])

        def s4b(u, un):
            c, j, h, jmax = un
            b = u % NB
            S.op("act", lambda e: e.activation(out=att[b][:], in_=t1[b][:], func=AF.Exp), reads=[f"t1{b}"], writes=[f"att{b}"])
            if j >= 4 * c:
                o = j - 4 * c
                S.op("pool", lambda e: e.tensor_tensor(out=att[b][:], in0=att[b][:], in1=cmb[:, o * 512:(o + 1) * 512], op=ALU.mult),
                     reads=[f"att{b}", "cmb"], writes=[f"att{b}"])

        def s5(u, un):
            c, j, h, jmax = un
            b = u % NB
            S.op("pe", lambda e: e.matmul(pso[h][0:64, :], lhsT=V[:, j, h * 64:(h + 1) * 64], rhs=att[b][:],
                                          start=(j == jmax), stop=(j == 0)), reads=[f"att{b}", "V"], writes=[f"pso{h}"])
            if j == 0:
                ob = osb[h]
                S.op("act", lambda e: e.copy(out=ob[:], in_=pso[h][0:64, :]), reads=[f"pso{h}"], writes=[f"osb{h}"])
                r0 = pair * 128 + h * 64
                S.dma("sp", lambda e: e.dma_start(out=oT[r0:r0 + 64, c * 512:(c + 1) * 512], in_=ob[:]), reads=[f"osb{h}"])

        n_u = len(units)
        stages = ((s2a, 0), (s2b, 2), (s3, 3), (s4a, 4), (s3b, 3), (s4b, 5), (s5, 6))
        for t in range(n_u + 7):
            if t % 2 == 0:
                for u in (t, t + 1):
                    if u < n_u:
                        s1(u, units[u])
            for fn_, sk_ in stages:
                u = t - sk_
                if 0 <= u < n_u:
                    fn_(u, units[u])
    return kb.finish()


def sb_consts():
    cm = np.zeros((128, 4 * 512 + 256), np.float32)
    k = np.arange(128)[:, None]
    q = np.arange(512)[None, :]
    for o in range(4):
        cm[:, o * 512:(o + 1) * 512] = ((o * 128 + k) < q).astype(np.float32)
    cm[:, 2048:2176] = 1.0
    jj = np.arange(128)[:, None]
    ss = np.arange(128)[None, :]
    cm[:, 2176:2304] = (jj >= ss).astype(np.float32)
    return cm


class SoftmaxPipe:
    def __init__(self, kb, nS=3, nP=3):
        import os
        nP = int(os.environ.get('SM_NP', nP))
        self.kb = kb
        self.psS = [kb.ps(f"psS{i}") for i in range(nS)]
        self.PT = [kb.sb(f"PT{i}", [128, 512], BF16) for i in range(nP)]
        self.nS, self.nP = nS, nP
        self.cnt = 0

    def run(self, units):
        S = self.kb.S
        base = self.cnt
        self.cnt += len(units)

        def s1(u, un):
            i = (base + u) % self.nS
            ps, pk = self.psS[i], f"psS{i}"
            n = len(un["mm"])
            kp, N = un.get("kp", 128), un["N"]
            for m, mmx in enumerate(un["mm"]):
                (lhsT, rhs, reads) = mmx[:3]
                r0, r1 = mmx[3] if len(mmx) > 3 else (0, kp)
                S.op("pe", lambda e, lhsT=lhsT, rhs=rhs, m=m, r0=r0, r1=r1: e.matmul(ps[r0:r1, 0:N], lhsT=lhsT, rhs=rhs, start=(m == 0), stop=(m == n - 1)),
                     reads=reads, writes=[pk])

        def s2(u, un):
            i = (base + u) % self.nS
            ps, pk = self.psS[i], f"psS{i}"
            b = (base + u) % self.nP
            kp, N = un.get("kp", 128), un["N"]
            cb = un.get("cb")
            if cb is None:
                S.op("act", lambda e: e.activation(out=self.PT[b][0:kp, 0:N], in_=ps[0:kp, 0:N], func=AF.Exp),
                     reads=[pk], writes=[f"PT{b}"])
            else:
                S.op("act", lambda e: e.activation(out=self.PT[b][0:kp, 0:N], in_=ps[0:kp, 0:N], func=AF.Exp, bias=cb[0]),
                     reads=[pk, cb[1]], writes=[f"PT{b}"])
            if un.get("post"):
                un["post"](self.PT[b], f"PT{b}")

        def s3(u, un):
            b = (base + u) % self.nP
            kp, N = un.get("kp", 128), un["N"]
            for pv in un["pv"]:
                (lhsT_v, reads, acc, acck, start, stop) = pv[:6]
                c0, c1 = pv[6] if len(pv) > 6 else (0, N)
                S.op("pe", lambda e, lhsT_v=lhsT_v, acc=acc, start=start, stop=stop, c0=c0, c1=c1: e.matmul(
                    acc, lhsT=lhsT_v, rhs=self.PT[b][0:kp, c0:c1], start=start, stop=stop),
                    reads=[f"PT{b}"] + list(reads), writes=[acck])
            if un.get("fin"):
                un["fin"]()

        import os
        pipeline(units, [s1, s2, s3], [int(v) for v in os.environ.get('SM_SKEW', '0,1,2').split(',')])


class SoftmaxPairPipe:
    def __init__(self, kb, nU=2, nP=3):
        import os
        nP = int(os.environ.get('SMP_NP', 4))
        self.kb = kb
        self.psS = [[kb.ps(f"psS{u}_{h}") for h in range(2)] for u in range(nU)]
        self.PT = [[kb.sb(f"PT{u}_{h}", [128, 512], BF16) for h in range(2)] for u in range(nP)]
        self.nU, self.nP = nU, nP
        self.cnt = 0

    def run(self, units):
        S = self.kb.S
        base = self.cnt
        self.cnt += len(units)

        def s1(u, un):
            i = (base + u) % self.nU
            N = un["N"]
            cnt = [0, 0]
            tot = [sum(1 for m in un["mm"] if m[3] == h) for h in range(2)]
            for mmx in un["mm"]:
                (lhsT, rhs, reads, h) = mmx[:4]
                r0, r1 = mmx[4] if len(mmx) > 4 else (0, 128)
                ps, pk = self.psS[i][h], f"psS{i}_{h}"
                first, last = cnt[h] == 0, cnt[h] == tot[h] - 1
                cnt[h] += 1
                S.op("pe", lambda e, lhsT=lhsT, rhs=rhs, ps=ps, first=first, last=last, r0=r0, r1=r1: e.matmul(ps[r0:r1, 0:N], lhsT=lhsT, rhs=rhs, start=first, stop=last),
                     reads=reads, writes=[pk])

        def s2(u, un):
            i = (base + u) % self.nU
            b = (base + u) % self.nP
            N = un["N"]
            for h in un.get("slots", (0, 1)):
                ps, pk = self.psS[i][h], f"psS{i}_{h}"
                cb = un["cb"][h]
                if cb is None:
                    S.op("act", lambda e, ps=ps, h=h: e.activation(out=self.PT[b][h][:, 0:N], in_=ps[:, 0:N], func=AF.Exp),
                         reads=[pk], writes=[f"PT{b}_{h}"])
                else:
                    S.op("act", lambda e, ps=ps, h=h, cb=cb: e.activation(out=self.PT[b][h][:, 0:N], in_=ps[:, 0:N], func=AF.Exp, bias=cb[0]),
                         reads=[pk, cb[1]], writes=[f"PT{b}_{h}"])
            if un.get("post"):
                un["post"](self.PT[b][0], f"PT{b}_0")

        def s3(u, un):
            b = (base + u) % self.nP
            N = un["N"]
            for (lhsT_v, reads, acc, acck, start, stop, h) in un["pv"]:
                S.op("pe", lambda e, lhsT_v=lhsT_v, acc=acc, start=start, stop=stop, h=h: e.matmul(
                    acc, lhsT=lhsT_v, rhs=self.PT[b][h][:, 0:N], start=start, stop=stop),
                    reads=[f"PT{b}_{h}"] + list(reads), writes=[acck])
            if un.get("fin"):
                un["fin"]()

        import os
        pipeline(units, [s1, s2, s3], [int(v) for v in os.environ.get('SMP_SKEW', '0,1,2').split(',')])


class Normalizer:
    def __init__(self, kb):
        self.kb = kb
        self.rdt = kb.sb("nz_rdt", [128, 512], F32)
        self.num = kb.sb("nz_num", [64, 512], F32)
        self.onesf = kb.sb("nz_ones", [128, 64], F32)
        self.psB = kb.ps("psB")
        kb.S.op("pool", lambda e: e.memset(self.onesf[:], 1.0), writes=["nz_ones"])

    def bcast_recip(self, acc, acck, N):
        S = self.kb.S
        S.op("dve", lambda e: e.reciprocal(out=self.rdt[64:65, 0:N], in_=acc[64:65, 0:N]), reads=[acck], writes=["nz_rdt"])
        S.op("pe", lambda e: e.matmul(self.psB[0:64, 0:N], lhsT=self.onesf[64:65, 0:64], rhs=self.rdt[64:65, 0:N],
                                      start=True, stop=True), reads=["nz_rdt", "nz_ones"], writes=["psB"])

    def normalize(self, acc, acck, N, out_ap, outk):
        S = self.kb.S
        self.bcast_recip(acc, acck, N)
        S.op("act", lambda e: e.copy(out=self.num[:, 0:N], in_=acc[0:64, 0:N]), reads=[acck], writes=["nz_num"])
        S.op("dve", lambda e: e.tensor_tensor(out=out_ap, in0=self.num[:, 0:N], in1=self.psB[0:64, 0:N], op=ALU.mult),
             reads=["nz_num", "psB"], writes=[outk])


NOFF = 16


def build_moba(Sq):
    kb = KB()
    S = kb.S
    NT, NCH, NBLK = Sq // 128, Sq // 512, Sq // 256
    GW = max(NBLK, 8)
    xT = kb.din("xT", [D, Sq])
    wq = kb.din("wq", [D, 256])
    wk = kb.din("wk", [D, 256])
    wv = kb.din("wv", [D, 256])
    bss = kb.din("bss", [4, 128, 2432])
    cfar = kb.din("cfar", [128, 4])
    oT = kb.dout("oT", [256, Sq], BF16)

    stg = kb.sb("stg", [128, 8 * 512], F32)
    wqb = load_w_bf16(kb, "wqb", wq, 256, stg, "stg")
    wkb = load_w_bf16(kb, "wkb", wk, 256, stg, "stg")
    wvb = load_w_bf16(kb, "wvb", wv, 256, stg, "stg")
    cf = kb.sb("cf", [128, 4], F32)
    S.dma("sp", lambda e: e.dma_start(out=cf[:], in_=cfar), writes=["cf"])
    identf = kb.sb("identf", [128, 128], F32)
    identb = kb.sb("identb", [128, 128], BF16)
    onesf = kb.sb("onesf", [128, 128], F32)
    S.op("pool", lambda e: e.memset(onesf[:], 1.0), writes=["onesf"])
    S.op("pool", lambda e: e.affine_select(out=identf[:], in_=onesf[:], pattern=[[-1, 128]], compare_op=ALU.is_equal,
                                           fill=0.0, base=0, channel_multiplier=1), reads=["onesf"], writes=["identf"])
    S.op("pool", lambda e: e.tensor_copy(out=identb[:], in_=identf[:]), reads=["identf"], writes=["identb"])

    QT = kb.sb("QT", [128, Sq], BF16)
    KT = kb.sb("KT", [128, Sq], BF16)
    Vh = [kb.sb(f"V{h}", [128, NT, 65], BF16) for h in range(2)]
    for h in range(2):
        S.op("pool", lambda e, h=h: e.memset(Vh[h][:, :, 64:65], 1.0), writes=["V"])
    maskT = kb.sb("maskT", [128, Sq], BF16)
    bs = kb.sb("bs", [128, 2, 2432], BF16)
    xs = [(stg[:].rearrange("p (k t) -> p k t", k=8), "stg")]
    xb = [kb.sb(f"xb{i}", [128, 8, 512], BF16) for i in range(2)]
    km = kb.sb("km", [128, NBLK], F32)
    kmb = kb.sb("kmb", [128, NBLK], BF16)
    gbuf = kb.sb("gbuf", [128, GW], F32)
    m8 = kb.sb("m8", [128, 8], F32)
    mb2 = kb.sb("mb2", [128, 128], F32)
    osb = [kb.sb(f"osb{i}", [64, 512], BF16) for i in range(2)]

    sp_ = SoftmaxPairPipe(kb)
    nz = Normalizer(kb)
    pso = [[kb.ps(f"pso{h}_0")] for h in range(2)]
    pss = [(sp_.psS[0][0], "psS0_0"), (sp_.psS[0][1], "psS0_1"), (sp_.psS[1][0], "psS1_0")]
    import os
    for i in range(int(os.environ.get("DUMMY_WARM", "0"))):
        S.op("pe", lambda e: e.matmul(sp_.psS[0][0][:, 0:256], lhsT=identb[:], rhs=wqb[:, 0, :], start=True, stop=True),
             reads=["identb", "wqb"], writes=["psS0_0"])

    for pair in range(2):
        fm = [(wqb[:, :, pair * 128:(pair + 1) * 128], "wqb", 128, QT, "QT", SCALE, "act"),
              (wkb[:, :, pair * 128:(pair + 1) * 128], "wkb", 128, KT, "KT", 1.0, "dve")]
        tm = [(wvb[:, :, pair * 128:(pair + 1) * 128], "wvb", 128,
               (lambda t: [(Vh[0][:, t, 0:64], 0, 64), (Vh[1][:, t, 0:64], 64, 128)]), "V")]
        project(kb, xT, Sq, fm, tm, pss, xs, xb)
        for h in range(2):
            for w0 in range(0, 2432, 2048):
                wn = min(2048, 2432 - w0)
                S.dma("sp", lambda e, h=h, pair=pair, w0=w0, wn=wn: e.dma_start(out=stg[:, 0:wn], in_=bss[pair * 2 + h, :, w0:w0 + wn]), writes=["stg"])
                S.op("pool", lambda e, h=h, w0=w0, wn=wn: e.tensor_copy(out=bs[:, h, w0:w0 + wn], in_=stg[:, 0:wn]), reads=["stg"], writes=["bs"])
        S.op("dve", lambda e: e.tensor_reduce(out=km[:], in_=KT[:].rearrange("p (b k) -> p b k", k=256), axis=AX.X, op=ALU.add),
             reads=["KT"], writes=["km"])
        S.op("dve", lambda e: e.tensor_copy(out=kmb[:], in_=km[:]), reads=["km"], writes=["kmb"])
        for qt in range(NT):
            nv = qt // 2
            S.op("pool", lambda e: e.memset(mb2[:], NEG), writes=["mb2"])
            for h in range(2):
                hp = slice(h * 64, (h + 1) * 64)
                c0 = h * 64
                pg, pgk = pss[(qt * 2 + h) % 3]
                if nv > 3:
                    S.op("pe", lambda e, pg=pg, hp=hp, qt=qt: e.matmul(pg[:, 0:NBLK], lhsT=QT[hp, qt * 128:(qt + 1) * 128], rhs=kmb[hp, :],
                                                                       start=True, stop=True), reads=["QT", "kmb"], writes=[pgk])
                    S.op("pool", lambda e: e.memset(gbuf[:], -1e30), writes=["gbuf"])
                    S.op("dve", lambda e, pg=pg, nv=nv: e.tensor_copy(out=gbuf[:, 0:nv], in_=pg[:, 0:nv]), reads=[pgk], writes=["gbuf"])
                    S.op("dve", lambda e: e.max(out=m8[:], in_=gbuf[:]), reads=["gbuf"], writes=["m8"])
                    S.op("dve", lambda e, c0=c0: e.tensor_scalar(out=mb2[:, c0:c0 + NBLK], in0=gbuf[:, 0:NBLK], scalar1=m8[:, 2:3], scalar2=NEG,
                                                                 op0=ALU.is_lt, op1=ALU.mult), reads=["gbuf", "m8"], writes=["mb2"])
                elif nv > 0:
                    S.op("pool", lambda e, nv=nv, c0=c0: e.memset(mb2[:, c0:c0 + nv], 0.0), writes=["mb2"])
                S.op("pool", lambda e, nv=nv, c0=c0: e.memset(mb2[:, c0 + nv:c0 + nv + 1], 0.0), writes=["mb2"])
            pt, ptk = pss[(qt * 2 + 2) % 3]
            S.op("pe", lambda e, pt=pt: e.transpose(pt[:, 0:128], mb2[:], identf[:]), reads=["mb2", "identf"], writes=[ptk])
            S.op("act", lambda e, pt=pt, qt=qt: e.copy(out=maskT[:, qt * 128:(qt + 1) * 128], in_=pt[:, 0:128]),
                 reads=[ptk], writes=["maskT"])
        units = []
        for c in range(NCH):
            jmax = 4 * c + 3
            qs = slice(c * 512, (c + 1) * 512)
            for j in range(jmax + 1):
                o = 4 * c - j
                x = j // 2
                mm, cbs, pv = [], [], []
                for h in range(2):
                    hp = slice(h * 64, (h + 1) * 64)
                    mm.append((KT[hp, j * 128:(j + 1) * 128], QT[hp, qs], ["KT", "QT"], h))
                for h in range(2):
                    hp = slice(h * 64, (h + 1) * 64)
                    mm.append((identb[hp, h * 64 + x:h * 64 + x + 1].to_broadcast([64, 128]), maskT[hp, qs], ["identb", "maskT"], h))
                for h in range(2):
                    if o <= 12:
                        mm.append((identb[:], bs[:, h, (o + 3) * 128:(o + 3) * 128 + 512], ["identb", "bs"], h))
                        cbs.append(None)
                    else:
                        cbs.append((cf[:, pair * 2 + h:pair * 2 + h + 1], "cf"))
                    pv.append((Vh[h][:, j, :], ["V"], pso[h][0][0:65, :], f"pso{h}_0", j == 0, j == jmax, h))
                un = dict(mm=mm, N=512, cb=cbs, pv=pv)
                if j == jmax:
                    def fin(c=c, pair=pair):
                        for h in range(2):
                            ob = osb[h]
                            nz.normalize(pso[h][0], f"pso{h}_0", 512, ob[:], f"osb{h}")
                            r0 = pair * 128 + h * 64
                            S.dma("sp", lambda e, ob=ob, r0=r0: e.dma_start(out=oT[r0:r0 + 64, c * 512:(c + 1) * 512], in_=ob[:]), reads=[f"osb{h}"])
                    un["fin"] = fin
                units.append(un)
        sp_.run(units)
    return kb.finish()


def rel_bucket_np(dist):
    n = np.maximum(dist, 0)
    exact = 16
    logf = np.log(np.maximum(n, 1).astype(np.float32) / exact) / np.float32(np.log(2048 / exact))
    large = np.minimum(exact + (logf * 16).astype(np.int32), 31)
    return np.where(n < exact, n, large)


def moba_bias_tiles(rel_table, heads):
    k = np.arange(128)[:, None]
    q = np.arange(512)[None, :]
    out = np.empty((len(heads), NOFF, 128, 512), np.float32)
    for oi in range(NOFF):
        o = oi - 3
        dist = o * 128 + q - k
        bk = rel_bucket_np(dist)
        for i, h in enumerate(heads):
            out[i, oi] = np.where(dist >= 0, rel_table[bk, h], np.float32(NEG))
    return out


DIL = ((128, 1), (512, 4), (2048, 16))


def build_dil(Sq):
    kb = KB()
    S = kb.S
    NT, NCH = Sq // 128, Sq // 512
    xTg = [kb.din(f"xT{g}", [D, Sq]) for g in range(3)]
    wA = kb.din("wA", [3, 4, D, 192])
    btA = kb.din("btA", [3, 4, 128, 256])
    oT = kb.dout("oT", [256, Sq], BF16)

    stg = kb.sb("stg", [128, 8 * 192], F32)
    identf = kb.sb("identf", [128, 128], F32)
    identb = kb.sb("identb", [128, 128], BF16)
    onesf = kb.sb("onesf", [128, 128], F32)
    S.op("pool", lambda e: e.memset(onesf[:], 1.0), writes=["onesf"])
    S.op("pool", lambda e: e.affine_select(out=identf[:], in_=onesf[:], pattern=[[-1, 128]], compare_op=ALU.is_equal,
                                           fill=0.0, base=0, channel_multiplier=1), reads=["onesf"], writes=["identf"])
    S.op("pool", lambda e: e.tensor_copy(out=identb[:], in_=identf[:]), reads=["identf"], writes=["identb"])
    QT = kb.sb("QT", [64, Sq], BF16)
    KT = kb.sb("KT", [64, Sq], BF16)
    V = kb.sb("V", [128, NT, 65], BF16)
    S.op("pool", lambda e: e.memset(V[:, :, 64:65], 1.0), writes=["V"])
    accS = kb.sb("accS", [65, Sq], F32)
    wab = kb.sb("wab", [128, 8, 192], BF16)
    btf = kb.sb("btf", [128, 256], F32)
    btb = kb.sb("btb", [128, 256], BF16)
    xs = [kb.sb(f"xs{i}", [128, 8, 512], F32) for i in range(1)]
    xb = [kb.sb(f"xb{i}", [128, 8, 512], BF16) for i in range(2)]
    osb = [kb.sb(f"osb{i}", [64, 512], BF16) for i in range(2)]
    sp_ = SoftmaxPipe(kb)
    nz = Normalizer(kb)
    pso = [kb.ps(f"pso{i}") for i in range(2)]
    pss = [(sp_.psS[0], "psS0"), (sp_.psS[1], "psS1"), (sp_.psS[2], "psS2")]

    for hh in range(4):
        for g, (win, d) in enumerate(DIL):
            L = Sq // d
            Lt = L // 128
            S.dma("sp", lambda e, g=g, hh=hh: e.dma_start(out=stg[:].rearrange("p (k n) -> p k n", k=8),
                                                         in_=wA[g, hh].rearrange("(k p) n -> p k n", p=128)), writes=["stg"])
            S.op("pool", lambda e: e.tensor_copy(out=wab[:].rearrange("p k n -> p (k n)"), in_=stg[:]), reads=["stg"], writes=["wab"])
            S.dma("sp", lambda e, g=g, hh=hh: e.dma_start(out=btf[:], in_=btA[g, hh]), writes=["btf"])
            S.op("pool", lambda e: e.tensor_copy(out=btb[:], in_=btf[:]), reads=["btf"], writes=["btb"])
            fm = [(wab[:, :, 0:64], "wab", 64, QT, "QT", SCALE, "act"), (wab[:, :, 64:128], "wab", 64, KT, "KT", 1.0, "dve")]
            tm = [(wab[:, :, 128:192], "wab", 64, (lambda t: [(V[:, t, 0:64], 0, 64)]), "V")]
            project(kb, xTg[g], Sq, fm, tm, pss, xs, xb)
            accv = accS[:].rearrange("p (i r) -> p r i", r=d)
            units = []
            for T in range(NT):
                r, jl = T // Lt, T % Lt
                has_next = jl + 1 < Lt
                N = 256 if has_next else 128
                mm = [(KT[:, T * 128:(T + 1) * 128], QT[:, T * 128:T * 128 + N], ["KT", "QT"]),
                      (identb[:], btb[:, 0:N], ["identb", "btb"])]
                pv = [(V[:, T, :], ["V"], pso[T % 2][0:65, 0:128], f"pso{T % 2}", jl == 0, True, (0, 128))]
                if has_next:
                    pv.append((V[:, T, :], ["V"], pso[(T + 1) % 2][0:65, 0:128], f"pso{(T + 1) % 2}", True, False, (128, 256)))

                def fin(T=T, r=r, jl=jl, g=g, accv=accv):
                    dst = accv[:, r, jl * 128:(jl + 1) * 128]
                    acc, acck = pso[T % 2], f"pso{T % 2}"
                    if g == 0:
                        S.op("dve", lambda e: e.tensor_copy(out=dst, in_=acc[0:65, 0:128]), reads=[acck], writes=["accS"])
                    else:
                        S.op("dve", lambda e: e.tensor_tensor(out=dst, in0=dst, in1=acc[0:65, 0:128], op=ALU.add),
                             reads=[acck, "accS"], writes=["accS"])
                units.append(dict(mm=mm, N=N, kp=128, pv=pv, fin=fin))
            sp_.run(units)
        for c in range(NCH):
            ob = osb[c % 2]
            cs = slice(c * 512, (c + 1) * 512)
            nz.bcast_recip(accS[:, cs], "accS", 512)
            S.op("dve", lambda e, ob=ob, cs=cs: e.tensor_tensor(out=ob[:], in0=accS[0:64, cs], in1=nz.psB[0:64, :], op=ALU.mult),
                 reads=["accS", "psB"], writes=[f"osb{c % 2}"])
            S.dma("sp", lambda e, ob=ob, cs=cs, hh=hh: e.dma_start(out=oT[hh * 64:(hh + 1) * 64, cs], in_=ob[:]), reads=[f"osb{c % 2}"])
    return kb.finish()


def dil_bias_tiles(rel_table, heads):
    k = np.arange(128)[:, None]
    q = np.arange(256)[None, :]
    steps = q - k
    out = np.empty((3, len(heads), 128, 256), np.float32)
    for g, (win, d) in enumerate(DIL):
        span = win // d
        ok = (steps >= 0) & (steps <= span)
        bk = rel_bucket_np(steps * d)
        for i, h in enumerate(heads):
            out[g, i] = np.where(ok, rel_table[bk, h], np.float32(NEG))
    return out


def dil_perm(Sq, d):
    L = Sq // d
    return (np.arange(d)[:, None] + d * np.arange(L)[None, :]).reshape(-1)


def nsa_consts(Sq):
    NSEL = Sq // 64
    n_cmp = Sq // 16 - 1
    NCT = (n_cmp + 127) // 128
    c = np.arange(NCT * 128)[:, None]
    j = np.arange(NSEL)[None, :]
    ov = ((c >= 4 * j - 1) & (c <= 4 * j + 3) & (c < n_cmp)).astype(np.float32)
    return ov


def nsa_bias_strips(rel_table, heads):
    k = np.arange(128)[:, None]
    H = len(heads)
    def strip(dist, ok):
        bk = rel_bucket_np(dist)
        out = np.empty((H,) + dist.shape, np.float32)
        for i, h in enumerate(heads):
            out[i] = np.where(ok, rel_table[bk, h], np.float32(NEG))
        return out
    ds = np.arange(2432)[None, :] - 384 - k
    dw = np.arange(1408)[None, :] - 384 - k
    dc = np.arange(3584)[None, :] - 16 * k - 31
    return strip(ds, ds >= 0), strip(dw, (dw >= 0) & (dw <= 511)), strip(dc, dc >= 0)


def build_nsa(Sq):
    kb = KB()
    S = kb.S
    NT, NCH, NSEL = Sq // 128, Sq // 512, Sq // 64
    n_cmp = Sq // 16 - 1
    NCT = (n_cmp + 127) // 128
    NCC = NCT * 128
    NH = (NSEL + 127) // 128
    NSP = min(NSEL, 128)
    SW = max(NSEL, 8)
    xT = kb.din("xT", [D, Sq])
    wq = kb.din("wq", [D, 256])
    wkv = kb.din("wkv", [D, 512])
    wgp = kb.din("wgp", [D, 76])
    w1 = kb.din("w1", [128, 32, 256])
    posT = kb.din("posT", [128, 32])
    w2 = kb.din("w2", [128, 2, 128])
    ovm = kb.din("ovm", [NCC, NSEL])
    bss = kb.din("bss", [4, 128, 2432])
    bsw = kb.din("bsw", [4, 128, 1408])
    bsc = kb.din("bsc", [4, 128, 3584])
    cfar = kb.din("cfar", [128, 4])
    oT = kb.dout("oT", [256, Sq], BF16)

    identf = kb.sb("identf", [128, 128], F32)
    identb = kb.sb("identb", [128, 128], BF16)
    onesf = kb.sb("onesf", [128, 128], F32)
    S.op("pool", lambda e: e.memset(onesf[:], 1.0), writes=["onesf"])
    S.op("pool", lambda e: e.affine_select(out=identf[:], in_=onesf[:], pattern=[[-1, 128]], compare_op=ALU.is_equal,
                                           fill=0.0, base=0, channel_multiplier=1), reads=["onesf"], writes=["identf"])
    S.op("pool", lambda e: e.tensor_copy(out=identb[:], in_=identf[:]), reads=["identf"], writes=["identb"])
    cf = kb.sb("cf", [128, 4], F32)
    S.dma("sp", lambda e: e.dma_start(out=cf[:], in_=cfar), writes=["cf"])
    selgb = kb.sb("selgb", [128, 12 * 64], BF16)
    stg = kb.sb("stg", [128, 8 * 512], F32)
    zerob = kb.sb("zerob", [128, 128], BF16)
    S.op("pool", lambda e: e.memset(zerob[:], 0.0), writes=["zerob"])
    wqb = load_w_bf16(kb, "wqb", wq, 256, stg, "stg")
    wgb = load_w_bf16(kb, "wgb", wgp, 76, stg, "stg")
    w2b = kb.sb("w2b", [128, 2, 128], BF16)
    S.dma("sp", lambda e: e.dma_start(out=stg[:, 0:256].rearrange("p (a b) -> p a b", a=2), in_=w2), writes=["stg"])
    S.op("pool", lambda e: e.tensor_copy(out=w2b[:].rearrange("p a b -> p (a b)"), in_=stg[:, 0:256]), reads=["stg"], writes=["w2b"])
    ovb = kb.sb("ovb", [128, NCT, NSEL], BF16)
    for ct in range(NCT):
        S.dma("sp", lambda e, ct=ct: e.dma_start(out=stg[:, 0:NSEL], in_=ovm[ct * 128:(ct + 1) * 128, :]), writes=["stg"])
        S.op("pool", lambda e, ct=ct: e.tensor_copy(out=ovb[:, ct, :], in_=stg[:, 0:NSEL]), reads=["stg"], writes=["ovb"])

    KKs = kb.sb("KKs", [128, Sq // 2], BF16)
    KKw = kb.sb("KKw", [128, Sq // 2], BF16)
    Vs = kb.sb("Vs", [128, NT, 76], BF16)
    Vw = kb.sb("Vw", [128, NT, 76], BF16)
    vc = kb.sb("vc", [128, NCT, 76], BF16)
    kcT = kb.sb("kcT", [64, NCC], BF16)
    for t_, k_ in ((Vs, "Vs"), (Vw, "Vw"), (vc, "vc")):
        S.op("pool", lambda e, t_=t_: e.memset(t_[:, :, 64:76], 1.0), writes=[k_])
    xs = [(stg[:].rearrange("p (k t) -> p k t", k=8), "stg")]
    xb0 = kb.sb("xb0", [128, 8, 512], BF16)
    xb = [(xb0, "xb0")]
    sp_ = SoftmaxPairPipe(kb, nU=2, nP=2)
    pss = [(sp_.psS[0][0], "psS0_0"), (sp_.psS[0][1], "psS0_1"), (sp_.psS[1][0], "psS1_0"), (sp_.psS[1][1], "psS1_1")]
    accs = [kb.ps("acc0")]
    psB = kb.ps("psB")
    psI = [kb.ps(f"psI{i}") for i in range(2)]
    psR = psB

    with contextlib.ExitStack() as st0:
        selg = kb.sb("selg", [128, 12 * 64], F32, st0)
        S.op("pool", lambda e: e.memset(stg[:, 0:768], 1.0), writes=["stg"])
        S.op("pool", lambda e: e.affine_select(out=selg[:].rearrange("p (i m) -> p i m", m=64), in_=stg[:, 0:768].rearrange("p (i m) -> p i m", m=64),
                                               pattern=[[-1, 12], [0, 64]], compare_op=ALU.is_equal, fill=0.0, base=-64,
                                               channel_multiplier=1), reads=["stg"], writes=["selg"])
        S.op("pool", lambda e: e.tensor_copy(out=selgb[:], in_=selg[:]), reads=["selg"], writes=["selgb"])
        KVc = kb.sb("KVc", [128, Sq], BF16, st0)
        wkvb = kb.sb("wkvb", [128, 8, 512], BF16, st0)
        S.dma("sp", lambda e: e.dma_start(out=stg[:, 0:4096].rearrange("p (k n) -> p k n", k=8), in_=wkv.rearrange("(k p) n -> p k n", p=128)), writes=["stg"])
        S.op("pool", lambda e: e.tensor_copy(out=wkvb[:].rearrange("p k n -> p (k n)"), in_=stg[:, 0:4096]), reads=["stg"], writes=["wkvb"])
        w1b = kb.sb("w1b", [128, 32, 256], BF16, st0)
        hT = kb.sb("hT", [128, 2, 2, NCC], BF16, st0)
        posb = kb.sb("posb", [128, 32], BF16, st0)
        pbias = kb.sb("pbias", [128, 4], F32, st0)
        gx = kb.sb("gx", [128, 512], F32, st0)
        gu = kb.sb("gu", [128, 512], F32, st0)
        for q4 in range(4):
            S.dma("sp", lambda e, q4=q4: e.dma_start(out=stg[:, 0:2048].rearrange("p (a b) -> p a b", a=8), in_=w1[:, q4 * 8:(q4 + 1) * 8, :]),
                  writes=["stg"])
            S.op("pool", lambda e, q4=q4: e.tensor_copy(out=w1b[:, q4 * 8:(q4 + 1) * 8, :].rearrange("p a b -> p (a b)"), in_=stg[:, 0:2048]),
                 reads=["stg"], writes=["w1b"])
        S.dma("sp", lambda e: e.dma_start(out=stg[:, 0:32], in_=posT), writes=["stg"])
        S.op("pool", lambda e: e.tensor_copy(out=posb[:], in_=stg[:, 0:32]), reads=["stg"], writes=["posb"])
        S.op("pool", lambda e: e.memset(hT[:], 0.0), writes=["hT"])
        S.op("pool", lambda e: e.memset(kcT[:], 0.0), writes=["kcT"])
        def scatter(dst, dkey, eng):
            def ev(p, pk, c):
                for tt in range(4):
                    rows = slice(0, 64) if tt % 2 == 0 else slice(64, 128)
                    m = (4 * c + tt) // 2
                    if eng == "act":
                        S.op("act", lambda e, rows=rows, m=m, tt=tt, p=p: e.copy(out=dst[rows, m * 128:(m + 1) * 128], in_=p[rows, tt * 128:(tt + 1) * 128]),
                             reads=[pk], writes=[dkey])
                    else:
                        S.op("dve", lambda e, rows=rows, m=m, tt=tt, p=p: e.tensor_copy(out=dst[rows, m * 128:(m + 1) * 128], in_=p[rows, tt * 128:(tt + 1) * 128]),
                             reads=[pk], writes=[dkey])
            return ev
        fm = [(wkvb[:, :, 0:128], "wkvb", 128, KVc, "KVc", 1.0, "act"),
              (wkvb[:, :, 128:256], "wkvb", 128, KKs, "KKs", 1.0, scatter(KKs, "KKs", "dve")),
              (wkvb[:, :, 256:384], "wkvb", 128, KKw, "KKw", 1.0, scatter(KKw, "KKw", "act"))]
        tm = [(wkvb[:, :, 384:512], "wkvb", 128, (lambda t: [(Vs[:, t, 0:64], 0, 64), (Vw[:, t, 0:64], 64, 128)]), "V")]
        project(kb, xT, Sq, fm, tm, pss + [(accs[0], "acc0")], xs, xb)
        for kv in range(2):
            rows = slice(kv * 64, (kv + 1) * 64)
            for hc in range(2):
                for p in range(32):
                    S.op("pe", lambda e, rows=rows, hc=hc, p=p, kv=kv: e.matmul(
                        psR[:, kv * 2 + hc:kv * 2 + hc + 1], lhsT=w1b[rows, p, hc * 128:(hc + 1) * 128], rhs=posb[rows, p:p + 1],
                        start=(p == 0), stop=(p == 31)), reads=["w1b", "posb"], writes=["psB"])
        S.op("act", lambda e: e.copy(out=pbias[:], in_=psR[:, 0:4]), reads=["psB"], writes=["pbias"])
        ccs = [(c0, min(512, n_cmp - c0)) for c0 in range(0, n_cmp, 512)]
        KVv = KVc[:].rearrange("p (c s) -> p s c", s=16)
        ui = 0
        for kv in range(2):
            rows = slice(kv * 64, (kv + 1) * 64)
            for hc in range(2):
                for (c0, cn) in ccs:
                    ps_, pk_ = pss[ui % 2]
                    ui += 1
                    for p in range(32):
                        S.op("pe", lambda e, ps_=ps_, rows=rows, hc=hc, p=p, c0=c0, cn=cn: e.matmul(
                            ps_[:, 0:cn], lhsT=w1b[rows, p, hc * 128:(hc + 1) * 128],
                            rhs=KVv[rows, p % 16, c0 + p // 16:c0 + p // 16 + cn], start=(p == 0), stop=(p == 31)),
                            reads=["w1b", "KVc"], writes=[pk_])
                    col = kv * 2 + hc
                    S.op("act", lambda e, ps_=ps_, cn=cn, col=col: e.activation(out=gx[:, 0:cn], in_=ps_[:, 0:cn], func=AF.Identity,
                                                                                bias=pbias[:, col:col + 1]), reads=[pk_, "pbias"], writes=["gx"])
                    S.op("dve", lambda e, cn=cn: e.tensor_tensor(out=gu[:, 0:cn], in0=gx[:, 0:cn], in1=gx[:, 0:cn], op=ALU.mult), reads=["gx"], writes=["gu"])
                    S.op("dve", lambda e, cn=cn: e.tensor_scalar(out=gu[:, 0:cn], in0=gu[:, 0:cn], scalar1=0.044715, scalar2=1.0, op0=ALU.mult, op1=ALU.add),
                         reads=["gu"], writes=["gu"])
                    S.op("dve", lambda e, cn=cn: e.tensor_tensor(out=gu[:, 0:cn], in0=gu[:, 0:cn], in1=gx[:, 0:cn], op=ALU.mult), reads=["gu", "gx"], writes=["gu"])
                    S.op("act", lambda e, cn=cn: e.activation(out=gu[:, 0:cn], in_=gu[:, 0:cn], func=AF.Tanh, scale=0.7978845608028654), reads=["gu"], writes=["gu"])
                    S.op("dve", lambda e, cn=cn: e.tensor_scalar(out=gu[:, 0:cn], in0=gu[:, 0:cn], scalar1=1.0, scalar2=0.5, op0=ALU.add, op1=ALU.mult),
                         reads=["gu"], writes=["gu"])
                    S.op("dve", lambda e, cn=cn, kv=kv, hc=hc, c0=c0: e.tensor_tensor(out=hT[:, kv, hc, c0:c0 + cn], in0=gu[:, 0:cn], in1=gx[:, 0:cn], op=ALU.mult),
                         reads=["gu", "gx"], writes=["hT"])
        for c0 in range(0, NCC, 512):
            cn = min(512, NCC - c0)
            ps_, pk_ = pss[ui % 2]
            ui += 1
            for hc in range(2):
                S.op("pe", lambda e, ps_=ps_, hc=hc, c0=c0, cn=cn: e.matmul(ps_[0:64, 0:cn], lhsT=w2b[:, hc, 0:64], rhs=hT[:, 0, hc, c0:c0 + cn],
                                                                            start=(hc == 0), stop=(hc == 1)), reads=["w2b", "hT"], writes=[pk_])
            S.op("act", lambda e, ps_=ps_, c0=c0, cn=cn: e.copy(out=kcT[:, c0:c0 + cn], in_=ps_[0:64, 0:cn]), reads=[pk_], writes=["kcT"])
        for ct in range(NCT):
            ps_, pk_ = pss[ui % 2]
            ui += 1
            for hc in range(2):
                S.op("pe", lambda e, ps_=ps_, hc=hc, ct=ct: e.matmul(ps_[:, 0:64], lhsT=hT[:, 1, hc, ct * 128:(ct + 1) * 128], rhs=w2b[:, hc, 64:128],
                                                                     start=(hc == 0), stop=(hc == 1)), reads=["w2b", "hT"], writes=[pk_])
            S.op("dve", lambda e, ps_=ps_, ct=ct: e.tensor_copy(out=vc[:, ct, 0:64], in_=ps_[:, 0:64]), reads=[pk_], writes=["vc"])

    S.fence()
    strips = {}
    for nm, src, W in (("bs_s", bss, 2432), ("bs_w", bsw, 1408), ("bs_c", bsc, 3584)):
        t_ = kb.sb(nm, [128, 4, W], BF16)
        strips[nm] = t_
        for r in range(4):
            for w0 in range(0, W, 2048):
                wn = min(2048, W - w0)
                S.dma("sp", lambda e, src=src, r=r, w0=w0, wn=wn: e.dma_start(out=stg[:, 0:wn], in_=src[r, :, w0:w0 + wn]), writes=["stg"])
                S.op("pool", lambda e, t_=t_, r=r, w0=w0, wn=wn: e.tensor_copy(out=t_[:, r, w0:w0 + wn], in_=stg[:, 0:wn]), reads=["stg"], writes=[nm])
    bs_s, bs_w, bs_c = strips["bs_s"], strips["bs_w"], strips["bs_c"]
    Qc = [kb.sb(f"Qc{i}", [128, 512], BF16) for i in range(4)]
    ng_ = kb.sb("ng_", [128, 512], F32)
    tf_ = kb.sb("tf_", [128, 512], F32)
    gsb, fgd = ng_, tf_
    fgdb = kb.sb("fgdb", [128, 512], BF16)
    numt, tmpc = ng_, tf_
    res = [kb.sb(f"res{r}", [64, 512], F32) for r in range(4)]
    osb0 = kb.sb("osb0", [64, 512], BF16)
    osb = [osb0, osb0]
    impsum = [kb.sb(f"imps{t}", [128, SW], F32) for t in range(4)]
    sc2 = kb.sb("sc2", [128, SW], F32)
    mbias = kb.sb("mbias", [128, SW], F32)
    m8a = kb.sb("m8a", [128, 8], F32)
    m8b = kb.sb("m8b", [128, 8], F32)
    thr = kb.sb("thr", [128, 1], F32)
    rdcol = kb.sb("rdcol", [128, 4], F32)
    maskT = kb.sb("maskT", [128, NH, 512], BF16)
    xv = xT.rearrange("(k p) t -> p k t", p=128)

    def qsel(r):
        return [Qc[0][0:64, :], Qc[2][0:64, :], Qc[1][0:64, :], Qc[3][0:64, :]][r], ["Qc0", "Qc2", "Qc1", "Qc3"][r]

    def qwin(r):
        return [Qc[2][64:128, :], Qc[0][64:128, :], Qc[3][64:128, :], Qc[1][64:128, :]][r], ["Qc2", "Qc0", "Qc3", "Qc1"][r]

    acc_i = [0]

    def finalize(acc, acck, br, r, first, last, c):
        i = br * 4 + r
        S.op("dve", lambda e: e.tensor_scalar(out=fgd[64:76, :], in0=acc[64:76, :], scalar1=1e-30, scalar2=None, op0=ALU.max),
             reads=[acck], writes=["fgd"])
        S.op("dve", lambda e: e.reciprocal(out=fgd[64:76, :], in_=fgd[64:76, :]), reads=["fgd"], writes=["fgd"])
        if br == 0:
            for tt in range(4):
                S.op("pe", lambda e, tt=tt: e.matmul(psR[:, tt:tt + 1], lhsT=fgd[64:65, tt * 128:(tt + 1) * 128], rhs=onesf[64:65, 0:1],
                                                     start=True, stop=True), reads=["fgd", "onesf"], writes=["psB"])
            S.op("act", lambda e: e.copy(out=rdcol[:], in_=psR[:, 0:4]), reads=["psB"], writes=["rdcol"])
        S.op("dve", lambda e: e.tensor_tensor(out=fgdb[64:76, :], in0=fgd[64:76, :], in1=gsb[64:76, :], op=ALU.mult),
             reads=["fgd", "gsb"], writes=["fgdb"])
        S.op("pe", lambda e: e.matmul(psB[0:64, :], lhsT=selgb[64:76, i * 64:(i + 1) * 64], rhs=fgdb[64:76, :], start=True, stop=True),
             reads=["fgdb", "selgb"], writes=["psB"])
        S.op("act", lambda e: e.copy(out=numt[0:64, :], in_=acc[0:64, :]), reads=[acck], writes=["numt"])
        if first:
            S.op("dve", lambda e: e.tensor_tensor(out=res[r][:], in0=numt[0:64, :], in1=psB[0:64, :], op=ALU.mult),
                 reads=["numt", "psB"], writes=[f"res{r}"])
        else:
            S.op("dve", lambda e: e.tensor_tensor(out=tmpc[0:64, :], in0=numt[0:64, :], in1=psB[0:64, :], op=ALU.mult),
                 reads=["numt", "psB"], writes=["tmpc"])
            if not last:
                S.op("pool", lambda e: e.tensor_tensor(out=res[r][:], in0=res[r][:], in1=tmpc[0:64, :], op=ALU.add),
                     reads=["tmpc", f"res{r}"], writes=[f"res{r}"])
            else:
                ob = osb[r % 2]
                S.op("pool", lambda e: e.tensor_tensor(out=ob[:], in0=res[r][:], in1=tmpc[0:64, :], op=ALU.add),
                     reads=["tmpc", f"res{r}"], writes=["osb0"])
                S.dma("sp", lambda e: e.dma_start(out=oT[r * 64:(r + 1) * 64, c * 512:(c + 1) * 512], in_=ob[:]), reads=["osb0"])

    for c in range(NCH):
        qs = slice(c * 512, (c + 1) * 512)
        xbi, xbk = xb0, "xb0"
        S.dma("sp", lambda e, c=c: e.dma_start(out=xs[0][0], in_=xv[:, :, c * 512:(c + 1) * 512]), writes=["stg"])
        S.op("dve", lambda e, xbi=xbi: e.tensor_copy(out=xbi[:, 0:4, :], in_=xs[0][0][:, 0:4, :]), reads=["stg"], writes=[xbk])
        S.op("act", lambda e, xbi=xbi: e.copy(out=xbi[:, 4:8, :], in_=xs[0][0][:, 4:8, :]), reads=["stg"], writes=[xbk])
        for qi in range(4):
            ps_, pk_ = pss[qi % 4]
            base = (qi % 2) * 128
            if qi < 2:
                for k in range(8):
                    S.op("pe", lambda e, ps_=ps_, base=base, k=k, xbi=xbi: e.matmul(ps_[:], lhsT=wqb[:, k, base:base + 128], rhs=xbi[:, k, :],
                                                                                  start=(k == 0), stop=(k == 7)), reads=["wqb", xbk], writes=[pk_])
            else:
                for half in range(2):
                    cols = base + 64 * (1 - half)
                    for k in range(8):
                        S.op("pe", lambda e, ps_=ps_, cols=cols, half=half, k=k, xbi=xbi: e.matmul(
                            ps_[half * 64:(half + 1) * 64, :], lhsT=wqb[:, k, cols:cols + 64], rhs=xbi[:, k, :],
                            start=(k == 0), stop=(k == 7)), reads=["wqb", xbk], writes=[pk_])
            S.op("act", lambda e, ps_=ps_, qi=qi: e.mul(Qc[qi][:], ps_[:], SCALE), reads=[pk_], writes=[f"Qc{qi}"])
        for k in range(8):
            S.op("pe", lambda e, k=k, xbi=xbi: e.matmul(psB[0:76, :], lhsT=wgb[:, k, 0:76], rhs=xbi[:, k, :], start=(k == 0), stop=(k == 7)),
                 reads=["wgb", xbk], writes=["psB"])
        S.op("act", lambda e: e.activation(out=gsb[64:76, :], in_=psB[64:76, :], func=AF.Sigmoid), reads=["psB"], writes=["gsb"])

        ctmax = min(NCT - 1, (32 * c + 30) // 128)
        for r in range(4):
            acc, acck = accs[0], "acc0"
            acc_i[0] += 1
            qa, qk = qsel(r)
            units = []
            for ct in range(ctmax + 1):
                o = c - 4 * ct
                mm = [(kcT[:, ct * 128:(ct + 1) * 128], qa, ["kcT", qk], 0)]
                cb = None
                if o <= 6:
                    mm.append((identb[:], bs_c[:, r, o * 512:(o + 1) * 512], ["identb", "bs_c"], 0))
                else:
                    cb = (cf[:, r:r + 1], "cf")
                un = dict(mm=mm, N=512, cb=[cb, None], slots=(0,), pv=[(vc[:, ct, :], ["vc"], acc[0:76, :], acck, ct == 0, ct == ctmax, 0)])
                un["imp"] = (ct, ctmax)
                units.append(un)
            for bnk in range(2):
                S.op("pe", lambda e, bnk=bnk: e.matmul(psI[bnk][:], lhsT=zerob[:], rhs=bs_c[:, 0, 0:512], start=True, stop=False),
                     reads=["zerob", "bs_c"], writes=[f"psI{bnk}"])
            run_cmp(kb, sp_, units, psI, ovb, NSEL)
            finalize(acc, acck, 0, r, True, False, c)
            for tt in range(4):
                src = psI[tt // 2][:, (tt % 2) * 256:(tt % 2) * 256 + NSEL]
                if r == 0:
                    S.op("dve", lambda e, tt=tt, src=src: e.tensor_scalar(out=impsum[tt][:, 0:NSEL], in0=src, scalar1=rdcol[:, tt:tt + 1], scalar2=None,
                                                                          op0=ALU.mult), reads=[f"psI{tt // 2}", "rdcol"], writes=[f"imps{tt}"])
                else:
                    S.op("dve", lambda e, tt=tt, src=src: e.scalar_tensor_tensor(out=impsum[tt][:, 0:NSEL], in0=src, scalar=rdcol[:, tt:tt + 1],
                                                                                 in1=impsum[tt][:, 0:NSEL], op0=ALU.mult, op1=ALU.add),
                         reads=[f"psI{tt // 2}", "rdcol", f"imps{tt}"], writes=[f"imps{tt}"])
        for tt in range(4):
            qt = 4 * c + tt
            sc = impsum[tt]
            sk = f"imps{tt}"
            if SW > NSEL:
                S.op("pool", lambda e, sc=sc: e.memset(sc[:, NSEL:SW], -1e30), writes=[sk])
            if 2 * qt + 2 < NSEL:
                S.op("pool", lambda e, sc=sc, qt=qt: e.memset(sc[:, 2 * qt + 2:NSEL], -1e30), writes=[sk])
            S.op("pool", lambda e, sc=sc, qt=qt: e.memset(sc[0:64, 2 * qt + 1:2 * qt + 2], -1e30), writes=[sk])
            S.op("pool", lambda e, sc=sc: e.memset(sc[:, 0:1], 1e9), writes=[sk])
            lo = max(2 * qt - 1, 0)
            S.op("pool", lambda e, sc=sc, qt=qt, lo=lo: e.memset(sc[0:64, lo:2 * qt + 1], 1e9), writes=[sk])
            S.op("pool", lambda e, sc=sc, qt=qt: e.memset(sc[64:128, 2 * qt:2 * qt + 2], 1e9), writes=[sk])
            S.op("dve", lambda e, sc=sc: e.max(out=m8a[:], in_=sc[:]), reads=[sk], writes=["m8a"])
            S.op("dve", lambda e, sc=sc: e.match_replace(out=sc2[:], in_to_replace=m8a[:], in_values=sc[:], imm_value=-1e30),
                 reads=[sk, "m8a"], writes=["sc2"])
            S.op("dve", lambda e: e.max(out=m8b[:], in_=sc2[:]), reads=["sc2"], writes=["m8b"])
            S.op("dve", lambda e: e.tensor_scalar(out=thr[:], in0=m8b[:, 7:8], scalar1=-1e29, scalar2=None, op0=ALU.max),
                 reads=["m8b"], writes=["thr"])
            S.op("dve", lambda e, sc=sc: e.tensor_scalar(out=mbias[:], in0=sc[:], scalar1=thr[:, 0:1], scalar2=NEG, op0=ALU.is_lt, op1=ALU.mult),
                 reads=[sk, "thr"], writes=["mbias"])
            for jh in range(NH):
                ps_, pk_ = pss[(tt * NH + jh) % 2]
                S.op("pe", lambda e, ps_=ps_, jh=jh: e.transpose(ps_[0:NSP, 0:128], mbias[:, jh * 128:jh * 128 + NSP], identf[:]),
                     reads=["mbias", "identf"], writes=[pk_])
                S.op("act", lambda e, ps_=ps_, jh=jh, tt=tt: e.copy(out=maskT[0:NSP, jh, tt * 128:(tt + 1) * 128], in_=ps_[0:NSP, 0:128]),
                     reads=[pk_], writes=["maskT"])
        jmax = 4 * c + 3
        for r in range(4):
            acc, acck = accs[0], "acc0"
            (qa0, qk0), (qa1, qk1) = qsel(r), qwin(r)
            units = []
            npair = (jmax + 1) // 2
            for m in range(npair):
                mm, cbs, pv = [], [], []
                mm.append((KKs[0:64, m * 128:(m + 1) * 128], qa0, ["KKs", qk0], 0))
                mm.append((KKs[64:128, m * 128:(m + 1) * 128], qa1, ["KKs", qk1], 1))
                for sl in range(2):
                    j = 2 * m + sl
                    col = 2 * (j % 64)
                    mm.append((identb[0:NSP, col:col + 1].to_broadcast([NSP, 64]), maskT[0:NSP, j // 64, :], ["identb", "maskT"], sl, (0, 64)))
                    mm.append((identb[0:NSP, col + 1:col + 2].to_broadcast([NSP, 64]), maskT[0:NSP, j // 64, :], ["identb", "maskT"], sl, (64, 128)))
                for sl in range(2):
                    j = 2 * m + sl
                    o = 4 * c - j
                    if o <= 12:
                        mm.append((identb[:], bs_s[:, r, (o + 3) * 128:(o + 3) * 128 + 512], ["identb", "bs_s"], sl))
                        cbs.append(None)
                    else:
                        cbs.append((cf[:, r:r + 1], "cf"))
                    pv.append((Vs[:, j, :], ["Vs"], acc[0:76, :], acck, j == 0, j == jmax, sl))
                units.append(dict(mm=mm, N=512, cb=cbs, pv=pv))
            sp_.run(units)
            finalize(acc, acck, 1, r, False, False, c)
        for r in range(4):
            acc, acck = accs[0], "acc0"
            (qa0, qk0), (qa1, qk1) = qsel(r), qwin(r)
            units = []
            j0 = max(0, 4 * c - 4)
            for m in range(j0 // 2, (jmax + 1) // 2):
                mm, pv = [], []
                mm.append((KKw[0:64, m * 128:(m + 1) * 128], qa0, ["KKw", qk0], 0))
                mm.append((KKw[64:128, m * 128:(m + 1) * 128], qa1, ["KKw", qk1], 1))
                for sl in range(2):
                    j = 2 * m + sl
                    o = 4 * c - j
                    mm.append((identb[:], bs_w[:, r, (o + 3) * 128:(o + 3) * 128 + 512], ["identb", "bs_w"], sl))
                    pv.append((Vw[:, j, :], ["Vw"], acc[0:76, :], acck, j == j0, j == jmax, sl))
                units.append(dict(mm=mm, N=512, cb=[None, None], pv=pv))
            sp_.run(units)
            finalize(acc, acck, 2, r, False, True, c)
    return kb.finish()


def run_cmp(kb, sp_, units, psI, ovb, NSEL):
    S = kb.S
    for un in units:
        ct, ctmax = un["imp"]

        def post(PTb, PTk, ct=ct, ctmax=ctmax):
            for tt in range(4):
                S.op("pe", lambda e, tt=tt: e.matmul(psI[tt // 2][:, (tt % 2) * 256:(tt % 2) * 256 + NSEL],
                                                     lhsT=PTb[:, tt * 128:(tt + 1) * 128], rhs=ovb[:, ct, :],
                                                     start=False, stop=(ct == ctmax and tt % 2 == 1)),
                     reads=[PTk, "ovb"], writes=[f"psI{tt // 2}"])
        un["post"] = post
    sp_.run(units)


ALPHA = 8 ** 0.25
LN_EPS = 1e-5
D = 1024
NE = 16
DE = 256


def build_post(T, stage=9):
    nc = bass.Bass("TRN2", target_bir_lowering=False)
    TG = min(1024, T)
    NG = T // TG
    NT = TG // 128
    NCH = TG // 512
    oT = nc.dram_tensor("oT", [D, T], BF16, kind="ExternalInput").ap()
    xin = nc.dram_tensor("xin", [T, D], F32, kind="ExternalInput").ap()
    w_out = nc.dram_tensor("w_out", [D, D], F32, kind="ExternalInput").ap()
    lnp = nc.dram_tensor("lnp", [4, D], F32, kind="ExternalInput").ap()
    rw = nc.dram_tensor("rw", [D, NE], F32, kind="ExternalInput").ap()
    rb = nc.dram_tensor("rb", [1, NE], F32, kind="ExternalInput").ap()
    wg = nc.dram_tensor("wg", [NE, D, DE], F32, kind="ExternalInput").ap()
    wu = nc.dram_tensor("wu", [NE, D, DE], F32, kind="ExternalInput").ap()
    wd = nc.dram_tensor("wd", [NE, DE, D], F32, kind="ExternalInput").ap()
    xout = nc.dram_tensor("xout", [T, D], F32, kind="ExternalOutput").ap()

    S = Sched(nc)
    with contextlib.ExitStack() as st:
        def sb(name, shape, dt):
            return st.enter_context(nc.sbuf_tensor(name, shape, dt))

        def ps(name, shape, dt=F32):
            return st.enter_context(nc.psum_tensor(name, shape, dt))

        ident = sb("ident", [128, 128], F32)
        lnb = sb("lnb", [128, 4, D], F32)
        rbb = sb("rbb", [128, NE], F32)
        rwt = sb("rwt", [128, 8, NE], F32)
        wob = sb("wob", [128, 8, D], BF16)
        stg = [sb(f"stg{i}", [128, 2048], F32) for i in range(3)]
        wb = [[sb(f"wb{j}_{i}", [128, 2048], BF16) for i in range(3)] for j in range(2)]
        x1T = sb("x1T", [128, 8, TG], BF16)
        x1Tf = sb("x1Tf", [128, 8, 128], F32)
        yacc = sb("yacc", [128, NT, D], F32)
        xt = [sb(f"xt{i}", [128, D], F32) for i in range(2)]
        ot = [sb(f"ot{i}", [128, 8, 128], BF16) for i in range(2)]
        r = sb("r", [128, D], F32)
        x1 = sb("x1", [128, D], F32)
        stats = sb("stats", [128, 2, 6], F32)
        mv = sb("mv", [128, 2], F32)
        rstd = sb("rstd", [128, 1], F32)
        G = sb("G", [128, NT, NE], F32)
        rt = [sb(f"rt{i}", [128, NE], F32) for i in range(6)]
        rs = [sb(f"rs{i}", [128, 4], F32) for i in range(4)]
        sg = [sb(f"sg{i}", [128, 512], F32) for i in range(2)]
        aT = [[sb(f"aT{j}_{i}", [128, 512], BF16) for i in range(2)] for j in range(2)]
        outt = [sb(f"outt{i}", [128, D], F32) for i in range(2)]

        psD = [[ps(f"psD{j}_{i}", [128, 512]) for i in range(2)] for j in range(2)]
        psG = [ps(f"psG{i}", [128, 512]) for i in range(2)]
        psU = [ps(f"psU{i}", [128, 512]) for i in range(2)]

        S.op("pool", lambda e: e.memset(ident[:], 0.0), writes=["ident"])
        S.op("pool", lambda e: e.memset(r[:, 0:128], 1.0), writes=["r"])
        S.op("pool", lambda e: e.affine_select(out=ident[:], in_=r[:, 0:128], pattern=[[-1, 128]],
                                               compare_op=ALU.is_equal, fill=0.0, base=0, channel_multiplier=1),
             reads=["r"], writes=["ident"])
        S.dma("sp", lambda e: e.dma_start(out=lnb[:], in_=lnp.partition_broadcast(128)), writes=["lnb"])
        S.dma("sp", lambda e: e.dma_start(out=rbb[:], in_=rb[0, :].partition_broadcast(128)), writes=["rbb"])
        S.dma("sp", lambda e: e.dma_start(out=rwt[:], in_=rw.rearrange("(k p) e -> p k e", p=128)), writes=["rwt"])
        wo_v = w_out.rearrange("(k p) n -> p k n", p=128)
        for i in range(4):
            sname = f"stg{i % 3}"
            S.dma("sp", lambda e, i=i: e.dma_start(out=stg[i % 3][:].rearrange("p (k n) -> p k n", k=2),
                                                   in_=wo_v[:, 2 * i:2 * i + 2, :]), writes=[sname])
            S.op("pool", lambda e, i=i: e.tensor_copy(out=wob[:, 2 * i:2 * i + 2, :].rearrange("p k n -> p (k n)"),
                                                      in_=stg[i % 3][:]), reads=[sname], writes=["wob"])

        xin_v = xin.rearrange("(n p) d -> n p d", p=128)
        xout_v = xout.rearrange("(n p) d -> n p d", p=128)
        oT_v = oT.rearrange("(k p) t -> p k t", p=128)
        wg_v = wg.rearrange("e (k p) f -> e p k f", p=128)
        wu_v = wu.rearrange("e (k p) f -> e p k f", p=128)
        wd_v = wd.rearrange("e (k p) n -> e p k n", p=128)

        def layer_norm(src_ap_fn, src_key, dst_ap, dst_key, gi, tag):
            for h in range(2):
                S.op("dve", lambda e, h=h: e.bn_stats(out=stats[:, h, :], in_=src_ap_fn()[:, h * 512:(h + 1) * 512]),
                     reads=[src_key], writes=["stats"])
            S.op("dve", lambda e: e.bn_aggr(out=mv[:], in_=stats[:].rearrange("p a b -> p (a b)")),
                 reads=["stats"], writes=["mv"])
            S.op("dve", lambda e: e.tensor_scalar(out=rstd[:], in0=mv[:, 1:2], scalar1=LN_EPS, scalar2=None,
                                                  op0=ALU.add), reads=["mv"], writes=["rstd"])
            S.op("act", lambda e: e.sqrt(rstd[:], rstd[:]), reads=["rstd"], writes=["rstd"])
            S.op("dve", lambda e: e.reciprocal(out=rstd[:], in_=rstd[:]), reads=["rstd"], writes=["rstd"])
            S.op("dve", lambda e: e.tensor_scalar(out=dst_ap, in0=src_ap_fn(), scalar1=mv[:, 0:1], scalar2=rstd[:, 0:1],
                                                  op0=ALU.subtract, op1=ALU.mult),
                 reads=[src_key, "mv", "rstd"], writes=[dst_key])
            S.op("pool", lambda e: e.tensor_tensor(out=dst_ap, in0=dst_ap, in1=lnb[:, gi, :], op=ALU.mult),
                 reads=[dst_key, "lnb"], writes=[dst_key])
            S.op("pool", lambda e: e.tensor_tensor(out=dst_ap, in0=dst_ap, in1=lnb[:, gi + 1, :], op=ALU.add),
                 reads=[dst_key, "lnb"], writes=[dst_key])

        wcount = 0
        for g in range(NG if stage >= 1 else 0):
            t0 = g * TG
            for t in range(NT):
                gt = g * NT + t
                xi, oi = xt[gt % 2], ot[gt % 2]
                xk, ok = f"xt{gt % 2}", f"ot{gt % 2}"
                S.dma("sp", lambda e, xi=xi, gt=gt: e.dma_start(out=xi[:], in_=xin_v[gt]), writes=[xk])
                S.dma("sp", lambda e, oi=oi, gt=gt: e.dma_start(out=oi[:], in_=oT_v[:, :, gt * 128:(gt + 1) * 128]),
                      writes=[ok])
                pY = psD[0]
                for h in range(2):
                    for k in range(8):
                        S.op("pe", lambda e, h=h, k=k, oi=oi: e.matmul(pY[h][:], lhsT=oi[:, k, :],
                                                                       rhs=wob[:, k, h * 512:(h + 1) * 512],
                                                                       start=(k == 0), stop=(k == 7)),
                             reads=[ok, "wob"], writes=[f"psD0_{h}"])
                for h in range(2):
                    S.op("dve", lambda e, h=h, xi=xi: e.scalar_tensor_tensor(
                        out=r[:, h * 512:(h + 1) * 512], in0=xi[:, h * 512:(h + 1) * 512], scalar=ALPHA,
                        in1=pY[h][:], op0=ALU.mult, op1=ALU.add), reads=[xk, f"psD0_{h}"], writes=["r"])
                if stage < 1.2: continue
                layer_norm(lambda: r[:], "r", x1[:], "x1", 0, "ln1")
                S.op("act", lambda e, t=t: e.mul(yacc[:, t, :], x1[:], ALPHA), reads=["x1"], writes=[f"yacc{t}"])
                if stage < 1.3: continue
                for k in range(8):
                    S.op("pe", lambda e, k=k: e.transpose(psD[1][k // 4][:, (k % 4) * 128:(k % 4 + 1) * 128],
                                                          x1[:, k * 128:(k + 1) * 128], ident[:]),
                         reads=["x1", "ident"], writes=[f"psD1_{k // 4}"])
                for h in range(2 if stage >= 1.32 else 0):
                    S.op("act", lambda e, h=h: e.copy(out=x1Tf[:, 4 * h:4 * h + 4, :].rearrange("p k t -> p (k t)"),
                                                      in_=psD[1][h][:]), reads=[f"psD1_{h}"], writes=["x1Tf"])
                    if stage < 1.33: continue
                    S.op("pool", lambda e, h=h, t=t: e.tensor_copy(
                        out=x1T[:, 4 * h:4 * h + 4, t * 128:(t + 1) * 128],
                        in_=x1Tf[:, 4 * h:4 * h + 4, :]), reads=["x1Tf"], writes=["x1T"])
                if stage < 1.4: continue
                for k in range(8):
                    S.op("pe", lambda e, k=k: e.matmul(psG[0][:, 0:NE], lhsT=x1Tf[:, k, :], rhs=rwt[:, k, :],
                                                       start=(k == 0), stop=(k == 7)),
                         reads=["x1Tf", "rwt"], writes=["psG0"])
                if stage < 1.5: continue
                sc, bi, eq, b2, sel, ws = rt
                m1, m2, gs, gsel = rs
                S.op("act", lambda e: e.activation(out=sc[:], in_=psG[0][:, 0:NE], func=AF.Sigmoid),
                     reads=["psG0"], writes=["sc"])
                S.op("dve", lambda e: e.tensor_tensor(out=bi[:], in0=sc[:], in1=rbb[:], op=ALU.add),
                     reads=["sc", "rbb"], writes=["bi"])
                v3 = lambda a: a[:].rearrange("p (g j) -> p g j", g=4)
                S.op("dve", lambda e: e.tensor_reduce(out=m1[:], in_=v3(bi), axis=AX.X, op=ALU.max),
                     reads=["bi"], writes=["m1"])
                S.op("dve", lambda e: e.tensor_tensor(out=v3(eq), in0=v3(bi), in1=m1[:].unsqueeze(2).to_broadcast([128, 4, 4]),
                                                      op=ALU.is_equal), reads=["bi", "m1"], writes=["eq"])
                S.op("dve", lambda e: e.scalar_tensor_tensor(out=b2[:], in0=eq[:], scalar=-1e9, in1=bi[:],
                                                             op0=ALU.mult, op1=ALU.add), reads=["eq", "bi"], writes=["b2"])
                S.op("dve", lambda e: e.tensor_reduce(out=m2[:], in_=v3(b2), axis=AX.X, op=ALU.max),
                     reads=["b2"], writes=["m2"])
                S.op("dve", lambda e: e.tensor_tensor(out=gs[:], in0=m1[:], in1=m2[:], op=ALU.add),
                     reads=["m1", "m2"], writes=["gs"])
                S.op("dve", lambda e: e.tensor_reduce(out=rstd[:], in_=gs[:], axis=AX.X, op=ALU.max),
                     reads=["gs"], writes=["rstd"])
                S.op("dve", lambda e: e.tensor_scalar(out=gsel[:], in0=gs[:], scalar1=rstd[:, 0:1], scalar2=None,
                                                      op0=ALU.is_ge), reads=["gs", "rstd"], writes=["gsel"])
                S.op("dve", lambda e: e.tensor_tensor(out=v3(sel), in0=v3(bi), in1=m2[:].unsqueeze(2).to_broadcast([128, 4, 4]),
                                                      op=ALU.is_ge), reads=["bi", "m2"], writes=["sel"])
                S.op("dve", lambda e: e.tensor_tensor(out=v3(sel), in0=v3(sel), in1=gsel[:].unsqueeze(2).to_broadcast([128, 4, 4]),
                                                      op=ALU.mult), reads=["sel", "gsel"], writes=["sel"])
                S.op("dve", lambda e: e.tensor_tensor(out=ws[:], in0=sel[:], in1=sc[:], op=ALU.mult),
                     reads=["sel", "sc"], writes=["ws"])
                S.op("dve", lambda e: e.tensor_reduce(out=rstd[:], in_=ws[:], axis=AX.X, op=ALU.add),
                     reads=["ws"], writes=["rstd"])
                S.op("dve", lambda e: e.reciprocal(out=rstd[:], in_=rstd[:]), reads=["rstd"], writes=["rstd"])
                S.op("dve", lambda e, t=t: e.tensor_scalar(out=G[:, t, :], in0=ws[:], scalar1=rstd[:, 0:1], scalar2=None,
                                                           op0=ALU.mult), reads=["ws", "rstd"], writes=[f"G{t}"])
            for ex in range(NE if stage >= 2 else 0):
                wset = wb[wcount % 2]
                wk = [f"wb{wcount % 2}_{i}" for i in range(3)]
                wcount += 1
                srcs = [wg_v[ex], wu_v[ex], wd_v[ex]]
                for i in range(3):
                    kk = 8 if i < 2 else 2
                    S.dma("sp", lambda e, i=i, kk=kk, src=srcs[i]: e.dma_start(
                        out=stg[i][:].rearrange("p (k n) -> p k n", k=kk), in_=src), writes=[f"stg{i}"])
                    if i < 2:
                        S.op("act", lambda e, i=i, wset=wset: e.copy(out=wset[i][:], in_=stg[i][:]),
                             reads=[f"stg{i}"], writes=[wk[i]])
                    else:
                        S.op("dve", lambda e, i=i, wset=wset: e.tensor_copy(out=wset[i][:], in_=stg[i][:]),
                             reads=[f"stg{i}"], writes=[wk[i]])
                wgb = wset[0][:].rearrange("p (k f) -> p k f", k=8)
                wub = wset[1][:].rearrange("p (k f) -> p k f", k=8)
                wdb = wset[2][:].rearrange("p (k n) -> p k n", k=2)
                for c in range(NCH):
                    pi = (ex * NCH + c) % 2
                    for fh in range(2):
                        for (pt, pk, wv, wkey) in ((psG[fh], f"psG{fh}", wgb, wk[0]), (psU[fh], f"psU{fh}", wub, wk[1])):
                            for k in range(8):
                                S.op("pe", lambda e, pt=pt, wv=wv, k=k, fh=fh, c=c: e.matmul(
                                    pt[:], lhsT=wv[:, k, fh * 128:(fh + 1) * 128], rhs=x1T[:, k, c * 512:(c + 1) * 512],
                                    start=(k == 0), stop=(k == 7)), reads=[wkey, "x1T"], writes=[pk])
                        S.op("act", lambda e, fh=fh: e.activation(out=sg[fh][:], in_=psG[fh][:], func=AF.Silu),
                             reads=[f"psG{fh}"], writes=[f"sg{fh}"])
                        S.op("dve", lambda e, fh=fh, pi=pi: e.tensor_tensor(out=aT[pi][fh][:], in0=sg[fh][:], in1=psU[fh][:],
                                                                           op=ALU.mult),
                             reads=[f"sg{fh}", f"psU{fh}"], writes=[f"aT{pi}_{fh}"])
                    for tt in range(4):
                        t = c * 4 + tt
                        dj = (c * 4 + tt) % 2
                        for h in range(2):
                            for fh in range(2):
                                S.op("pe", lambda e, dj=dj, h=h, fh=fh, tt=tt, pi=pi, wdb=wdb: e.matmul(
                                    psD[dj][h][:], lhsT=aT[pi][fh][:, tt * 128:(tt + 1) * 128],
                                    rhs=wdb[:, fh, h * 512:(h + 1) * 512], start=(fh == 0), stop=(fh == 1)),
                                    reads=[f"aT{pi}_{fh}", wk[2]], writes=[f"psD{dj}_{h}"])
                            S.op("dve", lambda e, dj=dj, h=h, t=t, ex=ex: e.scalar_tensor_tensor(
                                out=yacc[:, t, h * 512:(h + 1) * 512], in0=psD[dj][h][:], scalar=G[:, t, ex:ex + 1],
                                in1=yacc[:, t, h * 512:(h + 1) * 512], op0=ALU.mult, op1=ALU.add),
                                reads=[f"psD{dj}_{h}", f"G{t}", f"yacc{t}"], writes=[f"yacc{t}"])
            for t in range(NT if stage >= 3 else 0):
                gt = g * NT + t
                oo = outt[gt % 2]
                layer_norm(lambda t=t: yacc[:, t, :], f"yacc{t}", oo[:], f"outt{gt % 2}", 2, "ln2")
                S.dma("sp", lambda e, oo=oo, gt=gt: e.dma_start(out=xout_v[gt], in_=oo[:]), reads=[f"outt{gt % 2}"])
        if stage < 3:
            S.dma('sp', lambda e: e.dma_start(out=xout_v[0], in_=lnb[:, 0, :]), reads=['lnb'])
        S.emit()
    return nc


_NCS = {}


def _nc(name, fn):
    if name not in _NCS:
        _NCS[name] = fn()
    return _NCS[name]


def _c(a):
    return np.ascontiguousarray(a)


def _nsa_in_map(xb_, G, w_in, tbl, inp, ov):
    heads = list(range(G * 4, G * 4 + 4))
    wq = _c(w_in[:, G * 256:(G + 1) * 256])
    wqs = _c(np.concatenate([wq[:, 64:128], wq[:, 0:64], wq[:, 192:256], wq[:, 128:192]], axis=1))
    kvp = lambda i: w_in[:, 1024 + i * 256 + G * 64: 1024 + i * 256 + (G + 1) * 64]
    wkv = _c(np.concatenate([kvp(0), kvp(1), kvp(2), kvp(2), kvp(4), kvp(4), kvp(3), kvp(5)], axis=1))
    wgp = np.zeros((1024, 76), np.float32)
    for br in range(3):
        for r in range(4):
            wgp[:, 64 + br * 4 + r] = w_in[:, 2560 + br * 16 + G * 4 + r]
    w1 = np.concatenate([inp['nsa_cmp_w1_k'][0].reshape(32, 64, 256).transpose(1, 0, 2),
                         inp['nsa_cmp_w1_v'][0].reshape(32, 64, 256).transpose(1, 0, 2)], axis=0)
    posT = np.concatenate([inp['nsa_cmp_pos_k'][0].T, inp['nsa_cmp_pos_v'][0].T], axis=0)
    w2 = np.concatenate([inp['nsa_cmp_w2_k'][0].reshape(2, 128, 64).transpose(1, 0, 2),
                         inp['nsa_cmp_w2_v'][0].reshape(2, 128, 64).transpose(1, 0, 2)], axis=2)
    bss, bsw, bsc = nsa_bias_strips(tbl, heads)
    return dict(xT=xb_, wq=wq, wkv=wkv, wgp=wgp, w1=_c(w1), posT=_c(posT), w2=_c(w2), ovm=ov, bss=bss, bsw=bsw, bsc=bsc,
                cfar=_c(np.broadcast_to(tbl[31, heads][None, :], (128, 4))))


def kernel(**inputs):
    inp = {k: np.asarray(v) for k, v in inputs.items()}
    x = inp['x']
    B, Sq, _ = x.shape
    tbl = inp['rel_table']
    T = B * Sq // 8
    cur = x
    for layer in range(4):
        kind = layer % 4
        xTs = [_c(cur[b].T) for b in range(B)]
        maps = []
        if kind == 0:
            nca = _nc('dil', lambda: build_dil(Sq))
            w_in, w_out = inp['dil_w_in'][0], inp['dil_w_out'][0]
            xperm = [[_c(xTs[b][:, dil_perm(Sq, d)]) for (_, d) in DIL] for b in range(B)]
            for c in range(8):
                b, hg = c // 4, c % 4
                heads = list(range(hg * 4, hg * 4 + 4))
                m = {f"xT{g}": xperm[b][g] for g in range(3)}
                wA = np.empty((3, 4, 1024, 192), np.float32)
                for g in range(3):
                    for i, h in enumerate(heads):
                        for j in range(3):
                            wA[g, i, :, j * 64:(j + 1) * 64] = w_in[:, g * 3072 + j * 1024 + h * 64: g * 3072 + j * 1024 + (h + 1) * 64]
                m["wA"] = wA
                m["btA"] = dil_bias_tiles(tbl, heads)
                maps.append(m)
        elif kind == 1:
            nca = _nc('sb', lambda: build_sb(Sq))
            w_in, w_out = inp['sb_w_in'][0], inp['sb_w_out'][0]
            cm = sb_consts()
            for c in range(8):
                b, hg = c // 4, c % 4
                cols = slice(hg * 256, (hg + 1) * 256)
                maps.append(dict(xT=xTs[b], wq=_c(w_in[:, 0:1024][:, cols]), wk=_c(w_in[:, 1024:2048][:, cols]),
                                 wv=_c(w_in[:, 2048:3072][:, cols]), cm=cm))
        elif kind == 2:
            nca = _nc('nsa', lambda: build_nsa(Sq))
            w_in, w_out = inp['nsa_w_in'][0], inp['nsa_w_out'][0]
            ov = nsa_consts(Sq)
            for c in range(8):
                maps.append(_nsa_in_map(xTs[c // 4], c % 4, w_in, tbl, inp, ov))
        else:
            nca = _nc('moba', lambda: build_moba(Sq))
            w_in, w_out = inp['moba_w_in'][0], inp['moba_w_out'][0]
            for c in range(8):
                b, hg = c // 4, c % 4
                cols = slice(hg * 256, (hg + 1) * 256)
                heads = list(range(hg * 4, hg * 4 + 4))
                maps.append(dict(xT=xTs[b], wq=_c(w_in[:, 0:1024][:, cols]), wk=_c(w_in[:, 1024:2048][:, cols]),
                                 wv=_c(w_in[:, 2048:3072][:, cols]), bss=nsa_bias_strips(tbl, heads)[0],
                                 cfar=_c(np.broadcast_to(tbl[31, heads][None, :], (128, 4)))))
        res = run_bass_kernel_spmd(nca, maps, core_ids=list(range(8)))
        oTf = [np.concatenate([res.results[b * 4 + i]['oT'] for i in range(4)], axis=0) for b in range(B)]
        del res, maps, xTs
        ncp = _nc('post', lambda: build_post(T))
        lnp = _c(np.stack([inp['ln1_g'][layer], inp['ln1_b'][layer], inp['ln2_g'][layer], inp['ln2_b'][layer]]))
        curf = cur.reshape(B * Sq, 1024)
        pmaps = []
        for c in range(8):
            b, s0 = (c * T) // Sq, (c * T) % Sq
            pmaps.append(dict(oT=_c(oTf[b][:, s0:s0 + T]), xin=_c(curf[c * T:(c + 1) * T]), w_out=_c(w_out), lnp=lnp,
                              rw=inp['router_w'], rb=_c(inp['router_b'][None, :]), wg=inp['exp_w_gate'][layer],
                              wu=inp['exp_w_up'][layer], wd=inp['exp_w_down'][layer]))
        res = run_bass_kernel_spmd(ncp, pmaps, core_ids=list(range(8)))
        cur = np.concatenate([res.results[c]['xout'] for c in range(8)], axis=0).reshape(B, Sq, 1024)
        del res, pmaps
    return np.asarray(cur, dtype=np.float32)
```

```python
import contextlib
import numpy as np
import concourse.bass as bass
import concourse.mybir as mybir
from concourse.bass_utils import run_bass_kernel_spmd

F32 = mybir.dt.float32
BF16 = mybir.dt.bfloat16
I32 = mybir.dt.int32
AF = mybir.ActivationFunctionType
ALU = mybir.AluOpType
AX = mybir.AxisListType

ENGS = ("pe", "act", "dve", "pool", "sp")
NDSEM = 8


class Sched:
    def __init__(self, nc):
        self.nc = nc
        self.ops = {e: [] for e in ENGS}
        self.lastw = {}
        self.readers = {}
        self.ndma = {e: 0 for e in ENGS}
        self.waited = {e: {} for e in ENGS}

    def _add(self, eng, fn, reads, writes, dma):
        idx = len(self.ops[eng])
        deps = set()
        for b in reads:
            if b in self.lastw:
                deps.add(self.lastw[b])
        for b in writes:
            if b in self.lastw:
                deps.add(self.lastw[b])
            for r in self.readers.get(b, ()):
                deps.add(r)
        op = dict(fn=fn, waits=[], dma=None, sig=False)
        me = (eng, idx)
        if dma:
            k = self.ndma[eng]
            self.ndma[eng] += 1
            slot, val = k % NDSEM, 16 * (k // NDSEM + 1)
            op["dma"] = (slot, val)
            me = ("dma", eng, slot, val)
            if k >= NDSEM:
                deps.add(("dma", eng, slot, val - 16))
        best = {}
        for d in deps:
            if d[0] == "dma":
                key = ("dma", d[1], d[2]); v = d[3]
            else:
                key = d[0]; v = d[1]
                if key == eng and eng == "pe" and not dma:
                    continue
            if v > best.get(key, -1):
                best[key] = v
        w = self.waited[eng]
        for key, v in best.items():
            if w.get(key, -1) >= v:
                continue
            w[key] = v
            op["waits"].append((key, v))
            if key not in ("dma",) and not isinstance(key, tuple):
                self.ops[key][v]["sig"] = True
        self.ops[eng].append(op)
        for b in reads:
            self.readers.setdefault(b, []).append(me)
        for b in writes:
            self.lastw[b] = me
            self.readers[b] = []
        return me

    def op(self, eng, fn, reads=(), writes=()):
        return self._add(eng, fn, tuple(reads), tuple(writes), False)

    def dma(self, eng, fn, reads=(), writes=()):
        return self._add(eng, fn, tuple(reads), tuple(writes), True)

    def fence(self):
        targets = []
        for e in ENGS:
            n = len(self.ops[e])
            for i in range(n - 1, -1, -1):
                o = self.ops[e][i]
                if o["fn"] is not None and o["dma"] is None:
                    targets.append((e, i))
                    break
            k = self.ndma[e]
            for slot in range(min(k, NDSEM)):
                cnt = (k - 1 - slot) // NDSEM + 1
                targets.append((("dma", e, slot), 16 * cnt))
        for e in ENGS:
            w = self.waited[e]
            waits = []
            for key, v in targets:
                if key == e and not isinstance(key, tuple):
                    continue
                if w.get(key, -1) >= v:
                    continue
                w[key] = v
                waits.append((key, v))
                if not isinstance(key, tuple):
                    self.ops[key][v]["sig"] = True
            if waits:
                self.ops[e].append(dict(fn=None, waits=waits, dma=None, sig=False))

    def emit(self, final_engine="sp"):
        nc = self.nc
        fin_waits = []
        for e in ENGS:
            n = self.ndma[e]
            for slot in range(min(n, NDSEM)):
                cnt = (n - 1 - slot) // NDSEM + 1
                fin_waits.append((("dma", e, slot), 16 * cnt))
        self.ops[final_engine].append(dict(fn=None, waits=fin_waits, dma=None, sig=False))
        sigcnt = {}
        for e in ENGS:
            c = 0
            arr = []
            for o in self.ops[e]:
                if o["sig"]:
                    c += 1
                arr.append(c)
            sigcnt[e] = arr
        import contextlib
        with contextlib.ExitStack() as st:
            esem = {e: st.enter_context(nc.semaphore(f"s_{e}")) for e in ENGS}
            dsem = {e: [st.enter_context(nc.semaphore(f"d_{e}{i}")) for i in range(NDSEM)]
                    for e in ENGS if self.ndma[e] > 0}
            block = st.enter_context(nc.Block())

            def run(e, eng):
                for i, o in enumerate(self.ops[e]):
                    for key, v in o["waits"]:
                        if isinstance(key, tuple):
                            eng.wait_ge(dsem[key[1]][key[2]], v)
                        else:
                            eng.wait_ge(esem[key], sigcnt[key][v])
                    if o["fn"] is None:
                        continue
                    ins = o["fn"](eng)
                    if o["dma"] is not None:
                        ins.then_inc(dsem[e][o["dma"][0]], 16)
                    elif o["sig"]:
                        ins.then_inc(esem[e], 1)

            if self.ops["pe"]:
                @block.tensor
                def _(eng):
                    run("pe", eng)
            if self.ops["act"]:
                @block.scalar
                def _(eng):
                    run("act", eng)
            if self.ops["dve"]:
                @block.vector
                def _(eng):
                    run("dve", eng)
            if self.ops["pool"]:
                @block.gpsimd
                def _(eng):
                    run("pool", eng)
            if self.ops["sp"]:
                @block.sync
                def _(eng):
                    run("sp", eng)


D = 1024
HD = 64
SCALE = 0.125
NEG = -30000.0


class KB:
    def __init__(self):
        self.nc = bass.Bass("TRN2", target_bir_lowering=False)
        self.S = Sched(self.nc)
        self.st = contextlib.ExitStack()
        self._n = 0

    def din(self, name, shape, dt=F32):
        return self.nc.dram_tensor(name, list(shape), dt, kind="ExternalInput").ap()

    def dout(self, name, shape, dt=F32):
        return self.nc.dram_tensor(name, list(shape), dt, kind="ExternalOutput").ap()

    def sb(self, name, shape, dt, st=None):
        t = (st or self.st).enter_context(self.nc.sbuf_tensor(name, list(shape), dt))
        return t

    def ps(self, name, shape=(128, 512), dt=F32):
        return self.st.enter_context(self.nc.psum_tensor(name, list(shape), dt))

    def finish(self):
        self.S.emit()
        self.st.close()
        return self.nc


def pipeline(units, stages, skews):
    n = len(units)
    if n == 0:
        return
    for t in range(n + max(skews)):
        for s, fn in enumerate(stages):
            u = t - skews[s]
            if 0 <= u < n:
                fn(u, units[u])


def load_w_bf16(kb, name, w_ap, ncols, stg, stg_key):
    S = kb.S
    wb = kb.sb(name, [128, 8, ncols], BF16)
    S.dma("sp", lambda e: e.dma_start(out=stg[:, 0:8 * ncols].rearrange("p (k n) -> p k n", k=8),
                                      in_=w_ap.rearrange("(k p) n -> p k n", p=128)), writes=[stg_key])
    S.op("pool", lambda e: e.tensor_copy(out=wb[:].rearrange("p k n -> p (k n)"), in_=stg[:, 0:8 * ncols]),
         reads=[stg_key], writes=[name])
    return wb


def project(kb, xT, Sq, fm_outs, tm_outs, pss, xs, xb):
    S = kb.S
    xv = xT.rearrange("(k p) t -> p k t", p=128)
    NC = Sq // 512
    pi = 0
    for c in range(NC):
        xsi = xs[c % len(xs)]
        if isinstance(xsi, tuple):
            xsi, xsk = xsi
        else:
            xsk = f"xs{c % len(xs)}"
        xbi = xb[c % len(xb)]
        if isinstance(xbi, tuple):
            xbi, xbk = xbi
        else:
            xbk = f"xb{c % len(xb)}"
        S.dma("sp", lambda e, xsi=xsi, c=c: e.dma_start(out=xsi[:], in_=xv[:, :, c * 512:(c + 1) * 512]), writes=[xsk])
        S.op("dve", lambda e, xsi=xsi, xbi=xbi: e.tensor_copy(out=xbi[:, 0:4, :], in_=xsi[:, 0:4, :]), reads=[xsk], writes=[xbk])
        S.op("act", lambda e, xsi=xsi, xbi=xbi: e.copy(out=xbi[:, 4:8, :], in_=xsi[:, 4:8, :]), reads=[xsk], writes=[xbk])
        for (w, wkey, M, dst, dkey, scale, ev) in fm_outs:
            p, pk = pss[pi % len(pss)]
            pi += 1
            for k in range(8):
                S.op("pe", lambda e, p=p, w=w, k=k, M=M, xbi=xbi: e.matmul(p[0:M, :], lhsT=w[:, k, 0:M], rhs=xbi[:, k, :],
                                                                          start=(k == 0), stop=(k == 7)),
                     reads=[wkey, xbk], writes=[pk])
            if callable(ev):
                ev(p, pk, c)
            elif ev == "act":
                S.op("act", lambda e, p=p, M=M, dst=dst, c=c, scale=scale: e.mul(dst[0:M, c * 512:(c + 1) * 512], p[0:M, :], scale),
                     reads=[pk], writes=[dkey])
            else:
                S.op("dve", lambda e, p=p, M=M, dst=dst, c=c, scale=scale: e.tensor_scalar(
                    out=dst[0:M, c * 512:(c + 1) * 512], in0=p[0:M, :], scalar1=scale, scalar2=None, op0=ALU.mult),
                    reads=[pk], writes=[dkey])
        for (w, wkey, N, dst_fn, dkey) in tm_outs:
            p, pk = pss[pi % len(pss)]
            pi += 1
            for tt in range(4):
                for k in range(8):
                    S.op("pe", lambda e, p=p, w=w, k=k, N=N, tt=tt, xbi=xbi: e.matmul(
                        p[:, tt * N:(tt + 1) * N], lhsT=xbi[:, k, tt * 128:(tt + 1) * 128], rhs=w[:, k, 0:N],
                        start=(k == 0), stop=(k == 7)), reads=[wkey, xbk], writes=[pk])
            for tt in range(4):
                for (dap, lo, hi) in dst_fn(c * 4 + tt):
                    S.op("dve", lambda e, p=p, N=N, tt=tt, dap=dap, lo=lo, hi=hi: e.tensor_copy(out=dap, in_=p[:, tt * N + lo:tt * N + hi]),
                         reads=[pk], writes=[dkey])


def build_sb(Sq):
    kb = KB()
    S = kb.S
    NT, NCH = Sq // 128, Sq // 512
    xT = kb.din("xT", [D, Sq])
    wq = kb.din("wq", [D, 256])
    wk = kb.din("wk", [D, 256])
    wv = kb.din("wv", [D, 256])
    cm = kb.din("cm", [128, 4 * 512 + 256])
    oT = kb.dout("oT", [256, Sq], BF16)

    stg = kb.sb("stg", [128, 8 * 256], F32)
    cmf = kb.sb("cmf", [128, 4 * 512 + 256], F32)
    cmb = kb.sb("cmb", [128, 4 * 512 + 256], BF16)
    one1 = kb.sb("one1", [128, 1], F32)
    S.dma("sp", lambda e: e.dma_start(out=cmf[:], in_=cm), writes=["cmf"])
    S.op("pool", lambda e: e.tensor_copy(out=cmb[:], in_=cmf[:]), reads=["cmf"], writes=["cmb"])
    S.op("pool", lambda e: e.memset(one1[:], 1.0), writes=["one1"])
    ones_b = cmb[:, 2048:2176]
    umat_b = cmb[:, 2176:2304]
    wqb = load_w_bf16(kb, "wqb", wq, 256, stg, "stg")
    wkb = load_w_bf16(kb, "wkb", wk, 256, stg, "stg")
    wvb = load_w_bf16(kb, "wvb", wv, 256, stg, "stg")

    QT = kb.sb("QT", [128, Sq], BF16)
    KT = kb.sb("KT", [128, Sq], BF16)
    V = kb.sb("V", [128, NT, 128], BF16)
    xs = [kb.sb(f"xs{i}", [128, 8, 512], F32) for i in range(1)]
    xb = [kb.sb(f"xb{i}", [128, 8, 512], BF16) for i in range(2)]
    NB = 3
    NZ = 6
    zc = [kb.sb(f"zc{i}", [128, 512], F32) for i in range(NZ)]
    ee = [kb.sb(f"ee{i}", [128, 512], F32) for i in range(2)]
    sp = [kb.sb(f"sp{i}", [128, 512], BF16) for i in range(NB)]
    t1 = [kb.sb(f"t1{i}", [128, 512], F32) for i in range(NB)]
    att = [kb.sb(f"att{i}", [128, 512], BF16) for i in range(NB)]
    osb = [kb.sb(f"osb{i}", [64, 512], BF16) for i in range(2)]
    totb = [kb.sb(f"totb{i}", [128, 512], F32) for i in range(2)]

    psz = [kb.ps(f"psz{i}") for i in range(2)]
    psT = [kb.ps(f"psT{i}") for i in range(2)]
    psL = [kb.ps(f"psL{i}") for i in range(2)]
    pso = [kb.ps(f"pso{i}") for i in range(2)]
    pss = [(psz[0], "psz0"), (psz[1], "psz1"), (psL[0], "psL0"), (psL[1], "psL1")]

    for pair in range(2):
        fm = [(wqb[:, :, pair * 128:(pair + 1) * 128], "wqb", 128, QT, "QT", SCALE, "act"),
              (wkb[:, :, pair * 128:(pair + 1) * 128], "wkb", 128, KT, "KT", 1.0, "dve")]
        tm = [(wvb[:, :, pair * 128:(pair + 1) * 128], "wvb", 128, (lambda t: [(V[:, t, :], 0, 128)]), "V")]
        project(kb, xT, Sq, fm, tm, pss, xs, xb)

        units = []
        for c in range(NCH):
            jmax = 4 * c + 3
            for j in range(jmax, -1, -1):
                for h in range(2):
                    units.append((c, j, h, jmax))

        def s1(u, un):
            c, j, h, jmax = un
            pz, pzk = psz[u % 2], f"psz{u % 2}"
            hp = slice(h * 64, (h + 1) * 64)
            S.op("pe", lambda e: e.matmul(pz[:], lhsT=KT[hp, j * 128:(j + 1) * 128], rhs=QT[hp, c * 512:(c + 1) * 512],
                                          start=True, stop=True), reads=["KT", "QT"], writes=[pzk])

        def s2a(u, un):
            c, j, h, jmax = un
            pz, pzk = psz[u % 2], f"psz{u % 2}"
            bz = u % NZ
            S.op("dve", lambda e: e.tensor_scalar(out=zc[bz][:], in0=pz[:], scalar1=-60.0, scalar2=60.0,
                                                  op0=ALU.max, op1=ALU.min), reads=[pzk], writes=[f"zc{bz}"])

        def s2b(u, un):
            c, j, h, jmax = un
            bz, be, b = u % NZ, u % 2, u % NB
            S.op("act", lambda e: e.activation(out=ee[be][:], in_=zc[bz][:], func=AF.Exp), reads=[f"zc{bz}"], writes=[f"ee{be}"])
            S.op("act", lambda e: e.activation(out=sp[b][:], in_=ee[be][:], func=AF.Ln, bias=one1[:, 0:1]),
                 reads=[f"ee{be}", "one1"], writes=[f"sp{b}"])
            if j >= 4 * c:
                o = j - 4 * c
                S.op("pool", lambda e: e.tensor_tensor(out=sp[b][:], in0=sp[b][:], in1=cmb[:, o * 512:(o + 1) * 512], op=ALU.mult),
                     reads=[f"sp{b}", "cmb"], writes=[f"sp{b}"])

        def s3b(u, un):
            c, j, h, jmax = un
            bz = u % NZ
            if j != jmax:
                S.op("pool", lambda e: e.tensor_tensor(out=zc[bz][:], in0=zc[bz][:], in1=totb[h][:], op=ALU.subtract),
                     reads=[f"zc{bz}", f"totb{h}"], writes=[f"zc{bz}"])

        def s3(u, un):
            c, j, h, jmax = un
            b = u % NB
            pl, plk = psL[u % 2], f"psL{u % 2}"
            p1, p1k = psT[u % 2], f"psT{u % 2}"
            S.op("pe", lambda e: e.matmul(pl[:], lhsT=umat_b, rhs=sp[b][:], start=True, stop=True),
                 reads=[f"sp{b}", "cmb"], writes=[plk])
            S.op("pe", lambda e: e.matmul(p1[:], lhsT=ones_b, rhs=sp[b][:], start=True, stop=True),
                 reads=[f"sp{b}", "cmb"], writes=[p1k])

        def s4a(u, un):
            c, j, h, jmax = un
            bz, b = u % NZ, u % NB
            pl, plk = psL[u % 2], f"psL{u % 2}"
            p1, p1k = psT[u % 2], f"psT{u % 2}"
            S.op("dve", lambda e: e.tensor_tensor(out=t1[b][:], in0=zc[bz][:], in1=pl[:], op=ALU.subtract),
                 reads=[f"zc{bz}", plk], writes=[f"t1{b}"])
            if j != jmax:
                S.op("dve", lambda e: e.tensor_tensor(out=totb[h][:], in0=totb[h][:], in1=p1[:], op=ALU.add),
                     reads=[f"totb{h}", p1k], writes=[f"totb{h}"])
            else:
                S.op("dve", lambda e: e.tensor_copy(out=totb[h][:], in_=p1[:]), reads=[p1k], writes=[f"totb{h}"])

        def s4b(u, un):
            c, j, h, jmax = un
            b = u % NB
            S.op("act", lambda e: e.activation(out=att[b][:], in_=t1[b][:], func=AF.Exp), reads=[f"t1{b}"], writes=[f"att{b}"])
            if j >= 4 * c:
                o = j - 4 * c
                S.op("pool", lambda e: e.tensor_tensor(out=att[b][:], in0=att[b][:], in1=cmb[:, o * 512:(o + 1) * 512], op=ALU.mult),
                     reads=[f"att{b}", "cmb"], writes=[f"att{b}"])

        def s5(u, un):
            c, j, h, jmax = un
            b = u % NB
            S.op("pe", lambda e: e.matmul(pso[h][0:64, :], lhsT=V[:, j, h * 64:(h + 1) * 64], rhs=att[b][:],
                                          start=(j == jmax), stop=(j == 0)), reads=[f"att{b}", "V"], writes=[f"pso{h}"])
            if j == 0:
                ob = osb[h]
                S.op("act", lambda e: e.copy(out=ob[:], in_=pso[h][0:64, :]), reads=[f"pso{h}"], writes=[f"osb{h}"])
                r0 = pair * 128 + h * 64
                S.dma("sp", lambda e: e.dma_start(out=oT[r0:r0 + 64, c * 512:(c + 1) * 512], in_=ob[:]), reads=[f"osb{h}"])

        n_u = len(units)
        stages = ((s2a, 0), (s2b, 2), (s3, 3), (s4a, 4), (s3b, 3), (s4b, 5), (s5, 6))
        for t in range(n_u + 7):
            if t % 2 == 0:
                for u in (t, t + 1):
                    if u < n_u:
                        s1(u, units[u])
            for fn_, sk_ in stages:
                u = t - sk_
                if 0 <= u < n_u:
                    fn_(u, units[u])
    return kb.finish()


def sb_consts():
    cm = np.zeros((128, 4 * 512 + 256), np.float32)
    k = np.arange(128)[:, None]
    q = np.arange(512)[None, :]
    for o in range(4):
        cm[:, o * 512:(o + 1) * 512] = ((o * 128 + k) < q).astype(np.float32)
    cm[:, 2048:2176] = 1.0
    jj = np.arange(128)[:, None]
    ss = np.arange(128)[None, :]
    cm[:, 2176:2304] = (jj >= ss).astype(np.float32)
    return cm


class SoftmaxPipe:
    def __init__(self, kb, nS=3, nP=3):
        import os
        nP = int(os.environ.get('SM_NP', nP))
        self.kb = kb
        self.psS = [kb.ps(f"psS{i}") for i in range(nS)]
        self.PT = [kb.sb(f"PT{i}", [128, 512], BF16) for i in range(nP)]
        self.nS, self.nP = nS, nP
        self.cnt = 0

    def run(self, units):
        S = self.kb.S
        base = self.cnt
        self.cnt += len(units)

        def s1(u, un):
            i = (base + u) % self.nS
            ps, pk = self.psS[i], f"psS{i}"
            n = len(un["mm"])
            kp, N = un.get("kp", 128), un["N"]
            for m, mmx in enumerate(un["mm"]):
                (lhsT, rhs, reads) = mmx[:3]
                r0, r1 = mmx[3] if len(mmx) > 3 else (0, kp)
                S.op("pe", lambda e, lhsT=lhsT, rhs=rhs, m=m, r0=r0, r1=r1: e.matmul(ps[r0:r1, 0:N], lhsT=lhsT, rhs=rhs, start=(m == 0), stop=(m == n - 1)),
                     reads=reads, writes=[pk])

        def s2(u, un):
            i = (base + u) % self.nS
            ps, pk = self.psS[i], f"psS{i}"
            b = (base + u) % self.nP
            kp, N = un.get("kp", 128), un["N"]
            cb = un.get("cb")
            if cb is None:
                S.op("act", lambda e: e.activation(out=self.PT[b][0:kp, 0:N], in_=ps[0:kp, 0:N], func=AF.Exp),
                     reads=[pk], writes=[f"PT{b}"])
            else:
                S.op("act", lambda e: e.activation(out=self.PT[b][0:kp, 0:N], in_=ps[0:kp, 0:N], func=AF.Exp, bias=cb[0]),
                     reads=[pk, cb[1]], writes=[f"PT{b}"])
            if un.get("post"):
                un["post"](self.PT[b], f"PT{b}")

        def s3(u, un):
            b = (base + u) % self.nP
            kp, N = un.get("kp", 128), un["N"]
            for pv in un["pv"]:
                (lhsT_v, reads, acc, acck, start, stop) = pv[:6]
                c0, c1 = pv[6] if len(pv) > 6 else (0, N)
                S.op("pe", lambda e, lhsT_v=lhsT_v, acc=acc, start=start, stop=stop, c0=c0, c1=c1: e.matmul(
                    acc, lhsT=lhsT_v, rhs=self.PT[b][0:kp, c0:c1], start=start, stop=stop),
                    reads=[f"PT{b}"] + list(reads), writes=[acck])
            if un.get("fin"):
                un["fin"]()

        import os
        pipeline(units, [s1, s2, s3], [int(v) for v in os.environ.get('SM_SKEW', '0,1,2').split(',')])


class SoftmaxPairPipe:
    def __init__(self, kb, nU=2, nP=3):
        import os
        nP = int(os.environ.get('SMP_NP', 4))
        self.kb = kb
        self.psS = [[kb.ps(f"psS{u}_{h}") for h in range(2)] for u in range(nU)]
        self.PT = [[kb.sb(f"PT{u}_{h}", [128, 512], BF16) for h in range(2)] for u in range(nP)]
        self.nU, self.nP = nU, nP
        self.cnt = 0

    def run(self, units):
        S = self.kb.S
        base = self.cnt
        self.cnt += len(units)

        def s1(u, un):
            i = (base + u) % self.nU
            N = un["N"]
            cnt = [0, 0]
            tot = [sum(1 for m in un["mm"] if m[3] == h) for h in range(2)]
            for mmx in un["mm"]:
                (lhsT, rhs, reads, h) = mmx[:4]
                r0, r1 = mmx[4] if len(mmx) > 4 else (0, 128)
                ps, pk = self.psS[i][h], f"psS{i}_{h}"
                first, last = cnt[h] == 0, cnt[h] == tot[h] - 1
                cnt[h] += 1
                S.op("pe", lambda e, lhsT=lhsT, rhs=rhs, ps=ps, first=first, last=last, r0=r0, r1=r1: e.matmul(ps[r0:r1, 0:N], lhsT=lhsT, rhs=rhs, start=first, stop=last),
                     reads=reads, writes=[pk])

        def s2(u, un):
            i = (base + u) % self.nU
            b = (base + u) % self.nP
            N = un["N"]
            for h in un.get("slots", (0, 1)):
                ps, pk = self.psS[i][h], f"psS{i}_{h}"
                cb = un["cb"][h]
                if cb is None:
                    S.op("act", lambda e, ps=ps, h=h: e.activation(out=self.PT[b][h][:, 0:N], in_=ps[:, 0:N], func=AF.Exp),
                         reads=[pk], writes=[f"PT{b}_{h}"])
                else:
                    S.op("act", lambda e, ps=ps, h=h, cb=cb: e.activation(out=self.PT[b][h][:, 0:N], in_=ps[:, 0:N], func=AF.Exp, bias=cb[0]),
                         reads=[pk, cb[1]], writes=[f"PT{b}_{h}"])
            if un.get("post"):
                un["post"](self.PT[b][0], f"PT{b}_0")

        def s3(u, un):
            b = (base + u) % self.nP
            N = un["N"]
            for (lhsT_v, reads, acc, acck, start, stop, h) in un["pv"]:
                S.op("pe", lambda e, lhsT_v=lhsT_v, acc=acc, start=start, stop=stop, h=h: e.matmul(
                    acc, lhsT=lhsT_v, rhs=self.PT[b][h][:, 0:N], start=start, stop=stop),
                    reads=[f"PT{b}_{h}"] + list(reads), writes=[acck])
            if un.get("fin"):
                un["fin"]()

        import os
        pipeline(units, [s1, s2, s3], [int(v) for v in os.environ.get('SMP_SKEW', '0,1,2').split(',')])


class Normalizer:
    def __init__(self, kb):
        self.kb = kb
        self.rdt = kb.sb("nz_rdt", [128, 512], F32)
        self.num = kb.sb("nz_num", [64, 512], F32)
        self.onesf = kb.sb("nz_ones", [128, 64], F32)
        self.psB = kb.ps("psB")
        kb.S.op("pool", lambda e: e.memset(self.onesf[:], 1.0), writes=["nz_ones"])

    def bcast_recip(self, acc, acck, N):
        S = self.kb.S
        S.op("act", lambda e: e.activation(out=self.rdt[64:65, 0:N], in_=acc[64:65, 0:N], func=AF.Ln), reads=[acck], writes=["nz_rdt"])
        S.op("act", lambda e: e.activation(out=self.rdt[64:65, 0:N], in_=self.rdt[64:65, 0:N], func=AF.Exp, scale=-1.0), reads=["nz_rdt"], writes=["nz_rdt"])
        S.op("pe", lambda e: e.matmul(self.psB[0:64, 0:N], lhsT=self.onesf[64:65, 0:64], rhs=self.rdt[64:65, 0:N],
                                      start=True, stop=True), reads=["nz_rdt", "nz_ones"], writes=["psB"])

    def normalize(self, acc, acck, N, out_ap, outk):
        S = self.kb.S
        self.bcast_recip(acc, acck, N)
        S.op("act", lambda e: e.copy(out=self.num[:, 0:N], in_=acc[0:64, 0:N]), reads=[acck], writes=["nz_num"])
        S.op("dve", lambda e: e.tensor_tensor(out=out_ap, in0=self.num[:, 0:N], in1=self.psB[0:64, 0:N], op=ALU.mult),
             reads=["nz_num", "psB"], writes=[outk])


NOFF = 16


def build_moba(Sq):
    kb = KB()
    S = kb.S
    NT, NCH, NBLK = Sq // 128, Sq // 512, Sq // 256
    GW = max(NBLK, 8)
    xT = kb.din("xT", [D, Sq])
    wq = kb.din("wq", [D, 256])
    wk = kb.din("wk", [D, 256])
    wv = kb.din("wv", [D, 256])
    bss = kb.din("bss", [4, 128, 2432])
    cfar = kb.din("cfar", [128, 4])
    oT = kb.dout("oT", [256, Sq], BF16)

    stg = kb.sb("stg", [128, 8 * 512], F32)
    wqb = load_w_bf16(kb, "wqb", wq, 256, stg, "stg")
    wkb = load_w_bf16(kb, "wkb", wk, 256, stg, "stg")
    wvb = load_w_bf16(kb, "wvb", wv, 256, stg, "stg")
    cf = kb.sb("cf", [128, 4], F32)
    S.dma("sp", lambda e: e.dma_start(out=cf[:], in_=cfar), writes=["cf"])
    identf = kb.sb("identf", [128, 128], F32)
    identb = kb.sb("identb", [128, 128], BF16)
    onesf = kb.sb("onesf", [128, 128], F32)
    S.op("pool", lambda e: e.memset(onesf[:], 1.0), writes=["onesf"])
    S.op("pool", lambda e: e.affine_select(out=identf[:], in_=onesf[:], pattern=[[-1, 128]], compare_op=ALU.is_equal,
                                           fill=0.0, base=0, channel_multiplier=1), reads=["onesf"], writes=["identf"])
    S.op("pool", lambda e: e.tensor_copy(out=identb[:], in_=identf[:]), reads=["identf"], writes=["identb"])

    QT = kb.sb("QT", [128, Sq], BF16)
    KT = kb.sb("KT", [128, Sq], BF16)
    Vh = [kb.sb(f"V{h}", [128, NT, 65], BF16) for h in range(2)]
    for h in range(2):
        S.op("pool", lambda e, h=h: e.memset(Vh[h][:, :, 64:65], 1.0), writes=["V"])
    maskT = kb.sb("maskT", [128, Sq], BF16)
    bs = kb.sb("bs", [128, 2, 2432], BF16)
    xs = [(stg[:].rearrange("p (k t) -> p k t", k=8), "stg")]
    xb = [kb.sb(f"xb{i}", [128, 8, 512], BF16) for i in range(2)]
    km = kb.sb("km", [128, NBLK], F32)
    kmb = kb.sb("kmb", [128, NBLK], BF16)
    gbuf = kb.sb("gbuf", [128, GW], F32)
    m8 = kb.sb("m8", [128, 8], F32)
    mb2 = kb.sb("mb2", [128, 128], F32)
    osb = [kb.sb(f"osb{i}", [64, 512], BF16) for i in range(2)]

    sp_ = SoftmaxPairPipe(kb)
    nz = Normalizer(kb)
    pso = [[kb.ps(f"pso{h}_0")] for h in range(2)]
    pss = [(sp_.psS[0][0], "psS0_0"), (sp_.psS[0][1], "psS0_1"), (sp_.psS[1][0], "psS1_0")]
    import os
    for i in range(int(os.environ.get("DUMMY_WARM", "0"))):
        S.op("pe", lambda e: e.matmul(sp_.psS[0][0][:, 0:256], lhsT=identb[:], rhs=wqb[:, 0, :], start=True, stop=True),
             reads=["identb", "wqb"], writes=["psS0_0"])

    for pair in range(2):
        fm = [(wqb[:, :, pair * 128:(pair + 1) * 128], "wqb", 128, QT, "QT", SCALE, "act"),
              (wkb[:, :, pair * 128:(pair + 1) * 128], "wkb", 128, KT, "KT", 1.0, "dve")]
        tm = [(wvb[:, :, pair * 128:(pair + 1) * 128], "wvb", 128,
               (lambda t: [(Vh[0][:, t, 0:64], 0, 64), (Vh[1][:, t, 0:64], 64, 128)]), "V")]
        project(kb, xT, Sq, fm, tm, pss, xs, xb)
        for h in range(2):
            for w0 in range(0, 2432, 2048):
                wn = min(2048, 2432 - w0)
                S.dma("sp", lambda e, h=h, pair=pair, w0=w0, wn=wn: e.dma_start(out=stg[:, 0:wn], in_=bss[pair * 2 + h, :, w0:w0 + wn]), writes=["stg"])
                S.op("pool", lambda e, h=h, w0=w0, wn=wn: e.tensor_copy(out=bs[:, h, w0:w0 + wn], in_=stg[:, 0:wn]), reads=["stg"], writes=["bs"])
        S.op("dve", lambda e: e.tensor_reduce(out=km[:], in_=KT[:].rearrange("p (b k) -> p b k", k=256), axis=AX.X, op=ALU.add),
             reads=["KT"], writes=["km"])
        S.op("dve", lambda e: e.tensor_copy(out=kmb[:], in_=km[:]), reads=["km"], writes=["kmb"])
        for qt in range(NT):
            nv = qt // 2
            S.op("pool", lambda e: e.memset(mb2[:], NEG), writes=["mb2"])
            for h in range(2):
                hp = slice(h * 64, (h + 1) * 64)
                c0 = h * 64
                pg, pgk = pss[(qt * 2 + h) % 3]
                if nv > 3:
                    S.op("pe", lambda e, pg=pg, hp=hp, qt=qt: e.matmul(pg[:, 0:NBLK], lhsT=QT[hp, qt * 128:(qt + 1) * 128], rhs=kmb[hp, :],
                                                                       start=True, stop=True), reads=["QT", "kmb"], writes=[pgk])
                    S.op("pool", lambda e: e.memset(gbuf[:], -1e30), writes=["gbuf"])
                    S.op("dve", lambda e, pg=pg, nv=nv: e.tensor_copy(out=gbuf[:, 0:nv], in_=pg[:, 0:nv]), reads=[pgk], writes=["gbuf"])
                    S.op("dve", lambda e: e.max(out=m8[:], in_=gbuf[:]), reads=["gbuf"], writes=["m8"])
                    S.op("dve", lambda e, c0=c0: e.tensor_scalar(out=mb2[:, c0:c0 + NBLK], in0=gbuf[:, 0:NBLK], scalar1=m8[:, 2:3], scalar2=NEG,
                                                                 op0=ALU.is_lt, op1=ALU.mult), reads=["gbuf", "m8"], writes=["mb2"])
                elif nv > 0:
                    S.op("pool", lambda e, nv=nv, c0=c0: e.memset(mb2[:, c0:c0 + nv], 0.0), writes=["mb2"])
                S.op("pool", lambda e, nv=nv, c0=c0: e.memset(mb2[:, c0 + nv:c0 + nv + 1], 0.0), writes=["mb2"])
            pt, ptk = pss[(qt * 2 + 2) % 3]
            S.op("pe", lambda e, pt=pt: e.transpose(pt[:, 0:128], mb2[:], identf[:]), reads=["mb2", "identf"], writes=[ptk])
            S.op("act", lambda e, pt=pt, qt=qt: e.copy(out=maskT[:, qt * 128:(qt + 1) * 128], in_=pt[:, 0:128]),
                 reads=[ptk], writes=["maskT"])
        units = []
        for c in range(NCH):
            jmax = 4 * c + 3
            qs = slice(c * 512, (c + 1) * 512)
            for j in range(jmax + 1):
                o = 4 * c - j
                x = j // 2
                mm, cbs, pv = [], [], []
                for h in range(2):
                    hp = slice(h * 64, (h + 1) * 64)
                    mm.append((KT[hp, j * 128:(j + 1) * 128], QT[hp, qs], ["KT", "QT"], h))
                for h in range(2):
                    hp = slice(h * 64, (h + 1) * 64)
                    mm.append((identb[hp, h * 64 + x:h * 64 + x + 1].to_broadcast([64, 128]), maskT[hp, qs], ["identb", "maskT"], h))
                for h in range(2):
                    if o <= 12:
                        mm.append((identb[:], bs[:, h, (o + 3) * 128:(o + 3) * 128 + 512], ["identb", "bs"], h))
                        cbs.append(None)
                    else:
                        cbs.append((cf[:, pair * 2 + h:pair * 2 + h + 1], "cf"))
                    pv.append((Vh[h][:, j, :], ["V"], pso[h][0][0:65, :], f"pso{h}_0", j == 0, j == jmax, h))
                un = dict(mm=mm, N=512, cb=cbs, pv=pv)
                if j == jmax:
                    def fin(c=c, pair=pair):
                        for h in range(2):
                            ob = osb[h]
                            nz.normalize(pso[h][0], f"pso{h}_0", 512, ob[:], f"osb{h}")
                            r0 = pair * 128 + h * 64
                            S.dma("sp", lambda e, ob=ob, r0=r0: e.dma_start(out=oT[r0:r0 + 64, c * 512:(c + 1) * 512], in_=ob[:]), reads=[f"osb{h}"])
                    un["fin"] = fin
                units.append(un)
        sp_.run(units)
    return kb.finish()


def rel_bucket_np(dist):
    n = np.maximum(dist, 0)
    exact = 16
    logf = np.log(np.maximum(n, 1).astype(np.float32) / exact) / np.float32(np.log(2048 / exact))
    large = np.minimum(exact + (logf * 16).astype(np.int32), 31)
    return np.where(n < exact, n, large)


def moba_bias_tiles(rel_table, heads):
    k = np.arange(128)[:, None]
    q = np.arange(512)[None, :]
    out = np.empty((len(heads), NOFF, 128, 512), np.float32)
    for oi in range(NOFF):
        o = oi - 3
        dist = o * 128 + q - k
        bk = rel_bucket_np(dist)
        for i, h in enumerate(heads):
            out[i, oi] = np.where(dist >= 0, rel_table[bk, h], np.float32(NEG))
    return out


DIL = ((128, 1), (512, 4), (2048, 16))


def build_dil(Sq):
    kb = KB()
    S = kb.S
    NT, NCH = Sq // 128, Sq // 512
    xTg = [kb.din(f"xT{g}", [D, Sq]) for g in range(3)]
    wA = kb.din("wA", [3, 4, D, 192])
    btA = kb.din("btA", [3, 4, 128, 256])
    oT = kb.dout("oT", [256, Sq], BF16)

    stg = kb.sb("stg", [128, 8 * 192], F32)
    identf = kb.sb("identf", [128, 128], F32)
    identb = kb.sb("identb", [128, 128], BF16)
    onesf = kb.sb("onesf", [128, 128], F32)
    S.op("pool", lambda e: e.memset(onesf[:], 1.0), writes=["onesf"])
    S.op("pool", lambda e: e.affine_select(out=identf[:], in_=onesf[:], pattern=[[-1, 128]], compare_op=ALU.is_equal,
                                           fill=0.0, base=0, channel_multiplier=1), reads=["onesf"], writes=["identf"])
    S.op("pool", lambda e: e.tensor_copy(out=identb[:], in_=identf[:]), reads=["identf"], writes=["identb"])
    QT = kb.sb("QT", [64, Sq], BF16)
    KT = kb.sb("KT", [64, Sq], BF16)
    V = kb.sb("V", [128, NT, 65], BF16)
    S.op("pool", lambda e: e.memset(V[:, :, 64:65], 1.0), writes=["V"])
    accS = kb.sb("accS", [65, Sq], F32)
    wab = kb.sb("wab", [128, 8, 192], BF16)
    btf = kb.sb("btf", [128, 256], F32)
    btb = kb.sb("btb", [128, 256], BF16)
    xs = [kb.sb(f"xs{i}", [128, 8, 512], F32) for i in range(1)]
    xb = [kb.sb(f"xb{i}", [128, 8, 512], BF16) for i in range(2)]
    osb = [kb.sb(f"osb{i}", [64, 512], BF16) for i in range(2)]
    sp_ = SoftmaxPipe(kb)
    nz = Normalizer(kb)
    pso = [kb.ps(f"pso{i}") for i in range(2)]
    pss = [(sp_.psS[0], "psS0"), (sp_.psS[1], "psS1"), (sp_.psS[2], "psS2")]

    for hh in range(4):
        for g, (win, d) in enumerate(DIL):
            L = Sq // d
            Lt = L // 128
            S.dma("sp", lambda e, g=g, hh=hh: e.dma_start(out=stg[:].rearrange("p (k n) -> p k n", k=8),
                                                         in_=wA[g, hh].rearrange("(k p) n -> p k n", p=128)), writes=["stg"])
            S.op("pool", lambda e: e.tensor_copy(out=wab[:].rearrange("p k n -> p (k n)"), in_=stg[:]), reads=["stg"], writes=["wab"])
            S.dma("sp", lambda e, g=g, hh=hh: e.dma_start(out=btf[:], in_=btA[g, hh]), writes=["btf"])
            S.op("pool", lambda e: e.tensor_copy(out=btb[:], in_=btf[:]), reads=["btf"], writes=["btb"])
            fm = [(wab[:, :, 0:64], "wab", 64, QT, "QT", SCALE, "act"), (wab[:, :, 64:128], "wab", 64, KT, "KT", 1.0, "dve")]
            tm = [(wab[:, :, 128:192], "wab", 64, (lambda t: [(V[:, t, 0:64], 0, 64)]), "V")]
            project(kb, xTg[g], Sq, fm, tm, pss, xs, xb)
            accv = accS[:].rearrange("p (i r) -> p r i", r=d)
            units = []
            for T in range(NT):
                r, jl = T // Lt, T % Lt
                has_next = jl + 1 < Lt
                N = 256 if has_next else 128
                mm = [(KT[:, T * 128:(T + 1) * 128], QT[:, T * 128:T * 128 + N], ["KT", "QT"]),
                      (identb[:], btb[:, 0:N], ["identb", "btb"])]
                pv = [(V[:, T, :], ["V"], pso[T % 2][0:65, 0:128], f"pso{T % 2}", jl == 0, True, (0, 128))]
                if has_next:
                    pv.append((V[:, T, :], ["V"], pso[(T + 1) % 2][0:65, 0:128], f"pso{(T + 1) % 2}", True, False, (128, 256)))

                def fin(T=T, r=r, jl=jl, g=g, accv=accv):
                    dst = accv[:, r, jl * 128:(jl + 1) * 128]
                    acc, acck = pso[T % 2], f"pso{T % 2}"
                    if g == 0:
                        S.op("dve", lambda e: e.tensor_copy(out=dst, in_=acc[0:65, 0:128]), reads=[acck], writes=["accS"])
                    else:
                        S.op("dve", lambda e: e.tensor_tensor(out=dst, in0=dst, in1=acc[0:65, 0:128], op=ALU.add),
                             reads=[acck, "accS"], writes=["accS"])
                units.append(dict(mm=mm, N=N, kp=128, pv=pv, fin=fin))
            sp_.run(units)
        for c in range(NCH):
            ob = osb[c % 2]
            cs = slice(c * 512, (c + 1) * 512)
            nz.bcast_recip(accS[:, cs], "accS", 512)
            S.op("dve", lambda e, ob=ob, cs=cs: e.tensor_tensor(out=ob[:], in0=accS[0:64, cs], in1=nz.psB[0:64, :], op=ALU.mult),
                 reads=["accS", "psB"], writes=[f"osb{c % 2}"])
            S.dma("sp", lambda e, ob=ob, cs=cs, hh=hh: e.dma_start(out=oT[hh * 64:(hh + 1) * 64, cs], in_=ob[:]), reads=[f"osb{c % 2}"])
    return kb.finish()


def dil_bias_tiles(rel_table, heads):
    k = np.arange(128)[:, None]
    q = np.arange(256)[None, :]
    steps = q - k
    out = np.empty((3, len(heads), 128, 256), np.float32)
    for g, (win, d) in enumerate(DIL):
        span = win // d
        ok = (steps >= 0) & (steps <= span)
        bk = rel_bucket_np(steps * d)
        for i, h in enumerate(heads):
            out[g, i] = np.where(ok, rel_table[bk, h], np.float32(NEG))
    return out


def dil_perm(Sq, d):
    L = Sq // d
    return (np.arange(d)[:, None] + d * np.arange(L)[None, :]).reshape(-1)


def nsa_consts(Sq):
    NSEL = Sq // 64
    n_cmp = Sq // 16 - 1
    NCT = (n_cmp + 127) // 128
    c = np.arange(NCT * 128)[:, None]
    j = np.arange(NSEL)[None, :]
    ov = ((c >= 4 * j - 1) & (c <= 4 * j + 3) & (c < n_cmp)).astype(np.float32)
    return ov


def nsa_bias_strips(rel_table, heads):
    k = np.arange(128)[:, None]
    H = len(heads)
    def strip(dist, ok):
        bk = rel_bucket_np(dist)
        out = np.empty((H,) + dist.shape, np.float32)
        for i, h in enumerate(heads):
            out[i] = np.where(ok, rel_table[bk, h], np.float32(NEG))
        return out
    ds = np.arange(2432)[None, :] - 384 - k
    dw = np.arange(1408)[None, :] - 384 - k
    dc = np.arange(3584)[None, :] - 16 * k - 31
    return strip(ds, ds >= 0), strip(dw, (dw >= 0) & (dw <= 511)), strip(dc, dc >= 0)


def build_nsa(Sq):
    kb = KB()
    S = kb.S
    NT, NCH, NSEL = Sq // 128, Sq // 512, Sq // 64
    n_cmp = Sq // 16 - 1
    NCT = (n_cmp + 127) // 128
    NCC = NCT * 128
    NH = (NSEL + 127) // 128
    NSP = min(NSEL, 128)
    SW = max(NSEL, 8)
    xT = kb.din("xT", [D, Sq])
    wq = kb.din("wq", [D, 256])
    wkv = kb.din("wkv", [D, 512])
    wgp = kb.din("wgp", [D, 76])
    w1 = kb.din("w1", [128, 32, 256])
    posT = kb.din("posT", [128, 32])
    w2 = kb.din("w2", [128, 2, 128])
    ovm = kb.din("ovm", [NCC, NSEL])
    bss = kb.din("bss", [4, 128, 2432])
    bsw = kb.din("bsw", [4, 128, 1408])
    bsc = kb.din("bsc", [4, 128, 3584])
    cfar = kb.din("cfar", [128, 4])
    oT = kb.dout("oT", [256, Sq], BF16)

    identf = kb.sb("identf", [128, 128], F32)
    identb = kb.sb("identb", [128, 128], BF16)
    onesf = kb.sb("onesf", [128, 128], F32)
    S.op("pool", lambda e: e.memset(onesf[:], 1.0), writes=["onesf"])
    S.op("pool", lambda e: e.affine_select(out=identf[:], in_=onesf[:], pattern=[[-1, 128]], compare_op=ALU.is_equal,
                                           fill=0.0, base=0, channel_multiplier=1), reads=["onesf"], writes=["identf"])
    S.op("pool", lambda e: e.tensor_copy(out=identb[:], in_=identf[:]), reads=["identf"], writes=["identb"])
    cf = kb.sb("cf", [128, 4], F32)
    S.dma("sp", lambda e: e.dma_start(out=cf[:], in_=cfar), writes=["cf"])
    selgb = kb.sb("selgb", [128, 12 * 64], BF16)
    stg = kb.sb("stg", [128, 8 * 512], F32)
    zerob = kb.sb("zerob", [128, 128], BF16)
    S.op("pool", lambda e: e.memset(zerob[:], 0.0), writes=["zerob"])
    wqb = load_w_bf16(kb, "wqb", wq, 256, stg, "stg")
    wgb = load_w_bf16(kb, "wgb", wgp, 76, stg, "stg")
    w2b = kb.sb("w2b", [128, 2, 128], BF16)
    S.dma("sp", lambda e: e.dma_start(out=stg[:, 0:256].rearrange("p (a b) -> p a b", a=2), in_=w2), writes=["stg"])
    S.op("pool", lambda e: e.tensor_copy(out=w2b[:].rearrange("p a b -> p (a b)"), in_=stg[:, 0:256]), reads=["stg"], writes=["w2b"])
    ovb = kb.sb("ovb", [128, NCT, NSEL], BF16)
    for ct in range(NCT):
        S.dma("sp", lambda e, ct=ct: e.dma_start(out=stg[:, 0:NSEL], in_=ovm[ct * 128:(ct + 1) * 128, :]), writes=["stg"])
        S.op("pool", lambda e, ct=ct: e.tensor_copy(out=ovb[:, ct, :], in_=stg[:, 0:NSEL]), reads=["stg"], writes=["ovb"])

    KKs = kb.sb("KKs", [128, Sq // 2], BF16)
    KKw = kb.sb("KKw", [128, Sq // 2], BF16)
    Vs = kb.sb("Vs", [128, NT, 76], BF16)
    Vw = kb.sb("Vw", [128, NT, 76], BF16)
    vc = kb.sb("vc", [128, NCT, 76], BF16)
    kcT = kb.sb("kcT", [64, NCC], BF16)
    for t_, k_ in ((Vs, "Vs"), (Vw, "Vw"), (vc, "vc")):
        S.op("pool", lambda e, t_=t_: e.memset(t_[:, :, 64:76], 1.0), writes=[k_])
    xs = [(stg[:].rearrange("p (k t) -> p k t", k=8), "stg")]
    xb0 = kb.sb("xb0", [128, 8, 512], BF16)
    xb = [(xb0, "xb0")]
    sp_ = SoftmaxPairPipe(kb, nU=2, nP=2)
    pss = [(sp_.psS[0][0], "psS0_0"), (sp_.psS[0][1], "psS0_1"), (sp_.psS[1][0], "psS1_0"), (sp_.psS[1][1], "psS1_1")]
    accs = [kb.ps("acc0")]
    psB = kb.ps("psB")
    psI = [kb.ps(f"psI{i}") for i in range(2)]
    psR = psB

    with contextlib.ExitStack() as st0:
        selg = kb.sb("selg", [128, 12 * 64], F32, st0)
        S.op("pool", lambda e: e.memset(stg[:, 0:768], 1.0), writes=["stg"])
        S.op("pool", lambda e: e.affine_select(out=selg[:].rearrange("p (i m) -> p i m", m=64), in_=stg[:, 0:768].rearrange("p (i m) -> p i m", m=64),
                                               pattern=[[-1, 12], [0, 64]], compare_op=ALU.is_equal, fill=0.0, base=-64,
                                               channel_multiplier=1), reads=["stg"], writes=["selg"])
        S.op("pool", lambda e: e.tensor_copy(out=selgb[:], in_=selg[:]), reads=["selg"], writes=["selgb"])
        KVc = kb.sb("KVc", [128, Sq], BF16, st0)
        wkvb = kb.sb("wkvb", [128, 8, 512], BF16, st0)
        S.dma("sp", lambda e: e.dma_start(out=stg[:, 0:4096].rearrange("p (k n) -> p k n", k=8), in_=wkv.rearrange("(k p) n -> p k n", p=128)), writes=["stg"])
        S.op("pool", lambda e: e.tensor_copy(out=wkvb[:].rearrange("p k n -> p (k n)"), in_=stg[:, 0:4096]), reads=["stg"], writes=["wkvb"])
        w1b = kb.sb("w1b", [128, 32, 256], BF16, st0)
        hT = kb.sb("hT", [128, 2, 2, NCC], BF16, st0)
        posb = kb.sb("posb", [128, 32], BF16, st0)
        pbias = kb.sb("pbias", [128, 4], F32, st0)
        gx = kb.sb("gx", [128, 512], F32, st0)
        gu = kb.sb("gu", [128, 512], F32, st0)
        for q4 in range(4):
            S.dma("sp", lambda e, q4=q4: e.dma_start(out=stg[:, 0:2048].rearrange("p (a b) -> p a b", a=8), in_=w1[:, q4 * 8:(q4 + 1) * 8, :]),
                  writes=["stg"])
            S.op("pool", lambda e, q4=q4: e.tensor_copy(out=w1b[:, q4 * 8:(q4 + 1) * 8, :].rearrange("p a b -> p (a b)"), in_=stg[:, 0:2048]),
                 reads=["stg"], writes=["w1b"])
        S.dma("sp", lambda e: e.dma_start(out=stg[:, 0:32], in_=posT), writes=["stg"])
        S.op("pool", lambda e: e.tensor_copy(out=posb[:], in_=stg[:, 0:32]), reads=["stg"], writes=["posb"])
        S.op("pool", lambda e: e.memset(hT[:], 0.0), writes=["hT"])
        S.op("pool", lambda e: e.memset(kcT[:], 0.0), writes=["kcT"])
        def scatter(dst, dkey, eng):
            def ev(p, pk, c):
                for tt in range(4):
                    rows = slice(0, 64) if tt % 2 == 0 else slice(64, 128)
                    m = (4 * c + tt) // 2
                    if eng == "act":
                        S.op("act", lambda e, rows=rows, m=m, tt=tt, p=p: e.copy(out=dst[rows, m * 128:(m + 1) * 128], in_=p[rows, tt * 128:(tt + 1) * 128]),
                             reads=[pk], writes=[dkey])
                    else:
                        S.op("dve", lambda e, rows=rows, m=m, tt=tt, p=p: e.tensor_copy(out=dst[rows, m * 128:(m + 1) * 128], in_=p[rows, tt * 128:(tt + 1) * 128]),
                             reads=[pk], writes=[dkey])
            return ev
        fm = [(wkvb[:, :, 0:128], "wkvb", 128, KVc, "KVc", 1.0, "act"),
              (wkvb[:, :, 128:256], "wkvb", 128, KKs, "KKs", 1.0, scatter(KKs, "KKs", "dve")),
              (wkvb[:, :, 256:384], "wkvb", 128, KKw, "KKw", 1.0, scatter(KKw, "KKw", "act"))]
        tm = [(wkvb[:, :, 384:512], "wkvb", 128, (lambda t: [(Vs[:, t, 0:64], 0, 64), (Vw[:, t, 0:64], 64, 128)]), "V")]
        project(kb, xT, Sq, fm, tm, pss + [(accs[0], "acc0")], xs, xb)
        for kv in range(2):
            rows = slice(kv * 64, (kv + 1) * 64)
            for hc in range(2):
                for p in range(32):
                    S.op("pe", lambda e, rows=rows, hc=hc, p=p, kv=kv: e.matmul(
                        psR[:, kv * 2 + hc:kv * 2 + hc + 1], lhsT=w1b[rows, p, hc * 128:(hc + 1) * 128], rhs=posb[rows, p:p + 1],
                        start=(p == 0), stop=(p == 31)), reads=["w1b", "posb"], writes=["psB"])
        S.op("act", lambda e: e.copy(out=pbias[:], in_=psR[:, 0:4]), reads=["psB"], writes=["pbias"])
        ccs = [(c0, min(512, n_cmp - c0)) for c0 in range(0, n_cmp, 512)]
        KVv = KVc[:].rearrange("p (c s) -> p s c", s=16)
        ui = 0
        for kv in range(2):
            rows = slice(kv * 64, (kv + 1) * 64)
            for hc in range(2):
                for (c0, cn) in ccs:
                    ps_, pk_ = pss[ui % 2]
                    ui += 1
                    for p in range(32):
                        S.op("pe", lambda e, ps_=ps_, rows=rows, hc=hc, p=p, c0=c0, cn=cn: e.matmul(
                            ps_[:, 0:cn], lhsT=w1b[rows, p, hc * 128:(hc + 1) * 128],
                            rhs=KVv[rows, p % 16, c0 + p // 16:c0 + p // 16 + cn], start=(p == 0), stop=(p == 31)),
                            reads=["w1b", "KVc"], writes=[pk_])
                    col = kv * 2 + hc
                    S.op("act", lambda e, ps_=ps_, cn=cn, col=col: e.activation(out=gx[:, 0:cn], in_=ps_[:, 0:cn], func=AF.Identity,
                                                                                bias=pbias[:, col:col + 1]), reads=[pk_, "pbias"], writes=["gx"])
                    S.op("dve", lambda e, cn=cn: e.tensor_tensor(out=gu[:, 0:cn], in0=gx[:, 0:cn], in1=gx[:, 0:cn], op=ALU.mult), reads=["gx"], writes=["gu"])
                    S.op("dve", lambda e, cn=cn: e.tensor_scalar(out=gu[:, 0:cn], in0=gu[:, 0:cn], scalar1=0.044715, scalar2=1.0, op0=ALU.mult, op1=ALU.add),
                         reads=["gu"], writes=["gu"])
                    S.op("dve", lambda e, cn=cn: e.tensor_tensor(out=gu[:, 0:cn], in0=gu[:, 0:cn], in1=gx[:, 0:cn], op=ALU.mult), reads=["gu", "gx"], writes=["gu"])
                    S.op("act", lambda e, cn=cn: e.activation(out=gu[:, 0:cn], in_=gu[:, 0:cn], func=AF.Tanh, scale=0.7978845608028654), reads=["gu"], writes=["gu"])
                    S.op("dve", lambda e, cn=cn: e.tensor_scalar(out=gu[:, 0:cn], in0=gu[:, 0:cn], scalar1=1.0, scalar2=0.5, op0=ALU.add, op1=ALU.mult),
                         reads=["gu"], writes=["gu"])
                    S.op("dve", lambda e, cn=cn, kv=kv, hc=hc, c0=c0: e.tensor_tensor(out=hT[:, kv, hc, c0:c0 + cn], in0=gu[:, 0:cn], in1=gx[:, 0:cn], op=ALU.mult),
                         reads=["gu", "gx"], writes=["hT"])
        for c0 in range(0, NCC, 512):
            cn = min(512, NCC - c0)
            ps_, pk_ = pss[ui % 2]
            ui += 1
            for hc in range(2):
                S.op("pe", lambda e, ps_=ps_, hc=hc, c0=c0, cn=cn: e.matmul(ps_[0:64, 0:cn], lhsT=w2b[:, hc, 0:64], rhs=hT[:, 0, hc, c0:c0 + cn],
                                                                            start=(hc == 0), stop=(hc == 1)), reads=["w2b", "hT"], writes=[pk_])
            S.op("act", lambda e, ps_=ps_, c0=c0, cn=cn: e.copy(out=kcT[:, c0:c0 + cn], in_=ps_[0:64, 0:cn]), reads=[pk_], writes=["kcT"])
        for ct in range(NCT):
            ps_, pk_ = pss[ui % 2]
            ui += 1
            for hc in range(2):
                S.op("pe", lambda e, ps_=ps_, hc=hc, ct=ct: e.matmul(ps_[:, 0:64], lhsT=hT[:, 1, hc, ct * 128:(ct + 1) * 128], rhs=w2b[:, hc, 64:128],
                                                                     start=(hc == 0), stop=(hc == 1)), reads=["w2b", "hT"], writes=[pk_])
            S.op("dve", lambda e, ps_=ps_, ct=ct: e.tensor_copy(out=vc[:, ct, 0:64], in_=ps_[:, 0:64]), reads=[pk_], writes=["vc"])

    S.fence()
    strips = {}
    for nm, src, W in (("bs_s", bss, 2432), ("bs_w", bsw, 1408), ("bs_c", bsc, 3584)):
        t_ = kb.sb(nm, [128, 4, W], BF16)
        strips[nm] = t_
        for r in range(4):
            for w0 in range(0, W, 2048):
                wn = min(2048, W - w0)
                S.dma("sp", lambda e, src=src, r=r, w0=w0, wn=wn: e.dma_start(out=stg[:, 0:wn], in_=src[r, :, w0:w0 + wn]), writes=["stg"])
                S.op("pool", lambda e, t_=t_, r=r, w0=w0, wn=wn: e.tensor_copy(out=t_[:, r, w0:w0 + wn], in_=stg[:, 0:wn]), reads=["stg"], writes=[nm])
    bs_s, bs_w, bs_c = strips["bs_s"], strips["bs_w"], strips["bs_c"]
    Qc = [kb.sb(f"Qc{i}", [128, 512], BF16) for i in range(4)]
    ng_ = kb.sb("ng_", [128, 512], F32)
    tf_ = kb.sb("tf_", [128, 512], F32)
    gsb, fgd = ng_, tf_
    fgdb = kb.sb("fgdb", [128, 512], BF16)
    numt, tmpc = ng_, tf_
    res = [kb.sb(f"res{r}", [64, 512], F32) for r in range(4)]
    osb0 = kb.sb("osb0", [64, 512], BF16)
    osb = [osb0, osb0]
    impsum = [kb.sb(f"imps{t}", [128, SW], F32) for t in range(4)]
    sc2 = kb.sb("sc2", [128, SW], F32)
    mbias = kb.sb("mbias", [128, SW], F32)
    m8a = kb.sb("m8a", [128, 8], F32)
    m8b = kb.sb("m8b", [128, 8], F32)
    thr = kb.sb("thr", [128, 1], F32)
    rdcol = kb.sb("rdcol", [128, 4], F32)
    maskT = kb.sb("maskT", [128, NH, 512], BF16)
    xv = xT.rearrange("(k p) t -> p k t", p=128)

    def qsel(r):
        return [Qc[0][0:64, :], Qc[2][0:64, :], Qc[1][0:64, :], Qc[3][0:64, :]][r], ["Qc0", "Qc2", "Qc1", "Qc3"][r]

    def qwin(r):
        return [Qc[2][64:128, :], Qc[0][64:128, :], Qc[3][64:128, :], Qc[1][64:128, :]][r], ["Qc2", "Qc0", "Qc3", "Qc1"][r]

    acc_i = [0]

    def finalize(acc, acck, br, r, first, last, c):
        i = br * 4 + r
        S.op("dve", lambda e: e.tensor_scalar(out=fgd[64:76, :], in0=acc[64:76, :], scalar1=1e-18, scalar2=None, op0=ALU.max),
             reads=[acck], writes=["fgd"])
        S.op("act", lambda e: e.activation(out=fgd[64:76, :], in_=fgd[64:76, :], func=AF.Ln), reads=["fgd"], writes=["fgd"])
        S.op("act", lambda e: e.activation(out=fgd[64:76, :], in_=fgd[64:76, :], func=AF.Exp, scale=-1.0), reads=["fgd"], writes=["fgd"])
        if br == 0:
            for tt in range(4):
                S.op("pe", lambda e, tt=tt: e.matmul(psR[:, tt:tt + 1], lhsT=fgd[64:65, tt * 128:(tt + 1) * 128], rhs=onesf[64:65, 0:1],
                                                     start=True, stop=True), reads=["fgd", "onesf"], writes=["psB"])
            S.op("act", lambda e: e.copy(out=rdcol[:], in_=psR[:, 0:4]), reads=["psB"], writes=["rdcol"])
        S.op("dve", lambda e: e.tensor_tensor(out=fgdb[64:76, :], in0=fgd[64:76, :], in1=gsb[64:76, :], op=ALU.mult),
             reads=["fgd", "gsb"], writes=["fgdb"])
        S.op("pe", lambda e: e.matmul(psB[0:64, :], lhsT=selgb[64:76, i * 64:(i + 1) * 64], rhs=fgdb[64:76, :], start=True, stop=True),
             reads=["fgdb", "selgb"], writes=["psB"])
        S.op("act", lambda e: e.copy(out=numt[0:64, :], in_=acc[0:64, :]), reads=[acck], writes=["numt"])
        if first:
            S.op("dve", lambda e: e.tensor_tensor(out=res[r][:], in0=numt[0:64, :], in1=psB[0:64, :], op=ALU.mult),
                 reads=["numt", "psB"], writes=[f"res{r}"])
        else:
            S.op("dve", lambda e: e.tensor_tensor(out=tmpc[0:64, :], in0=numt[0:64, :], in1=psB[0:64, :], op=ALU.mult),
                 reads=["numt", "psB"], writes=["tmpc"])
            if not last:
                S.op("pool", lambda e: e.tensor_tensor(out=res[r][:], in0=res[r][:], in1=tmpc[0:64, :], op=ALU.add),
                     reads=["tmpc", f"res{r}"], writes=[f"res{r}"])
            else:
                ob = osb[r % 2]
                S.op("pool", lambda e: e.tensor_tensor(out=ob[:], in0=res[r][:], in1=tmpc[0:64, :], op=ALU.add),
                     reads=["tmpc", f"res{r}"], writes=["osb0"])
                S.dma("sp", lambda e: e.dma_start(out=oT[r * 64:(r + 1) * 64, c * 512:(c + 1) * 512], in_=ob[:]), reads=["osb0"])

    for c in range(NCH):
        qs = slice(c * 512, (c + 1) * 512)
        xbi, xbk = xb0, "xb0"
        S.dma("sp", lambda e, c=c: e.dma_start(out=xs[0][0], in_=xv[:, :, c * 512:(c + 1) * 512]), writes=["stg"])
        S.op("dve", lambda e, xbi=xbi: e.tensor_copy(out=xbi[:, 0:4, :], in_=xs[0][0][:, 0:4, :]), reads=["stg"], writes=[xbk])
        S.op("act", lambda e, xbi=xbi: e.copy(out=xbi[:, 4:8, :], in_=xs[0][0][:, 4:8, :]), reads=["stg"], writes=[xbk])
        for qi in range(4):
            ps_, pk_ = pss[qi % 4]
            base = (qi % 2) * 128
            if qi < 2:
                for k in range(8):
                    S.op("pe", lambda e, ps_=ps_, base=base, k=k, xbi=xbi: e.matmul(ps_[:], lhsT=wqb[:, k, base:base + 128], rhs=xbi[:, k, :],
                                                                                  start=(k == 0), stop=(k == 7)), reads=["wqb", xbk], writes=[pk_])
            else:
                for half in range(2):
                    cols = base + 64 * (1 - half)
                    for k in range(8):
                        S.op("pe", lambda e, ps_=ps_, cols=cols, half=half, k=k, xbi=xbi: e.matmul(
                            ps_[half * 64:(half + 1) * 64, :], lhsT=wqb[:, k, cols:cols + 64], rhs=xbi[:, k, :],
                            start=(k == 0), stop=(k == 7)), reads=["wqb", xbk], writes=[pk_])
            S.op("act", lambda e, ps_=ps_, qi=qi: e.mul(Qc[qi][:], ps_[:], SCALE), reads=[pk_], writes=[f"Qc{qi}"])
        for k in range(8):
            S.op("pe", lambda e, k=k, xbi=xbi: e.matmul(psB[0:76, :], lhsT=wgb[:, k, 0:76], rhs=xbi[:, k, :], start=(k == 0), stop=(k == 7)),
                 reads=["wgb", xbk], writes=["psB"])
        S.op("act", lambda e: e.activation(out=gsb[64:76, :], in_=psB[64:76, :], func=AF.Sigmoid), reads=["psB"], writes=["gsb"])

        ctmax = min(NCT - 1, (32 * c + 30) // 128)
        for r in range(4):
            acc, acck = accs[0], "acc0"
            acc_i[0] += 1
            qa, qk = qsel(r)
            units = []
            for ct in range(ctmax + 1):
                o = c - 4 * ct
                mm = [(kcT[:, ct * 128:(ct + 1) * 128], qa, ["kcT", qk], 0)]
                cb = None
                if o <= 6:
                    mm.append((identb[:], bs_c[:, r, o * 512:(o + 1) * 512], ["identb", "bs_c"], 0))
                else:
                    cb = (cf[:, r:r + 1], "cf")
                un = dict(mm=mm, N=512, cb=[cb, None], slots=(0,), pv=[(vc[:, ct, :], ["vc"], acc[0:76, :], acck, ct == 0, ct == ctmax, 0)])
                un["imp"] = (ct, ctmax)
                units.append(un)
            for bnk in range(2):
                S.op("pe", lambda e, bnk=bnk: e.matmul(psI[bnk][:], lhsT=zerob[:], rhs=bs_c[:, 0, 0:512], start=True, stop=False),
                     reads=["zerob", "bs_c"], writes=[f"psI{bnk}"])
            run_cmp(kb, sp_, units, psI, ovb, NSEL)
            finalize(acc, acck, 0, r, True, False, c)
            for tt in range(4):
                src = psI[tt // 2][:, (tt % 2) * 256:(tt % 2) * 256 + NSEL]
                if r == 0:
                    S.op("dve", lambda e, tt=tt, src=src: e.tensor_scalar(out=impsum[tt][:, 0:NSEL], in0=src, scalar1=rdcol[:, tt:tt + 1], scalar2=None,
                                                                          op0=ALU.mult), reads=[f"psI{tt // 2}", "rdcol"], writes=[f"imps{tt}"])
                else:
                    S.op("dve", lambda e, tt=tt, src=src: e.scalar_tensor_tensor(out=impsum[tt][:, 0:NSEL], in0=src, scalar=rdcol[:, tt:tt + 1],
                                                                                 in1=impsum[tt][:, 0:NSEL], op0=ALU.mult, op1=ALU.add),
                         reads=[f"psI{tt // 2}", "rdcol", f"imps{tt}"], writes=[f"imps{tt}"])
        for tt in range(4):
            qt = 4 * c + tt
            sc = impsum[tt]
            sk = f"imps{tt}"
            if SW > NSEL:
                S.op("pool", lambda e, sc=sc: e.memset(sc[:, NSEL:SW], -1e30), writes=[sk])
            if 2 * qt + 2 < NSEL:
                S.op("pool", lambda e, sc=sc, qt=qt: e.memset(sc[:, 2 * qt + 2:NSEL], -1e30), writes=[sk])
            S.op("pool", lambda e, sc=sc, qt=qt: e.memset(sc[0:64, 2 * qt + 1:2 * qt + 2], -1e30), writes=[sk])
            S.op("pool", lambda e, sc=sc: e.memset(sc[:, 0:1], 1e9), writes=[sk])
            lo = max(2 * qt - 1, 0)
            S.op("pool", lambda e, sc=sc, qt=qt, lo=lo: e.memset(sc[0:64, lo:2 * qt + 1], 1e9), writes=[sk])
            S.op("pool", lambda e, sc=sc, qt=qt: e.memset(sc[64:128, 2 * qt:2 * qt + 2], 1e9), writes=[sk])
            S.op("dve", lambda e, sc=sc: e.max(out=m8a[:], in_=sc[:]), reads=[sk], writes=["m8a"])
            S.op("dve", lambda e, sc=sc: e.match_replace(out=sc2[:], in_to_replace=m8a[:], in_values=sc[:], imm_value=-1e30),
                 reads=[sk, "m8a"], writes=["sc2"])
            S.op("dve", lambda e: e.max(out=m8b[:], in_=sc2[:]), reads=["sc2"], writes=["m8b"])
            S.op("dve", lambda e: e.tensor_scalar(out=thr[:], in0=m8b[:, 7:8], scalar1=-1e29, scalar2=None, op0=ALU.max),
                 reads=["m8b"], writes=["thr"])
            S.op("dve", lambda e, sc=sc: e.tensor_scalar(out=mbias[:], in0=sc[:], scalar1=thr[:, 0:1], scalar2=NEG, op0=ALU.is_lt, op1=ALU.mult),
                 reads=[sk, "thr"], writes=["mbias"])
            for jh in range(NH):
                ps_, pk_ = pss[(tt * NH + jh) % 2]
                S.op("pe", lambda e, ps_=ps_, jh=jh: e.transpose(ps_[0:NSP, 0:128], mbias[:, jh * 128:jh * 128 + NSP], identf[:]),
                     reads=["mbias", "identf"], writes=[pk_])
                S.op("act", lambda e, ps_=ps_, jh=jh, tt=tt: e.copy(out=maskT[0:NSP, jh, tt * 128:(tt + 1) * 128], in_=ps_[0:NSP, 0:128]),
                     reads=[pk_], writes=["maskT"])
        jmax = 4 * c + 3
        for r in range(4):
            acc, acck = accs[0], "acc0"
            (qa0, qk0), (qa1, qk1) = qsel(r), qwin(r)
            units = []
            npair = (jmax + 1) // 2
            for m in range(npair):
                mm, cbs, pv = [], [], []
                mm.append((KKs[0:64, m * 128:(m + 1) * 128], qa0, ["KKs", qk0], 0))
                mm.append((KKs[64:128, m * 128:(m + 1) * 128], qa1, ["KKs", qk1], 1))
                for sl in range(2):
                    j = 2 * m + sl
                    col = 2 * (j % 64)
                    mm.append((identb[0:NSP, col:col + 1].to_broadcast([NSP, 64]), maskT[0:NSP, j // 64, :], ["identb", "maskT"], sl, (0, 64)))
                    mm.append((identb[0:NSP, col + 1:col + 2].to_broadcast([NSP, 64]), maskT[0:NSP, j // 64, :], ["identb", "maskT"], sl, (64, 128)))
                for sl in range(2):
                    j = 2 * m + sl
                    o = 4 * c - j
                    if o <= 12:
                        mm.append((identb[:], bs_s[:, r, (o + 3) * 128:(o + 3) * 128 + 512], ["identb", "bs_s"], sl))
                        cbs.append(None)
                    else:
                        cbs.append((cf[:, r:r + 1], "cf"))
                    pv.append((Vs[:, j, :], ["Vs"], acc[0:76, :], acck, j == 0, j == jmax, sl))
                units.append(dict(mm=mm, N=512, cb=cbs, pv=pv))
            sp_.run(units)
            finalize(acc, acck, 1, r, False, False, c)
        for r in range(4):
            acc, acck = accs[0], "acc0"
            (qa0, qk0), (qa1, qk1) = qsel(r), qwin(r)
            units = []
            j0 = max(0, 4 * c - 4)
            for m in range(j0 // 2, (jmax + 1) // 2):
                mm, pv = [], []
                mm.append((KKw[0:64, m * 128:(m + 1) * 128], qa0, ["KKw", qk0], 0))
                mm.append((KKw[64:128, m * 128:(m + 1) * 128], qa1, ["KKw", qk1], 1))
                for sl in range(2):
                    j = 2 * m + sl
                    o = 4 * c - j
                    mm.append((identb[:], bs_w[:, r, (o + 3) * 128:(o + 3) * 128 + 512], ["identb", "bs_w"], sl))
                    pv.append((Vw[:, j, :], ["Vw"], acc[0:76, :], acck, j == j0, j == jmax, sl))
                units.append(dict(mm=mm, N=512, cb=[None, None], pv=pv))
            sp_.run(units)
            finalize(acc, acck, 2, r, False, True, c)
    return kb.finish()


def run_cmp(kb, sp_, units, psI, ovb, NSEL):
    S = kb.S
    for un in units:
        ct, ctmax = un["imp"]

        def post(PTb, PTk, ct=ct, ctmax=ctmax):
            for tt in range(4):
                S.op("pe", lambda e, tt=tt: e.matmul(psI[tt // 2][:, (tt % 2) * 256:(tt % 2) * 256 + NSEL],
                                                     lhsT=PTb[:, tt * 128:(tt + 1) * 128], rhs=ovb[:, ct, :],
                                                     start=False, stop=(ct == ctmax and tt % 2 == 1)),
                     reads=[PTk, "ovb"], writes=[f"psI{tt // 2}"])
        un["post"] = post
    sp_.run(units)


ALPHA = 8 ** 0.25
LN_EPS = 1e-5
D = 1024
NE = 16
DE = 256


def build_post(T, stage=9):
    nc = bass.Bass("TRN2", target_bir_lowering=False)
    TG = min(1024, T)
    NG = T // TG
    NT = TG // 128
    NCH = TG // 512
    oT = nc.dram_tensor("oT", [D, T], BF16, kind="ExternalInput").ap()
    xin = nc.dram_tensor("xin", [T, D], F32, kind="ExternalInput").ap()
    w_out = nc.dram_tensor("w_out", [D, D], F32, kind="ExternalInput").ap()
    lnp = nc.dram_tensor("lnp", [4, D], F32, kind="ExternalInput").ap()
    rw = nc.dram_tensor("rw", [D, NE], F32, kind="ExternalInput").ap()
    rb = nc.dram_tensor("rb", [1, NE], F32, kind="ExternalInput").ap()
    wg = nc.dram_tensor("wg", [NE, D, DE], F32, kind="ExternalInput").ap()
    wu = nc.dram_tensor("wu", [NE, D, DE], F32, kind="ExternalInput").ap()
    wd = nc.dram_tensor("wd", [NE, DE, D], F32, kind="ExternalInput").ap()
    xout = nc.dram_tensor("xout", [T, D], F32, kind="ExternalOutput").ap()

    S = Sched(nc)
    with contextlib.ExitStack() as st:
        def sb(name, shape, dt):
            return st.enter_context(nc.sbuf_tensor(name, shape, dt))

        def ps(name, shape, dt=F32):
            return st.enter_context(nc.psum_tensor(name, shape, dt))

        ident = sb("ident", [128, 128], F32)
        lnb = sb("lnb", [128, 4, D], F32)
        rbb = sb("rbb", [128, NE], F32)
        rwt = sb("rwt", [128, 8, NE], F32)
        wob = sb("wob", [128, 8, D], BF16)
        stg = [sb(f"stg{i}", [128, 2048], F32) for i in range(3)]
        wb = [[sb(f"wb{j}_{i}", [128, 2048], BF16) for i in range(3)] for j in range(2)]
        x1T = sb("x1T", [128, 8, TG], BF16)
        x1Tf = sb("x1Tf", [128, 8, 128], F32)
        yacc = sb("yacc", [128, NT, D], F32)
        xt = [sb(f"xt{i}", [128, D], F32) for i in range(2)]
        ot = [sb(f"ot{i}", [128, 8, 128], BF16) for i in range(2)]
        r = sb("r", [128, D], F32)
        x1 = sb("x1", [128, D], F32)
        stats = sb("stats", [128, 2, 6], F32)
        mv = sb("mv", [128, 2], F32)
        rstd = sb("rstd", [128, 1], F32)
        G = sb("G", [128, NT, NE], F32)
        rt = [sb(f"rt{i}", [128, NE], F32) for i in range(6)]
        rs = [sb(f"rs{i}", [128, 4], F32) for i in range(4)]
        sg = [sb(f"sg{i}", [128, 512], F32) for i in range(2)]
        aT = [[sb(f"aT{j}_{i}", [128, 512], BF16) for i in range(2)] for j in range(2)]
        outt = [sb(f"outt{i}", [128, D], F32) for i in range(2)]

        psD = [[ps(f"psD{j}_{i}", [128, 512]) for i in range(2)] for j in range(2)]
        psG = [ps(f"psG{i}", [128, 512]) for i in range(2)]
        psU = [ps(f"psU{i}", [128, 512]) for i in range(2)]

        S.op("pool", lambda e: e.memset(ident[:], 0.0), writes=["ident"])
        S.op("pool", lambda e: e.memset(r[:, 0:128], 1.0), writes=["r"])
        S.op("pool", lambda e: e.affine_select(out=ident[:], in_=r[:, 0:128], pattern=[[-1, 128]],
                                               compare_op=ALU.is_equal, fill=0.0, base=0, channel_multiplier=1),
             reads=["r"], writes=["ident"])
        S.dma("sp", lambda e: e.dma_start(out=lnb[:], in_=lnp.partition_broadcast(128)), writes=["lnb"])
        S.dma("sp", lambda e: e.dma_start(out=rbb[:], in_=rb[0, :].partition_broadcast(128)), writes=["rbb"])
        S.dma("sp", lambda e: e.dma_start(out=rwt[:], in_=rw.rearrange("(k p) e -> p k e", p=128)), writes=["rwt"])
        wo_v = w_out.rearrange("(k p) n -> p k n", p=128)
        for i in range(4):
            sname = f"stg{i % 3}"
            S.dma("sp", lambda e, i=i: e.dma_start(out=stg[i % 3][:].rearrange("p (k n) -> p k n", k=2),
                                                   in_=wo_v[:, 2 * i:2 * i + 2, :]), writes=[sname])
            S.op("pool", lambda e, i=i: e.tensor_copy(out=wob[:, 2 * i:2 * i + 2, :].rearrange("p k n -> p (k n)"),
                                                      in_=stg[i % 3][:]), reads=[sname], writes=["wob"])

        xin_v = xin.rearrange("(n p) d -> n p d", p=128)
        xout_v = xout.rearrange("(n p) d -> n p d", p=128)
        oT_v = oT.rearrange("(k p) t -> p k t", p=128)
        wg_v = wg.rearrange("e (k p) f -> e p k f", p=128)
        wu_v = wu.rearrange("e (k p) f -> e p k f", p=128)
        wd_v = wd.rearrange("e (k p) n -> e p k n", p=128)

        def layer_norm(src_ap_fn, src_key, dst_ap, dst_key, gi, tag):
            for h in range(2):
                S.op("dve", lambda e, h=h: e.bn_stats(out=stats[:, h, :], in_=src_ap_fn()[:, h * 512:(h + 1) * 512]),
                     reads=[src_key], writes=["stats"])
            S.op("dve", lambda e: e.bn_aggr(out=mv[:], in_=stats[:].rearrange("p a b -> p (a b)")),
                 reads=["stats"], writes=["mv"])
            S.op("dve", lambda e: e.tensor_scalar(out=rstd[:], in0=mv[:, 1:2], scalar1=LN_EPS, scalar2=None,
                                                  op0=ALU.add), reads=["mv"], writes=["rstd"])
            S.op("act", lambda e: e.sqrt(rstd[:], rstd[:]), reads=["rstd"], writes=["rstd"])
            S.op("dve", lambda e: e.reciprocal(out=rstd[:], in_=rstd[:]), reads=["rstd"], writes=["rstd"])
            S.op("dve", lambda e: e.tensor_scalar(out=dst_ap, in0=src_ap_fn(), scalar1=mv[:, 0:1], scalar2=rstd[:, 0:1],
                                                  op0=ALU.subtract, op1=ALU.mult),
                 reads=[src_key, "mv", "rstd"], writes=[dst_key])
            S.op("pool", lambda e: e.tensor_tensor(out=dst_ap, in0=dst_ap, in1=lnb[:, gi, :], op=ALU.mult),
                 reads=[dst_key, "lnb"], writes=[dst_key])
            S.op("pool", lambda e: e.tensor_tensor(out=dst_ap, in0=dst_ap, in1=lnb[:, gi + 1, :], op=ALU.add),
                 reads=[dst_key, "lnb"], writes=[dst_key])

        wcount = 0
        for g in range(NG if stage >= 1 else 0):
            t0 = g * TG
            for t in range(NT):
                gt = g * NT + t
                xi, oi = xt[gt % 2], ot[gt % 2]
                xk, ok = f"xt{gt % 2}", f"ot{gt % 2}"
                S.dma("sp", lambda e, xi=xi, gt=gt: e.dma_start(out=xi[:], in_=xin_v[gt]), writes=[xk])
                S.dma("sp", lambda e, oi=oi, gt=gt: e.dma_start(out=oi[:], in_=oT_v[:, :, gt * 128:(gt + 1) * 128]),
                      writes=[ok])
                pY = psD[0]
                for h in range(2):
                    for k in range(8):
                        S.op("pe", lambda e, h=h, k=k, oi=oi: e.matmul(pY[h][:], lhsT=oi[:, k, :],
                                                                       rhs=wob[:, k, h * 512:(h + 1) * 512],
                                                                       start=(k == 0), stop=(k == 7)),
                             reads=[ok, "wob"], writes=[f"psD0_{h}"])
                for h in range(2):
                    S.op("dve", lambda e, h=h, xi=xi: e.scalar_tensor_tensor(
                        out=r[:, h * 512:(h + 1) * 512], in0=xi[:, h * 512:(h + 1) * 512], scalar=ALPHA,
                        in1=pY[h][:], op0=ALU.mult, op1=ALU.add), reads=[xk, f"psD0_{h}"], writes=["r"])
                if stage < 1.2: continue
                layer_norm(lambda: r[:], "r", x1[:], "x1", 0, "ln1")
                S.op("act", lambda e, t=t: e.mul(yacc[:, t, :], x1[:], ALPHA), reads=["x1"], writes=[f"yacc{t}"])
                if stage < 1.3: continue
                for k in range(8):
                    S.op("pe", lambda e, k=k: e.transpose(psD[1][k // 4][:, (k % 4) * 128:(k % 4 + 1) * 128],
                                                          x1[:, k * 128:(k + 1) * 128], ident[:]),
                         reads=["x1", "ident"], writes=[f"psD1_{k // 4}"])
                for h in range(2 if stage >= 1.32 else 0):
                    S.op("act", lambda e, h=h: e.copy(out=x1Tf[:, 4 * h:4 * h + 4, :].rearrange("p k t -> p (k t)"),
                                                      in_=psD[1][h][:]), reads=[f"psD1_{h}"], writes=["x1Tf"])
                    if stage < 1.33: continue
                    S.op("pool", lambda e, h=h, t=t: e.tensor_copy(
                        out=x1T[:, 4 * h:4 * h + 4, t * 128:(t + 1) * 128],
                        in_=x1Tf[:, 4 * h:4 * h + 4, :]), reads=["x1Tf"], writes=["x1T"])
                if stage < 1.4: continue
                for k in range(8):
                    S.op("pe", lambda e, k=k: e.matmul(psG[0][:, 0:NE], lhsT=x1Tf[:, k, :], rhs=rwt[:, k, :],
                                                       start=(k == 0), stop=(k == 7)),
                         reads=["x1Tf", "rwt"], writes=["psG0"])
                if stage < 1.5: continue
                sc, bi, eq, b2, sel, ws = rt
                m1, m2, gs, gsel = rs
                S.op("act", lambda e: e.activation(out=sc[:], in_=psG[0][:, 0:NE], func=AF.Sigmoid),
                     reads=["psG0"], writes=["sc"])
                S.op("dve", lambda e: e.tensor_tensor(out=bi[:], in0=sc[:], in1=rbb[:], op=ALU.add),
                     reads=["sc", "rbb"], writes=["bi"])
                v3 = lambda a: a[:].rearrange("p (g j) -> p g j", g=4)
                S.op("dve", lambda e: e.tensor_reduce(out=m1[:], in_=v3(bi), axis=AX.X, op=ALU.max),
                     reads=["bi"], writes=["m1"])
                S.op("dve", lambda e: e.tensor_tensor(out=v3(eq), in0=v3(bi), in1=m1[:].unsqueeze(2).to_broadcast([128, 4, 4]),
                                                      op=ALU.is_equal), reads=["bi", "m1"], writes=["eq"])
                S.op("dve", lambda e: e.scalar_tensor_tensor(out=b2[:], in0=eq[:], scalar=-1e9, in1=bi[:],
                                                             op0=ALU.mult, op1=ALU.add), reads=["eq", "bi"], writes=["b2"])
                S.op("dve", lambda e: e.tensor_reduce(out=m2[:], in_=v3(b2), axis=AX.X, op=ALU.max),
                     reads=["b2"], writes=["m2"])
                S.op("dve", lambda e: e.tensor_tensor(out=gs[:], in0=m1[:], in1=m2[:], op=ALU.add),
                     reads=["m1", "m2"], writes=["gs"])
                S.op("dve", lambda e: e.tensor_reduce(out=rstd[:], in_=gs[:], axis=AX.X, op=ALU.max),
                     reads=["gs"], writes=["rstd"])
                S.op("dve", lambda e: e.tensor_scalar(out=gsel[:], in0=gs[:], scalar1=rstd[:, 0:1], scalar2=None,
                                                      op0=ALU.is_ge), reads=["gs", "rstd"], writes=["gsel"])
                S.op("dve", lambda e: e.tensor_tensor(out=v3(sel), in0=v3(bi), in1=m2[:].unsqueeze(2).to_broadcast([128, 4, 4]),
                                                      op=ALU.is_ge), reads=["bi", "m2"], writes=["sel"])
                S.op("dve", lambda e: e.tensor_tensor(out=v3(sel), in0=v3(sel), in1=gsel[:].unsqueeze(2).to_broadcast([128, 4, 4]),
                                                      op=ALU.mult), reads=["sel", "gsel"], writes=["sel"])
                S.op("dve", lambda e: e.tensor_tensor(out=ws[:], in0=sel[:], in1=sc[:], op=ALU.mult),
                     reads=["sel", "sc"], writes=["ws"])
                S.op("dve", lambda e: e.tensor_reduce(out=rstd[:], in_=ws[:], axis=AX.X, op=ALU.add),
                     reads=["ws"], writes=["rstd"])
                S.op("dve", lambda e: e.reciprocal(out=rstd[:], in_=rstd[:]), reads=["rstd"], writes=["rstd"])
                S.op("dve", lambda e, t=t: e.tensor_scalar(out=G[:, t, :], in0=ws[:], scalar1=rstd[:, 0:1], scalar2=None,
                                                           op0=ALU.mult), reads=["ws", "rstd"], writes=[f"G{t}"])
            for ex in range(NE if stage >= 2 else 0):
                wset = wb[wcount % 2]
                wk = [f"wb{wcount % 2}_{i}" for i in range(3)]
                wcount += 1
                srcs = [wg_v[ex], wu_v[ex], wd_v[ex]]
                for i in range(3):
                    kk = 8 if i < 2 else 2
                    S.dma("sp", lambda e, i=i, kk=kk, src=srcs[i]: e.dma_start(
                        out=stg[i][:].rearrange("p (k n) -> p k n", k=kk), in_=src), writes=[f"stg{i}"])
                    if i < 2:
                        S.op("act", lambda e, i=i, wset=wset: e.copy(out=wset[i][:], in_=stg[i][:]),
                             reads=[f"stg{i}"], writes=[wk[i]])
                    else:
                        S.op("dve", lambda e, i=i, wset=wset: e.tensor_copy(out=wset[i][:], in_=stg[i][:]),
                             reads=[f"stg{i}"], writes=[wk[i]])
                wgb = wset[0][:].rearrange("p (k f) -> p k f", k=8)
                wub = wset[1][:].rearrange("p (k f) -> p k f", k=8)
                wdb = wset[2][:].rearrange("p (k n) -> p k n", k=2)
                for c in range(NCH):
                    pi = (ex * NCH + c) % 2
                    for fh in range(2):
                        for (pt, pk, wv, wkey) in ((psG[fh], f"psG{fh}", wgb, wk[0]), (psU[fh], f"psU{fh}", wub, wk[1])):
                            for k in range(8):
                                S.op("pe", lambda e, pt=pt, wv=wv, k=k, fh=fh, c=c: e.matmul(
                                    pt[:], lhsT=wv[:, k, fh * 128:(fh + 1) * 128], rhs=x1T[:, k, c * 512:(c + 1) * 512],
                                    start=(k == 0), stop=(k == 7)), reads=[wkey, "x1T"], writes=[pk])
                        S.op("act", lambda e, fh=fh: e.activation(out=sg[fh][:], in_=psG[fh][:], func=AF.Silu),
                             reads=[f"psG{fh}"], writes=[f"sg{fh}"])
                        S.op("dve", lambda e, fh=fh, pi=pi: e.tensor_tensor(out=aT[pi][fh][:], in0=sg[fh][:], in1=psU[fh][:],
                                                                           op=ALU.mult),
                             reads=[f"sg{fh}", f"psU{fh}"], writes=[f"aT{pi}_{fh}"])
                    for tt in range(4):
                        t = c * 4 + tt
                        dj = (c * 4 + tt) % 2
                        for h in range(2):
                            for fh in range(2):
                                S.op("pe", lambda e, dj=dj, h=h, fh=fh, tt=tt, pi=pi, wdb=wdb: e.matmul(
                                    psD[dj][h][:], lhsT=aT[pi][fh][:, tt * 128:(tt + 1) * 128],
                                    rhs=wdb[:, fh, h * 512:(h + 1) * 512], start=(fh == 0), stop=(fh == 1)),
                                    reads=[f"aT{pi}_{fh}", wk[2]], writes=[f"psD{dj}_{h}"])
                            S.op("dve", lambda e, dj=dj, h=h, t=t, ex=ex: e.scalar_tensor_tensor(
                                out=yacc[:, t, h * 512:(h + 1) * 512], in0=psD[dj][h][:], scalar=G[:, t, ex:ex + 1],
                                in1=yacc[:, t, h * 512:(h + 1) * 512], op0=ALU.mult, op1=ALU.add),
                                reads=[f"psD{dj}_{h}", f"G{t}", f"yacc{t}"], writes=[f"yacc{t}"])
            for t in range(NT if stage >= 3 else 0):
                gt = g * NT + t
                oo = outt[gt % 2]
                layer_norm(lambda t=t: yacc[:, t, :], f"yacc{t}", oo[:], f"outt{gt % 2}", 2, "ln2")
                S.dma("sp", lambda e, oo=oo, gt=gt: e.dma_start(out=xout_v[gt], in_=oo[:]), reads=[f"outt{gt % 2}"])
        if stage < 3:
            S.dma('sp', lambda e: e.dma_start(out=xout_v[0], in_=lnb[:, 0, :]), reads=['lnb'])
        S.emit()
    return nc


_NCS = {}


def _nc(name, fn):
    if name not in _NCS:
        _NCS[name] = fn()
    return _NCS[name]


def _c(a):
    return np.ascontiguousarray(a)


def _nsa_in_map(xb_, G, w_in, tbl, inp, ov):
    heads = list(range(G * 4, G * 4 + 4))
    wq = _c(w_in[:, G * 256:(G + 1) * 256])
    wqs = _c(np.concatenate([wq[:, 64:128], wq[:, 0:64], wq[:, 192:256], wq[:, 128:192]], axis=1))
    kvp = lambda i: w_in[:, 1024 + i * 256 + G * 64: 1024 + i * 256 + (G + 1) * 64]
    wkv = _c(np.concatenate([kvp(0), kvp(1), kvp(2), kvp(2), kvp(4), kvp(4), kvp(3), kvp(5)], axis=1))
    wgp = np.zeros((1024, 76), np.float32)
    for br in range(3):
        for r in range(4):
            wgp[:, 64 + br * 4 + r] = w_in[:, 2560 + br * 16 + G * 4 + r]
    w1 = np.concatenate([inp['nsa_cmp_w1_k'][0].reshape(32, 64, 256).transpose(1, 0, 2),
                         inp['nsa_cmp_w1_v'][0].reshape(32, 64, 256).transpose(1, 0, 2)], axis=0)
    posT = np.concatenate([inp['nsa_cmp_pos_k'][0].T, inp['nsa_cmp_pos_v'][0].T], axis=0)
    w2 = np.concatenate([inp['nsa_cmp_w2_k'][0].reshape(2, 128, 64).transpose(1, 0, 2),
                         inp['nsa_cmp_w2_v'][0].reshape(2, 128, 64).transpose(1, 0, 2)], axis=2)
    bss, bsw, bsc = nsa_bias_strips(tbl, heads)
    return dict(xT=xb_, wq=wq, wkv=wkv, wgp=wgp, w1=_c(w1), posT=_c(posT), w2=_c(w2), ovm=ov, bss=bss, bsw=bsw, bsc=bsc,
                cfar=_c(np.broadcast_to(tbl[31, heads][None, :], (128, 4))))


def kernel(**inputs):
    inp = {k: np.asarray(v) for k, v in inputs.items()}
    x = inp['x']
    B, Sq, _ = x.shape
    tbl = inp['rel_table']
    T = B * Sq // 8
    cur = x
    for layer in range(4):
        kind = layer % 4
        xTs = [_c(cur[b].T) for b in range(B)]
        maps = []
        if kind == 0:
            nca = _nc('dil', lambda: build_dil(Sq))
            w_in, w_out = inp['dil_w_in'][0], inp['dil_w_out'][0]
            xperm = [[_c(xTs[b][:, dil_perm(Sq, d)]) for (_, d) in DIL] for b in range(B)]
            for c in range(8):
                b, hg = c // 4, c % 4
                heads = list(range(hg * 4, hg * 4 + 4))
                m = {f"xT{g}": xperm[b][g] for g in range(3)}
                wA = np.empty((3, 4, 1024, 192), np.float32)
                for g in range(3):
                    for i, h in enumerate(heads):
                        for j in range(3):
                            wA[g, i, :, j * 64:(j + 1) * 64] = w_in[:, g * 3072 + j * 1024 + h * 64: g * 3072 + j * 1024 + (h + 1) * 64]
                m["wA"] = wA
                m["btA"] = dil_bias_tiles(tbl, heads)
                maps.append(m)
        elif kind == 1:
            nca = _nc('sb', lambda: build_sb(Sq))
            w_in, w_out = inp['sb_w_in'][0], inp['sb_w_out'][0]
            cm = sb_consts()
            for c in range(8):
                b, hg = c // 4, c % 4
                cols = slice(hg * 256, (hg + 1) * 256)
                maps.append(dict(xT=xTs[b], wq=_c(w_in[:, 0:1024][:, cols]), wk=_c(w_in[:, 1024:2048][:, cols]),
                                 wv=_c(w_in[:, 2048:3072][:, cols]), cm=cm))
        elif kind == 2:
            nca = _nc('nsa', lambda: build_nsa(Sq))
            w_in, w_out = inp['nsa_w_in'][0], inp['nsa_w_out'][0]
            ov = nsa_consts(Sq)
            for c in range(8):
                maps.append(_nsa_in_map(xTs[c // 4], c % 4, w_in, tbl, inp, ov))
        else:
            nca = _nc('moba', lambda: build_moba(Sq))
            w_in, w_out = inp['moba_w_in'][0], inp['moba_w_out'][0]
            for c in range(8):
                b, hg = c // 4, c % 4
                cols = slice(hg * 256, (hg + 1) * 256)
                heads = list(range(hg * 4, hg * 4 + 4))
                maps.append(dict(xT=xTs[b], wq=_c(w_in[:, 0:1024][:, cols]), wk=_c(w_in[:, 1024:2048][:, cols]),
                                 wv=_c(w_in[:, 2048:3072][:, cols]), bss=nsa_bias_strips(tbl, heads)[0],
                                 cfar=_c(np.broadcast_to(tbl[31, heads][None, :], (128, 4)))))
        res = run_bass_kernel_spmd(nca, maps, core_ids=list(range(8)))
        oTf = [np.concatenate([res.results[b * 4 + i]['oT'] for i in range(4)], axis=0) for b in range(B)]
        del res, maps, xTs
        ncp = _nc('post', lambda: build_post(T))
        lnp = _c(np.stack([inp['ln1_g'][layer], inp['ln1_b'][layer], inp['ln2_g'][layer], inp['ln2_b'][layer]]))
        curf = cur.reshape(B * Sq, 1024)
        pmaps = []
        for c in range(8):
            b, s0 = (c * T) // Sq, (c * T) % Sq
            pmaps.append(dict(oT=_c(oTf[b][:, s0:s0 + T]), xin=_c(curf[c * T:(c + 1) * T]), w_out=_c(w_out), lnp=lnp,
                              rw=inp['router_w'], rb=_c(inp['router_b'][None, :]), wg=inp['exp_w_gate'][layer],
                              wu=inp['exp_w_up'][layer], wd=inp['exp_w_down'][layer]))
        res = run_bass_kernel_spmd(ncp, pmaps, core_ids=list(range(8)))
        cur = np.concatenate([res.results[c]['xout'] for c in range(8)], axis=0).reshape(B, Sq, 1024)
        del res, pmaps
    return np.asarray(cur, dtype=np.float32)
```
